# Optimizing a Trainium2 kernel written in Bass

```python
import jax, jax.numpy as jnp
from jax import lax
import numpy as np

D_MODEL = 2048
BATCH = 8
SEQ = 2048
DEPTH = 1
DEC_BATCH = 128
DEC_SEQ = 4
PAST_LEN = 2048
PAGE_SIZE = 128

N_HEADS = 8
HEAD_DIM = 128
N_KV_HEADS = 2
ATTN_WIDTH = N_HEADS * HEAD_DIM
CONV_CH = D_MODEL - ATTN_WIDTH
CONV_WIDTH = 31
IDX_HEADS = 8
IDX_DIM = 64
INDEX_TOPK = 256
Q_BLOCK = 128
ROPE_THETA = 500000.0
ROT_FRACTION = 4
PEER_HEADS = 8
PEER_KEYS = 128
PEER_N = PEER_KEYS * PEER_KEYS
PEER_QDIM = 256
PEER_HALF = PEER_QDIM // 2
PEER_TOPK = 16
PEER_CHUNK = 128
PLE_DIM = 256
EPS = 1e-6

OFF_K = N_HEADS * HEAD_DIM
OFF_V = OFF_K + N_KV_HEADS * HEAD_DIM
OFF_QI = OFF_V + N_KV_HEADS * HEAD_DIM
OFF_KI = OFF_QI + IDX_HEADS * IDX_DIM
OFF_WI = OFF_KI + IDX_DIM
OFF_GLU = OFF_WI + IDX_HEADS
N_IN = OFF_GLU + 2 * CONV_CH

kernel_name = 'hymba_dsa_conformer_peer_step'


def rmsnorm(x, g):
    xf = x.astype(jnp.float32)
    y = xf * lax.rsqrt(jnp.mean(xf * xf, axis=-1, keepdims=True) + EPS)
    return (y * g.astype(jnp.float32)).astype(x.dtype)


def partial_rope(x, pos):
    rot = x.shape[-1] // ROT_FRACTION
    half = rot // 2
    inv = ROPE_THETA ** (-jnp.arange(half, dtype=jnp.float32) * 2.0 / rot)
    ang = pos.astype(jnp.float32)[:, None] * inv[None, :]
    cos = jnp.cos(ang)[:, None, :]
    sin = jnp.sin(ang)[:, None, :]
    xf = x.astype(jnp.float32)
    x1 = xf[..., :half]
    x2 = xf[..., half:rot]
    out = jnp.concatenate([x1 * cos - x2 * sin, x2 * cos + x1 * sin, xf[..., rot:]], axis=-1)
    return out.astype(x.dtype)


def dsa_attend(q, qi, wi, k, v, ki, q_pos, top_k):
    b, nq = q.shape[:2]
    L = k.shape[1]
    s = jnp.einsum('bqhe,ble->bqhl', qi.astype(jnp.float32), ki.astype(jnp.float32)) * (IDX_DIM ** -0.5)
    iscore = jnp.einsum('bqhl,bqh->bql', jax.nn.relu(s), wi.astype(jnp.float32)) * (IDX_HEADS ** -0.5)
    causal = jnp.arange(L)[None, :] <= q_pos[:, None]
    iscore = jnp.where(causal[None], iscore, -jnp.inf)
    _, idx = lax.top_k(iscore, top_k)
    valid = idx <= q_pos[None, :, None]
    k_sel = jax.vmap(lambda kb, ib: kb[ib])(k, idx)
    v_sel = jax.vmap(lambda vb, ib: vb[ib])(v, idx)
    qg = q.reshape(b, nq, N_KV_HEADS, N_HEADS // N_KV_HEADS, HEAD_DIM)
    logits = jnp.einsum('bqgrd,bqkgd->bqgrk', qg, k_sel).astype(jnp.float32) * (HEAD_DIM ** -0.5)
    logits = jnp.where(valid[:, :, None, None, :], logits, -jnp.inf)
    p = jax.nn.softmax(logits, axis=-1).astype(v.dtype)
    out = jnp.einsum('bqgrk,bqkgd->bqgrd', p, v_sel)
    return out.reshape(b, nq, N_HEADS, HEAD_DIM)


def attend_prompt(q, k, v, qi, ki, wi):
    b, s = q.shape[:2]
    n_blocks = s // Q_BLOCK
    top_k = min(INDEX_TOPK, s // 4)

    def block(j):
        s0 = j * Q_BLOCK
        qb = lax.dynamic_slice_in_dim(q, s0, Q_BLOCK, axis=1)
        qib = lax.dynamic_slice_in_dim(qi, s0, Q_BLOCK, axis=1)
        wib = lax.dynamic_slice_in_dim(wi, s0, Q_BLOCK, axis=1)
        q_pos = s0 + jnp.arange(Q_BLOCK)
        return dsa_attend(qb, qib, wib, k, v, ki, q_pos, top_k)

    out = lax.map(block, jnp.arange(n_blocks))
    return out.transpose(1, 0, 2, 3, 4).reshape(b, s, N_HEADS, HEAD_DIM)


def make_sample_attend(k_past, v_past, ki_past):
    def attend(q, k, v, qi, ki, wi):
        k_all = jnp.concatenate([k_past.astype(k.dtype), k], axis=1)
        v_all = jnp.concatenate([v_past.astype(v.dtype), v], axis=1)
        ki_all = jnp.concatenate([ki_past.astype(ki.dtype), ki], axis=1)
        L = k_all.shape[1]
        nq = q.shape[1]
        q_pos = (L - nq) + jnp.arange(nq)
        return dsa_attend(q, qi, wi, k_all, v_all, ki_all, q_pos, min(INDEX_TOPK, L // 4))
    return attend


def conformer_conv(a, prev, conv_w, conv_b, ln_g, ln_b):
    glu = a[..., :CONV_CH] * jax.nn.sigmoid(a[..., CONV_CH:])
    seq = jnp.concatenate([prev.astype(glu.dtype), glu], axis=1)
    y = lax.conv_general_dilated(seq, conv_w[:, None, :].astype(seq.dtype), (1,), 'VALID',
                                 dimension_numbers=('NWC', 'WIO', 'NWC'), feature_group_count=CONV_CH)
    yf = (y + conv_b).astype(jnp.float32)
    mu = jnp.mean(yf, axis=-1, keepdims=True)
    var = jnp.mean(jnp.square(yf - mu), axis=-1, keepdims=True)
    yn = (yf - mu) * lax.rsqrt(var + EPS) * ln_g.astype(jnp.float32) + ln_b.astype(jnp.float32)
    return jax.nn.silu(yn).astype(a.dtype), seq[:, -(CONV_WIDTH - 1):]


def peer_ffn(x, wq, subkeys, u, v):
    shp = x.shape
    xt = x.reshape(-1, D_MODEL)
    t = xt.shape[0]
    tp = -(-t // PEER_CHUNK) * PEER_CHUNK
    xt = jnp.pad(xt, ((0, tp - t), (0, 0)))

    def chunk(xc):
        c = xc.shape[0]
        qh = (xc @ wq).reshape(c, PEER_HEADS, 2, PEER_HALF)
        s = jnp.einsum('chnd,hnkd->chnk', qh, subkeys).astype(jnp.float32)
        s1, i1 = lax.top_k(s[:, :, 0], PEER_TOPK)
        s2, i2 = lax.top_k(s[:, :, 1], PEER_TOPK)
        cand = (s1[..., :, None] + s2[..., None, :]).reshape(c, PEER_HEADS, PEER_TOPK * PEER_TOPK)
        cidx = (i1[..., :, None] * PEER_KEYS + i2[..., None, :]).reshape(c, PEER_HEADS, PEER_TOPK * PEER_TOPK)
        best, sel = lax.top_k(cand, PEER_TOPK)
        eidx = jnp.take_along_axis(cidx, sel, axis=-1).reshape(c, PEER_HEADS * PEER_TOPK)
        g = jax.nn.softmax(best, axis=-1).reshape(c, PEER_HEADS * PEER_TOPK)
        hpre = jnp.einsum('cd,ced->ce', xc, u[eidx]).astype(jnp.float32)
        act = (jax.nn.gelu(hpre, approximate=False) * g).astype(xc.dtype)
        return jnp.einsum('ce,ced->cd', act, v[eidx])

    out = lax.map(chunk, xt.reshape(-1, PEER_CHUNK, D_MODEL))
    return out.reshape(tp, D_MODEL)[:t].reshape(shp)


def layer_forward(h, p_i, pos, attend, conv_prev, lw):
    (attn_norm, w_in, conv_w, conv_b, conv_ln_g, conv_ln_b, w_out, ffn_norm,
     peer_wq, peer_subkeys, peer_u, peer_v, ple_norm, ple_gate, ple_proj) = lw
    b, l = h.shape[:2]
    z = rmsnorm(h, attn_norm) @ w_in
    q, k, v, qi, ki, wi, a = jnp.split(z, [OFF_K, OFF_V, OFF_QI, OFF_KI, OFF_WI, OFF_GLU], axis=-1)
    q = partial_rope(q.reshape(b, l, N_HEADS, HEAD_DIM), pos)
    k = partial_rope(k.reshape(b, l, N_KV_HEADS, HEAD_DIM), pos)
    v = v.reshape(b, l, N_KV_HEADS, HEAD_DIM)
    qi = partial_rope(qi.reshape(b, l, IDX_HEADS, IDX_DIM), pos)
    ki = partial_rope(ki[:, :, None, :], pos)[:, :, 0, :]
    attn = attend(q, k, v, qi, ki, wi).reshape(b, l, ATTN_WIDTH)
    conv_out, conv_state = conformer_conv(a, conv_prev, conv_w, conv_b, conv_ln_g, conv_ln_b)
    h = h + jnp.concatenate([attn, conv_out], axis=-1) @ w_out
    h = h + peer_ffn(rmsnorm(h, ffn_norm), peer_wq, peer_subkeys, peer_u, peer_v)
    gate = jax.nn.sigmoid(rmsnorm(h, ple_norm) @ ple_gate)
    h = h + gate * (p_i @ ple_proj)
    return h, k, v, ki, conv_state


def setup_inputs(seed: int = 0) -> dict:
    key = jax.random.key(seed)
    ks = jax.random.split(key, 32)
    n_pages = PAST_LEN // PAGE_SIZE
    n_used = DEC_BATCH * n_pages
    n_pool = n_used + max(1, n_used // 4)
    f32 = jnp.float32

    def nrm(k, shape, s):
        return jax.random.normal(k, shape, f32) * s

    page_table = jax.random.permutation(ks[6], n_pool)[:n_used].reshape(DEC_BATCH, n_pages).astype(jnp.int32)
    return {
        'x_prompt': nrm(ks[0], (BATCH, SEQ, D_MODEL), 1.0),
        'x_sample': nrm(ks[1], (DEC_BATCH, DEC_SEQ, D_MODEL), 1.0),
        'cache_k': nrm(ks[2], (DEPTH, n_pool, PAGE_SIZE, N_KV_HEADS, HEAD_DIM), 1.0),
        'cache_v': nrm(ks[3], (DEPTH, n_pool, PAGE_SIZE, N_KV_HEADS, HEAD_DIM), 1.0),
        'cache_kidx': nrm(ks[4], (DEPTH, n_pool, PAGE_SIZE, IDX_DIM), 1.0),
        'state_conv': nrm(ks[5], (DEPTH, DEC_BATCH, CONV_WIDTH - 1, CONV_CH), 0.5),
        'page_table': page_table,
        'p_prompt': nrm(ks[7], (DEPTH, BATCH, SEQ, PLE_DIM), 1.0),
        'p_sample': nrm(ks[8], (DEPTH, DEC_BATCH, DEC_SEQ, PLE_DIM), 1.0),
        'attn_norm': 1.0 + nrm(ks[9], (DEPTH, D_MODEL), 0.02),
        'w_in': nrm(ks[10], (DEPTH, D_MODEL, N_IN), D_MODEL ** -0.5),
        'conv_w': nrm(ks[11], (DEPTH, CONV_WIDTH, CONV_CH), CONV_WIDTH ** -0.5),
        'conv_b': nrm(ks[12], (DEPTH, CONV_CH), 0.01),
        'conv_ln_g': 1.0 + nrm(ks[13], (DEPTH, CONV_CH), 0.02),
        'conv_ln_b': nrm(ks[14], (DEPTH, CONV_CH), 0.01),
        'w_out': nrm(ks[15], (DEPTH, D_MODEL, D_MODEL), D_MODEL ** -0.5),
        'ffn_norm': 1.0 + nrm(ks[16], (DEPTH, D_MODEL), 0.02),
        'peer_wq': nrm(ks[17], (DEPTH, D_MODEL, PEER_HEADS * PEER_QDIM), D_MODEL ** -0.5),
        'peer_subkeys': nrm(ks[18], (DEPTH, PEER_HEADS, 2, PEER_KEYS, PEER_HALF), PEER_HALF ** -0.5),
        'peer_u': nrm(ks[19], (DEPTH, PEER_N, D_MODEL), D_MODEL ** -0.5),
        'peer_v': nrm(ks[20], (DEPTH, PEER_N, D_MODEL), PEER_HEADS ** -0.5),
        'ple_norm': 1.0 + nrm(ks[21], (DEPTH, D_MODEL), 0.02),
        'ple_gate': nrm(ks[22], (DEPTH, D_MODEL, D_MODEL), D_MODEL ** -0.5),
        'ple_proj': nrm(ks[23], (DEPTH, PLE_DIM, D_MODEL), PLE_DIM ** -0.5),
        'final_norm': 1.0 + nrm(ks[24], (D_MODEL,), 0.02),
    }


def reference(x_prompt, x_sample, cache_k, cache_v, cache_kidx, state_conv, page_table, p_prompt, p_sample,
              attn_norm, w_in, conv_w, conv_b, conv_ln_g, conv_ln_b, w_out, ffn_norm,
              peer_wq, peer_subkeys, peer_u, peer_v, ple_norm, ple_gate, ple_proj, final_norm):
    n_seq = page_table.shape[0]
    past_len = page_table.shape[1] * cache_k.shape[2]
    pos_p = jnp.arange(x_prompt.shape[1])
    pos_s = past_len + jnp.arange(x_sample.shape[1])
    conv_prev_p = jnp.zeros((x_prompt.shape[0], CONV_WIDTH - 1, CONV_CH), x_prompt.dtype)
    hp, hs = x_prompt, x_sample
    kp_l, vp_l, kip_l, cp_l = [], [], [], []
    ks_l, vs_l, kis_l, cs_l = [], [], [], []
    for i in range(DEPTH):
        lw = (attn_norm[i], w_in[i], conv_w[i], conv_b[i], conv_ln_g[i], conv_ln_b[i], w_out[i], ffn_norm[i],
              peer_wq[i], peer_subkeys[i], peer_u[i], peer_v[i], ple_norm[i], ple_gate[i], ple_proj[i])
        hp, kp, vp, kip, cp = layer_forward(hp, p_prompt[i], pos_p, attend_prompt, conv_prev_p, lw)
        k_past = cache_k[i][page_table].reshape(n_seq, past_len, N_KV_HEADS, HEAD_DIM)
        v_past = cache_v[i][page_table].reshape(n_seq, past_len, N_KV_HEADS, HEAD_DIM)
        ki_past = cache_kidx[i][page_table].reshape(n_seq, past_len, IDX_DIM)
        hs, k_s, v_s, ki_s, c_s = layer_forward(hs, p_sample[i], pos_s, make_sample_attend(k_past, v_past, ki_past),
                                                state_conv[i], lw)
        kp_l.append(kp); vp_l.append(vp); kip_l.append(kip); cp_l.append(cp)
        ks_l.append(k_s); vs_l.append(v_s); kis_l.append(ki_s); cs_l.append(c_s)
    y_prompt = rmsnorm(hp, final_norm)
    y_sample = rmsnorm(hs, final_norm)
    return (y_prompt, y_sample, jnp.stack(kp_l), jnp.stack(vp_l), jnp.stack(kip_l), jnp.stack(cp_l),
            jnp.stack(ks_l), jnp.stack(vs_l), jnp.stack(kis_l), jnp.stack(cs_l))
```

```python
import contextlib
import numpy as np
import concourse.bass as bass
import concourse.mybir as mybir
from concourse.bass_utils import run_bass_kernel_spmd

F32 = mybir.dt.float32
BF16 = mybir.dt.bfloat16
I32 = mybir.dt.int32
U32 = mybir.dt.uint32
AF = mybir.ActivationFunctionType
ALU = mybir.AluOpType
AX_ = mybir.AxisListType

D = 2048
NC = 8
SEQ = 2048
NS = 16
TS = 64
T = SEQ + TS
N_IN = 4168
OFF_K, OFF_V, OFF_QI, OFF_KI, OFF_WI, OFF_GLU = 1024, 1280, 1536, 2048, 2112, 2120
EPS = 1e-6
NEG = -1.0e30


class Res:
    __slots__ = ("name", "w", "r", "excl")

    def __init__(self, name, excl=False):
        self.name = name
        self.w = None
        self.r = {}
        self.excl = excl


class Buf:
    def __init__(self, t, name):
        self.t = t
        self.res = Res(name)
        self.subs = []

    def sub(self, name, excl=False):
        r = Res(name, excl)
        self.subs.append(r)
        return r

    def __getitem__(self, idx):
        return self.t[idx]


class KB:
    def __init__(self, nc, es):
        self.nc = nc
        self.es = es
        self.E = {"pe": nc.tensor, "act": nc.scalar, "dve": nc.vector, "pool": nc.gpsimd, "sp": nc.sync}
        self.esem = {k: es.enter_context(nc.semaphore("sem_" + k)) for k in self.E}
        self.ecnt = {k: 0 for k in self.E}
        self.seen = {k: {} for k in self.E}
        self.dsem = {}
        self.dcnt = {}
        self.semname = {}
        self.out_tags = []
        self.nsem = 5

    def sb(self, es, name, shape, dtype):
        self.nalloc = getattr(self, "nalloc", 0) + 1
        b = Buf(es.enter_context(self.nc.sbuf_tensor("s%d_%s" % (self.nalloc, name), list(shape), dtype)), name)
        es.callback(self.release, b)
        return b

    def release(self, buf):
        tags = []
        for res in [buf.res] + buf.subs:
            tags += list(res.r.values()) + ([res.w] if res.w is not None else [])
        for eng in self.E:
            for tg in tags:
                self._wait(eng, tg)
        for res in [buf.res] + buf.subs:
            for kind in ("w", "r"):
                s_ = self.dsem.pop((id(res), kind), None)
                if s_ is not None:
                    self.free_sems = getattr(self, "free_sems", [])
                    self.free_sems.append(s_)

    def _wait(self, eng, dep):
        sem, val = dep
        key = id(sem)
        if eng == "pe" and sem is self.esem["pe"]:
            return
        if self.seen[eng].get(key, 0) >= val:
            return
        self.E[eng].wait_ge(sem, val)
        self.seen[eng][key] = val

    def _deps(self, eng, reads, writes, skip_waw_sem=None):
        for r in reads:
            if r.w is not None:
                self._wait(eng, r.w)
            if r.excl:
                for d in r.r.values():
                    if d[0] is not self.esem.get(eng):
                        self._wait(eng, d)
        for w in writes:
            if w.w is not None and not (skip_waw_sem is not None and w.w[0] is skip_waw_sem):
                self._wait(eng, w.w)
            for d in w.r.values():
                self._wait(eng, d)

    @staticmethod
    def _res(xs):
        return [x.res if isinstance(x, Buf) else x for x in xs]

    def op(self, eng, fn, reads=(), writes=()):
        reads = self._res(reads)
        writes = self._res(writes)
        self._deps(eng, reads, writes)
        ins = fn(self.E[eng])
        self.ecnt[eng] += 1
        ins.then_inc(self.esem[eng], 1)
        tag = (self.esem[eng], self.ecnt[eng])
        for r in reads:
            r.r[id(tag[0])] = tag
        for w in writes:
            w.w = tag
            w.r = {}
        return ins

    def _dma_sem(self, res, kind):
        key = (id(res), kind)
        if key not in self.dsem:
            free = getattr(self, "free_sems", None)
            if free is None:
                free = self.free_sems = []
            if free:
                s = free.pop()
            else:
                self.nsem += 1
                s = self.es.enter_context(self.nc.semaphore("d%d" % self.nsem))
                self.dcnt[id(s)] = 0
            self.dsem[key] = s
        return self.dsem[key]

    def dma(self, q, out, in_, reads=(), writes=(), is_output=False, **kw):
        reads = self._res(reads)
        writes = self._res(writes)
        if writes:
            sem = self._dma_sem(writes[0], "w")
        else:
            sem = self._dma_sem(reads[0], "r")
        self._deps(q, reads, writes, skip_waw_sem=sem)
        if q == "pool":
            kw.setdefault("max_dma_last_dim", 2048)
        ins = self.E[q].dma_start(out=out, in_=in_, **kw)
        self.dcnt[id(sem)] += 16
        ins.then_inc(sem, 16)
        tag = (sem, self.dcnt[id(sem)])
        for r in reads:
            r.r[id(sem)] = tag
        for w in writes:
            w.w = tag
            w.r = {}
        if is_output:
            self.out_tags.append(tag)
        return ins

    def idma(self, out, in_, idx_ap, reads=(), writes=()):
        reads = self._res(reads)
        writes = self._res(writes)
        sem = self._dma_sem(writes[0], "w")
        self._deps("pool", reads, writes, skip_waw_sem=sem)
        ins = self.nc.gpsimd.indirect_dma_start(out=out, out_offset=None, in_=in_,
                                                in_offset=bass.IndirectOffsetOnAxis(ap=idx_ap, axis=0))
        self.dcnt[id(sem)] += 16
        ins.then_inc(sem, 16)
        tag = (sem, self.dcnt[id(sem)])
        for r in reads:
            r.r[id(sem)] = tag
        for w in writes:
            w.w = tag
            w.r = {}
        return ins

    def finish(self):
        last = {}
        for sem, val in self.out_tags:
            k = id(sem)
            if k not in last or last[k][1] < val:
                last[k] = (sem, val)
        for tag in last.values():
            self._wait("sp", tag)
        for e in ("pe", "act", "dve", "pool"):
            if self.ecnt[e]:
                self._wait("sp", (self.esem[e], self.ecnt[e]))


def rope_tables():
    pos = np.concatenate([np.arange(SEQ, dtype=np.float32), np.tile(2048.0 + np.arange(4, dtype=np.float32), NS)])
    out = np.zeros((T, 96), np.float32)
    for rot, off in ((32, 0), (16, 64)):
        half = rot // 2
        inv = (np.float32(500000.0) ** (-np.arange(half, dtype=np.float32) * np.float32(2.0) / np.float32(rot))).astype(np.float32)
        ang = pos[:, None] * inv[None, :]
        c, s = np.cos(ang), np.sin(ang)
        out[:, off:off + rot] = np.concatenate([c, c], 1)
        out[:, off + rot:off + 2 * rot] = np.concatenate([-s, s], 1)
    return out


def consts():
    cm = np.where(np.arange(128)[None, :] <= np.arange(128)[:, None], 0.0, NEG).astype(np.float32)
    r_ = np.arange(64)
    cms = np.where((r_[:, None] // 4 == r_[None, :] // 4) & (r_[None, :] % 4 <= r_[:, None] % 4), 0.0, NEG).astype(np.float32)
    sel = np.zeros((64, 16, 4, 4), np.float32)
    for s_ in range(16):
        for i_ in range(4):
            sel[4 * s_ + i_, s_, :, i_] = 1.0
    return {"ident_f": np.eye(128, dtype=np.float32), "cmask": cm, "rope": rope_tables(), "cmaskS": cms, "selS": sel.reshape(64, 256),
            "pidx": np.arange(128, dtype=np.float32).reshape(128, 1),
            "iota128": np.tile(np.arange(128, dtype=np.float32)[None, :], (128, 1))}


class _Stop(Exception):
    pass


def build(stage=99, debug=()):
    nc = bass.Bass("TRN2", target_bir_lowering=False)
    try:
        _build(nc, stage, debug)
    except _Stop:
        pass
    return nc


def _build(nc, stage, debug):
    import os

    def chk(tag):
        if os.environ.get("KSTOP") == tag:
            kb.finish()
            raise _Stop()

    def din(name, shape, dt=F32):
        return nc.dram_tensor(name, list(shape), dt, kind="ExternalInput").ap()

    def dout(name, shape, dt=F32):
        return nc.dram_tensor(name, list(shape), dt, kind="ExternalOutput").ap()

    x = din("x", [T, D])
    w_in = din("w_in", [D, N_IN])
    rope = din("rope", [T, 96])
    ident_f_d = din("ident_f", [128, 128])
    cmask_d = din("cmask", [128, 128])
    k_out = dout("k_out", [T, 256])
    v_out = dout("v_out", [T, 256])
    ki_out = dout("ki_out", [T, 64])
    w_out = din("w_out", [D, D])
    norms = din("norms", [4, D])
    peer_wq = din("peer_wq", [D, D])
    peer_sk = din("peer_sk", [16 * 128, 128])
    peer_u = din("peer_u", [16384, D])
    peer_v = din("peer_v", [16384, D])
    ple_gate = din("ple_gate", [D, D])
    ple_proj = din("ple_proj", [256, D])
    pvec = din("pvec", [T, 256])
    iota_d = din("iota128", [128, 128])
    ck = din("ck", [2560 * 128, 256])
    cv = din("cv", [2560 * 128, 256])
    cki = din("cki", [2560 * 128, 64])
    pt_d = din("pt", [NS * 16], I32)
    cmaskS_d = din("cmaskS", [64, 64])
    selS_d = din("selS", [64, 256])
    pidx_d = din("pidx", [128, 1])
    y_out = dout("y", [T, D])
    Gd = nc.dram_tensor("Gd", [128, 128, T], BF16, kind="Internal").ap()
    conv_w = din("conv_w", [31, 1024])
    conv_b = din("conv_b", [1024])
    conv_ln_g = din("conv_ln_g", [1024])
    conv_ln_b = din("conv_ln_b", [1024])
    state = din("state", [NS * 30, 1024])
    conv_p = dout("conv_p", [30, 1024])
    conv_s = dout("conv_s", [NS, 30, 1024])
    dbg = {n: dout("dbg_" + n, shp) for n, shp in debug}

    with contextlib.ExitStack() as es:
        kb = KB(nc, es)
        op, dma = kb.op, kb.dma
        banks = [Buf(es.enter_context(nc.psum_tensor("bank%d" % i, [128, 512], F32)), "bank%d" % i) for i in range(8)]
        for b_ in banks:
            b_.res.excl = True

        ident_f = kb.sb(es, "ident_f", [128, 128], F32)
        ident_b = kb.sb(es, "ident_b", [128, 128], BF16)
        cmask = kb.sb(es, "cmask", [128, 128], F32)
        gcols = kb.sb(es, "gcols", [128, 4, 16], F32)
        gcol = gcols[:, 0, :]
        gcol_r = gcols
        fgb = kb.sb(es, "fgb", [128, D], F32)
        iotaC = kb.sb(es, "iotaC", [128, 128], BF16)
        iota16 = kb.sb(es, "iota16", [128, 16], F32)
        skT = kb.sb(es, "skT", [128, 16, 128], BF16)
        dma("sp", ident_f[:], ident_f_d, writes=[ident_f])
        dma("sp", cmask[:], cmask_d, writes=[cmask])
        for w_ in range(4):
            dma("sp", gcols[:, w_, :], norms[w_].rearrange("(kc p) -> p kc", p=128), writes=[gcols], allow_slow_non_contiguous=True)
        dma("sp", fgb[:], norms[3].partition_broadcast(128), writes=[fgb])
        op("dve", lambda e: e.tensor_copy(out=ident_b[:], in_=ident_f[:]), reads=[ident_f], writes=[ident_b])
        with contextlib.ExitStack() as c1s:
            io32 = kb.sb(c1s, "io32", [128, 128], F32)
            dma("sp", io32[:], iota_d, writes=[io32])
            op("dve", lambda e: e.tensor_copy(out=iotaC[:], in_=io32[:]), reads=[io32], writes=[iotaC])
            op("dve", lambda e: e.tensor_copy(out=iota16[:], in_=io32[:, 0:16]), reads=[io32], writes=[iota16])
            sk32 = kb.sb(c1s, "sk32", [128, 16, 128], F32)
            dma("sp", sk32[:], peer_sk.rearrange("(b k) d -> k b d", k=128), writes=[sk32])
            for q4 in range(4):
                for b4 in range(4):
                    op("pe", lambda e: e.transpose(out=banks[q4].t[:, b4 * 128:(b4 + 1) * 128], in_=sk32[:, q4 * 4 + b4, :], identity=ident_f[:]),
                       reads=[sk32, ident_f], writes=[banks[q4]])
                op("act", lambda e: e.activation(out=skT[:, q4 * 4:q4 * 4 + 4, :], in_=banks[q4].t[:, :].rearrange("p (b k) -> p b k", b=4), func=AF.Copy),
                   reads=[banks[q4]], writes=[skT])
        chk("p0")
        ones_f = kb.sb(es, "ones_f", [128, 128], F32)
        ones_b = kb.sb(es, "ones_b", [128, 128], BF16)
        I4 = kb.sb(es, "I4", [128, 4, 128], BF16)
        thr0 = kb.sb(es, "thr0", [128, 1], F32)
        op("pool", lambda e: e.memset(ones_b[:], 1.0), writes=[ones_b])
        op("pool", lambda e: e.memset(thr0[:], -1.0e29), writes=[thr0])
        op("dve", lambda e: e.tensor_copy(out=I4[:], in_=ident_f[:].unsqueeze(1).broadcast_to([128, 4, 128])), reads=[ident_f], writes=[I4])
        op("pool", lambda e: e.memset(ones_f[:], 1.0), writes=[ones_f])
        cwT = kb.sb(es, "cwT", [128, 8, 31], F32)
        ccol = kb.sb(es, "ccol", [128, 3, 8], F32)
        halo = kb.sb(es, "halo", [128, 8, 30], BF16)
        with contextlib.ExitStack() as c0s:
            cw_sb = kb.sb(c0s, "cw_sb", [31, 1024], F32)
            dma("sp", cw_sb[:], conv_w, writes=[cw_sb])
            for i_, src in enumerate((conv_b, conv_ln_g, conv_ln_b)):
                dma("sp", ccol[:, i_, :], src.rearrange("(g p) -> p g", p=128), writes=[ccol], allow_slow_non_contiguous=True)
            for g in range(8):
                op("pe", lambda e: e.transpose(out=banks[0].t[:, g * 31:(g + 1) * 31], in_=cw_sb[:31, g * 128:(g + 1) * 128],
                                               identity=ident_f[:31, :31]), reads=[cw_sb, ident_f], writes=[banks[0]])
            op("act", lambda e: e.activation(out=cwT[:], in_=banks[0].t[:, 0:248].rearrange("p (g j) -> p g j", g=8), func=AF.Copy),
               reads=[banks[0]], writes=[cwT])

        Gd_res = Res("Gd")
        kT = kb.sb(es, "kT", [128, 2, T], BF16)
        Vb = kb.sb(es, "Vb", [128, 17, 256], BF16)
        kiT = kb.sb(es, "kiT", [64, T], BF16)
        wiS = kb.sb(es, "wiS", [128, 17, 8], F32)

        tiles_all = [(i * 128, 128) for i in range(16)] + [(SEQ, TS)]
        passes = [tiles_all[:6], tiles_all[6:12], tiles_all[12:]]
        import os
        if os.environ.get("KTILES"):
            passes = [tiles_all[:int(os.environ["KTILES"])]]

        for pi, tiles in enumerate(passes):
            c0 = tiles[0][0]
            Tp = sum(r for _, r in tiles)
            Tpp = sum(r for t0_, r in tiles if t0_ < SEQ)
            has_s = any(t0_ >= SEQ for t0_, _ in tiles)
            last_pass = (tiles[-1][0] + tiles[-1][1] >= SEQ)
            chunks = []
            cc_ = 0
            while cc_ < Tpp:
                n_ = min(512, Tpp - cc_)
                chunks.append((cc_, n_, False))
                cc_ += n_
            if has_s:
                chunks.append((Tpp, TS, True))

            def tiles_of(cl0, n):
                return [i for i, (t0_, r_) in enumerate(tiles) if (t0_ - c0) < cl0 + n and (t0_ - c0 + r_) > cl0]

            with contextlib.ExitStack() as ps:
                AX = kb.sb(ps, "AX%d" % pi, [128, 16, Tp], BF16)
                xnT = AX
                actT = AX
                xnT_t = [AX.sub("AX%d_%d" % (pi, i)) for i in range(len(tiles))]
                actT_t = xnT_t
                pa = contextlib.ExitStack()
                ps_real = ps
                ps = pa
                gluT = kb.sb(ps, "gluT%d" % pi, [128, 8, 30 + Tpp], BF16)
                gluT_g = [gluT.sub("gluT%d_%d" % (pi, g)) for g in range(8)]
                if has_s:
                    gluS = kb.sb(ps, "gluS", [128, 8, NS, 34], BF16)
                    gluS_g = [gluS.sub("gluS_%d" % g) for g in range(8)]
                if last_pass:
                    gl32 = kb.sb(ps, "gl32", [128, 8, 96], F32)
                qT = kb.sb(ps, "qT%d" % pi, [128, 8, Tp], BF16)
                qiT = kb.sb(ps, "qiT%d" % pi, [64, 8, Tp], BF16)
                ps = ps_real
                qT_t = [qT.sub("qT%d_%d" % (pi, i)) for i in range(len(tiles))]
                qiT_t = [qiT.sub("qiT%d_%d" % (pi, i)) for i in range(len(tiles))]
                with contextlib.ExitStack() as p1:
                    xt = [kb.sb(p1, "xt%d" % i, [128, D], F32) for i in range(2)]
                    junk = kb.sb(p1, "junk", [128, D], BF16)
                    xs = [kb.sb(p1, "xs%d" % i, [128, D], BF16) for i in range(2)]
                    st = [kb.sb(p1, "st%d" % i, [128, 4], F32) for i in range(2)]
                    for ti, (t0, rows) in enumerate(tiles):
                        s = ti % 2
                        lc = t0 - c0
                        dma("sp", xt[s][:rows, :], x[t0:t0 + rows, :], writes=[xt[s]])
                        op("act", lambda e: e.activation(out=junk[:rows, :], in_=xt[s][:rows, :], func=AF.Square,
                                                         accum_out=st[s][:rows, 0:1]), reads=[xt[s]], writes=[junk, st[s]])
                        op("dve", lambda e: e.tensor_scalar(out=st[s][:rows, 1:2], in0=st[s][:rows, 0:1], scalar1=1.0 / D,
                                                            scalar2=EPS, op0=ALU.mult, op1=ALU.add), reads=[st[s]], writes=[st[s]])
                        op("act", lambda e: e.activation(out=st[s][:rows, 2:3], in_=st[s][:rows, 1:2], func=AF.Sqrt),
                           reads=[st[s]], writes=[st[s]])
                        op("dve", lambda e: e.reciprocal(out=st[s][:rows, 3:4], in_=st[s][:rows, 2:3]), reads=[st[s]], writes=[st[s]])
                        op("act", lambda e: e.activation(out=xs[s][:rows, :], in_=xt[s][:rows, :], func=AF.Copy,
                                                         scale=st[s][:rows, 3:4]), reads=[xt[s], st[s]], writes=[xs[s]])
                        for half in range(2):
                            bk = banks[(2 * ti + half) % 4]
                            tp = bk.t[:].bitcast(BF16)
                            for k8 in range(8):
                                kc = half * 8 + k8
                                op("pe", lambda e: e.transpose(out=tp[:, k8 * 128:k8 * 128 + rows], in_=xs[s][:rows, kc * 128:(kc + 1) * 128],
                                                               identity=ident_b[:rows, :rows]), reads=[xs[s], ident_b], writes=[bk])
                            op("dve", lambda e: e.tensor_tensor(
                                out=xnT[:, half * 8:half * 8 + 8, lc:lc + rows],
                                in0=tp.rearrange("p (k t) -> p k t", k=8)[:, :, :rows],
                                in1=gcols[:, 0, half * 8:half * 8 + 8].unsqueeze(2).broadcast_to([128, 8, rows]), op=ALU.mult),
                               reads=[bk, gcols], writes=[xnT_t[ti]])

                chk("p1")
                if "xnT" in dbg and pi == 0:
                    dma("pool", dbg["xnT"][:, :, 0:Tp], xnT[:], reads=xnT_t, is_output=True)

                chk("p1d")
                with contextlib.ExitStack() as p2:
                    wblk = [kb.sb(p2, "wblk%d" % i, [128, 16, 512], BF16) for i in range(2)]
                    rp = [kb.sb(p2, "rp%d" % i, [128, 96], F32) for i in range(2)]
                    zb = [kb.sb(p2, "zb%d" % i, [128, 512], BF16) for i in range(2)]
                    z32 = [kb.sb(p2, "z32%d" % i, [128, 512], F32) for i in range(2)]
                    ra = [kb.sb(p2, "ra%d" % i, [128, 256], F32) for i in range(2)]
                    rb = [kb.sb(p2, "rb%d" % i, [128, 256], F32) for i in range(2)]
                    blocks = [("q", 0, 512), ("q", 512, 512), ("kv", 1024, 512), ("qi", 1536, 512), ("kw", 2048, 72)]
                    it = 0
                    for bi, (kind, col0, ncol) in enumerate(blocks):
                        wb = wblk[bi % 2]
                        dma("pool", wb[:, :, :ncol], w_in[:, col0:col0 + ncol].rearrange("(kc p) n -> p kc n", p=128), writes=[wb])
                        for ti, (t0, rows) in enumerate(tiles):
                            lc = t0 - c0
                            tile_id = t0 // 128
                            s = it % 2
                            it += 1
                            zp = banks[4 + s]
                            for kc in range(16):
                                op("pe", lambda e: e.matmul(zp.t[:rows, :ncol], lhsT=xnT[:, kc, lc:lc + rows], rhs=wb[:, kc, :ncol],
                                                            start=(kc == 0), stop=(kc == 15)), reads=[xnT_t[ti], wb], writes=[zp])
                            chk("m")
                            dma("sp", rp[s][:rows, :], rope[t0:t0 + rows, :], writes=[rp[s]])
                            chk("rd")

                            def do_rope(dst, H, Dh, R, tb, col_off=0):
                                half = R // 2
                                zv = zp.t[:rows, col_off:col_off + H * Dh].rearrange("p (h d) -> p h d", h=H)
                                cs = rp[s][:rows, tb:tb + R].unsqueeze(1).broadcast_to([rows, H, R])
                                sn1 = rp[s][:rows, tb + R:tb + R + half].unsqueeze(1).broadcast_to([rows, H, half])
                                sn2 = rp[s][:rows, tb + R + half:tb + 2 * R].unsqueeze(1).broadcast_to([rows, H, half])
                                A = ra[s][:rows, :H * R].rearrange("p (h r) -> p h r", h=H)
                                B = rb[s][:rows, :H * R].rearrange("p (h r) -> p h r", h=H)
                                op("dve", lambda e: e.tensor_tensor(out=A, in0=zv[:, :, 0:R], in1=cs, op=ALU.mult), reads=[zp, rp[s]], writes=[ra[s]])
                                op("dve", lambda e: e.tensor_tensor(out=B[:, :, 0:half], in0=zv[:, :, half:R], in1=sn1, op=ALU.mult), reads=[zp, rp[s]], writes=[rb[s]])
                                op("dve", lambda e: e.tensor_tensor(out=B[:, :, half:R], in0=zv[:, :, 0:half], in1=sn2, op=ALU.mult), reads=[zp, rp[s]], writes=[rb[s]])
                                return A, B

                            if kind == "q":
                                h0 = col0 // 128
                                A, B = do_rope(None, 4, 128, 32, 0)
                                chk("r")
                                op("act", lambda e: e.activation(out=zb[s][:rows, :], in_=zp.t[:rows, :], func=AF.Copy), reads=[zp], writes=[zb[s]])
                                op("dve", lambda e: e.tensor_tensor(out=zb[s][:rows, :].rearrange("p (h d) -> p h d", h=4)[:, :, 0:32], in0=A, in1=B, op=ALU.add),
                                   reads=[ra[s], rb[s]], writes=[zb[s]])
                                chk("z")
                                tb_ = banks[(it % 2)]
                                tpv = tb_.t[:].bitcast(BF16)
                                for h in range(4):
                                    op("pe", lambda e: e.transpose(out=tpv[:, h * 128:h * 128 + rows], in_=zb[s][:rows, h * 128:(h + 1) * 128],
                                                                   identity=ident_b[:rows, :rows]), reads=[zb[s], ident_b], writes=[tb_])
                                chk("t")
                                op("act", lambda e: e.activation(out=qT[:, h0:h0 + 4, lc:lc + rows],
                                                                 in_=tpv[:, 0:512].rearrange("p (h t) -> p h t", h=4)[:, :, :rows], func=AF.Copy),
                                   reads=[tb_], writes=[qT_t[ti]])
                            elif kind == "kv":
                                A, B = do_rope(None, 2, 128, 32, 0)
                                op("act", lambda e: e.activation(out=z32[s][:rows, :], in_=zp.t[:rows, :], func=AF.Copy), reads=[zp], writes=[z32[s]])
                                op("dve", lambda e: e.tensor_tensor(out=z32[s][:rows, 0:256].rearrange("p (h d) -> p h d", h=2)[:, :, 0:32], in0=A, in1=B, op=ALU.add),
                                   reads=[ra[s], rb[s]], writes=[z32[s]])
                                dma("sp", k_out[t0:t0 + rows, :], z32[s][:rows, 0:256], reads=[z32[s]], is_output=True)
                                dma("sp", v_out[t0:t0 + rows, :], z32[s][:rows, 256:512], reads=[z32[s]], is_output=True)
                                op("act", lambda e: e.activation(out=Vb[:rows, tile_id, :], in_=z32[s][:rows, 256:512], func=AF.Copy), reads=[z32[s]], writes=[Vb])
                                op("act", lambda e: e.activation(out=zb[s][:rows, 0:256], in_=z32[s][:rows, 0:256], func=AF.Copy), reads=[z32[s]], writes=[zb[s]])
                                tb_ = banks[(it % 2)]
                                tpv = tb_.t[:].bitcast(BF16)
                                for g in range(2):
                                    op("pe", lambda e: e.transpose(out=tpv[:, g * 128:g * 128 + rows], in_=zb[s][:rows, g * 128:(g + 1) * 128],
                                                                   identity=ident_b[:rows, :rows]), reads=[zb[s], ident_b], writes=[tb_])
                                op("act", lambda e: e.activation(out=kT[:, :, t0:t0 + rows],
                                                                 in_=tpv[:, 0:256].rearrange("p (h t) -> p h t", h=2)[:, :, :rows], func=AF.Copy),
                                   reads=[tb_], writes=[kT])
                            elif kind == "qi":
                                A, B = do_rope(None, 8, 64, 16, 64)
                                op("act", lambda e: e.activation(out=zb[s][:rows, :], in_=zp.t[:rows, :], func=AF.Copy), reads=[zp], writes=[zb[s]])
                                op("dve", lambda e: e.tensor_tensor(out=zb[s][:rows, :].rearrange("p (h d) -> p h d", h=8)[:, :, 0:16], in0=A, in1=B, op=ALU.add),
                                   reads=[ra[s], rb[s]], writes=[zb[s]])
                                tb_ = banks[(it % 2)]
                                tpv = tb_.t[:].bitcast(BF16)
                                for h in range(8):
                                    op("pe", lambda e: e.transpose(out=tpv[0:64, h * 128:h * 128 + rows], in_=zb[s][:rows, h * 64:(h + 1) * 64],
                                                                   identity=ident_b[:rows, :rows]), reads=[zb[s], ident_b], writes=[tb_])
                                op("act", lambda e: e.activation(out=qiT[:, :, lc:lc + rows],
                                                                 in_=tpv[0:64, :].rearrange("p (h t) -> p h t", h=8)[:, :, :rows], func=AF.Copy),
                                   reads=[tb_], writes=[qiT_t[ti]])
                            else:
                                A, B = do_rope(None, 1, 64, 16, 64)
                                op("act", lambda e: e.activation(out=z32[s][:rows, 0:72], in_=zp.t[:rows, 0:72], func=AF.Copy), reads=[zp], writes=[z32[s]])
                                op("dve", lambda e: e.tensor_tensor(out=z32[s][:rows, 0:16], in0=A[:, 0, :], in1=B[:, 0, :], op=ALU.add),
                                   reads=[ra[s], rb[s]], writes=[z32[s]])
                                dma("sp", ki_out[t0:t0 + rows, :], z32[s][:rows, 0:64], reads=[z32[s]], is_output=True)
                                op("dve", lambda e: e.tensor_scalar(out=wiS[:rows, tile_id, :], in0=z32[s][:rows, 64:72], scalar1=float(64 ** -0.5 * 8 ** -0.5),
                                                                    scalar2=None, op0=ALU.mult), reads=[z32[s]], writes=[wiS])
                                op("act", lambda e: e.activation(out=zb[s][:rows, 0:64], in_=z32[s][:rows, 0:64], func=AF.Copy), reads=[z32[s]], writes=[zb[s]])
                                tb_ = banks[(it % 2)]
                                tpv = tb_.t[:].bitcast(BF16)
                                op("pe", lambda e: e.transpose(out=tpv[0:64, 0:rows], in_=zb[s][:rows, 0:64],
                                                               identity=ident_b[:rows, :rows]), reads=[zb[s], ident_b], writes=[tb_])
                                op("act", lambda e: e.activation(out=kiT[:, t0:t0 + rows], in_=tpv[0:64, 0:rows], func=AF.Copy), reads=[tb_], writes=[kiT])

                        chk("b%d" % bi)
                with contextlib.ExitStack() as p3:
                    wga = [kb.sb(p3, "wga%d" % i, [128, 16, 256], BF16) for i in range(2)]
                    sg = [kb.sb(p3, "sg%d" % i, [128, 512], F32) for i in range(2)]
                    if pi == 0:
                        op("pool", lambda e: e.memset(gluT[:, :, 0:30], 0.0), writes=gluT_g)
                    else:
                        op("dve", lambda e: e.tensor_copy(out=gluT[:, :, 0:30], in_=halo[:]), reads=[halo], writes=gluT_g)
                    if has_s:
                        stt = [kb.sb(p3, "stt%d" % i, [120, 1024], F32) for i in range(2)]
                        for q4 in range(4):
                            st_ = stt[q4 % 2]
                            dma("sp", st_[:, :], state[q4 * 120:(q4 + 1) * 120, :], writes=[st_])
                            for sl in range(4):
                                dma("sp", conv_s[q4 * 4 + sl, 0:26, :], st_[sl * 30 + 4:sl * 30 + 30, :], reads=[st_], is_output=True)
                            for gh in range(2):
                                bk = banks[gh]
                                for g4 in range(4):
                                    g = gh * 4 + g4
                                    op("pe", lambda e: e.transpose(out=bk.t[:, g4 * 128:g4 * 128 + 120], in_=st_[:120, g * 128:(g + 1) * 128],
                                                                   identity=ident_f[:120, :120]), reads=[st_, ident_f], writes=[bk])
                                op("act", lambda e: e.activation(
                                    out=gluS[:, gh * 4:gh * 4 + 4, q4 * 4:q4 * 4 + 4, 0:30],
                                    in_=bk.t[:, :].rearrange("p (g x) -> p g x", g=4)[:, :, 0:120].rearrange("p g (s r) -> p g s r", s=4),
                                    func=AF.Copy), reads=[bk], writes=gluS_g[gh * 4:gh * 4 + 4])
                    it3 = 0
                    for g in range(8):
                        wg = wga[g % 2]
                        ca = OFF_GLU + g * 128
                        dma("pool", wg[:, :, 0:128], w_in[:, ca:ca + 128].rearrange("(kc p) n -> p kc n", p=128), writes=[wg])
                        dma("pool", wg[:, :, 128:256], w_in[:, ca + 1024:ca + 1152].rearrange("(kc p) n -> p kc n", p=128), writes=[wg])
                        for (cl0, n, is_s) in chunks:
                            s3 = it3 % 2
                            it3 += 1
                            bA, bB = banks[4 + s3], banks[6 + s3]
                            rt = [xnT_t[i] for i in tiles_of(cl0, n)]
                            for kc in range(16):
                                op("pe", lambda e: e.matmul(bA.t[:, :n], lhsT=wg[:, kc, 0:128], rhs=xnT[:, kc, cl0:cl0 + n],
                                                            start=(kc == 0), stop=(kc == 15)), reads=rt + [wg], writes=[bA])
                            for kc in range(16):
                                op("pe", lambda e: e.matmul(bB.t[:, :n], lhsT=wg[:, kc, 128:256], rhs=xnT[:, kc, cl0:cl0 + n],
                                                            start=(kc == 0), stop=(kc == 15)), reads=rt + [wg], writes=[bB])
                            op("act", lambda e: e.activation(out=sg[s3][:, :n], in_=bB.t[:, :n], func=AF.Sigmoid), reads=[bB], writes=[sg[s3]])
                            if not is_s:
                                op("dve", lambda e: e.tensor_tensor(out=gluT[:, g, 30 + cl0:30 + cl0 + n], in0=bA.t[:, :n], in1=sg[s3][:, :n], op=ALU.mult),
                                   reads=[bA, sg[s3]], writes=[gluT_g[g]])
                                if last_pass and cl0 + n == Tpp:
                                    op("dve", lambda e: e.tensor_tensor(out=gl32[:, g, 0:32], in0=bA.t[:, n - 32:n], in1=sg[s3][:, n - 32:n], op=ALU.mult),
                                       reads=[bA, sg[s3]], writes=[gl32])
                            else:
                                op("dve", lambda e: e.tensor_tensor(out=gluS[:, g, :, 30:34], in0=bA.t[:, :n].rearrange("p (s i) -> p s i", i=4),
                                                                    in1=sg[s3][:, :n].rearrange("p (s i) -> p s i", i=4), op=ALU.mult),
                                   reads=[bA, sg[s3]], writes=[gluS_g[g]])
                                op("dve", lambda e: e.tensor_tensor(out=gl32[:, g, 32:96], in0=bA.t[:, :n], in1=sg[s3][:, :n], op=ALU.mult),
                                   reads=[bA, sg[s3]], writes=[gl32])
                    if not last_pass:
                        op("dve", lambda e: e.tensor_copy(out=halo[:], in_=gluT[:, :, Tpp:Tpp + 30]), reads=gluT_g, writes=[halo])
                chk("p3a")
                actT_c = [[xnT_t[i] for i in tiles_of(cl0_, n_)] for (cl0_, n_, _s) in chunks]

                with contextlib.ExitStack() as p3:
                    Dg = [kb.sb(p3, "Dg%d" % i, [128, 31, 128], BF16) for i in range(2)]
                    yb = kb.sb(p3, "yb", [128, 8, 512], F32)
                    yb_g = [yb.sub("yb_%d" % g) for g in range(8)]
                    ysq = [kb.sb(p3, "ysq%d" % i, [128, 512], F32) for i in range(2)]
                    mu = kb.sb(p3, "mu", [128, 512], F32)
                    rs = kb.sb(p3, "rs", [128, 512], F32)
                    tmp = kb.sb(p3, "tmp", [128, 512], F32)
                    it3 = 0
                    for ci, (cl0, n, is_s) in enumerate(chunks):
                        S1, S2 = banks[2], banks[3]
                        for g in range(8):
                            s3 = it3 % 2
                            it3 += 1
                            dg = Dg[s3]
                            op("pool", lambda e: e.tensor_tensor(out=dg[:], in0=ident_b[:].unsqueeze(1).broadcast_to([128, 31, 128]),
                                                                 in1=cwT[:, g, :].unsqueeze(2).broadcast_to([128, 31, 128]), op=ALU.mult),
                               reads=[ident_b, cwT], writes=[dg])
                            bY = banks[s3]
                            for j in range(31):
                                if not is_s:
                                    rhs = gluT[:, g, cl0 + j:cl0 + j + n]
                                    rr = [gluT_g[g]]
                                    outp = bY.t[:, :n]
                                else:
                                    rhs = gluS[:, g, :, j:j + 4]
                                    rr = [gluS_g[g]]
                                    outp = bY.t[:, :n].rearrange("p (s i) -> p s i", i=4)
                                op("pe", lambda e: e.matmul(outp, lhsT=dg[:, j, :], rhs=rhs, start=(j == 0), stop=(j == 30)),
                                   reads=rr + [dg], writes=[bY])
                            op("act", lambda e: e.activation(out=yb[:, g, :n], in_=bY.t[:, :n], func=AF.Identity, bias=ccol[:, 0, g:g + 1]),
                               reads=[bY, ccol], writes=[yb_g[g]])
                            op("act", lambda e: e.activation(out=ysq[s3][:, :n], in_=bY.t[:, :n], func=AF.Square, bias=ccol[:, 0, g:g + 1]),
                               reads=[bY, ccol], writes=[ysq[s3]])
                            op("pe", lambda e: e.matmul(S1.t[:, :n], lhsT=ones_f[:], rhs=yb[:, g, :n], start=(g == 0), stop=(g == 7)),
                               reads=[ones_f, yb_g[g]], writes=[S1])
                            op("pe", lambda e: e.matmul(S2.t[:, :n], lhsT=ones_f[:], rhs=ysq[s3][:, :n], start=(g == 0), stop=(g == 7)),
                               reads=[ones_f, ysq[s3]], writes=[S2])
                        op("dve", lambda e: e.tensor_scalar(out=mu[:, :n], in0=S1.t[:, :n], scalar1=1.0 / 1024, scalar2=None, op0=ALU.mult), reads=[S1], writes=[mu])
                        op("dve", lambda e: e.tensor_tensor(out=tmp[:, :n], in0=mu[:, :n], in1=mu[:, :n], op=ALU.mult), reads=[mu], writes=[tmp])
                        op("dve", lambda e: e.scalar_tensor_tensor(out=tmp[:, :n], in0=S2.t[:, :n], scalar=1.0 / 1024, in1=tmp[:, :n], op0=ALU.mult, op1=ALU.subtract),
                           reads=[S2, tmp], writes=[tmp])
                        op("dve", lambda e: e.tensor_scalar(out=tmp[:, :n], in0=tmp[:, :n], scalar1=EPS, scalar2=None, op0=ALU.add), reads=[tmp], writes=[tmp])
                        op("act", lambda e: e.activation(out=tmp[:, :n], in_=tmp[:, :n], func=AF.Sqrt), reads=[tmp], writes=[tmp])
                        op("dve", lambda e: e.reciprocal(out=rs[:, :n], in_=tmp[:, :n]), reads=[tmp], writes=[rs])
                        for g in range(8):
                            op("dve", lambda e: e.tensor_tensor(out=yb[:, g, :n], in0=yb[:, g, :n], in1=mu[:, :n], op=ALU.subtract), reads=[yb_g[g], mu], writes=[yb_g[g]])
                            op("dve", lambda e: e.tensor_tensor(out=yb[:, g, :n], in0=yb[:, g, :n], in1=rs[:, :n], op=ALU.mult), reads=[yb_g[g], rs], writes=[yb_g[g]])
                            op("act", lambda e: e.activation(out=actT[:, 8 + g, cl0:cl0 + n], in_=yb[:, g, :n], func=AF.Silu,
                                                             scale=ccol[:, 1, g:g + 1], bias=ccol[:, 2, g:g + 1]),
                               reads=[yb_g[g], ccol], writes=actT_c[ci])
                    if last_pass:
                        cst = kb.sb(p3, "cst", [64, 1024], F32)
                        for (c_lo, c_n, which) in ((0, 32, "p"), (32, 64, "s")):
                            if which == "s" and not has_s:
                                continue
                            for gh in range(2):
                                bk = banks[4 + gh]
                                for g4 in range(4):
                                    g = gh * 4 + g4
                                    op("pe", lambda e: e.transpose(out=bk.t[:c_n, g4 * 128:(g4 + 1) * 128], in_=gl32[:, g, c_lo:c_lo + c_n],
                                                                   identity=ident_f[:, :]), reads=[gl32, ident_f], writes=[bk])
                                op("act", lambda e: e.activation(out=cst[:c_n, gh * 512:(gh + 1) * 512], in_=bk.t[:c_n, :], func=AF.Copy), reads=[bk], writes=[cst])
                            if which == "p":
                                dma("sp", conv_p[:, :], cst[2:32, :], reads=[cst], is_output=True)
                            else:
                                for s_ in range(NS):
                                    dma("sp", conv_s[s_, 26:30, :], cst[4 * s_:4 * s_ + 4, :], reads=[cst], is_output=True)
                chk("p3b")
                if "convT" in dbg:
                    for g in range(8):
                        dma("pool", dbg["convT"][:, g, c0:c0 + Tp], actT[:, 8 + g, :], reads=xnT_t, is_output=True)
                with contextlib.ExitStack() as p4:
                    Rh = [kb.sb(p4, "Rh%d" % i, [128, 512], BF16) for i in range(3)]
                    Dw = [kb.sb(p4, "Dw%d" % i, [128, 8, 128], BF16) for i in range(2)]
                    iscA = [kb.sb(p4, "iscA%d" % i, [128, 2048], F32) for i in range(2)]
                    iscW = kb.sb(p4, "iscW", [128, 2048], F32)
                    mx8 = [kb.sb(p4, "mx8_%d" % i, [128, 8], F32) for i in range(2)]
                    maskb = [kb.sb(p4, "maskb%d" % i, [128, 2048], BF16) for i in range(2)]
                    PT = [kb.sb(p4, "PT%d" % i, [128, 512], BF16) for i in range(3)]
                    rsum = [kb.sb(p4, "rsum%d" % i, [128, 512], F32) for i in range(2)]
                    itR = itS = itL = itP = itG = 0
                    for ti, (t0, rows) in enumerate(tiles):
                        if t0 >= SEQ:
                            continue
                        j = t0 // 128
                        lc = t0 - c0
                        L = (j + 1) * 128
                        dw = Dw[ti % 2]
                        ia = iscA[ti % 2]
                        mb = maskb[ti % 2]
                        op("pool", lambda e: e.tensor_tensor(out=dw[:], in0=ident_b[:].unsqueeze(1).broadcast_to([128, 8, 128]),
                                                             in1=wiS[:, j, :].unsqueeze(2).broadcast_to([128, 8, 128]), op=ALU.mult),
                           reads=[ident_b, wiS], writes=[dw])
                        nch = (L + 511) // 512
                        for c4 in range(nch):
                            l0 = c4 * 512
                            n = min(512, L - l0)
                            iP = banks[2 + c4 % 2]
                            for h in range(8):
                                Sb = banks[itS % 2]
                                itS += 1
                                rh = Rh[itR % 3]
                                itR += 1
                                op("pe", lambda e: e.matmul(Sb.t[:, :n], lhsT=qiT[:, h, lc:lc + 128], rhs=kiT[:, l0:l0 + n], start=True, stop=True),
                                   reads=[qiT_t[ti], kiT], writes=[Sb])
                                op("act", lambda e: e.activation(out=rh[:, :n], in_=Sb.t[:, :n], func=AF.Relu), reads=[Sb], writes=[rh])
                                op("pe", lambda e: e.matmul(iP.t[:, :n], lhsT=dw[:, h, :], rhs=rh[:, :n], start=(h == 0), stop=(h == 7)),
                                   reads=[dw, rh], writes=[iP])
                            op("act", lambda e: e.activation(out=ia[:, l0:l0 + n], in_=iP.t[:, :n], func=AF.Copy), reads=[iP], writes=[ia])
                        op("dve", lambda e: e.tensor_tensor(out=ia[:, j * 128:(j + 1) * 128], in0=ia[:, j * 128:(j + 1) * 128], in1=cmask[:], op=ALU.add),
                           reads=[ia, cmask], writes=[ia])
                        if "isc" in dbg and j == int(os.environ.get("KDBGJ", "3")):
                            dma("sp", dbg["isc"][:, 0:L], ia[:, 0:L], reads=[ia], is_output=True)
                        if j >= 2:
                            src = ia
                            for r in range(32):
                                m8 = mx8[r % 2]
                                op("dve", lambda e: e.max(out=m8[:], in_=src[:, :L]), reads=[src], writes=[m8])
                                if r < 31:
                                    op("dve", lambda e: e.match_replace(out=iscW[:, :L], in_to_replace=m8[:], in_values=src[:, :L], imm_value=NEG),
                                       reads=[src, m8], writes=[iscW])
                                    src = iscW
                            thr_ap = m8[:, 7:8]
                            thr_r = m8
                        else:
                            thr_ap = thr0[:, 0:1]
                            thr_r = thr0
                        op("dve", lambda e: e.tensor_scalar(out=mb[:, :L], in0=ia[:, :L], scalar1=thr_ap, scalar2=NEG, op0=ALU.is_lt, op1=ALU.mult),
                           reads=[ia, thr_r], writes=[mb])
                        for g in range(2):
                            OT, SM = banks[6], banks[7]
                            for lb in range(j + 1):
                                LT = banks[4 + itL % 2]
                                itL += 1
                                pt = PT[itP % 3]
                                itP += 1
                                op("pe", lambda e: e.matmul(LT.t[:, :], lhsT=kT[:, g, lb * 128:(lb + 1) * 128], rhs=qT[:, 4 * g:4 * g + 4, lc:lc + 128],
                                                            start=True, stop=False), reads=[kT, qT_t[ti]], writes=[LT])
                                op("pe", lambda e: e.matmul(LT.t[:, :], lhsT=mb[:, lb * 128:(lb + 1) * 128], rhs=I4[:], start=False, stop=True),
                                   reads=[mb, I4], writes=[LT])
                                op("act", lambda e: e.activation(out=pt[:], in_=LT.t[:, :], func=AF.Exp, scale=float(128 ** -0.5)), reads=[LT], writes=[pt])
                                op("pe", lambda e: e.matmul(OT.t[:, :], lhsT=Vb[:, lb, g * 128:(g + 1) * 128], rhs=pt[:], start=(lb == 0), stop=(lb == j)),
                                   reads=[Vb, pt], writes=[OT])
                                op("pe", lambda e: e.matmul(SM.t[:, :], lhsT=ones_b[:], rhs=pt[:], start=(lb == 0), stop=(lb == j)),
                                   reads=[ones_b, pt], writes=[SM])
                            rsm = rsum[itG % 2]
                            itG += 1
                            op("dve", lambda e: e.reciprocal(out=rsm[:], in_=SM.t[:, :]), reads=[SM], writes=[rsm])
                            op("dve", lambda e: e.tensor_tensor(out=actT[:, 4 * g:4 * g + 4, lc:lc + 128], in0=OT.t[:, :].rearrange("p (h t) -> p h t", h=4),
                                                                in1=rsm[:].rearrange("p (h t) -> p h t", h=4), op=ALU.mult),
                               reads=[OT, rsm], writes=[actT_t[ti]])
                if has_s:
                    lcS = SEQ - c0
                    tiS = len(tiles) - 1
                    with contextlib.ExitStack() as p4s:
                        ptb_ = kb.sb(p4s, "ptb_", [128, 256], I32)
                        ptf = kb.sb(p4s, "ptf", [128, 256], F32)
                        pcol = kb.sb(p4s, "pcol", [128, 1], F32)
                        idxs = kb.sb(p4s, "idxs", [128, 256], U32)
                        cmS = kb.sb(p4s, "cmS", [64, 64], F32)
                        sel32 = kb.sb(p4s, "sel32", [64, 256], F32)
                        selS = kb.sb(p4s, "selS", [64, 256], BF16)
                        dma("sp", ptb_[:], pt_d.partition_broadcast(128), writes=[ptb_])
                        dma("sp", pcol[:], pidx_d, writes=[pcol])
                        dma("sp", cmS[:], cmaskS_d, writes=[cmS])
                        dma("sp", sel32[:], selS_d, writes=[sel32])
                        op("dve", lambda e: e.tensor_copy(out=selS[:], in_=sel32[:]), reads=[sel32], writes=[selS])
                        op("dve", lambda e: e.tensor_copy(out=ptf[:], in_=ptb_[:]), reads=[ptb_], writes=[ptf])
                        op("dve", lambda e: e.tensor_scalar(out=ptf[:], in0=ptf[:], scalar1=128.0, scalar2=pcol[:, 0:1], op0=ALU.mult, op1=ALU.add), reads=[ptf, pcol], writes=[ptf])
                        op("dve", lambda e: e.tensor_copy(out=idxs[:], in_=ptf[:]), reads=[ptf], writes=[idxs])
                        iscS = kb.sb(p4s, "iscS", [64, 2112], F32)
                        iscWs = kb.sb(p4s, "iscWs", [64, 2112], F32)
                        mbS = kb.sb(p4s, "mbS", [64, 2112], BF16)
                        m8s = [kb.sb(p4s, "m8s%d" % i, [64, 8], F32) for i in range(2)]
                        DwS = kb.sb(p4s, "DwS", [64, 8, 64], BF16)
                        op("pool", lambda e: e.tensor_tensor(out=DwS[:], in0=ident_b[0:64, 0:64].unsqueeze(1).broadcast_to([64, 8, 64]),
                                                             in1=wiS[0:64, 16, :].unsqueeze(2).broadcast_to([64, 8, 64]), op=ALU.mult), reads=[ident_b, wiS], writes=[DwS])
                        with contextlib.ExitStack() as pix:
                            qiZ = kb.sb(pix, "qiZ", [64, 8, NS, 64], BF16)
                            op("pool", lambda e: e.memset(qiZ[:], 0.0), writes=[qiZ])
                            for s_ in range(NS):
                                op("act", lambda e: e.activation(out=qiZ[:, :, s_, 4 * s_:4 * s_ + 4], in_=qiT[:, :, lcS + 4 * s_:lcS + 4 * s_ + 4], func=AF.Copy),
                                   reads=[qiT_t[tiS]], writes=[qiZ])
                            kis = [kb.sb(pix, "kis%d" % i, [128, 16, 64], BF16) for i in range(2)]
                            kiTs = [kb.sb(pix, "kiTs%d" % i, [64, 512], BF16) for i in range(2)]
                            RhS = [kb.sb(pix, "RhS%d" % i, [64, 512], BF16) for i in range(3)]
                            for h in range(8):
                                Sb = banks[4 + h % 2]
                                rh = RhS[h % 3]
                                op("pe", lambda e: e.matmul(Sb.t[0:64, 0:64], lhsT=qiT[:, h, lcS:lcS + 64], rhs=kiT[:, SEQ:SEQ + 64], start=True, stop=True),
                                   reads=[qiT_t[tiS], kiT], writes=[Sb])
                                op("act", lambda e: e.activation(out=rh[:, 0:64], in_=Sb.t[0:64, 0:64], func=AF.Relu), reads=[Sb], writes=[rh])
                                op("pe", lambda e: e.matmul(banks[6].t[0:64, 0:64], lhsT=DwS[:, h, :], rhs=rh[:, 0:64], start=(h == 0), stop=(h == 7)),
                                   reads=[DwS, rh], writes=[banks[6]])
                            op("dve", lambda e: e.tensor_tensor(out=iscS[:, 2048:2112], in0=banks[6].t[0:64, 0:64], in1=cmS[:], op=ALU.add), reads=[banks[6], cmS], writes=[iscS])
                            itq = 0
                            for s_ in range(NS):
                                ks_ = kis[s_ % 2]
                                for pg in range(16):
                                    kb.idma(ks_[:, pg, :], cki, idxs[:, s_ * 16 + pg:s_ * 16 + pg + 1], reads=[idxs], writes=[ks_])
                                for c4 in range(4):
                                    tb_ = banks[6 + itq % 2]
                                    kt_ = kiTs[itq % 2]
                                    itq += 1
                                    tpv = tb_.t[:].bitcast(BF16)
                                    for p4_ in range(4):
                                        op("pe", lambda e: e.transpose(out=tpv[0:64, p4_ * 128:(p4_ + 1) * 128], in_=ks_[:, c4 * 4 + p4_, :], identity=ident_b[:]),
                                           reads=[ks_, ident_b], writes=[tb_])
                                    op("dve", lambda e: e.tensor_copy(out=kt_[:], in_=tpv[0:64, 0:512]), reads=[tb_], writes=[kt_])
                                    for h in range(8):
                                        Sb = banks[4 + h % 2]
                                        rh = RhS[h % 3]
                                        op("pe", lambda e: e.matmul(Sb.t[0:64, :], lhsT=qiZ[:, h, s_, :], rhs=kt_[:], start=True, stop=True), reads=[qiZ, kt_], writes=[Sb])
                                        op("act", lambda e: e.activation(out=rh[:], in_=Sb.t[0:64, :], func=AF.Relu), reads=[Sb], writes=[rh])
                                        op("pe", lambda e: e.matmul(banks[c4].t[0:64, :], lhsT=DwS[:, h, :], rhs=rh[:], start=(s_ == 0 and h == 0), stop=(s_ == NS - 1 and h == 7)),
                                           reads=[DwS, rh], writes=[banks[c4]])
                            for c4 in range(4):
                                op("act", lambda e: e.activation(out=iscS[:, c4 * 512:(c4 + 1) * 512], in_=banks[c4].t[0:64, :], func=AF.Copy), reads=[banks[c4]], writes=[iscS])
                        if "iscS" in dbg:
                            dma("sp", dbg["iscS"], iscS[:], reads=[iscS], is_output=True)
                        src = iscS
                        for r in range(32):
                            m8 = m8s[r % 2]
                            op("dve", lambda e: e.max(out=m8[:], in_=src[:, :]), reads=[src], writes=[m8])
                            if r < 31:
                                op("dve", lambda e: e.match_replace(out=iscWs[:, :], in_to_replace=m8[:], in_values=src[:, :], imm_value=NEG), reads=[src, m8], writes=[iscWs])
                                src = iscWs
                        op("dve", lambda e: e.tensor_scalar(out=mbS[:], in0=iscS[:], scalar1=m8[:, 7:8], scalar2=NEG, op0=ALU.is_lt, op1=ALU.mult), reads=[iscS, m8], writes=[mbS])
                        with contextlib.ExitStack() as pat:
                            Ks = [kb.sb(pat, "Ks%d" % i, [128, 16, 256], BF16) for i in range(2)]
                            Vs = [kb.sb(pat, "Vs%d" % i, [128, 16, 256], BF16) for i in range(2)]
                            kTs = [kb.sb(pat, "kTs%d" % i, [128, 2, 2048], BF16) for i in range(2)]
                            PTs = [kb.sb(pat, "PTs%d" % i, [128, 272], BF16) for i in range(2)]
                            rsS = [kb.sb(pat, "rsS%d" % i, [128, 32], F32) for i in range(2)]
                            itt = itl = 0
                            for s_ in range(NS):
                                K_, V_, kT_ = Ks[s_ % 2], Vs[s_ % 2], kTs[s_ % 2]
                                for pg in range(16):
                                    kb.idma(K_[:, pg, :], ck, idxs[:, s_ * 16 + pg:s_ * 16 + pg + 1], reads=[idxs], writes=[K_])
                                    kb.idma(V_[:, pg, :], cv, idxs[:, s_ * 16 + pg:s_ * 16 + pg + 1], reads=[idxs], writes=[V_])
                                for g in range(2):
                                    for p8 in range(2):
                                        tb_ = banks[itt % 2]
                                        itt += 1
                                        tpv = tb_.t[:].bitcast(BF16)
                                        for k8 in range(8):
                                            pg = p8 * 8 + k8
                                            op("pe", lambda e: e.transpose(out=tpv[:, k8 * 128:(k8 + 1) * 128], in_=K_[:, pg, g * 128:(g + 1) * 128], identity=ident_b[:]),
                                               reads=[K_, ident_b], writes=[tb_])
                                        op("act", lambda e: e.activation(out=kT_[:, g, p8 * 1024:(p8 + 1) * 1024], in_=tpv[:, :], func=AF.Copy), reads=[tb_], writes=[kT_])
                                OT, SM = banks[4], banks[5]
                                for g in range(2):
                                    LT = banks[2 + itl % 2]
                                    pt = PTs[itl % 2]
                                    itl += 1
                                    qv = qT[:, 4 * g:4 * g + 4, lcS + 4 * s_:lcS + 4 * s_ + 4]
                                    sv = selS[:, s_ * 16:(s_ + 1) * 16]
                                    for lb in range(16):
                                        op("pe", lambda e: e.matmul(LT.t[:, lb * 16:(lb + 1) * 16], lhsT=kT_[:, g, lb * 128:(lb + 1) * 128], rhs=qv, start=True, stop=False),
                                           reads=[kT_, qT_t[tiS]], writes=[LT])
                                        op("pe", lambda e: e.matmul(LT.t[:, lb * 16:(lb + 1) * 16], lhsT=mbS[:, lb * 128:(lb + 1) * 128], rhs=sv, start=False, stop=True),
                                           reads=[mbS, selS], writes=[LT])
                                    op("pe", lambda e: e.matmul(LT.t[0:64, 256:272], lhsT=kT[:, g, SEQ:SEQ + 64], rhs=qv, start=True, stop=False), reads=[kT, qT_t[tiS]], writes=[LT])
                                    op("pe", lambda e: e.matmul(LT.t[0:64, 256:272], lhsT=mbS[:, 2048:2112], rhs=sv, start=False, stop=True), reads=[mbS, selS], writes=[LT])
                                    op("act", lambda e: e.activation(out=pt[:, 0:256], in_=LT.t[:, 0:256], func=AF.Exp, scale=float(128 ** -0.5)), reads=[LT], writes=[pt])
                                    op("act", lambda e: e.activation(out=pt[0:64, 256:272], in_=LT.t[0:64, 256:272], func=AF.Exp, scale=float(128 ** -0.5)), reads=[LT], writes=[pt])
                                    for lb in range(17):
                                        if lb < 16:
                                            lv, pv, on = V_[:, lb, g * 128:(g + 1) * 128], pt[:, lb * 16:(lb + 1) * 16], ones_b[:]
                                        else:
                                            lv, pv, on = Vb[0:64, 16, g * 128:(g + 1) * 128], pt[0:64, 256:272], ones_b[0:64, :]
                                        op("pe", lambda e: e.matmul(OT.t[:, g * 16:(g + 1) * 16], lhsT=lv, rhs=pv, start=(lb == 0), stop=(lb == 16)), reads=[V_, Vb, pt], writes=[OT])
                                        op("pe", lambda e: e.matmul(SM.t[:, g * 16:(g + 1) * 16], lhsT=on, rhs=pv, start=(lb == 0), stop=(lb == 16)), reads=[ones_b, pt], writes=[SM])
                                rs_ = rsS[s_ % 2]
                                op("dve", lambda e: e.reciprocal(out=rs_[:], in_=SM.t[:, 0:32]), reads=[SM], writes=[rs_])
                                op("dve", lambda e: e.tensor_tensor(out=actT[:, 0:8, lcS + 4 * s_:lcS + 4 * s_ + 4], in0=OT.t[:, 0:32].rearrange("p (h i) -> p h i", i=4),
                                                                    in1=rs_[:].rearrange("p (h i) -> p h i", i=4), op=ALU.mult), reads=[OT, rs_], writes=[actT_t[tiS]])
                chk("p4")
                if "attnT" in dbg:
                    for h in range(8):
                        dma("pool", dbg["attnT"][:, h, c0:c0 + Tp], actT[:, h, :], reads=actT_t, is_output=True)
                if "qT" in dbg and pi == 0:
                    dma("pool", dbg["qT"][:, :, 0:Tp], qT[:], reads=qT_t, is_output=True)
                if "qiT" in dbg and pi == 0:
                    dma("pool", dbg["qiT"][:, :, 0:Tp], qiT[:], reads=qiT_t, is_output=True)
                pa.close()

                acc = kb.sb(ps, "acc%d" % pi, [128, len(tiles), D], F32)
                acc_t = [acc.sub("acc%d_%d" % (pi, i)) for i in range(len(tiles))]

                def proj_resid(wsrc, nm):
                    with contextlib.ExitStack() as p5:
                        wblk = [kb.sb(p5, "w5_%d" % i, [128, 16, 512], BF16) for i in range(2)]
                        xb = [kb.sb(p5, "xb%d" % i, [128, 512], F32) for i in range(3)]
                        it5 = 0
                        for nb in range(4):
                            wb = wblk[nb % 2]
                            dma("pool", wb[:], wsrc[:, nb * 512:(nb + 1) * 512].rearrange("(kc p) n -> p kc n", p=128), writes=[wb])
                            for ti, (t0, rows) in enumerate(tiles):
                                lc = t0 - c0
                                bk = banks[it5 % 4]
                                xs_ = xb[it5 % 3]
                                it5 += 1
                                for kc in range(16):
                                    op("pe", lambda e: e.matmul(bk.t[:rows, :], lhsT=AX[:, kc, lc:lc + rows], rhs=wb[:, kc, :],
                                                                start=(kc == 0), stop=(kc == 15)), reads=[xnT_t[ti], wb], writes=[bk])
                                dma("sp", xs_[:rows, :], x[t0:t0 + rows, nb * 512:(nb + 1) * 512], writes=[xs_])
                                op("dve", lambda e: e.tensor_tensor(out=acc[:rows, ti, nb * 512:(nb + 1) * 512], in0=bk.t[:rows, :], in1=xs_[:rows, :], op=ALU.add),
                                   reads=[bk, xs_], writes=[acc_t[ti]])

                proj_resid(w_out, "wout")
                chk("p5")

                def norm_to_AX(which):
                    with contextlib.ExitStack() as pn:
                        junk = kb.sb(pn, "njunk", [128, D], BF16)
                        xs = [kb.sb(pn, "nxs%d" % i, [128, D], BF16) for i in range(2)]
                        st = [kb.sb(pn, "nst%d" % i, [128, 4], F32) for i in range(2)]
                        for ti, (t0, rows) in enumerate(tiles):
                            s_ = ti % 2
                            lc = t0 - c0
                            op("act", lambda e: e.activation(out=junk[:rows, :], in_=acc[:rows, ti, :], func=AF.Square,
                                                             accum_out=st[s_][:rows, 0:1]), reads=[acc_t[ti]], writes=[junk, st[s_]])
                            op("dve", lambda e: e.tensor_scalar(out=st[s_][:rows, 1:2], in0=st[s_][:rows, 0:1], scalar1=1.0 / D,
                                                                scalar2=EPS, op0=ALU.mult, op1=ALU.add), reads=[st[s_]], writes=[st[s_]])
                            op("act", lambda e: e.activation(out=st[s_][:rows, 2:3], in_=st[s_][:rows, 1:2], func=AF.Sqrt), reads=[st[s_]], writes=[st[s_]])
                            op("dve", lambda e: e.reciprocal(out=st[s_][:rows, 3:4], in_=st[s_][:rows, 2:3]), reads=[st[s_]], writes=[st[s_]])
                            op("act", lambda e: e.activation(out=xs[s_][:rows, :], in_=acc[:rows, ti, :], func=AF.Copy,
                                                             scale=st[s_][:rows, 3:4]), reads=[acc_t[ti], st[s_]], writes=[xs[s_]])
                            for half in range(2):
                                bk = banks[(2 * ti + half) % 4]
                                tp = bk.t[:].bitcast(BF16)
                                for k8 in range(8):
                                    kc = half * 8 + k8
                                    op("pe", lambda e: e.transpose(out=tp[:, k8 * 128:k8 * 128 + rows], in_=xs[s_][:rows, kc * 128:(kc + 1) * 128],
                                                                   identity=ident_b[:rows, :rows]), reads=[xs[s_], ident_b], writes=[bk])
                                op("dve", lambda e: e.tensor_tensor(
                                    out=AX[:, half * 8:half * 8 + 8, lc:lc + rows],
                                    in0=tp.rearrange("p (k t) -> p k t", k=8)[:, :, :rows],
                                    in1=gcols[:, which, half * 8:half * 8 + 8].unsqueeze(2).broadcast_to([128, 8, rows]), op=ALU.mult),
                                   reads=[bk, gcols], writes=[xnT_t[ti]])

                norm_to_AX(1)
                nt = len(tiles)
                with contextlib.ExitStack() as p6:
                    v16 = kb.sb(p6, "v16", [128, nt, 16, 16], F32)
                    I16 = kb.sb(p6, "I16", [128, nt, 16, 16], U32)
                    v16_t = [v16.sub("v16_%d" % i) for i in range(nt)]
                    with contextlib.ExitStack() as p6a:
                        wqb = [kb.sb(p6a, "wqb%d" % i, [128, 16, 512], BF16) for i in range(2)]
                        qhb = [kb.sb(p6a, "qhb%d" % i, [128, Tp], BF16) for i in range(2)]
                        stmp = [kb.sb(p6a, "stmp%d" % i, [128, 128], F32) for i in range(2)]
                        it6 = 0
                        for b4 in range(4):
                            wb = wqb[b4 % 2]
                            dma("pool", wb[:], peer_wq[:, b4 * 512:(b4 + 1) * 512].rearrange("(kc p) n -> p kc n", p=128), writes=[wb])
                            for bl in range(4):
                                blk = b4 * 4 + bl
                                qb = qhb[blk % 2]
                                for (cl0, n, is_s) in chunks:
                                    bk = banks[it6 % 2]
                                    it6 += 1
                                    rt = [xnT_t[i] for i in tiles_of(cl0, n)]
                                    for kc in range(16):
                                        op("pe", lambda e: e.matmul(bk.t[:, :n], lhsT=wb[:, kc, bl * 128:(bl + 1) * 128], rhs=AX[:, kc, cl0:cl0 + n],
                                                                    start=(kc == 0), stop=(kc == 15)), reads=rt + [wb], writes=[bk])
                                    op("act", lambda e: e.activation(out=qb[:, cl0:cl0 + n], in_=bk.t[:, :n], func=AF.Copy), reads=[bk], writes=[qb])
                                for ti, (t0, rows) in enumerate(tiles):
                                    lc = t0 - c0
                                    sP = banks[2 + (it6 % 2)]
                                    it6 += 1
                                    sm = stmp[it6 % 2]
                                    op("pe", lambda e: e.matmul(sP.t[:rows, 0:128], lhsT=qb[:, lc:lc + rows], rhs=skT[:, blk, :], start=True, stop=True),
                                       reads=[qb, skT], writes=[sP])
                                    op("dve", lambda e: e.max(out=v16[:rows, ti, blk, 0:8], in_=sP.t[:rows, 0:128]), reads=[sP], writes=[v16_t[ti]])
                                    op("dve", lambda e: e.max_index(out=I16[:rows, ti, blk, 0:8], in_max=v16[:rows, ti, blk, 0:8], in_values=sP.t[:rows, 0:128]),
                                       reads=[sP, v16_t[ti]], writes=[v16_t[ti]])
                                    op("dve", lambda e: e.match_replace(out=sm[:rows, :], in_to_replace=v16[:rows, ti, blk, 0:8], in_values=sP.t[:rows, 0:128], imm_value=NEG),
                                       reads=[sP, v16_t[ti]], writes=[sm])
                                    op("dve", lambda e: e.max(out=v16[:rows, ti, blk, 8:16], in_=sm[:rows, :]), reads=[sm], writes=[v16_t[ti]])
                                    op("dve", lambda e: e.max_index(out=I16[:rows, ti, blk, 8:16], in_max=v16[:rows, ti, blk, 8:16], in_values=sm[:rows, :]),
                                       reads=[sm, v16_t[ti]], writes=[v16_t[ti]])
                    chk("p6a")
                    with contextlib.ExitStack() as p6b:
                        I16f = kb.sb(p6b, "I16f", [128, 16, 16], F32)
                        cand = kb.sb(p6b, "cand", [128, 8, 256], F32)
                        ctmp = kb.sb(p6b, "ctmp", [128, 256], F32)
                        c16 = kb.sb(p6b, "c16", [128, 8, 16], F32)
                        P16 = kb.sb(p6b, "P16", [128, 8, 16], U32)
                        ab_u = kb.sb(p6b, "ab_u", [128, 2, 8, 16], U32)
                        ab_f = kb.sb(p6b, "ab_f", [128, 2, 8, 16], F32)
                        eq = kb.sb(p6b, "eq", [128, 8, 16, 16], F32)
                        sel3 = kb.sb(p6b, "sel3", [128, 3, 128], F32)
                        zst = kb.sb(p6b, "zst", [128, 8, 2], F32)
                        selT = [kb.sb(p6b, "selT%d" % i, [128, 3, 128], BF16) for i in range(2)]
                        OH2 = [kb.sb(p6b, "OH2_%d" % i, [128, 16, 128], BF16) for i in range(2)]
                        OH1 = [kb.sb(p6b, "OH1_%d" % i, [128, 16, 128], BF16) for i in range(2)]
                        GS = kb.sb(p6b, "GS", [128, 128, 128], BF16)
                        itb = itg = 0
                        for ti, (t0, rows) in enumerate(tiles):
                            vv = v16[:rows, ti].rearrange("p (h n) a -> p h n a", n=2)
                            op("dve", lambda e: e.tensor_copy(out=I16f[:rows], in_=I16[:rows, ti]), reads=[v16_t[ti]], writes=[I16f])
                            op("dve", lambda e: e.tensor_tensor(out=cand[:rows].rearrange("p h (a b) -> p h a b", a=16),
                                                                in0=vv[:, :, 0, :].unsqueeze(3).broadcast_to([rows, 8, 16, 16]),
                                                                in1=vv[:, :, 1, :].unsqueeze(2).broadcast_to([rows, 8, 16, 16]), op=ALU.add),
                               reads=[v16_t[ti]], writes=[cand])
                            for h in range(8):
                                op("dve", lambda e: e.max(out=c16[:rows, h, 0:8], in_=cand[:rows, h, :]), reads=[cand], writes=[c16])
                                op("dve", lambda e: e.max_index(out=P16[:rows, h, 0:8], in_max=c16[:rows, h, 0:8], in_values=cand[:rows, h, :]), reads=[cand, c16], writes=[P16])
                                op("dve", lambda e: e.match_replace(out=ctmp[:rows, :], in_to_replace=c16[:rows, h, 0:8], in_values=cand[:rows, h, :], imm_value=NEG),
                                   reads=[cand, c16], writes=[ctmp])
                                op("dve", lambda e: e.max(out=c16[:rows, h, 8:16], in_=ctmp[:rows, :]), reads=[ctmp], writes=[c16])
                                op("dve", lambda e: e.max_index(out=P16[:rows, h, 8:16], in_max=c16[:rows, h, 8:16], in_values=ctmp[:rows, :]), reads=[ctmp, c16], writes=[P16])
                            op("dve", lambda e: e.tensor_scalar(out=ab_u[:rows, 0], in0=P16[:rows], scalar1=4, scalar2=None, op0=ALU.logical_shift_right), reads=[P16], writes=[ab_u])
                            op("dve", lambda e: e.tensor_scalar(out=ab_u[:rows, 1], in0=P16[:rows], scalar1=15, scalar2=None, op0=ALU.bitwise_and), reads=[P16], writes=[ab_u])
                            op("dve", lambda e: e.tensor_copy(out=ab_f[:rows], in_=ab_u[:rows]), reads=[ab_u], writes=[ab_f])
                            If = I16f[:rows].rearrange("p (h n) a -> p h n a", n=2)
                            for w_ in range(2):
                                op("dve", lambda e: e.tensor_tensor(out=eq[:rows], in0=ab_f[:rows, w_].unsqueeze(3).broadcast_to([rows, 8, 16, 16]),
                                                                    in1=iota16[:rows, :].unsqueeze(1).unsqueeze(1).broadcast_to([rows, 8, 16, 16]), op=ALU.is_equal),
                                   reads=[ab_f, iota16], writes=[eq])
                                op("dve", lambda e: e.tensor_tensor(out=eq[:rows], in0=eq[:rows], in1=If[:, :, w_, :].unsqueeze(2).broadcast_to([rows, 8, 16, 16]), op=ALU.mult),
                                   reads=[eq, I16f], writes=[eq])
                                op("dve", lambda e: e.tensor_reduce(out=sel3[:rows, w_, :].rearrange("p (h j) -> p h j", h=8), in_=eq[:rows], axis=AX_.X, op=ALU.add),
                                   reads=[eq], writes=[sel3])
                            wv = sel3[:rows, 2, :].rearrange("p (h j) -> p h j", h=8)
                            op("dve", lambda e: e.tensor_tensor(out=wv, in0=c16[:rows], in1=c16[:rows, :, 0:1].broadcast_to([rows, 8, 16]), op=ALU.subtract),
                               reads=[c16], writes=[sel3])
                            op("act", lambda e: e.activation(out=wv, in_=wv, func=AF.Exp), reads=[sel3], writes=[sel3])
                            op("dve", lambda e: e.tensor_reduce(out=zst[:rows, :, 0], in_=wv, axis=AX_.X, op=ALU.add), reads=[sel3], writes=[zst])
                            op("dve", lambda e: e.reciprocal(out=zst[:rows, :, 1], in_=zst[:rows, :, 0]), reads=[zst], writes=[zst])
                            op("dve", lambda e: e.tensor_tensor(out=wv, in0=wv, in1=zst[:rows, :, 1:2].broadcast_to([rows, 8, 16]), op=ALU.mult),
                               reads=[sel3, zst], writes=[sel3])
                            if "sel3" in dbg and t0 == 0:
                                dma("sp", dbg["sel3"], sel3[:], reads=[sel3], is_output=True)
                            sT = selT[ti % 2]
                            bkT = banks[4]
                            for w_ in range(3):
                                op("pe", lambda e: e.transpose(out=bkT.t[:, w_ * 128:w_ * 128 + rows], in_=sel3[:rows, w_, :], identity=ident_f[:rows, :rows]),
                                   reads=[sel3, ident_f], writes=[bkT])
                            op("act", lambda e: e.activation(out=sT[:, :, :rows], in_=bkT.t[:, 0:384].rearrange("p (w t) -> p w t", w=3)[:, :, :rows], func=AF.Copy),
                               reads=[bkT], writes=[sT])
                            for sub in range((rows + 15) // 16):
                                tl0 = sub * 16
                                o2, o1 = OH2[itb % 2], OH1[itb % 2]
                                itb += 1
                                io_b = iotaC[:].unsqueeze(1).broadcast_to([128, 16, 128])
                                op("dve", lambda e: e.tensor_tensor(out=o2[:], in0=io_b, in1=sT[:, 1, tl0:tl0 + 16].unsqueeze(2).broadcast_to([128, 16, 128]), op=ALU.is_equal),
                                   reads=[iotaC, sT], writes=[o2])
                                op("dve", lambda e: e.tensor_tensor(out=o1[:], in0=io_b, in1=sT[:, 0, tl0:tl0 + 16].unsqueeze(2).broadcast_to([128, 16, 128]), op=ALU.is_equal),
                                   reads=[iotaC, sT], writes=[o1])
                                op("pool", lambda e: e.tensor_tensor(out=o1[:], in0=o1[:], in1=sT[:, 2, tl0:tl0 + 16].unsqueeze(2).broadcast_to([128, 16, 128]), op=ALU.mult),
                                   reads=[o1, sT], writes=[o1])
                                for q4 in range(4):
                                    gb = banks[5 + itg % 3]
                                    itg += 1
                                    for t4 in range(4):
                                        tl = q4 * 4 + t4
                                        op("pe", lambda e: e.matmul(gb.t[:, t4 * 128:(t4 + 1) * 128], lhsT=o2[:, tl, :], rhs=o1[:, tl, :], start=True, stop=True),
                                           reads=[o2, o1], writes=[gb])
                                    tg = tl0 + q4 * 4
                                    op("act", lambda e: e.activation(out=GS[:, :, tg:tg + 4], in_=gb.t[:, :].rearrange("p (t c) -> p c t", t=4), func=AF.Copy),
                                       reads=[gb], writes=[GS])
                            for c8 in range(8):
                                dma("sp", Gd[c8 * 16:(c8 + 1) * 16, :, t0:t0 + rows].rearrange("c i t -> i c t"), GS[:, c8 * 16:(c8 + 1) * 16, :rows],
                                    reads=[GS], writes=[Gd_res], allow_slow_non_contiguous=False)
                    chk("p6b")
                    if "G" in dbg and pi == 0:
                        with contextlib.ExitStack() as pd:
                            gtmp = kb.sb(pd, "gtmp", [128, 128, 128], BF16)
                            dma("sp", gtmp[:], Gd[:, :, 0:128].rearrange("c i t -> i c t"), reads=[Gd_res], writes=[gtmp])
                            for c8 in range(8):
                                dma("pool", dbg["G"][:, c8 * 16:(c8 + 1) * 16, :], gtmp[:, c8 * 16:(c8 + 1) * 16, :], reads=[gtmp], is_output=True)
                    chk("p6c")
                    with contextlib.ExitStack() as p6d:
                        ES = 4
                        ub = [kb.sb(p6d, "ub%d" % i, [128, D], BF16) for i in range(2)]
                        uT = [kb.sb(p6d, "uT%d" % i, [128, 16, 128], BF16) for i in range(2)]
                        gt = [kb.sb(p6d, "gt%d" % i, [128, Tp], BF16) for i in range(2)]
                        gl = [kb.sb(p6d, "gl%d" % i, [128, 512], BF16) for i in range(2)]
                        actS = [kb.sb(p6d, "actS%d" % i, [128, ES, Tp], BF16) for i in range(2)]
                        vS = [kb.sb(p6d, "vS%d" % i, [128, ES, D], BF16) for i in range(2)]
                        ith = itv = 0
                        nsc = 128 // ES
                        if os.environ.get("KNSC"):
                            nsc = int(os.environ["KNSC"])
                        for sc in range(nsc):
                            aS, vs = actS[sc % 2], vS[sc % 2]
                            for ec in range(ES):
                                c = sc * ES + ec
                                u_, uT_, g_ = ub[c % 2], uT[c % 2], gt[c % 2]
                                dma("pool", u_[:], peer_u[c * 128:(c + 1) * 128, :], writes=[u_])
                                dma("pool", vs[:, ec, :], peer_v[c * 128:(c + 1) * 128, :], writes=[vs])
                                dma("sp", g_[:, :], Gd[c, :, c0:c0 + Tp], reads=[Gd_res], writes=[g_])
                                for half in range(2):
                                    bk = banks[half]
                                    tp = bk.t[:].bitcast(BF16)
                                    for k8 in range(8):
                                        kc = half * 8 + k8
                                        op("pe", lambda e: e.transpose(out=tp[:, k8 * 128:(k8 + 1) * 128], in_=u_[:, kc * 128:(kc + 1) * 128], identity=ident_b[:]),
                                           reads=[u_, ident_b], writes=[bk])
                                    op("act", lambda e: e.activation(out=uT_[:, half * 8:half * 8 + 8, :], in_=tp.rearrange("p (k e) -> p k e", k=8), func=AF.Copy),
                                       reads=[bk], writes=[uT_])
                                for (cl0, n, is_s) in chunks:
                                    hp = banks[2 + ith % 2]
                                    gl_ = gl[ith % 2]
                                    ith += 1
                                    rt = [xnT_t[i] for i in tiles_of(cl0, n)]
                                    for kc in range(16):
                                        op("pe", lambda e: e.matmul(hp.t[:, :n], lhsT=uT_[:, kc, :], rhs=AX[:, kc, cl0:cl0 + n], start=(kc == 0), stop=(kc == 15)),
                                           reads=rt + [uT_], writes=[hp])
                                    op("act", lambda e: e.activation(out=gl_[:, :n], in_=hp.t[:, :n], func=AF.Gelu), reads=[hp], writes=[gl_])
                                    op("pool", lambda e: e.tensor_tensor(out=aS[:, ec, cl0:cl0 + n], in0=gl_[:, :n], in1=g_[:, cl0:cl0 + n], op=ALU.mult),
                                       reads=[gl_, g_], writes=[aS])
                            for ti, (t0, rows) in enumerate(tiles):
                                lc = t0 - c0
                                for dh in range(2):
                                    for dq in range(2):
                                        bk = banks[4 + itv % 4]
                                        itv += 1
                                        d0 = dh * 1024 + dq * 512
                                        for ec in range(ES):
                                            op("pe", lambda e: e.matmul(bk.t[:rows, :], lhsT=aS[:, ec, lc:lc + rows], rhs=vs[:, ec, d0:d0 + 512], start=(ec == 0), stop=(ec == ES - 1)),
                                               reads=[aS, vs], writes=[bk])
                                        op("dve", lambda e: e.tensor_tensor(out=acc[:rows, ti, d0:d0 + 512], in0=bk.t[:rows, :], in1=acc[:rows, ti, d0:d0 + 512], op=ALU.add),
                                           reads=[bk, acc_t[ti]], writes=[acc_t[ti]])
                chk("p6")
                if "h3" in dbg:
                    for ti, (t0, rows) in enumerate(tiles):
                        dma("sp", dbg["h3"][t0:t0 + rows, :], acc[:rows, ti, :], reads=[acc_t[ti]], is_output=True)

                norm_to_AX(2)
                with contextlib.ExitStack() as p7:
                    pT = kb.sb(p7, "pT", [128, 2, Tp], BF16)
                    pt32 = [kb.sb(p7, "pt32_%d" % i, [128, 256], F32) for i in range(2)]
                    ptb = [kb.sb(p7, "ptb%d" % i, [128, 256], BF16) for i in range(2)]
                    for ti, (t0, rows) in enumerate(tiles):
                        lc = t0 - c0
                        s_ = ti % 2
                        dma("sp", pt32[s_][:rows, :], pvec[t0:t0 + rows, :], writes=[pt32[s_]])
                        op("act", lambda e: e.activation(out=ptb[s_][:rows, :], in_=pt32[s_][:rows, :], func=AF.Copy), reads=[pt32[s_]], writes=[ptb[s_]])
                        bk = banks[s_]
                        tp = bk.t[:].bitcast(BF16)
                        for k2 in range(2):
                            op("pe", lambda e: e.transpose(out=tp[:, k2 * 128:k2 * 128 + rows], in_=ptb[s_][:rows, k2 * 128:(k2 + 1) * 128], identity=ident_b[:rows, :rows]),
                               reads=[ptb[s_], ident_b], writes=[bk])
                        op("act", lambda e: e.activation(out=pT[:, :, lc:lc + rows], in_=tp[:, 0:256].rearrange("p (k t) -> p k t", k=2)[:, :, :rows], func=AF.Copy),
                           reads=[bk], writes=[pT])
                    wgb = [kb.sb(p7, "wgb%d" % i, [128, 16, 512], BF16) for i in range(2)]
                    wpb = [kb.sb(p7, "wpb%d" % i, [128, 2, 512], BF16) for i in range(2)]
                    gsb = [kb.sb(p7, "gsb%d" % i, [128, 512], F32) for i in range(2)]
                    it7 = 0
                    for nb in range(4):
                        wg_, wp_ = wgb[nb % 2], wpb[nb % 2]
                        dma("pool", wg_[:], ple_gate[:, nb * 512:(nb + 1) * 512].rearrange("(kc p) n -> p kc n", p=128), writes=[wg_])
                        dma("pool", wp_[:], ple_proj[:, nb * 512:(nb + 1) * 512].rearrange("(kc p) n -> p kc n", p=128), writes=[wp_])
                        for ti, (t0, rows) in enumerate(tiles):
                            lc = t0 - c0
                            bG, bP = banks[2 + it7 % 2], banks[4 + it7 % 2]
                            gs_ = gsb[it7 % 2]
                            it7 += 1
                            for kc in range(16):
                                op("pe", lambda e: e.matmul(bG.t[:rows, :], lhsT=AX[:, kc, lc:lc + rows], rhs=wg_[:, kc, :], start=(kc == 0), stop=(kc == 15)),
                                   reads=[xnT_t[ti], wg_], writes=[bG])
                            for k2 in range(2):
                                op("pe", lambda e: e.matmul(bP.t[:rows, :], lhsT=pT[:, k2, lc:lc + rows], rhs=wp_[:, k2, :], start=(k2 == 0), stop=(k2 == 1)),
                                   reads=[pT, wp_], writes=[bP])
                            op("act", lambda e: e.activation(out=gs_[:rows, :], in_=bG.t[:rows, :], func=AF.Sigmoid), reads=[bG], writes=[gs_])
                            op("dve", lambda e: e.tensor_tensor(out=gs_[:rows, :], in0=gs_[:rows, :], in1=bP.t[:rows, :], op=ALU.mult), reads=[gs_, bP], writes=[gs_])
                            op("dve", lambda e: e.tensor_tensor(out=acc[:rows, ti, nb * 512:(nb + 1) * 512], in0=acc[:rows, ti, nb * 512:(nb + 1) * 512], in1=gs_[:rows, :], op=ALU.add),
                               reads=[gs_, acc_t[ti]], writes=[acc_t[ti]])
                chk("p7")
                if "h4" in dbg:
                    for ti, (t0, rows) in enumerate(tiles):
                        dma("sp", dbg["h4"][t0:t0 + rows, :], acc[:rows, ti, :], reads=[acc_t[ti]], is_output=True)
                with contextlib.ExitStack() as pf:
                    junk = kb.sb(pf, "fjunk", [128, D], BF16)
                    yt = [kb.sb(pf, "yt%d" % i, [128, D], F32) for i in range(2)]
                    st = [kb.sb(pf, "fst%d" % i, [128, 4], F32) for i in range(2)]
                    for ti, (t0, rows) in enumerate(tiles):
                        s_ = ti % 2
                        op("act", lambda e: e.activation(out=junk[:rows, :], in_=acc[:rows, ti, :], func=AF.Square, accum_out=st[s_][:rows, 0:1]),
                           reads=[acc_t[ti]], writes=[junk, st[s_]])
                        op("dve", lambda e: e.tensor_scalar(out=st[s_][:rows, 1:2], in0=st[s_][:rows, 0:1], scalar1=1.0 / D, scalar2=EPS, op0=ALU.mult, op1=ALU.add),
                           reads=[st[s_]], writes=[st[s_]])
                        op("act", lambda e: e.activation(out=st[s_][:rows, 2:3], in_=st[s_][:rows, 1:2], func=AF.Sqrt), reads=[st[s_]], writes=[st[s_]])
                        op("dve", lambda e: e.reciprocal(out=st[s_][:rows, 3:4], in_=st[s_][:rows, 2:3]), reads=[st[s_]], writes=[st[s_]])
                        op("dve", lambda e: e.scalar_tensor_tensor(out=yt[s_][:rows, :], in0=acc[:rows, ti, :], scalar=st[s_][:rows, 3:4], in1=fgb[:rows, :], op0=ALU.mult, op1=ALU.mult),
                           reads=[acc_t[ti], st[s_], fgb], writes=[yt[s_]])
                        dma("sp", y_out[t0:t0 + rows, :], yt[s_][:rows, :], reads=[yt[s_]], is_output=True)

                if "h2" in dbg:
                    for ti, (t0, rows) in enumerate(tiles):
                        dma("sp", dbg["h2"][t0:t0 + rows, :], acc[:rows, ti, :], reads=[acc_t[ti]], is_output=True)
        kb.finish()


def make_in_maps(inputs):
    cst = consts()
    maps = []
    for c in range(NC):
        m = dict(cst)
        m["x"] = np.ascontiguousarray(np.concatenate(
            [inputs["x_prompt"][c], inputs["x_sample"][NS * c:NS * (c + 1)].reshape(TS, D)], 0))
        m["w_in"] = np.ascontiguousarray(inputs["w_in"][0])
        m["norms"] = np.ascontiguousarray(np.stack([inputs["attn_norm"][0], inputs["ffn_norm"][0], inputs["ple_norm"][0], inputs["final_norm"]]))
        m["peer_wq"] = np.ascontiguousarray(inputs["peer_wq"][0])
        m["peer_sk"] = np.ascontiguousarray(inputs["peer_subkeys"][0].reshape(16 * 128, 128))
        m["peer_u"] = np.ascontiguousarray(inputs["peer_u"][0])
        m["peer_v"] = np.ascontiguousarray(inputs["peer_v"][0])
        m["ple_gate"] = np.ascontiguousarray(inputs["ple_gate"][0])
        m["ple_proj"] = np.ascontiguousarray(inputs["ple_proj"][0])
        m["ck"] = inputs["cache_k"][0].reshape(2560 * 128, 256)
        m["cv"] = inputs["cache_v"][0].reshape(2560 * 128, 256)
        m["cki"] = inputs["cache_kidx"][0].reshape(2560 * 128, 64)
        m["pt"] = np.ascontiguousarray(inputs["page_table"][NS * c:NS * (c + 1)].reshape(-1).astype(np.int32))
        m["pvec"] = np.ascontiguousarray(np.concatenate([inputs["p_prompt"][0, c], inputs["p_sample"][0, NS * c:NS * (c + 1)].reshape(TS, 256)], 0))
        for k_ in ("conv_w", "conv_b", "conv_ln_g", "conv_ln_b", "w_out"):
            m[k_] = np.ascontiguousarray(inputs[k_][0])
        m["state"] = np.ascontiguousarray(inputs["state_conv"][0, NS * c:NS * (c + 1)].reshape(NS * 30, 1024))
        maps.append(m)
    return maps


def kernel(**inputs):
    inputs = {k: np.asarray(v) for k, v in inputs.items()}
    nc = build()
    maps = make_in_maps(inputs)
    res = run_bass_kernel_spmd(nc, maps, core_ids=list(range(NC)))
    R = res.results
    B = 8
    k_p = np.stack([R[c]["k_out"][:SEQ].reshape(SEQ, 2, 128) for c in range(NC)])[None]
    v_p = np.stack([R[c]["v_out"][:SEQ].reshape(SEQ, 2, 128) for c in range(NC)])[None]
    ki_p = np.stack([R[c]["ki_out"][:SEQ] for c in range(NC)])[None]
    k_s = np.concatenate([R[c]["k_out"][SEQ:].reshape(NS, 4, 2, 128) for c in range(NC)])[None]
    v_s = np.concatenate([R[c]["v_out"][SEQ:].reshape(NS, 4, 2, 128) for c in range(NC)])[None]
    ki_s = np.concatenate([R[c]["ki_out"][SEQ:].reshape(NS, 4, 64) for c in range(NC)])[None]
    y_p = np.stack([R[c]["y"][:SEQ] for c in range(NC)])
    y_s = np.concatenate([R[c]["y"][SEQ:].reshape(NS, 4, D) for c in range(NC)])
    cp = np.stack([R[c]["conv_p"] for c in range(NC)])[None]
    cs = np.concatenate([R[c]["conv_s"] for c in range(NC)])[None]
    return (y_p, y_s, k_p, v_p, ki_p, cp, k_s, v_s, ki_s, cs)
```

```python
import contextlib
import numpy as np
import concourse.bass as bass
import concourse.mybir as mybir
from concourse.bass_utils import run_bass_kernel_spmd

F32 = mybir.dt.float32
BF16 = mybir.dt.bfloat16
I32 = mybir.dt.int32
U32 = mybir.dt.uint32
AF = mybir.ActivationFunctionType
ALU = mybir.AluOpType
AX_ = mybir.AxisListType

D = 2048
NC = 8
SEQ = 2048
NS = 16
TS = 64
T = SEQ + TS
N_IN = 4168
OFF_K, OFF_V, OFF_QI, OFF_KI, OFF_WI, OFF_GLU = 1024, 1280, 1536, 2048, 2112, 2120
EPS = 1e-6
NEG = -1.0e30


class Res:
    __slots__ = ("name", "w", "r", "excl")

    def __init__(self, name, excl=False):
        self.name = name
        self.w = None
        self.r = {}
        self.excl = excl


class Buf:
    def __init__(self, t, name):
        self.t = t
        self.res = Res(name)
        self.subs = []

    def sub(self, name, excl=False):
        r = Res(name, excl)
        self.subs.append(r)
        return r

    def __getitem__(self, idx):
        return self.t[idx]


class KB:
    def __init__(self, nc, es):
        self.nc = nc
        self.es = es
        self.E = {"pe": nc.tensor, "act": nc.scalar, "dve": nc.vector, "pool": nc.gpsimd, "sp": nc.sync}
        self.esem = {k: es.enter_context(nc.semaphore("sem_" + k)) for k in self.E}
        self.ecnt = {k: 0 for k in self.E}
        self.seen = {k: {} for k in self.E}
        self.dsem = {}
        self.dcnt = {}
        self.semname = {}
        self.out_tags = []
        self.nsem = 5

    def sb(self, es, name, shape, dtype):
        self.nalloc = getattr(self, "nalloc", 0) + 1
        b = Buf(es.enter_context(self.nc.sbuf_tensor("s%d_%s" % (self.nalloc, name), list(shape), dtype)), name)
        es.callback(self.release, b)
        return b

    def release(self, buf):
        tags = []
        for res in [buf.res] + buf.subs:
            tags += list(res.r.values()) + ([res.w] if res.w is not None else [])
        for eng in self.E:
            for tg in tags:
                self._wait(eng, tg)
        for res in [buf.res] + buf.subs:
            for kind in ("w", "r"):
                for qc in ("sw", "hw"):
                    s_ = self.dsem.pop((id(res), kind, qc), None)
                    if s_ is not None:
                        if not hasattr(self, "free_sems"):
                            self.free_sems = {"sw": [], "hw": []}
                        self.free_sems[qc].append(s_)

    def _wait(self, eng, dep):
        sem, val = dep
        key = id(sem)
        if eng == "pe" and sem is self.esem["pe"]:
            return
        if self.seen[eng].get(key, 0) >= val:
            return
        self.E[eng].wait_ge(sem, val)
        self.seen[eng][key] = val

    def _deps(self, eng, reads, writes, skip_waw_sem=None):
        for r in reads:
            if r.w is not None:
                self._wait(eng, r.w)
            if r.excl:
                for d in r.r.values():
                    if d[0] is not self.esem.get(eng):
                        self._wait(eng, d)
        for w in writes:
            if w.w is not None and not (skip_waw_sem is not None and w.w[0] is skip_waw_sem):
                self._wait(eng, w.w)
            for d in w.r.values():
                self._wait(eng, d)

    @staticmethod
    def _res(xs):
        return [x.res if isinstance(x, Buf) else x for x in xs]

    def op(self, eng, fn, reads=(), writes=()):
        reads = self._res(reads)
        writes = self._res(writes)
        self._deps(eng, reads, writes)
        ins = fn(self.E[eng])
        self.ecnt[eng] += 1
        ins.then_inc(self.esem[eng], 1)
        tag = (self.esem[eng], self.ecnt[eng])
        for r in reads:
            r.r[id(tag[0])] = tag
        for w in writes:
            w.w = tag
            w.r = {}
        return ins

    def _dma_sem(self, res, kind, q="sp"):
        qc = "sw" if q == "pool" else "hw"
        key = (id(res), kind, qc)
        if key not in self.dsem:
            if not hasattr(self, "free_sems"):
                self.free_sems = {"sw": [], "hw": []}
            free = self.free_sems[qc]
            if free:
                s = free.pop()
            else:
                self.nsem += 1
                s = self.es.enter_context(self.nc.semaphore("d%d" % self.nsem))
                self.dcnt[id(s)] = 0
            self.dsem[key] = s
        return self.dsem[key]

    def dma(self, q, out, in_, reads=(), writes=(), is_output=False, **kw):
        reads = self._res(reads)
        writes = self._res(writes)
        if writes:
            sem = self._dma_sem(writes[0], "w", q)
        else:
            sem = self._dma_sem(reads[0], "r", q)
        self._deps(q, reads, writes, skip_waw_sem=sem)
        if q == "pool":
            kw.setdefault("max_dma_last_dim", 2048)
        ins = self.E[q].dma_start(out=out, in_=in_, **kw)
        self.dcnt[id(sem)] += 16
        ins.then_inc(sem, 16)
        tag = (sem, self.dcnt[id(sem)])
        for r in reads:
            r.r[id(sem)] = tag
        for w in writes:
            w.w = tag
            w.r = {}
        if is_output:
            self.out_tags.append(tag)
        return ins

    def idma(self, out, in_, idx_ap, reads=(), writes=()):
        reads = self._res(reads)
        writes = self._res(writes)
        sem = self._dma_sem(writes[0], "w", "pool")
        self._deps("pool", reads, writes, skip_waw_sem=sem)
        ins = self.nc.gpsimd.indirect_dma_start(out=out, out_offset=None, in_=in_,
                                                in_offset=bass.IndirectOffsetOnAxis(ap=idx_ap, axis=0))
        self.dcnt[id(sem)] += 16
        ins.then_inc(sem, 16)
        tag = (sem, self.dcnt[id(sem)])
        for r in reads:
            r.r[id(sem)] = tag
        for w in writes:
            w.w = tag
            w.r = {}
        return ins

    def finish(self):
        last = {}
        for sem, val in self.out_tags:
            k = id(sem)
            if k not in last or last[k][1] < val:
                last[k] = (sem, val)
        for tag in last.values():
            self._wait("sp", tag)
        for e in ("pe", "act", "dve", "pool"):
            if self.ecnt[e]:
                self._wait("sp", (self.esem[e], self.ecnt[e]))


def rope_tables():
    pos = np.concatenate([np.arange(SEQ, dtype=np.float32), np.tile(2048.0 + np.arange(4, dtype=np.float32), NS)])
    out = np.zeros((T, 96), np.float32)
    for rot, off in ((32, 0), (16, 64)):
        half = rot // 2
        inv = (np.float32(500000.0) ** (-np.arange(half, dtype=np.float32) * np.float32(2.0) / np.float32(rot))).astype(np.float32)
        ang = pos[:, None] * inv[None, :]
        c, s = np.cos(ang), np.sin(ang)
        out[:, off:off + rot] = np.concatenate([c, c], 1)
        out[:, off + rot:off + 2 * rot] = np.concatenate([-s, s], 1)
    return out


def consts():
    cm = np.where(np.arange(128)[None, :] <= np.arange(128)[:, None], 0.0, NEG).astype(np.float32)
    r_ = np.arange(64)
    cms = np.where((r_[:, None] // 4 == r_[None, :] // 4) & (r_[None, :] % 4 <= r_[:, None] % 4), 0.0, NEG).astype(np.float32)
    sel = np.zeros((64, 16, 4, 4), np.float32)
    for s_ in range(16):
        for i_ in range(4):
            sel[4 * s_ + i_, s_, :, i_] = 1.0
    return {"ident_f": np.eye(128, dtype=np.float32), "cmask": cm, "rope": rope_tables(), "cmaskS": cms, "selS": sel.reshape(64, 256),
            "pidx": np.arange(128, dtype=np.float32).reshape(128, 1),
            "iota128": np.tile(np.arange(128, dtype=np.float32)[None, :], (128, 1))}


class _Stop(Exception):
    pass


def build(stage=99, debug=()):
    nc = bass.Bass("TRN2", target_bir_lowering=False)
    try:
        _build(nc, stage, debug)
    except _Stop:
        pass
    return nc


def _build(nc, stage, debug):
    import os

    _cur = [None]

    def scope(name):
        if os.environ.get("KSCOPE"):
            if _cur[0] is not None:
                nc.leave_named_scope(_cur[0][0], _cur[0][1], False)
            _cur[0] = None
            if name is not None:
                sid, _ = nc.enter_named_scope(name, False)
                _cur[0] = (name, sid)

    def chk(tag):
        if os.environ.get("KSTOP") == tag:
            scope(None)
            kb.finish()
            raise _Stop()

    def din(name, shape, dt=F32):
        return nc.dram_tensor(name, list(shape), dt, kind="ExternalInput").ap()

    def dout(name, shape, dt=F32):
        return nc.dram_tensor(name, list(shape), dt, kind="ExternalOutput").ap()

    x = din("x", [T, D])
    w_in = din("w_in", [D, N_IN])
    rope = din("rope", [T, 96])
    ident_f_d = din("ident_f", [128, 128])
    cmask_d = din("cmask", [128, 128])
    k_out = dout("k_out", [T, 256])
    v_out = dout("v_out", [T, 256])
    ki_out = dout("ki_out", [T, 64])
    w_out = din("w_out", [D, D])
    norms = din("norms", [4, D])
    peer_wq = din("peer_wq", [D, D])
    peer_sk = din("peer_sk", [16 * 128, 128])
    peer_u = din("peer_u", [16384, D])
    peer_v = din("peer_v", [16384, D])
    ple_gate = din("ple_gate", [D, D])
    ple_proj = din("ple_proj", [256, D])
    pvec = din("pvec", [T, 256])
    iota_d = din("iota128", [128, 128])
    ck = din("ck", [2560 * 128, 256])
    cv = din("cv", [2560 * 128, 256])
    cki = din("cki", [2560 * 128, 64])
    pt_d = din("pt", [NS * 16], I32)
    cmaskS_d = din("cmaskS", [64, 64])
    selS_d = din("selS", [64, 256])
    pidx_d = din("pidx", [128, 1])
    y_out = dout("y", [T, D])
    Gd = nc.dram_tensor("Gd", [128, 128, T], BF16, kind="Internal").ap()
    conv_w = din("conv_w", [31, 1024])
    conv_b = din("conv_b", [1024])
    conv_ln_g = din("conv_ln_g", [1024])
    conv_ln_b = din("conv_ln_b", [1024])
    state = din("state", [NS * 30, 1024])
    conv_p = dout("conv_p", [30, 1024])
    conv_s = dout("conv_s", [NS, 30, 1024])
    dbg = {n: dout("dbg_" + n, shp) for n, shp in debug}

    with contextlib.ExitStack() as es:
        kb = KB(nc, es)
        op, dma = kb.op, kb.dma
        banks = [Buf(es.enter_context(nc.psum_tensor("bank%d" % i, [128, 512], F32)), "bank%d" % i) for i in range(8)]
        for b_ in banks:
            b_.res.excl = True

        ident_f = kb.sb(es, "ident_f", [128, 128], F32)
        ident_b = kb.sb(es, "ident_b", [128, 128], BF16)
        cmask = kb.sb(es, "cmask", [128, 128], F32)
        gcols = kb.sb(es, "gcols", [128, 4, 16], F32)
        gcol = gcols[:, 0, :]
        gcol_r = gcols
        fgb = kb.sb(es, "fgb", [128, D], F32)
        iotaC = kb.sb(es, "iotaC", [128, 128], BF16)
        iota16 = kb.sb(es, "iota16", [128, 16], F32)
        skT = kb.sb(es, "skT", [128, 16, 128], BF16)
        dma("sp", ident_f[:], ident_f_d, writes=[ident_f])
        dma("sp", cmask[:], cmask_d, writes=[cmask])
        for w_ in range(4):
            dma("sp", gcols[:, w_, :], norms[w_].rearrange("(kc p) -> p kc", p=128), writes=[gcols], allow_slow_non_contiguous=True)
        dma("sp", fgb[:], norms[3].partition_broadcast(128), writes=[fgb])
        op("dve", lambda e: e.tensor_copy(out=ident_b[:], in_=ident_f[:]), reads=[ident_f], writes=[ident_b])
        with contextlib.ExitStack() as c1s:
            io32 = kb.sb(c1s, "io32", [128, 128], F32)
            dma("sp", io32[:], iota_d, writes=[io32])
            op("dve", lambda e: e.tensor_copy(out=iotaC[:], in_=io32[:]), reads=[io32], writes=[iotaC])
            op("dve", lambda e: e.tensor_copy(out=iota16[:], in_=io32[:, 0:16]), reads=[io32], writes=[iota16])
            sk32 = kb.sb(c1s, "sk32", [128, 16, 128], F32)
            dma("sp", sk32[:], peer_sk.rearrange("(b k) d -> k b d", k=128), writes=[sk32])
            for q4 in range(4):
                for b4 in range(4):
                    op("pe", lambda e: e.transpose(out=banks[q4].t[:, b4 * 128:(b4 + 1) * 128], in_=sk32[:, q4 * 4 + b4, :], identity=ident_f[:]),
                       reads=[sk32, ident_f], writes=[banks[q4]])
                op("act", lambda e: e.activation(out=skT[:, q4 * 4:q4 * 4 + 4, :], in_=banks[q4].t[:, :].rearrange("p (b k) -> p b k", b=4), func=AF.Copy),
                   reads=[banks[q4]], writes=[skT])
        chk("p0")
        ones_f = kb.sb(es, "ones_f", [128, 128], F32)
        ones_b = kb.sb(es, "ones_b", [128, 128], BF16)
        I4 = kb.sb(es, "I4", [128, 4, 128], BF16)
        thr0 = kb.sb(es, "thr0", [128, 1], F32)
        op("pool", lambda e: e.memset(ones_b[:], 1.0), writes=[ones_b])
        op("pool", lambda e: e.memset(thr0[:], -1.0e29), writes=[thr0])
        op("dve", lambda e: e.tensor_copy(out=I4[:], in_=ident_f[:].unsqueeze(1).broadcast_to([128, 4, 128])), reads=[ident_f], writes=[I4])
        op("pool", lambda e: e.memset(ones_f[:], 1.0), writes=[ones_f])
        cwT = kb.sb(es, "cwT", [128, 8, 31], F32)
        ccol = kb.sb(es, "ccol", [128, 3, 8], F32)
        halo = kb.sb(es, "halo", [128, 8, 30], BF16)
        with contextlib.ExitStack() as c0s:
            cw_sb = kb.sb(c0s, "cw_sb", [31, 1024], F32)
            dma("sp", cw_sb[:], conv_w, writes=[cw_sb])
            for i_, src in enumerate((conv_b, conv_ln_g, conv_ln_b)):
                dma("sp", ccol[:, i_, :], src.rearrange("(g p) -> p g", p=128), writes=[ccol], allow_slow_non_contiguous=True)
            for g in range(8):
                op("pe", lambda e: e.transpose(out=banks[0].t[:, g * 31:(g + 1) * 31], in_=cw_sb[:31, g * 128:(g + 1) * 128],
                                               identity=ident_f[:31, :31]), reads=[cw_sb, ident_f], writes=[banks[0]])
            op("act", lambda e: e.activation(out=cwT[:], in_=banks[0].t[:, 0:248].rearrange("p (g j) -> p g j", g=8), func=AF.Copy),
               reads=[banks[0]], writes=[cwT])

        Gd_res = Res("Gd")
        kT = kb.sb(es, "kT", [128, 2, T], BF16)
        Vb = kb.sb(es, "Vb", [128, 17, 256], BF16)
        kiT = kb.sb(es, "kiT", [64, T], BF16)
        wiS = kb.sb(es, "wiS", [128, 17, 8], F32)

        tiles_all = [(i * 128, 128) for i in range(16)] + [(SEQ, TS)]
        passes = [tiles_all[:6], tiles_all[6:12], tiles_all[12:]]
        import os
        if os.environ.get("KTILES"):
            passes = [tiles_all[:int(os.environ["KTILES"])]]

        for pi, tiles in enumerate(passes):
            c0 = tiles[0][0]
            Tp = sum(r for _, r in tiles)
            Tpp = sum(r for t0_, r in tiles if t0_ < SEQ)
            has_s = any(t0_ >= SEQ for t0_, _ in tiles)
            last_pass = (tiles[-1][0] + tiles[-1][1] >= SEQ)
            chunks = []
            cc_ = 0
            while cc_ < Tpp:
                n_ = min(512, Tpp - cc_)
                chunks.append((cc_, n_, False))
                cc_ += n_
            if has_s:
                chunks.append((Tpp, TS, True))

            def tiles_of(cl0, n):
                return [i for i, (t0_, r_) in enumerate(tiles) if (t0_ - c0) < cl0 + n and (t0_ - c0 + r_) > cl0]

            with contextlib.ExitStack() as ps:
                AX = kb.sb(ps, "AX%d" % pi, [128, 16, Tp], BF16)
                xnT = AX
                actT = AX
                xnT_t = [AX.sub("AX%d_%d" % (pi, i)) for i in range(len(tiles))]
                actT_t = xnT_t
                pa = contextlib.ExitStack()
                ps_real = ps
                ps = pa
                gluT = kb.sb(ps, "gluT%d" % pi, [128, 8, 30 + Tpp], BF16)
                gluT_g = [gluT.sub("gluT%d_%d" % (pi, g)) for g in range(8)]
                if has_s:
                    gluS = kb.sb(ps, "gluS", [128, 8, NS, 34], BF16)
                    gluS_g = [gluS.sub("gluS_%d" % g) for g in range(8)]
                if last_pass:
                    gl32 = kb.sb(ps, "gl32", [128, 8, 96], F32)
                qT = kb.sb(ps, "qT%d" % pi, [128, 8, Tp], BF16)
                qiT = kb.sb(ps, "qiT%d" % pi, [64, 8, Tp], BF16)
                ps = ps_real
                qT_t = [qT.sub("qT%d_%d" % (pi, i)) for i in range(len(tiles))]
                qiT_t = [qiT.sub("qiT%d_%d" % (pi, i)) for i in range(len(tiles))]
                scope("P1_%d" % pi)
                with contextlib.ExitStack() as p1:
                    xt = [kb.sb(p1, "xt%d" % i, [128, D], F32) for i in range(2)]
                    junk = kb.sb(p1, "junk", [128, D], BF16)
                    xs = [kb.sb(p1, "xs%d" % i, [128, D], BF16) for i in range(2)]
                    st = [kb.sb(p1, "st%d" % i, [128, 4], F32) for i in range(2)]
                    for ti, (t0, rows) in enumerate(tiles):
                        s = ti % 2
                        lc = t0 - c0
                        dma("sp", xt[s][:rows, :], x[t0:t0 + rows, :], writes=[xt[s]])
                        op("act", lambda e: e.activation(out=junk[:rows, :], in_=xt[s][:rows, :], func=AF.Square,
                                                         accum_out=st[s][:rows, 0:1]), reads=[xt[s]], writes=[junk, st[s]])
                        op("dve", lambda e: e.tensor_scalar(out=st[s][:rows, 1:2], in0=st[s][:rows, 0:1], scalar1=1.0 / D,
                                                            scalar2=EPS, op0=ALU.mult, op1=ALU.add), reads=[st[s]], writes=[st[s]])
                        op("act", lambda e: e.activation(out=st[s][:rows, 2:3], in_=st[s][:rows, 1:2], func=AF.Sqrt),
                           reads=[st[s]], writes=[st[s]])
                        op("dve", lambda e: e.reciprocal(out=st[s][:rows, 3:4], in_=st[s][:rows, 2:3]), reads=[st[s]], writes=[st[s]])
                        op("act", lambda e: e.activation(out=xs[s][:rows, :], in_=xt[s][:rows, :], func=AF.Copy,
                                                         scale=st[s][:rows, 3:4]), reads=[xt[s], st[s]], writes=[xs[s]])
                        for half in range(2):
                            bk = banks[(2 * ti + half) % 4]
                            tp = bk.t[:].bitcast(BF16)
                            for k8 in range(8):
                                kc = half * 8 + k8
                                op("pe", lambda e: e.transpose(out=tp[:, k8 * 128:k8 * 128 + rows], in_=xs[s][:rows, kc * 128:(kc + 1) * 128],
                                                               identity=ident_b[:rows, :rows]), reads=[xs[s], ident_b], writes=[bk])
                            op("dve", lambda e: e.tensor_tensor(
                                out=xnT[:, half * 8:half * 8 + 8, lc:lc + rows],
                                in0=tp.rearrange("p (k t) -> p k t", k=8)[:, :, :rows],
                                in1=gcols[:, 0, half * 8:half * 8 + 8].unsqueeze(2).broadcast_to([128, 8, rows]), op=ALU.mult),
                               reads=[bk, gcols], writes=[xnT_t[ti]])

                chk("p1")
                if "xnT" in dbg and pi == 0:
                    dma("pool", dbg["xnT"][:, :, 0:Tp], xnT[:], reads=xnT_t, is_output=True)

                chk("p1d")
                scope("P2_%d" % pi)
                with contextlib.ExitStack() as p2:
                    wblk = [kb.sb(p2, "wblk%d" % i, [128, 16, 512], BF16) for i in range(2)]
                    rp = [kb.sb(p2, "rp%d" % i, [128, 96], F32) for i in range(2)]
                    zb = [kb.sb(p2, "zb%d" % i, [128, 512], BF16) for i in range(2)]
                    z32 = [kb.sb(p2, "z32%d" % i, [128, 512], F32) for i in range(2)]
                    ra = [kb.sb(p2, "ra%d" % i, [128, 256], F32) for i in range(2)]
                    rb = [kb.sb(p2, "rb%d" % i, [128, 256], F32) for i in range(2)]
                    blocks = [("q", 0, 512), ("q", 512, 512), ("kv", 1024, 512), ("qi", 1536, 512), ("kw", 2048, 72)]
                    it = 0
                    for bi, (kind, col0, ncol) in enumerate(blocks):
                        wb = wblk[bi % 2]
                        dma("pool", wb[:, :, :ncol], w_in[:, col0:col0 + ncol].rearrange("(kc p) n -> p kc n", p=128), writes=[wb])
                        for ti, (t0, rows) in enumerate(tiles):
                            lc = t0 - c0
                            tile_id = t0 // 128
                            s = it % 2
                            it += 1
                            zp = banks[4 + s]
                            for kc in range(16):
                                op("pe", lambda e: e.matmul(zp.t[:rows, :ncol], lhsT=xnT[:, kc, lc:lc + rows], rhs=wb[:, kc, :ncol],
                                                            start=(kc == 0), stop=(kc == 15)), reads=[xnT_t[ti], wb], writes=[zp])
                            chk("m")
                            dma("sp", rp[s][:rows, :], rope[t0:t0 + rows, :], writes=[rp[s]])
                            chk("rd")

                            def do_rope(dst, H, Dh, R, tb, col_off=0):
                                half = R // 2
                                zv = zp.t[:rows, col_off:col_off + H * Dh].rearrange("p (h d) -> p h d", h=H)
                                cs = rp[s][:rows, tb:tb + R].unsqueeze(1).broadcast_to([rows, H, R])
                                sn1 = rp[s][:rows, tb + R:tb + R + half].unsqueeze(1).broadcast_to([rows, H, half])
                                sn2 = rp[s][:rows, tb + R + half:tb + 2 * R].unsqueeze(1).broadcast_to([rows, H, half])
                                A = ra[s][:rows, :H * R].rearrange("p (h r) -> p h r", h=H)
                                B = rb[s][:rows, :H * R].rearrange("p (h r) -> p h r", h=H)
                                op("dve", lambda e: e.tensor_tensor(out=A, in0=zv[:, :, 0:R], in1=cs, op=ALU.mult), reads=[zp, rp[s]], writes=[ra[s]])
                                op("dve", lambda e: e.tensor_tensor(out=B[:, :, 0:half], in0=zv[:, :, half:R], in1=sn1, op=ALU.mult), reads=[zp, rp[s]], writes=[rb[s]])
                                op("dve", lambda e: e.tensor_tensor(out=B[:, :, half:R], in0=zv[:, :, 0:half], in1=sn2, op=ALU.mult), reads=[zp, rp[s]], writes=[rb[s]])
                                return A, B

                            if kind == "q":
                                h0 = col0 // 128
                                A, B = do_rope(None, 4, 128, 32, 0)
                                chk("r")
                                op("act", lambda e: e.activation(out=zb[s][:rows, :], in_=zp.t[:rows, :], func=AF.Copy), reads=[zp], writes=[zb[s]])
                                op("dve", lambda e: e.tensor_tensor(out=zb[s][:rows, :].rearrange("p (h d) -> p h d", h=4)[:, :, 0:32], in0=A, in1=B, op=ALU.add),
                                   reads=[ra[s], rb[s]], writes=[zb[s]])
                                chk("z")
                                tb_ = banks[(it % 2)]
                                tpv = tb_.t[:].bitcast(BF16)
                                for h in range(4):
                                    op("pe", lambda e: e.transpose(out=tpv[:, h * 128:h * 128 + rows], in_=zb[s][:rows, h * 128:(h + 1) * 128],
                                                                   identity=ident_b[:rows, :rows]), reads=[zb[s], ident_b], writes=[tb_])
                                chk("t")
                                op("act", lambda e: e.activation(out=qT[:, h0:h0 + 4, lc:lc + rows],
                                                                 in_=tpv[:, 0:512].rearrange("p (h t) -> p h t", h=4)[:, :, :rows], func=AF.Copy),
                                   reads=[tb_], writes=[qT_t[ti]])
                            elif kind == "kv":
                                A, B = do_rope(None, 2, 128, 32, 0)
                                op("act", lambda e: e.activation(out=z32[s][:rows, :], in_=zp.t[:rows, :], func=AF.Copy), reads=[zp], writes=[z32[s]])
                                op("dve", lambda e: e.tensor_tensor(out=z32[s][:rows, 0:256].rearrange("p (h d) -> p h d", h=2)[:, :, 0:32], in0=A, in1=B, op=ALU.add),
                                   reads=[ra[s], rb[s]], writes=[z32[s]])
                                dma("sp", k_out[t0:t0 + rows, :], z32[s][:rows, 0:256], reads=[z32[s]], is_output=True)
                                dma("sp", v_out[t0:t0 + rows, :], z32[s][:rows, 256:512], reads=[z32[s]], is_output=True)
                                op("act", lambda e: e.activation(out=Vb[:rows, tile_id, :], in_=z32[s][:rows, 256:512], func=AF.Copy), reads=[z32[s]], writes=[Vb])
                                op("act", lambda e: e.activation(out=zb[s][:rows, 0:256], in_=z32[s][:rows, 0:256], func=AF.Copy), reads=[z32[s]], writes=[zb[s]])
                                tb_ = banks[(it % 2)]
                                tpv = tb_.t[:].bitcast(BF16)
                                for g in range(2):
                                    op("pe", lambda e: e.transpose(out=tpv[:, g * 128:g * 128 + rows], in_=zb[s][:rows, g * 128:(g + 1) * 128],
                                                                   identity=ident_b[:rows, :rows]), reads=[zb[s], ident_b], writes=[tb_])
                                op("act", lambda e: e.activation(out=kT[:, :, t0:t0 + rows],
                                                                 in_=tpv[:, 0:256].rearrange("p (h t) -> p h t", h=2)[:, :, :rows], func=AF.Copy),
                                   reads=[tb_], writes=[kT])
                            elif kind == "qi":
                                A, B = do_rope(None, 8, 64, 16, 64)
                                op("act", lambda e: e.activation(out=zb[s][:rows, :], in_=zp.t[:rows, :], func=AF.Copy), reads=[zp], writes=[zb[s]])
                                op("dve", lambda e: e.tensor_tensor(out=zb[s][:rows, :].rearrange("p (h d) -> p h d", h=8)[:, :, 0:16], in0=A, in1=B, op=ALU.add),
                                   reads=[ra[s], rb[s]], writes=[zb[s]])
                                tb_ = banks[(it % 2)]
                                tpv = tb_.t[:].bitcast(BF16)
                                for h in range(8):
                                    op("pe", lambda e: e.transpose(out=tpv[0:64, h * 128:h * 128 + rows], in_=zb[s][:rows, h * 64:(h + 1) * 64],
                                                                   identity=ident_b[:rows, :rows]), reads=[zb[s], ident_b], writes=[tb_])
                                op("act", lambda e: e.activation(out=qiT[:, :, lc:lc + rows],
                                                                 in_=tpv[0:64, :].rearrange("p (h t) -> p h t", h=8)[:, :, :rows], func=AF.Copy),
                                   reads=[tb_], writes=[qiT_t[ti]])
                            else:
                                A, B = do_rope(None, 1, 64, 16, 64)
                                op("act", lambda e: e.activation(out=z32[s][:rows, 0:72], in_=zp.t[:rows, 0:72], func=AF.Copy), reads=[zp], writes=[z32[s]])
                                op("dve", lambda e: e.tensor_tensor(out=z32[s][:rows, 0:16], in0=A[:, 0, :], in1=B[:, 0, :], op=ALU.add),
                                   reads=[ra[s], rb[s]], writes=[z32[s]])
                                dma("sp", ki_out[t0:t0 + rows, :], z32[s][:rows, 0:64], reads=[z32[s]], is_output=True)
                                op("dve", lambda e: e.tensor_scalar(out=wiS[:rows, tile_id, :], in0=z32[s][:rows, 64:72], scalar1=float(64 ** -0.5 * 8 ** -0.5),
                                                                    scalar2=None, op0=ALU.mult), reads=[z32[s]], writes=[wiS])
                                op("act", lambda e: e.activation(out=zb[s][:rows, 0:64], in_=z32[s][:rows, 0:64], func=AF.Copy), reads=[z32[s]], writes=[zb[s]])
                                tb_ = banks[(it % 2)]
                                tpv = tb_.t[:].bitcast(BF16)
                                op("pe", lambda e: e.transpose(out=tpv[0:64, 0:rows], in_=zb[s][:rows, 0:64],
                                                               identity=ident_b[:rows, :rows]), reads=[zb[s], ident_b], writes=[tb_])
                                op("act", lambda e: e.activation(out=kiT[:, t0:t0 + rows], in_=tpv[0:64, 0:rows], func=AF.Copy), reads=[tb_], writes=[kiT])

                        chk("b%d" % bi)
                scope("P3a_%d" % pi)
                with contextlib.ExitStack() as p3:
                    wga = [kb.sb(p3, "wga%d" % i, [128, 16, 256], BF16) for i in range(2)]
                    sg = [kb.sb(p3, "sg%d" % i, [128, 512], F32) for i in range(2)]
                    if pi == 0:
                        op("pool", lambda e: e.memset(gluT[:, :, 0:30], 0.0), writes=gluT_g)
                    else:
                        op("dve", lambda e: e.tensor_copy(out=gluT[:, :, 0:30], in_=halo[:]), reads=[halo], writes=gluT_g)
                    if has_s:
                        stt = [kb.sb(p3, "stt%d" % i, [120, 1024], F32) for i in range(2)]
                        for q4 in range(4):
                            st_ = stt[q4 % 2]
                            dma("sp", st_[:, :], state[q4 * 120:(q4 + 1) * 120, :], writes=[st_])
                            for sl in range(4):
                                dma("sp", conv_s[q4 * 4 + sl, 0:26, :], st_[sl * 30 + 4:sl * 30 + 30, :], reads=[st_], is_output=True)
                            for gh in range(2):
                                bk = banks[gh]
                                for g4 in range(4):
                                    g = gh * 4 + g4
                                    op("pe", lambda e: e.transpose(out=bk.t[:, g4 * 128:g4 * 128 + 120], in_=st_[:120, g * 128:(g + 1) * 128],
                                                                   identity=ident_f[:120, :120]), reads=[st_, ident_f], writes=[bk])
                                op("act", lambda e: e.activation(
                                    out=gluS[:, gh * 4:gh * 4 + 4, q4 * 4:q4 * 4 + 4, 0:30],
                                    in_=bk.t[:, :].rearrange("p (g x) -> p g x", g=4)[:, :, 0:120].rearrange("p g (s r) -> p g s r", s=4),
                                    func=AF.Copy), reads=[bk], writes=gluS_g[gh * 4:gh * 4 + 4])
                    it3 = 0
                    for g in range(8):
                        wg = wga[g % 2]
                        ca = OFF_GLU + g * 128
                        dma("pool", wg[:, :, 0:128], w_in[:, ca:ca + 128].rearrange("(kc p) n -> p kc n", p=128), writes=[wg])
                        dma("pool", wg[:, :, 128:256], w_in[:, ca + 1024:ca + 1152].rearrange("(kc p) n -> p kc n", p=128), writes=[wg])
                        for (cl0, n, is_s) in chunks:
                            s3 = it3 % 2
                            it3 += 1
                            bA, bB = banks[4 + s3], banks[6 + s3]
                            rt = [xnT_t[i] for i in tiles_of(cl0, n)]
                            for kc in range(16):
                                op("pe", lambda e: e.matmul(bA.t[:, :n], lhsT=wg[:, kc, 0:128], rhs=xnT[:, kc, cl0:cl0 + n],
                                                            start=(kc == 0), stop=(kc == 15)), reads=rt + [wg], writes=[bA])
                            for kc in range(16):
                                op("pe", lambda e: e.matmul(bB.t[:, :n], lhsT=wg[:, kc, 128:256], rhs=xnT[:, kc, cl0:cl0 + n],
                                                            start=(kc == 0), stop=(kc == 15)), reads=rt + [wg], writes=[bB])
                            op("act", lambda e: e.activation(out=sg[s3][:, :n], in_=bB.t[:, :n], func=AF.Sigmoid), reads=[bB], writes=[sg[s3]])
                            if not is_s:
                                op("dve", lambda e: e.tensor_tensor(out=gluT[:, g, 30 + cl0:30 + cl0 + n], in0=bA.t[:, :n], in1=sg[s3][:, :n], op=ALU.mult),
                                   reads=[bA, sg[s3]], writes=[gluT_g[g]])
                                if last_pass and cl0 + n == Tpp:
                                    op("dve", lambda e: e.tensor_tensor(out=gl32[:, g, 0:32], in0=bA.t[:, n - 32:n], in1=sg[s3][:, n - 32:n], op=ALU.mult),
                                       reads=[bA, sg[s3]], writes=[gl32])
                            else:
                                op("dve", lambda e: e.tensor_tensor(out=gluS[:, g, :, 30:34], in0=bA.t[:, :n].rearrange("p (s i) -> p s i", i=4),
                                                                    in1=sg[s3][:, :n].rearrange("p (s i) -> p s i", i=4), op=ALU.mult),
                                   reads=[bA, sg[s3]], writes=[gluS_g[g]])
                                op("dve", lambda e: e.tensor_tensor(out=gl32[:, g, 32:96], in0=bA.t[:, :n], in1=sg[s3][:, :n], op=ALU.mult),
                                   reads=[bA, sg[s3]], writes=[gl32])
                    if not last_pass:
                        op("dve", lambda e: e.tensor_copy(out=halo[:], in_=gluT[:, :, Tpp:Tpp + 30]), reads=gluT_g, writes=[halo])
                chk("p3a")
                actT_c = [[xnT_t[i] for i in tiles_of(cl0_, n_)] for (cl0_, n_, _s) in chunks]

                scope("P3b_%d" % pi)
                with contextlib.ExitStack() as p3:
                    Dg = [kb.sb(p3, "Dg%d" % i, [128, 31, 128], BF16) for i in range(2)]
                    yb = kb.sb(p3, "yb", [128, 8, 512], F32)
                    yb_g = [yb.sub("yb_%d" % g) for g in range(8)]
                    ysq = [kb.sb(p3, "ysq%d" % i, [128, 512], F32) for i in range(2)]
                    mu = kb.sb(p3, "mu", [128, 512], F32)
                    rs = kb.sb(p3, "rs", [128, 512], F32)
                    tmp = kb.sb(p3, "tmp", [128, 512], F32)
                    it3 = 0
                    for ci, (cl0, n, is_s) in enumerate(chunks):
                        S1, S2 = banks[2], banks[3]
                        for g in range(8):
                            s3 = it3 % 2
                            it3 += 1
                            dg = Dg[s3]
                            op("pool", lambda e: e.tensor_tensor(out=dg[:], in0=ident_b[:].unsqueeze(1).broadcast_to([128, 31, 128]),
                                                                 in1=cwT[:, g, :].unsqueeze(2).broadcast_to([128, 31, 128]), op=ALU.mult),
                               reads=[ident_b, cwT], writes=[dg])
                            bY = banks[s3]
                            for j in range(31):
                                if not is_s:
                                    rhs = gluT[:, g, cl0 + j:cl0 + j + n]
                                    rr = [gluT_g[g]]
                                    outp = bY.t[:, :n]
                                else:
                                    rhs = gluS[:, g, :, j:j + 4]
                                    rr = [gluS_g[g]]
                                    outp = bY.t[:, :n].rearrange("p (s i) -> p s i", i=4)
                                op("pe", lambda e: e.matmul(outp, lhsT=dg[:, j, :], rhs=rhs, start=(j == 0), stop=(j == 30)),
                                   reads=rr + [dg], writes=[bY])
                            op("act", lambda e: e.activation(out=yb[:, g, :n], in_=bY.t[:, :n], func=AF.Identity, bias=ccol[:, 0, g:g + 1]),
                               reads=[bY, ccol], writes=[yb_g[g]])
                            op("act", lambda e: e.activation(out=ysq[s3][:, :n], in_=bY.t[:, :n], func=AF.Square, bias=ccol[:, 0, g:g + 1]),
                               reads=[bY, ccol], writes=[ysq[s3]])
                            op("pe", lambda e: e.matmul(S1.t[:, :n], lhsT=ones_f[:], rhs=yb[:, g, :n], start=(g == 0), stop=(g == 7)),
                               reads=[ones_f, yb_g[g]], writes=[S1])
                            op("pe", lambda e: e.matmul(S2.t[:, :n], lhsT=ones_f[:], rhs=ysq[s3][:, :n], start=(g == 0), stop=(g == 7)),
                               reads=[ones_f, ysq[s3]], writes=[S2])
                        op("dve", lambda e: e.tensor_scalar(out=mu[:, :n], in0=S1.t[:, :n], scalar1=1.0 / 1024, scalar2=None, op0=ALU.mult), reads=[S1], writes=[mu])
                        op("dve", lambda e: e.tensor_tensor(out=tmp[:, :n], in0=mu[:, :n], in1=mu[:, :n], op=ALU.mult), reads=[mu], writes=[tmp])
                        op("dve", lambda e: e.scalar_tensor_tensor(out=tmp[:, :n], in0=S2.t[:, :n], scalar=1.0 / 1024, in1=tmp[:, :n], op0=ALU.mult, op1=ALU.subtract),
                           reads=[S2, tmp], writes=[tmp])
                        op("dve", lambda e: e.tensor_scalar(out=tmp[:, :n], in0=tmp[:, :n], scalar1=EPS, scalar2=None, op0=ALU.add), reads=[tmp], writes=[tmp])
                        op("act", lambda e: e.activation(out=tmp[:, :n], in_=tmp[:, :n], func=AF.Sqrt), reads=[tmp], writes=[tmp])
                        op("dve", lambda e: e.reciprocal(out=rs[:, :n], in_=tmp[:, :n]), reads=[tmp], writes=[rs])
                        for g in range(8):
                            op("dve", lambda e: e.tensor_tensor(out=yb[:, g, :n], in0=yb[:, g, :n], in1=mu[:, :n], op=ALU.subtract), reads=[yb_g[g], mu], writes=[yb_g[g]])
                            op("dve", lambda e: e.tensor_tensor(out=yb[:, g, :n], in0=yb[:, g, :n], in1=rs[:, :n], op=ALU.mult), reads=[yb_g[g], rs], writes=[yb_g[g]])
                            op("act", lambda e: e.activation(out=actT[:, 8 + g, cl0:cl0 + n], in_=yb[:, g, :n], func=AF.Silu,
                                                             scale=ccol[:, 1, g:g + 1], bias=ccol[:, 2, g:g + 1]),
                               reads=[yb_g[g], ccol], writes=actT_c[ci])
                    if last_pass:
                        cst = kb.sb(p3, "cst", [64, 1024], F32)
                        for (c_lo, c_n, which) in ((0, 32, "p"), (32, 64, "s")):
                            if which == "s" and not has_s:
                                continue
                            for gh in range(2):
                                bk = banks[4 + gh]
                                for g4 in range(4):
                                    g = gh * 4 + g4
                                    op("pe", lambda e: e.transpose(out=bk.t[:c_n, g4 * 128:(g4 + 1) * 128], in_=gl32[:, g, c_lo:c_lo + c_n],
                                                                   identity=ident_f[:, :]), reads=[gl32, ident_f], writes=[bk])
                                op("act", lambda e: e.activation(out=cst[:c_n, gh * 512:(gh + 1) * 512], in_=bk.t[:c_n, :], func=AF.Copy), reads=[bk], writes=[cst])
                            if which == "p":
                                dma("sp", conv_p[:, :], cst[2:32, :], reads=[cst], is_output=True)
                            else:
                                for s_ in range(NS):
                                    dma("sp", conv_s[s_, 26:30, :], cst[4 * s_:4 * s_ + 4, :], reads=[cst], is_output=True)
                chk("p3b")
                if "convT" in dbg:
                    for g in range(8):
                        dma("pool", dbg["convT"][:, g, c0:c0 + Tp], actT[:, 8 + g, :], reads=xnT_t, is_output=True)
                scope("P4_%d" % pi)
                with contextlib.ExitStack() as p4:
                    Rh = [kb.sb(p4, "Rh%d" % i, [128, 512], BF16) for i in range(3)]
                    Dw = [kb.sb(p4, "Dw%d" % i, [128, 8, 128], BF16) for i in range(2)]
                    iscA = [kb.sb(p4, "iscA%d" % i, [128, 2048], F32) for i in range(2)]
                    iscW = kb.sb(p4, "iscW", [128, 2048], F32)
                    mx8 = [kb.sb(p4, "mx8_%d" % i, [128, 8], F32) for i in range(2)]
                    maskb = [kb.sb(p4, "maskb%d" % i, [128, 2048], BF16) for i in range(2)]
                    PT = [kb.sb(p4, "PT%d" % i, [128, 512], BF16) for i in range(3)]
                    rsum = [kb.sb(p4, "rsum%d" % i, [128, 512], F32) for i in range(2)]
                    cnt = {"S": 0, "R": 0, "L": 0, "P": 0, "G": 0}
                    ptl = [(ti, t0) for ti, (t0, rows) in enumerate(tiles) if t0 < SEQ]

                    def p4_indexer(ti, t0):
                        j = t0 // 128
                        lc = t0 - c0
                        L = (j + 1) * 128
                        dw, ia = Dw[ti % 2], iscA[ti % 2]
                        op("pool", lambda e: e.tensor_tensor(out=dw[:], in0=ident_b[:].unsqueeze(1).broadcast_to([128, 8, 128]),
                                                             in1=wiS[:, j, :].unsqueeze(2).broadcast_to([128, 8, 128]), op=ALU.mult),
                           reads=[ident_b, wiS], writes=[dw])
                        nch = (L + 511) // 512
                        items = [(c4, h) for c4 in range(nch) for h in range(8)]
                        slots = {}

                        def S_(i):
                            c4, h = items[i]
                            l0 = c4 * 512
                            n = min(512, L - l0)
                            Sb = banks[cnt["S"] % 2]
                            cnt["S"] += 1
                            slots[i] = Sb
                            op("pe", lambda e: e.matmul(Sb.t[:, :n], lhsT=qiT[:, h, lc:lc + 128], rhs=kiT[:, l0:l0 + n], start=True, stop=True),
                               reads=[qiT_t[ti], kiT], writes=[Sb])

                        S_(0)
                        for i, (c4, h) in enumerate(items):
                            if i + 1 < len(items):
                                S_(i + 1)
                            l0 = c4 * 512
                            n = min(512, L - l0)
                            Sb = slots.pop(i)
                            iP = banks[2 + c4 % 2]
                            rh = Rh[cnt["R"] % 3]
                            cnt["R"] += 1
                            op("act", lambda e: e.activation(out=rh[:, :n], in_=Sb.t[:, :n], func=AF.Relu), reads=[Sb], writes=[rh])
                            op("pe", lambda e: e.matmul(iP.t[:, :n], lhsT=dw[:, h, :], rhs=rh[:, :n], start=(h == 0), stop=(h == 7)),
                               reads=[dw, rh], writes=[iP])
                            if h == 7:
                                op("act", lambda e: e.activation(out=ia[:, l0:l0 + n], in_=iP.t[:, :n], func=AF.Copy), reads=[iP], writes=[ia])
                        op("dve", lambda e: e.tensor_tensor(out=ia[:, j * 128:(j + 1) * 128], in0=ia[:, j * 128:(j + 1) * 128], in1=cmask[:], op=ALU.add),
                           reads=[ia, cmask], writes=[ia])
                        if "isc" in dbg and j == int(os.environ.get("KDBGJ", "3")):
                            dma("sp", dbg["isc"][:, 0:L], ia[:, 0:L], reads=[ia], is_output=True)

                    def p4_topk(ti, t0):
                        j = t0 // 128
                        L = (j + 1) * 128
                        ia, mb = iscA[ti % 2], maskb[ti % 2]
                        if j >= 2:
                            src = ia
                            for r in range(32):
                                m8 = mx8[r % 2]
                                op("dve", lambda e: e.max(out=m8[:], in_=src[:, :L]), reads=[src], writes=[m8])
                                if r < 31:
                                    op("dve", lambda e: e.match_replace(out=iscW[:, :L], in_to_replace=m8[:], in_values=src[:, :L], imm_value=NEG),
                                       reads=[src, m8], writes=[iscW])
                                    src = iscW
                            thr_ap, thr_r = m8[:, 7:8], m8
                        else:
                            thr_ap, thr_r = thr0[:, 0:1], thr0
                        op("dve", lambda e: e.tensor_scalar(out=mb[:, :L], in0=ia[:, :L], scalar1=thr_ap, scalar2=NEG, op0=ALU.is_lt, op1=ALU.mult),
                           reads=[ia, thr_r], writes=[mb])

                    def p4_attn(ti, t0):
                        j = t0 // 128
                        lc = t0 - c0
                        mb = maskb[ti % 2]
                        OT, SM = banks[6], banks[7]
                        items = [(g, lb) for g in range(2) for lb in range(j + 1)]
                        slots = {}

                        def LT_(i):
                            g, lb = items[i]
                            LT = banks[4 + cnt["L"] % 2]
                            cnt["L"] += 1
                            slots[i] = LT
                            op("pe", lambda e: e.matmul(LT.t[:, :], lhsT=kT[:, g, lb * 128:(lb + 1) * 128], rhs=qT[:, 4 * g:4 * g + 4, lc:lc + 128],
                                                        start=True, stop=False), reads=[kT, qT_t[ti]], writes=[LT])
                            op("pe", lambda e: e.matmul(LT.t[:, :], lhsT=mb[:, lb * 128:(lb + 1) * 128], rhs=I4[:], start=False, stop=True),
                               reads=[mb, I4], writes=[LT])

                        LT_(0)
                        for i, (g, lb) in enumerate(items):
                            if i + 1 < len(items):
                                LT_(i + 1)
                            LT = slots.pop(i)
                            pt = PT[cnt["P"] % 3]
                            cnt["P"] += 1
                            op("act", lambda e: e.activation(out=pt[:], in_=LT.t[:, :], func=AF.Exp, scale=float(128 ** -0.5)), reads=[LT], writes=[pt])
                            op("pe", lambda e: e.matmul(OT.t[:, :], lhsT=Vb[:, lb, g * 128:(g + 1) * 128], rhs=pt[:], start=(lb == 0), stop=(lb == j)),
                               reads=[Vb, pt], writes=[OT])
                            op("pe", lambda e: e.matmul(SM.t[:, :], lhsT=ones_b[:], rhs=pt[:], start=(lb == 0), stop=(lb == j)),
                               reads=[ones_b, pt], writes=[SM])
                            if lb == j:
                                rsm = rsum[cnt["G"] % 2]
                                cnt["G"] += 1
                                op("dve", lambda e: e.reciprocal(out=rsm[:], in_=SM.t[:, :]), reads=[SM], writes=[rsm])
                                op("dve", lambda e: e.tensor_tensor(out=actT[:, 4 * g:4 * g + 4, lc:lc + 128], in0=OT.t[:, :].rearrange("p (h t) -> p h t", h=4),
                                                                    in1=rsm[:].rearrange("p (h t) -> p h t", h=4), op=ALU.mult),
                                   reads=[OT, rsm], writes=[actT_t[ti]])

                    if ptl:
                        p4_indexer(*ptl[0])
                    for k_, (ti, t0) in enumerate(ptl):
                        if k_ + 1 < len(ptl):
                            p4_indexer(*ptl[k_ + 1])
                        p4_topk(ti, t0)
                        p4_attn(ti, t0)
                scope("P4s_%d" % pi)
                if has_s:
                    lcS = SEQ - c0
                    tiS = len(tiles) - 1
                    with contextlib.ExitStack() as p4s:
                        ptb_ = kb.sb(p4s, "ptb_", [128, 256], I32)
                        ptf = kb.sb(p4s, "ptf", [128, 256], F32)
                        pcol = kb.sb(p4s, "pcol", [128, 1], F32)
                        idxs = kb.sb(p4s, "idxs", [128, 256], U32)
                        cmS = kb.sb(p4s, "cmS", [64, 64], F32)
                        sel32 = kb.sb(p4s, "sel32", [64, 256], F32)
                        selS = kb.sb(p4s, "selS", [64, 256], BF16)
                        dma("sp", ptb_[:], pt_d.partition_broadcast(128), writes=[ptb_])
                        dma("sp", pcol[:], pidx_d, writes=[pcol])
                        dma("sp", cmS[:], cmaskS_d, writes=[cmS])
                        dma("sp", sel32[:], selS_d, writes=[sel32])
                        op("dve", lambda e: e.tensor_copy(out=selS[:], in_=sel32[:]), reads=[sel32], writes=[selS])
                        op("dve", lambda e: e.tensor_copy(out=ptf[:], in_=ptb_[:]), reads=[ptb_], writes=[ptf])
                        op("dve", lambda e: e.tensor_scalar(out=ptf[:], in0=ptf[:], scalar1=128.0, scalar2=pcol[:, 0:1], op0=ALU.mult, op1=ALU.add), reads=[ptf, pcol], writes=[ptf])
                        op("dve", lambda e: e.tensor_copy(out=idxs[:], in_=ptf[:]), reads=[ptf], writes=[idxs])
                        iscS = kb.sb(p4s, "iscS", [64, 2112], F32)
                        iscWs = kb.sb(p4s, "iscWs", [64, 2112], F32)
                        mbS = kb.sb(p4s, "mbS", [64, 2112], BF16)
                        m8s = [kb.sb(p4s, "m8s%d" % i, [64, 8], F32) for i in range(2)]
                        DwS = kb.sb(p4s, "DwS", [64, 8, 64], BF16)
                        op("pool", lambda e: e.tensor_tensor(out=DwS[:], in0=ident_b[0:64, 0:64].unsqueeze(1).broadcast_to([64, 8, 64]),
                                                             in1=wiS[0:64, 16, :].unsqueeze(2).broadcast_to([64, 8, 64]), op=ALU.mult), reads=[ident_b, wiS], writes=[DwS])
                        with contextlib.ExitStack() as pix:
                            qiZ = kb.sb(pix, "qiZ", [64, 8, NS, 64], BF16)
                            op("pool", lambda e: e.memset(qiZ[:], 0.0), writes=[qiZ])
                            for s_ in range(NS):
                                op("act", lambda e: e.activation(out=qiZ[:, :, s_, 4 * s_:4 * s_ + 4], in_=qiT[:, :, lcS + 4 * s_:lcS + 4 * s_ + 4], func=AF.Copy),
                                   reads=[qiT_t[tiS]], writes=[qiZ])
                            kis = [kb.sb(pix, "kis%d" % i, [128, 16, 64], BF16) for i in range(2)]
                            kiTs = [kb.sb(pix, "kiTs%d" % i, [64, 512], BF16) for i in range(2)]
                            RhS = [kb.sb(pix, "RhS%d" % i, [64, 512], BF16) for i in range(3)]
                            for h in range(8):
                                Sb = banks[4 + h % 2]
                                rh = RhS[h % 3]
                                op("pe", lambda e: e.matmul(Sb.t[0:64, 0:64], lhsT=qiT[:, h, lcS:lcS + 64], rhs=kiT[:, SEQ:SEQ + 64], start=True, stop=True),
                                   reads=[qiT_t[tiS], kiT], writes=[Sb])
                                op("act", lambda e: e.activation(out=rh[:, 0:64], in_=Sb.t[0:64, 0:64], func=AF.Relu), reads=[Sb], writes=[rh])
                                op("pe", lambda e: e.matmul(banks[6].t[0:64, 0:64], lhsT=DwS[:, h, :], rhs=rh[:, 0:64], start=(h == 0), stop=(h == 7)),
                                   reads=[DwS, rh], writes=[banks[6]])
                            op("dve", lambda e: e.tensor_tensor(out=iscS[:, 2048:2112], in0=banks[6].t[0:64, 0:64], in1=cmS[:], op=ALU.add), reads=[banks[6], cmS], writes=[iscS])
                            groups = [(s_, c4) for s_ in range(NS) for c4 in range(4)]
                            gk = {}
                            cq = {"q": 0, "S": 0, "R": 0}

                            def prep_(gi):
                                s_, c4 = groups[gi]
                                ks_ = kis[s_ % 2]
                                if c4 == 0:
                                    for pg in range(16):
                                        kb.idma(ks_[:, pg, :], cki, idxs[:, s_ * 16 + pg:s_ * 16 + pg + 1], reads=[idxs], writes=[ks_])
                                tb_ = banks[6 + cq["q"] % 2]
                                kt_ = kiTs[cq["q"] % 2]
                                cq["q"] += 1
                                tpv = tb_.t[:].bitcast(BF16)
                                for p4_ in range(4):
                                    op("pe", lambda e: e.transpose(out=tpv[0:64, p4_ * 128:(p4_ + 1) * 128], in_=ks_[:, c4 * 4 + p4_, :], identity=ident_b[:]),
                                       reads=[ks_, ident_b], writes=[tb_])
                                op("dve", lambda e: e.tensor_copy(out=kt_[:], in_=tpv[0:64, 0:512]), reads=[tb_], writes=[kt_])
                                gk[gi] = kt_

                            prep_(0)
                            for gi, (s_, c4) in enumerate(groups):
                                if gi + 1 < len(groups):
                                    prep_(gi + 1)
                                kt_ = gk.pop(gi)
                                sl = {}

                                def S2_(h):
                                    Sb = banks[4 + cq["S"] % 2]
                                    cq["S"] += 1
                                    sl[h] = Sb
                                    op("pe", lambda e: e.matmul(Sb.t[0:64, :], lhsT=qiZ[:, h, s_, :], rhs=kt_[:], start=True, stop=True), reads=[qiZ, kt_], writes=[Sb])

                                S2_(0)
                                for h in range(8):
                                    if h + 1 < 8:
                                        S2_(h + 1)
                                    Sb = sl.pop(h)
                                    rh = RhS[cq["R"] % 3]
                                    cq["R"] += 1
                                    op("act", lambda e: e.activation(out=rh[:], in_=Sb.t[0:64, :], func=AF.Relu), reads=[Sb], writes=[rh])
                                    op("pe", lambda e: e.matmul(banks[c4].t[0:64, :], lhsT=DwS[:, h, :], rhs=rh[:], start=(s_ == 0 and h == 0), stop=(s_ == NS - 1 and h == 7)),
                                       reads=[DwS, rh], writes=[banks[c4]])
                            for c4 in range(4):
                                op("act", lambda e: e.activation(out=iscS[:, c4 * 512:(c4 + 1) * 512], in_=banks[c4].t[0:64, :], func=AF.Copy), reads=[banks[c4]], writes=[iscS])
                        if "iscS" in dbg:
                            dma("sp", dbg["iscS"], iscS[:], reads=[iscS], is_output=True)
                        src = iscS
                        for r in range(32):
                            m8 = m8s[r % 2]
                            op("dve", lambda e: e.max(out=m8[:], in_=src[:, :]), reads=[src], writes=[m8])
                            if r < 31:
                                op("dve", lambda e: e.match_replace(out=iscWs[:, :], in_to_replace=m8[:], in_values=src[:, :], imm_value=NEG), reads=[src, m8], writes=[iscWs])
                                src = iscWs
                        op("dve", lambda e: e.tensor_scalar(out=mbS[:], in0=iscS[:], scalar1=m8[:, 7:8], scalar2=NEG, op0=ALU.is_lt, op1=ALU.mult), reads=[iscS, m8], writes=[mbS])
                        with contextlib.ExitStack() as pat:
                            Ks = [kb.sb(pat, "Ks%d" % i, [128, 16, 256], BF16) for i in range(2)]
                            Vs = [kb.sb(pat, "Vs%d" % i, [128, 16, 256], BF16) for i in range(2)]
                            kTs = [kb.sb(pat, "kTs%d" % i, [128, 2, 2048], BF16) for i in range(2)]
                            PTs = [kb.sb(pat, "PTs%d" % i, [128, 272], BF16) for i in range(2)]
                            rsS = [kb.sb(pat, "rsS%d" % i, [128, 32], F32) for i in range(2)]
                            ca = {"t": 0, "l": 0}

                            def prepK(s_):
                                K_, V_, kT_ = Ks[s_ % 2], Vs[s_ % 2], kTs[s_ % 2]
                                for pg in range(16):
                                    kb.idma(K_[:, pg, :], ck, idxs[:, s_ * 16 + pg:s_ * 16 + pg + 1], reads=[idxs], writes=[K_])
                                    kb.idma(V_[:, pg, :], cv, idxs[:, s_ * 16 + pg:s_ * 16 + pg + 1], reads=[idxs], writes=[V_])
                                for g in range(2):
                                    for p8 in range(2):
                                        tb_ = banks[ca["t"] % 2]
                                        ca["t"] += 1
                                        tpv = tb_.t[:].bitcast(BF16)
                                        for k8 in range(8):
                                            pg = p8 * 8 + k8
                                            op("pe", lambda e: e.transpose(out=tpv[:, k8 * 128:(k8 + 1) * 128], in_=K_[:, pg, g * 128:(g + 1) * 128], identity=ident_b[:]),
                                               reads=[K_, ident_b], writes=[tb_])
                                        op("act", lambda e: e.activation(out=kT_[:, g, p8 * 1024:(p8 + 1) * 1024], in_=tpv[:, :], func=AF.Copy), reads=[tb_], writes=[kT_])

                            def attnS(s_):
                                K_, V_, kT_ = Ks[s_ % 2], Vs[s_ % 2], kTs[s_ % 2]
                                OT, SM = banks[4], banks[5]
                                sv = selS[:, s_ * 16:(s_ + 1) * 16]
                                lts = []
                                for g in range(2):
                                    LT = banks[2 + g]
                                    qv = qT[:, 4 * g:4 * g + 4, lcS + 4 * s_:lcS + 4 * s_ + 4]
                                    for lb in range(16):
                                        op("pe", lambda e: e.matmul(LT.t[:, lb * 16:(lb + 1) * 16], lhsT=kT_[:, g, lb * 128:(lb + 1) * 128], rhs=qv, start=True, stop=False),
                                           reads=[kT_, qT_t[tiS]], writes=[LT])
                                        op("pe", lambda e: e.matmul(LT.t[:, lb * 16:(lb + 1) * 16], lhsT=mbS[:, lb * 128:(lb + 1) * 128], rhs=sv, start=False, stop=True),
                                           reads=[mbS, selS], writes=[LT])
                                    op("pe", lambda e: e.matmul(LT.t[0:64, 256:272], lhsT=kT[:, g, SEQ:SEQ + 64], rhs=qv, start=True, stop=False), reads=[kT, qT_t[tiS]], writes=[LT])
                                    op("pe", lambda e: e.matmul(LT.t[0:64, 256:272], lhsT=mbS[:, 2048:2112], rhs=sv, start=False, stop=True), reads=[mbS, selS], writes=[LT])
                                for g in range(2):
                                    LT = banks[2 + g]
                                    pt = PTs[g]
                                    op("act", lambda e: e.activation(out=pt[:, 0:256], in_=LT.t[:, 0:256], func=AF.Exp, scale=float(128 ** -0.5)), reads=[LT], writes=[pt])
                                    op("act", lambda e: e.activation(out=pt[0:64, 256:272], in_=LT.t[0:64, 256:272], func=AF.Exp, scale=float(128 ** -0.5)), reads=[LT], writes=[pt])
                                for g in range(2):
                                    pt = PTs[g]
                                    for lb in range(17):
                                        if lb < 16:
                                            lv, pv, on = V_[:, lb, g * 128:(g + 1) * 128], pt[:, lb * 16:(lb + 1) * 16], ones_b[:]
                                        else:
                                            lv, pv, on = Vb[0:64, 16, g * 128:(g + 1) * 128], pt[0:64, 256:272], ones_b[0:64, :]
                                        op("pe", lambda e: e.matmul(OT.t[:, g * 16:(g + 1) * 16], lhsT=lv, rhs=pv, start=(lb == 0), stop=(lb == 16)), reads=[V_, Vb, pt], writes=[OT])
                                        op("pe", lambda e: e.matmul(SM.t[:, g * 16:(g + 1) * 16], lhsT=on, rhs=pv, start=(lb == 0), stop=(lb == 16)), reads=[ones_b, pt], writes=[SM])
                                rs_ = rsS[s_ % 2]
                                op("dve", lambda e: e.reciprocal(out=rs_[:], in_=SM.t[:, 0:32]), reads=[SM], writes=[rs_])
                                op("dve", lambda e: e.tensor_tensor(out=actT[:, 0:8, lcS + 4 * s_:lcS + 4 * s_ + 4], in0=OT.t[:, 0:32].rearrange("p (h i) -> p h i", i=4),
                                                                    in1=rs_[:].rearrange("p (h i) -> p h i", i=4), op=ALU.mult), reads=[OT, rs_], writes=[actT_t[tiS]])

                            prepK(0)
                            for s_ in range(NS):
                                if s_ + 1 < NS:
                                    prepK(s_ + 1)
                                attnS(s_)
                chk("p4")
                if "attnT" in dbg:
                    for h in range(8):
                        dma("pool", dbg["attnT"][:, h, c0:c0 + Tp], actT[:, h, :], reads=actT_t, is_output=True)
                if "qT" in dbg and pi == 0:
                    dma("pool", dbg["qT"][:, :, 0:Tp], qT[:], reads=qT_t, is_output=True)
                if "qiT" in dbg and pi == 0:
                    dma("pool", dbg["qiT"][:, :, 0:Tp], qiT[:], reads=qiT_t, is_output=True)
                pa.close()

                scope("P5_%d" % pi)
                acc = kb.sb(ps, "acc%d" % pi, [128, len(tiles), D], F32)
                acc_t = [acc.sub("acc%d_%d" % (pi, i)) for i in range(len(tiles))]

                def proj_resid(wsrc, nm):
                    with contextlib.ExitStack() as p5:
                        wblk = [kb.sb(p5, "w5_%d" % i, [128, 16, 512], BF16) for i in range(2)]
                        xb = [kb.sb(p5, "xb%d" % i, [128, 512], F32) for i in range(3)]
                        it5 = 0
                        for nb in range(4):
                            wb = wblk[nb % 2]
                            dma("pool", wb[:], wsrc[:, nb * 512:(nb + 1) * 512].rearrange("(kc p) n -> p kc n", p=128), writes=[wb])
                            for ti, (t0, rows) in enumerate(tiles):
                                lc = t0 - c0
                                bk = banks[it5 % 4]
                                xs_ = xb[it5 % 3]
                                it5 += 1
                                for kc in range(16):
                                    op("pe", lambda e: e.matmul(bk.t[:rows, :], lhsT=AX[:, kc, lc:lc + rows], rhs=wb[:, kc, :],
                                                                start=(kc == 0), stop=(kc == 15)), reads=[xnT_t[ti], wb], writes=[bk])
                                dma("sp", xs_[:rows, :], x[t0:t0 + rows, nb * 512:(nb + 1) * 512], writes=[xs_])
                                op("dve", lambda e: e.tensor_tensor(out=acc[:rows, ti, nb * 512:(nb + 1) * 512], in0=bk.t[:rows, :], in1=xs_[:rows, :], op=ALU.add),
                                   reads=[bk, xs_], writes=[acc_t[ti]])

                proj_resid(w_out, "wout")
                chk("p5")

                def norm_to_AX(which):
                    with contextlib.ExitStack() as pn:
                        junk = kb.sb(pn, "njunk", [128, D], BF16)
                        xs = [kb.sb(pn, "nxs%d" % i, [128, D], BF16) for i in range(2)]
                        st = [kb.sb(pn, "nst%d" % i, [128, 4], F32) for i in range(2)]
                        for ti, (t0, rows) in enumerate(tiles):
                            s_ = ti % 2
                            lc = t0 - c0
                            op("act", lambda e: e.activation(out=junk[:rows, :], in_=acc[:rows, ti, :], func=AF.Square,
                                                             accum_out=st[s_][:rows, 0:1]), reads=[acc_t[ti]], writes=[junk, st[s_]])
                            op("dve", lambda e: e.tensor_scalar(out=st[s_][:rows, 1:2], in0=st[s_][:rows, 0:1], scalar1=1.0 / D,
                                                                scalar2=EPS, op0=ALU.mult, op1=ALU.add), reads=[st[s_]], writes=[st[s_]])
                            op("act", lambda e: e.activation(out=st[s_][:rows, 2:3], in_=st[s_][:rows, 1:2], func=AF.Sqrt), reads=[st[s_]], writes=[st[s_]])
                            op("dve", lambda e: e.reciprocal(out=st[s_][:rows, 3:4], in_=st[s_][:rows, 2:3]), reads=[st[s_]], writes=[st[s_]])
                            op("act", lambda e: e.activation(out=xs[s_][:rows, :], in_=acc[:rows, ti, :], func=AF.Copy,
                                                             scale=st[s_][:rows, 3:4]), reads=[acc_t[ti], st[s_]], writes=[xs[s_]])
                            for half in range(2):
                                bk = banks[(2 * ti + half) % 4]
                                tp = bk.t[:].bitcast(BF16)
                                for k8 in range(8):
                                    kc = half * 8 + k8
                                    op("pe", lambda e: e.transpose(out=tp[:, k8 * 128:k8 * 128 + rows], in_=xs[s_][:rows, kc * 128:(kc + 1) * 128],
                                                                   identity=ident_b[:rows, :rows]), reads=[xs[s_], ident_b], writes=[bk])
                                op("dve", lambda e: e.tensor_tensor(
                                    out=AX[:, half * 8:half * 8 + 8, lc:lc + rows],
                                    in0=tp.rearrange("p (k t) -> p k t", k=8)[:, :, :rows],
                                    in1=gcols[:, which, half * 8:half * 8 + 8].unsqueeze(2).broadcast_to([128, 8, rows]), op=ALU.mult),
                                   reads=[bk, gcols], writes=[xnT_t[ti]])

                scope("P6n_%d" % pi)
                norm_to_AX(1)
                nt = len(tiles)
                with contextlib.ExitStack() as p6:
                    scope("P6a_%d" % pi)
                    v16 = kb.sb(p6, "v16", [128, nt, 16, 16], F32)
                    I16 = kb.sb(p6, "I16", [128, nt, 16, 16], U32)
                    v16_t = [v16.sub("v16_%d" % i) for i in range(nt)]
                    with contextlib.ExitStack() as p6a:
                        wqb = [kb.sb(p6a, "wqb%d" % i, [128, 16, 512], BF16) for i in range(2)]
                        qhb = [kb.sb(p6a, "qhb%d" % i, [128, Tp], BF16) for i in range(2)]
                        stmp = [kb.sb(p6a, "stmp%d" % i, [128, 128], F32) for i in range(2)]
                        it6 = 0
                        for b4 in range(4):
                            wb = wqb[b4 % 2]
                            dma("pool", wb[:], peer_wq[:, b4 * 512:(b4 + 1) * 512].rearrange("(kc p) n -> p kc n", p=128), writes=[wb])
                            for bl in range(4):
                                blk = b4 * 4 + bl
                                qb = qhb[blk % 2]
                                for (cl0, n, is_s) in chunks:
                                    bk = banks[it6 % 2]
                                    it6 += 1
                                    rt = [xnT_t[i] for i in tiles_of(cl0, n)]
                                    for kc in range(16):
                                        op("pe", lambda e: e.matmul(bk.t[:, :n], lhsT=wb[:, kc, bl * 128:(bl + 1) * 128], rhs=AX[:, kc, cl0:cl0 + n],
                                                                    start=(kc == 0), stop=(kc == 15)), reads=rt + [wb], writes=[bk])
                                    op("act", lambda e: e.activation(out=qb[:, cl0:cl0 + n], in_=bk.t[:, :n], func=AF.Copy), reads=[bk], writes=[qb])
                                for ti, (t0, rows) in enumerate(tiles):
                                    lc = t0 - c0
                                    sP = banks[2 + (it6 % 2)]
                                    it6 += 1
                                    sm = stmp[it6 % 2]
                                    op("pe", lambda e: e.matmul(sP.t[:rows, 0:128], lhsT=qb[:, lc:lc + rows], rhs=skT[:, blk, :], start=True, stop=True),
                                       reads=[qb, skT], writes=[sP])
                                    op("dve", lambda e: e.max(out=v16[:rows, ti, blk, 0:8], in_=sP.t[:rows, 0:128]), reads=[sP], writes=[v16_t[ti]])
                                    op("dve", lambda e: e.max_index(out=I16[:rows, ti, blk, 0:8], in_max=v16[:rows, ti, blk, 0:8], in_values=sP.t[:rows, 0:128]),
                                       reads=[sP, v16_t[ti]], writes=[v16_t[ti]])
                                    op("dve", lambda e: e.match_replace(out=sm[:rows, :], in_to_replace=v16[:rows, ti, blk, 0:8], in_values=sP.t[:rows, 0:128], imm_value=NEG),
                                       reads=[sP, v16_t[ti]], writes=[sm])
                                    op("dve", lambda e: e.max(out=v16[:rows, ti, blk, 8:16], in_=sm[:rows, :]), reads=[sm], writes=[v16_t[ti]])
                                    op("dve", lambda e: e.max_index(out=I16[:rows, ti, blk, 8:16], in_max=v16[:rows, ti, blk, 8:16], in_values=sm[:rows, :]),
                                       reads=[sm, v16_t[ti]], writes=[v16_t[ti]])
                    chk("p6a")
                    scope("P6b_%d" % pi)
                    with contextlib.ExitStack() as p6b:
                        I16f = kb.sb(p6b, "I16f", [128, 16, 16], F32)
                        cand = kb.sb(p6b, "cand", [128, 8, 256], F32)
                        ctmp = kb.sb(p6b, "ctmp", [128, 256], F32)
                        c16 = kb.sb(p6b, "c16", [128, 8, 16], F32)
                        P16 = kb.sb(p6b, "P16", [128, 8, 16], U32)
                        ab_u = kb.sb(p6b, "ab_u", [128, 2, 8, 16], U32)
                        ab_f = kb.sb(p6b, "ab_f", [128, 2, 8, 16], F32)
                        eq = kb.sb(p6b, "eq", [128, 8, 16, 16], F32)
                        sel3 = kb.sb(p6b, "sel3", [128, 3, 128], F32)
                        zst = kb.sb(p6b, "zst", [128, 8, 2], F32)
                        selT = [kb.sb(p6b, "selT%d" % i, [128, 3, 128], BF16) for i in range(2)]
                        OH2 = [kb.sb(p6b, "OH2_%d" % i, [128, 16, 128], BF16) for i in range(2)]
                        OH1 = [kb.sb(p6b, "OH1_%d" % i, [128, 16, 128], BF16) for i in range(2)]
                        GS = kb.sb(p6b, "GS", [128, 128, 128], BF16)
                        itb = itg = 0
                        for ti, (t0, rows) in enumerate(tiles):
                            vv = v16[:rows, ti].rearrange("p (h n) a -> p h n a", n=2)
                            op("dve", lambda e: e.tensor_copy(out=I16f[:rows], in_=I16[:rows, ti]), reads=[v16_t[ti]], writes=[I16f])
                            op("dve", lambda e: e.tensor_tensor(out=cand[:rows].rearrange("p h (a b) -> p h a b", a=16),
                                                                in0=vv[:, :, 0, :].unsqueeze(3).broadcast_to([rows, 8, 16, 16]),
                                                                in1=vv[:, :, 1, :].unsqueeze(2).broadcast_to([rows, 8, 16, 16]), op=ALU.add),
                               reads=[v16_t[ti]], writes=[cand])
                            for h in range(8):
                                op("dve", lambda e: e.max(out=c16[:rows, h, 0:8], in_=cand[:rows, h, :]), reads=[cand], writes=[c16])
                                op("dve", lambda e: e.max_index(out=P16[:rows, h, 0:8], in_max=c16[:rows, h, 0:8], in_values=cand[:rows, h, :]), reads=[cand, c16], writes=[P16])
                                op("dve", lambda e: e.match_replace(out=ctmp[:rows, :], in_to_replace=c16[:rows, h, 0:8], in_values=cand[:rows, h, :], imm_value=NEG),
                                   reads=[cand, c16], writes=[ctmp])
                                op("dve", lambda e: e.max(out=c16[:rows, h, 8:16], in_=ctmp[:rows, :]), reads=[ctmp], writes=[c16])
                                op("dve", lambda e: e.max_index(out=P16[:rows, h, 8:16], in_max=c16[:rows, h, 8:16], in_values=ctmp[:rows, :]), reads=[ctmp, c16], writes=[P16])
                            op("dve", lambda e: e.tensor_scalar(out=ab_u[:rows, 0], in0=P16[:rows], scalar1=4, scalar2=None, op0=ALU.logical_shift_right), reads=[P16], writes=[ab_u])
                            op("dve", lambda e: e.tensor_scalar(out=ab_u[:rows, 1], in0=P16[:rows], scalar1=15, scalar2=None, op0=ALU.bitwise_and), reads=[P16], writes=[ab_u])
                            op("dve", lambda e: e.tensor_copy(out=ab_f[:rows], in_=ab_u[:rows]), reads=[ab_u], writes=[ab_f])
                            If = I16f[:rows].rearrange("p (h n) a -> p h n a", n=2)
                            for w_ in range(2):
                                op("dve", lambda e: e.tensor_tensor(out=eq[:rows], in0=ab_f[:rows, w_].unsqueeze(3).broadcast_to([rows, 8, 16, 16]),
                                                                    in1=iota16[:rows, :].unsqueeze(1).unsqueeze(1).broadcast_to([rows, 8, 16, 16]), op=ALU.is_equal),
                                   reads=[ab_f, iota16], writes=[eq])
                                op("dve", lambda e: e.tensor_tensor(out=eq[:rows], in0=eq[:rows], in1=If[:, :, w_, :].unsqueeze(2).broadcast_to([rows, 8, 16, 16]), op=ALU.mult),
                                   reads=[eq, I16f], writes=[eq])
                                op("dve", lambda e: e.tensor_reduce(out=sel3[:rows, w_, :].rearrange("p (h j) -> p h j", h=8), in_=eq[:rows], axis=AX_.X, op=ALU.add),
                                   reads=[eq], writes=[sel3])
                            wv = sel3[:rows, 2, :].rearrange("p (h j) -> p h j", h=8)
                            op("dve", lambda e: e.tensor_tensor(out=wv, in0=c16[:rows], in1=c16[:rows, :, 0:1].broadcast_to([rows, 8, 16]), op=ALU.subtract),
                               reads=[c16], writes=[sel3])
                            op("act", lambda e: e.activation(out=wv, in_=wv, func=AF.Exp), reads=[sel3], writes=[sel3])
                            op("dve", lambda e: e.tensor_reduce(out=zst[:rows, :, 0], in_=wv, axis=AX_.X, op=ALU.add), reads=[sel3], writes=[zst])
                            op("dve", lambda e: e.reciprocal(out=zst[:rows, :, 1], in_=zst[:rows, :, 0]), reads=[zst], writes=[zst])
                            op("dve", lambda e: e.tensor_tensor(out=wv, in0=wv, in1=zst[:rows, :, 1:2].broadcast_to([rows, 8, 16]), op=ALU.mult),
                               reads=[sel3, zst], writes=[sel3])
                            if "sel3" in dbg and t0 == 0:
                                dma("sp", dbg["sel3"], sel3[:], reads=[sel3], is_output=True)
                            sT = selT[ti % 2]
                            bkT = banks[4]
                            for w_ in range(3):
                                op("pe", lambda e: e.transpose(out=bkT.t[:, w_ * 128:w_ * 128 + rows], in_=sel3[:rows, w_, :], identity=ident_f[:rows, :rows]),
                                   reads=[sel3, ident_f], writes=[bkT])
                            op("act", lambda e: e.activation(out=sT[:, :, :rows], in_=bkT.t[:, 0:384].rearrange("p (w t) -> p w t", w=3)[:, :, :rows], func=AF.Copy),
                               reads=[bkT], writes=[sT])
                            for sub in range((rows + 15) // 16):
                                tl0 = sub * 16
                                o2, o1 = OH2[itb % 2], OH1[itb % 2]
                                itb += 1
                                io_b = iotaC[:].unsqueeze(1).broadcast_to([128, 16, 128])
                                op("dve", lambda e: e.tensor_tensor(out=o2[:], in0=io_b, in1=sT[:, 1, tl0:tl0 + 16].unsqueeze(2).broadcast_to([128, 16, 128]), op=ALU.is_equal),
                                   reads=[iotaC, sT], writes=[o2])
                                op("dve", lambda e: e.tensor_tensor(out=o1[:], in0=io_b, in1=sT[:, 0, tl0:tl0 + 16].unsqueeze(2).broadcast_to([128, 16, 128]), op=ALU.is_equal),
                                   reads=[iotaC, sT], writes=[o1])
                                op("pool", lambda e: e.tensor_tensor(out=o1[:], in0=o1[:], in1=sT[:, 2, tl0:tl0 + 16].unsqueeze(2).broadcast_to([128, 16, 128]), op=ALU.mult),
                                   reads=[o1, sT], writes=[o1])
                                for q4 in range(4):
                                    gb = banks[5 + itg % 3]
                                    itg += 1
                                    for t4 in range(4):
                                        tl = q4 * 4 + t4
                                        op("pe", lambda e: e.matmul(gb.t[:, t4 * 128:(t4 + 1) * 128], lhsT=o2[:, tl, :], rhs=o1[:, tl, :], start=True, stop=True),
                                           reads=[o2, o1], writes=[gb])
                                    tg = tl0 + q4 * 4
                                    op("act", lambda e: e.activation(out=GS[:, :, tg:tg + 4], in_=gb.t[:, :].rearrange("p (t c) -> p c t", t=4), func=AF.Copy),
                                       reads=[gb], writes=[GS])
                            for c8 in range(8):
                                dma("sp", Gd[c8 * 16:(c8 + 1) * 16, :, t0:t0 + rows].rearrange("c i t -> i c t"), GS[:, c8 * 16:(c8 + 1) * 16, :rows],
                                    reads=[GS], writes=[Gd_res], allow_slow_non_contiguous=False)
                    chk("p6b")
                    if "G" in dbg and pi == 0:
                        with contextlib.ExitStack() as pd:
                            gtmp = kb.sb(pd, "gtmp", [128, 128, 128], BF16)
                            dma("sp", gtmp[:], Gd[:, :, 0:128].rearrange("c i t -> i c t"), reads=[Gd_res], writes=[gtmp])
                            for c8 in range(8):
                                dma("pool", dbg["G"][:, c8 * 16:(c8 + 1) * 16, :], gtmp[:, c8 * 16:(c8 + 1) * 16, :], reads=[gtmp], is_output=True)
                    chk("p6c")
                    scope("P6d_%d" % pi)
                    with contextlib.ExitStack() as p6d:
                        ES = 4
                        ub = [kb.sb(p6d, "ub%d" % i, [128, D], BF16) for i in range(2)]
                        uT = [kb.sb(p6d, "uT%d" % i, [128, 16, 128], BF16) for i in range(2)]
                        gt = [kb.sb(p6d, "gt%d" % i, [128, Tp], BF16) for i in range(2)]
                        gl = [kb.sb(p6d, "gl%d" % i, [128, 512], BF16) for i in range(2)]
                        actS = [kb.sb(p6d, "actS%d" % i, [128, ES, Tp], BF16) for i in range(2)]
                        vS = [kb.sb(p6d, "vS%d" % i, [128, ES, D], BF16) for i in range(2)]
                        ith = itv = 0
                        nsc = 128 // ES
                        if os.environ.get("KNSC"):
                            nsc = int(os.environ["KNSC"])
                        cu = {"h": 0, "v": 0}
                        prepped = {}

                        def prepU(c):
                            sc, ec = divmod(c, ES)
                            vs = vS[sc % 2]
                            u_, uT_, g_ = ub[c % 2], uT[c % 2], gt[c % 2]
                            dma("pool", u_[:], peer_u[c * 128:(c + 1) * 128, :], writes=[u_])
                            dma("pool", vs[:, ec, :], peer_v[c * 128:(c + 1) * 128, :], writes=[vs])
                            dma("sp", g_[:, :], Gd[c, :, c0:c0 + Tp], reads=[Gd_res], writes=[g_])
                            for half in range(2):
                                bk = banks[half]
                                tp = bk.t[:].bitcast(BF16)
                                for k8 in range(8):
                                    kc = half * 8 + k8
                                    op("pe", lambda e: e.transpose(out=tp[:, k8 * 128:(k8 + 1) * 128], in_=u_[:, kc * 128:(kc + 1) * 128], identity=ident_b[:]),
                                       reads=[u_, ident_b], writes=[bk])
                                op("act", lambda e: e.activation(out=uT_[:, half * 8:half * 8 + 8, :], in_=tp.rearrange("p (k e) -> p k e", k=8), func=AF.Copy),
                                   reads=[bk], writes=[uT_])

                        def uside(c):
                            sc, ec = divmod(c, ES)
                            aS = actS[sc % 2]
                            uT_, g_ = uT[c % 2], gt[c % 2]
                            for (cl0, n, is_s) in chunks:
                                hp = banks[2 + cu["h"] % 2]
                                gl_ = gl[cu["h"] % 2]
                                cu["h"] += 1
                                rt = [xnT_t[i] for i in tiles_of(cl0, n)]
                                for kc in range(16):
                                    op("pe", lambda e: e.matmul(hp.t[:, :n], lhsT=uT_[:, kc, :], rhs=AX[:, kc, cl0:cl0 + n], start=(kc == 0), stop=(kc == 15)),
                                       reads=rt + [uT_], writes=[hp])
                                op("act", lambda e: e.activation(out=gl_[:, :n], in_=hp.t[:, :n], func=AF.Gelu), reads=[hp], writes=[gl_])
                                op("pool", lambda e: e.tensor_tensor(out=aS[:, ec, cl0:cl0 + n], in0=gl_[:, :n], in1=g_[:, cl0:cl0 + n], op=ALU.mult),
                                   reads=[gl_, g_], writes=[aS])

                        def vside(sc):
                            aS, vs = actS[sc % 2], vS[sc % 2]
                            for ti, (t0, rows) in enumerate(tiles):
                                lc = t0 - c0
                                for dq4 in range(4):
                                    bk = banks[4 + cu["v"] % 4]
                                    cu["v"] += 1
                                    d0 = dq4 * 512
                                    for ec in range(ES):
                                        op("pe", lambda e: e.matmul(bk.t[:rows, :], lhsT=aS[:, ec, lc:lc + rows], rhs=vs[:, ec, d0:d0 + 512], start=(ec == 0), stop=(ec == ES - 1)),
                                           reads=[aS, vs], writes=[bk])
                                    op("dve", lambda e: e.tensor_tensor(out=acc[:rows, ti, d0:d0 + 512], in0=bk.t[:rows, :], in1=acc[:rows, ti, d0:d0 + 512], op=ALU.add),
                                       reads=[bk, acc_t[ti]], writes=[acc_t[ti]])

                        nchunk = nsc * ES
                        prepU(0)
                        for c in range(nchunk):
                            if c + 1 < nchunk:
                                prepU(c + 1)
                            uside(c)
                            if (c + 1) % ES == 0:
                                vside(c // ES)
                chk("p6")
                if "h3" in dbg:
                    for ti, (t0, rows) in enumerate(tiles):
                        dma("sp", dbg["h3"][t0:t0 + rows, :], acc[:rows, ti, :], reads=[acc_t[ti]], is_output=True)

                scope("P7_%d" % pi)
                norm_to_AX(2)
                with contextlib.ExitStack() as p7:
                    pT = kb.sb(p7, "pT", [128, 2, Tp], BF16)
                    pt32 = [kb.sb(p7, "pt32_%d" % i, [128, 256], F32) for i in range(2)]
                    ptb = [kb.sb(p7, "ptb%d" % i, [128, 256], BF16) for i in range(2)]
                    for ti, (t0, rows) in enumerate(tiles):
                        lc = t0 - c0
                        s_ = ti % 2
                        dma("sp", pt32[s_][:rows, :], pvec[t0:t0 + rows, :], writes=[pt32[s_]])
                        op("act", lambda e: e.activation(out=ptb[s_][:rows, :], in_=pt32[s_][:rows, :], func=AF.Copy), reads=[pt32[s_]], writes=[ptb[s_]])
                        bk = banks[s_]
                        tp = bk.t[:].bitcast(BF16)
                        for k2 in range(2):
                            op("pe", lambda e: e.transpose(out=tp[:, k2 * 128:k2 * 128 + rows], in_=ptb[s_][:rows, k2 * 128:(k2 + 1) * 128], identity=ident_b[:rows, :rows]),
                               reads=[ptb[s_], ident_b], writes=[bk])
                        op("act", lambda e: e.activation(out=pT[:, :, lc:lc + rows], in_=tp[:, 0:256].rearrange("p (k t) -> p k t", k=2)[:, :, :rows], func=AF.Copy),
                           reads=[bk], writes=[pT])
                    wgb = [kb.sb(p7, "wgb%d" % i, [128, 16, 512], BF16) for i in range(2)]
                    wpb = [kb.sb(p7, "wpb%d" % i, [128, 2, 512], BF16) for i in range(2)]
                    gsb = [kb.sb(p7, "gsb%d" % i, [128, 512], F32) for i in range(2)]
                    it7 = 0
                    for nb in range(4):
                        wg_, wp_ = wgb[nb % 2], wpb[nb % 2]
                        dma("pool", wg_[:], ple_gate[:, nb * 512:(nb + 1) * 512].rearrange("(kc p) n -> p kc n", p=128), writes=[wg_])
                        dma("pool", wp_[:], ple_proj[:, nb * 512:(nb + 1) * 512].rearrange("(kc p) n -> p kc n", p=128), writes=[wp_])
                        for ti, (t0, rows) in enumerate(tiles):
                            lc = t0 - c0
                            bG, bP = banks[2 + it7 % 2], banks[4 + it7 % 2]
                            gs_ = gsb[it7 % 2]
                            it7 += 1
                            for kc in range(16):
                                op("pe", lambda e: e.matmul(bG.t[:rows, :], lhsT=AX[:, kc, lc:lc + rows], rhs=wg_[:, kc, :], start=(kc == 0), stop=(kc == 15)),
                                   reads=[xnT_t[ti], wg_], writes=[bG])
                            for k2 in range(2):
                                op("pe", lambda e: e.matmul(bP.t[:rows, :], lhsT=pT[:, k2, lc:lc + rows], rhs=wp_[:, k2, :], start=(k2 == 0), stop=(k2 == 1)),
                                   reads=[pT, wp_], writes=[bP])
                            op("act", lambda e: e.activation(out=gs_[:rows, :], in_=bG.t[:rows, :], func=AF.Sigmoid), reads=[bG], writes=[gs_])
                            op("dve", lambda e: e.tensor_tensor(out=gs_[:rows, :], in0=gs_[:rows, :], in1=bP.t[:rows, :], op=ALU.mult), reads=[gs_, bP], writes=[gs_])
                            op("dve", lambda e: e.tensor_tensor(out=acc[:rows, ti, nb * 512:(nb + 1) * 512], in0=acc[:rows, ti, nb * 512:(nb + 1) * 512], in1=gs_[:rows, :], op=ALU.add),
                               reads=[gs_, acc_t[ti]], writes=[acc_t[ti]])
                chk("p7")
                if "h4" in dbg:
                    for ti, (t0, rows) in enumerate(tiles):
                        dma("sp", dbg["h4"][t0:t0 + rows, :], acc[:rows, ti, :], reads=[acc_t[ti]], is_output=True)
                scope("PF_%d" % pi)
                with contextlib.ExitStack() as pf:
                    junk = kb.sb(pf, "fjunk", [128, D], BF16)
                    yt = [kb.sb(pf, "yt%d" % i, [128, D], F32) for i in range(2)]
                    st = [kb.sb(pf, "fst%d" % i, [128, 4], F32) for i in range(2)]
                    for ti, (t0, rows) in enumerate(tiles):
                        s_ = ti % 2
                        op("act", lambda e: e.activation(out=junk[:rows, :], in_=acc[:rows, ti, :], func=AF.Square, accum_out=st[s_][:rows, 0:1]),
                           reads=[acc_t[ti]], writes=[junk, st[s_]])
                        op("dve", lambda e: e.tensor_scalar(out=st[s_][:rows, 1:2], in0=st[s_][:rows, 0:1], scalar1=1.0 / D, scalar2=EPS, op0=ALU.mult, op1=ALU.add),
                           reads=[st[s_]], writes=[st[s_]])
                        op("act", lambda e: e.activation(out=st[s_][:rows, 2:3], in_=st[s_][:rows, 1:2], func=AF.Sqrt), reads=[st[s_]], writes=[st[s_]])
                        op("dve", lambda e: e.reciprocal(out=st[s_][:rows, 3:4], in_=st[s_][:rows, 2:3]), reads=[st[s_]], writes=[st[s_]])
                        op("dve", lambda e: e.scalar_tensor_tensor(out=yt[s_][:rows, :], in0=acc[:rows, ti, :], scalar=st[s_][:rows, 3:4], in1=fgb[:rows, :], op0=ALU.mult, op1=ALU.mult),
                           reads=[acc_t[ti], st[s_], fgb], writes=[yt[s_]])
                        dma("sp", y_out[t0:t0 + rows, :], yt[s_][:rows, :], reads=[yt[s_]], is_output=True)

                if "h2" in dbg:
                    for ti, (t0, rows) in enumerate(tiles):
                        dma("sp", dbg["h2"][t0:t0 + rows, :], acc[:rows, ti, :], reads=[acc_t[ti]], is_output=True)
        scope(None)
        kb.finish()


def make_in_maps(inputs):
    cst = consts()
    maps = []
    for c in range(NC):
        m = dict(cst)
        m["x"] = np.ascontiguousarray(np.concatenate(
            [inputs["x_prompt"][c], inputs["x_sample"][NS * c:NS * (c + 1)].reshape(TS, D)], 0))
        m["w_in"] = np.ascontiguousarray(inputs["w_in"][0])
        m["norms"] = np.ascontiguousarray(np.stack([inputs["attn_norm"][0], inputs["ffn_norm"][0], inputs["ple_norm"][0], inputs["final_norm"]]))
        m["peer_wq"] = np.ascontiguousarray(inputs["peer_wq"][0])
        m["peer_sk"] = np.ascontiguousarray(inputs["peer_subkeys"][0].reshape(16 * 128, 128))
        m["peer_u"] = np.ascontiguousarray(inputs["peer_u"][0])
        m["peer_v"] = np.ascontiguousarray(inputs["peer_v"][0])
        m["ple_gate"] = np.ascontiguousarray(inputs["ple_gate"][0])
        m["ple_proj"] = np.ascontiguousarray(inputs["ple_proj"][0])
        m["ck"] = inputs["cache_k"][0].reshape(2560 * 128, 256)
        m["cv"] = inputs["cache_v"][0].reshape(2560 * 128, 256)
        m["cki"] = inputs["cache_kidx"][0].reshape(2560 * 128, 64)
        m["pt"] = np.ascontiguousarray(inputs["page_table"][NS * c:NS * (c + 1)].reshape(-1).astype(np.int32))
        m["pvec"] = np.ascontiguousarray(np.concatenate([inputs["p_prompt"][0, c], inputs["p_sample"][0, NS * c:NS * (c + 1)].reshape(TS, 256)], 0))
        for k_ in ("conv_w", "conv_b", "conv_ln_g", "conv_ln_b", "w_out"):
            m[k_] = np.ascontiguousarray(inputs[k_][0])
        m["state"] = np.ascontiguousarray(inputs["state_conv"][0, NS * c:NS * (c + 1)].reshape(NS * 30, 1024))
        maps.append(m)
    return maps


def kernel(**inputs):
    inputs = {k: np.asarray(v) for k, v in inputs.items()}
    nc = build()
    maps = make_in_maps(inputs)
    res = run_bass_kernel_spmd(nc, maps, core_ids=list(range(NC)))
    R = res.results
    B = 8
    k_p = np.stack([R[c]["k_out"][:SEQ].reshape(SEQ, 2, 128) for c in range(NC)])[None]
    v_p = np.stack([R[c]["v_out"][:SEQ].reshape(SEQ, 2, 128) for c in range(NC)])[None]
    ki_p = np.stack([R[c]["ki_out"][:SEQ] for c in range(NC)])[None]
    k_s = np.concatenate([R[c]["k_out"][SEQ:].reshape(NS, 4, 2, 128) for c in range(NC)])[None]
    v_s = np.concatenate([R[c]["v_out"][SEQ:].reshape(NS, 4, 2, 128) for c in range(NC)])[None]
    ki_s = np.concatenate([R[c]["ki_out"][SEQ:].reshape(NS, 4, 64) for c in range(NC)])[None]
    y_p = np.stack([R[c]["y"][:SEQ] for c in range(NC)])
    y_s = np.concatenate([R[c]["y"][SEQ:].reshape(NS, 4, D) for c in range(NC)])
    cp = np.stack([R[c]["conv_p"] for c in range(NC)])[None]
    cs = np.concatenate([R[c]["conv_s"] for c in range(NC)])[None]
    return (y_p, y_s, k_p, v_p, ki_p, cp, k_s, v_s, ki_s, cs)
```

```python
import contextlib
import numpy as np
import concourse.bass as bass
import concourse.mybir as mybir
from concourse.bass_utils import run_bass_kernel_spmd

F32 = mybir.dt.float32
BF16 = mybir.dt.bfloat16
I32 = mybir.dt.int32
U32 = mybir.dt.uint32
AF = mybir.ActivationFunctionType
ALU = mybir.AluOpType
AX_ = mybir.AxisListType

D = 2048
NC = 8
SEQ = 2048
NS = 16
TS = 64
T = SEQ + TS
N_IN = 4168
OFF_K, OFF_V, OFF_QI, OFF_KI, OFF_WI, OFF_GLU = 1024, 1280, 1536, 2048, 2112, 2120
EPS = 1e-6
NEG = -1.0e30


class Res:
    __slots__ = ("name", "w", "r", "excl")

    def __init__(self, name, excl=False):
        self.name = name
        self.w = None
        self.r = {}
        self.excl = excl


class Buf:
    def __init__(self, t, name):
        self.t = t
        self.res = Res(name)
        self.subs = []

    def sub(self, name, excl=False):
        r = Res(name, excl)
        self.subs.append(r)
        return r

    def __getitem__(self, idx):
        return self.t[idx]


class KB:
    def __init__(self, nc, es):
        self.nc = nc
        self.es = es
        self.E = {"pe": nc.tensor, "act": nc.scalar, "dve": nc.vector, "pool": nc.gpsimd, "sp": nc.sync}
        self.esem = {k: es.enter_context(nc.semaphore("sem_" + k)) for k in self.E}
        self.ecnt = {k: 0 for k in self.E}
        self.seen = {k: {} for k in self.E}
        self.dsem = {}
        self.dcnt = {}
        self.semname = {}
        self.out_tags = []
        self.nsem = 5

    def sb(self, es, name, shape, dtype):
        self.nalloc = getattr(self, "nalloc", 0) + 1
        b = Buf(es.enter_context(self.nc.sbuf_tensor("s%d_%s" % (self.nalloc, name), list(shape), dtype)), name)
        es.callback(self.release, b)
        return b

    def release(self, buf):
        tags = []
        for res in [buf.res] + buf.subs:
            tags += list(res.r.values()) + ([res.w] if res.w is not None else [])
        for eng in self.E:
            for tg in tags:
                self._wait(eng, tg)
        for res in [buf.res] + buf.subs:
            for kind in ("w", "r"):
                for qc in ("sw", "hw"):
                    s_ = self.dsem.pop((id(res), kind, qc), None)
                    if s_ is not None:
                        if not hasattr(self, "free_sems"):
                            self.free_sems = {"sw": [], "hw": []}
                        self.free_sems[qc].append(s_)

    def _wait(self, eng, dep):
        sem, val = dep
        key = id(sem)
        if eng == "pe" and sem is self.esem["pe"]:
            return
        if self.seen[eng].get(key, 0) >= val:
            return
        self.E[eng].wait_ge(sem, val)
        self.seen[eng][key] = val

    def _deps(self, eng, reads, writes, skip_waw_sem=None):
        for r in reads:
            if r.w is not None:
                self._wait(eng, r.w)
            if r.excl:
                for d in r.r.values():
                    if d[0] is not self.esem.get(eng):
                        self._wait(eng, d)
        for w in writes:
            if w.w is not None and not (skip_waw_sem is not None and w.w[0] is skip_waw_sem):
                self._wait(eng, w.w)
            for d in w.r.values():
                self._wait(eng, d)

    @staticmethod
    def _res(xs):
        return [x.res if isinstance(x, Buf) else x for x in xs]

    def op(self, eng, fn, reads=(), writes=()):
        reads = self._res(reads)
        writes = self._res(writes)
        self._deps(eng, reads, writes)
        ins = fn(self.E[eng])
        self.ecnt[eng] += 1
        ins.then_inc(self.esem[eng], 1)
        tag = (self.esem[eng], self.ecnt[eng])
        for r in reads:
            r.r[id(tag[0])] = tag
        for w in writes:
            w.w = tag
            w.r = {}
        return ins

    def _dma_sem(self, res, kind, q="sp"):
        qc = "sw" if q == "pool" else "hw"
        key = (id(res), kind, qc)
        if key not in self.dsem:
            if not hasattr(self, "free_sems"):
                self.free_sems = {"sw": [], "hw": []}
            free = self.free_sems[qc]
            if free:
                s = free.pop()
            else:
                self.nsem += 1
                s = self.es.enter_context(self.nc.semaphore("d%d" % self.nsem))
                self.dcnt[id(s)] = 0
            self.dsem[key] = s
        return self.dsem[key]

    def dma(self, q, out, in_, reads=(), writes=(), is_output=False, **kw):
        reads = self._res(reads)
        writes = self._res(writes)
        if writes:
            sem = self._dma_sem(writes[0], "w", q)
        else:
            sem = self._dma_sem(reads[0], "r", q)
        self._deps(q, reads, writes, skip_waw_sem=sem)
        if q == "pool":
            kw.setdefault("max_dma_last_dim", 2048)
        ins = self.E[q].dma_start(out=out, in_=in_, **kw)
        self.dcnt[id(sem)] += 16
        ins.then_inc(sem, 16)
        tag = (sem, self.dcnt[id(sem)])
        for r in reads:
            r.r[id(sem)] = tag
        for w in writes:
            w.w = tag
            w.r = {}
        if is_output:
            self.out_tags.append(tag)
        return ins

    def idma(self, out, in_, idx_ap, reads=(), writes=()):
        reads = self._res(reads)
        writes = self._res(writes)
        sem = self._dma_sem(writes[0], "w", "pool")
        self._deps("pool", reads, writes, skip_waw_sem=sem)
        ins = self.nc.gpsimd.indirect_dma_start(out=out, out_offset=None, in_=in_,
                                                in_offset=bass.IndirectOffsetOnAxis(ap=idx_ap, axis=0))
        self.dcnt[id(sem)] += 16
        ins.then_inc(sem, 16)
        tag = (sem, self.dcnt[id(sem)])
        for r in reads:
            r.r[id(sem)] = tag
        for w in writes:
            w.w = tag
            w.r = {}
        return ins

    def finish(self):
        last = {}
        for sem, val in self.out_tags:
            k = id(sem)
            if k not in last or last[k][1] < val:
                last[k] = (sem, val)
        for tag in last.values():
            self._wait("sp", tag)
        for e in ("pe", "act", "dve", "pool"):
            if self.ecnt[e]:
                self._wait("sp", (self.esem[e], self.ecnt[e]))


def rope_tables():
    pos = np.concatenate([np.arange(SEQ, dtype=np.float32), np.tile(2048.0 + np.arange(4, dtype=np.float32), NS)])
    out = np.zeros((T, 96), np.float32)
    for rot, off in ((32, 0), (16, 64)):
        half = rot // 2
        inv = (np.float32(500000.0) ** (-np.arange(half, dtype=np.float32) * np.float32(2.0) / np.float32(rot))).astype(np.float32)
        ang = pos[:, None] * inv[None, :]
        c, s = np.cos(ang), np.sin(ang)
        out[:, off:off + rot] = np.concatenate([c, c], 1)
        out[:, off + rot:off + 2 * rot] = np.concatenate([-s, s], 1)
    return out


def consts():
    cm = np.where(np.arange(128)[None, :] <= np.arange(128)[:, None], 0.0, NEG).astype(np.float32)
    r_ = np.arange(64)
    cms = np.where((r_[:, None] // 4 == r_[None, :] // 4) & (r_[None, :] % 4 <= r_[:, None] % 4), 0.0, NEG).astype(np.float32)
    sel = np.zeros((64, 16, 4, 4), np.float32)
    for s_ in range(16):
        for i_ in range(4):
            sel[4 * s_ + i_, s_, :, i_] = 1.0
    return {"ident_f": np.eye(128, dtype=np.float32), "cmask": cm, "rope": rope_tables(), "cmaskS": cms, "selS": sel.reshape(64, 256),
            "pidx": np.arange(128, dtype=np.float32).reshape(128, 1),
            "iota128": np.tile(np.arange(128, dtype=np.float32)[None, :], (128, 1))}


class _Stop(Exception):
    pass


def build(stage=99, debug=()):
    nc = bass.Bass("TRN2", target_bir_lowering=False)
    try:
        _build(nc, stage, debug)
    except _Stop:
        pass
    return nc


def _build(nc, stage, debug):
    import os

    _cur = [None]

    def scope(name):
        if os.environ.get("KSCOPE"):
            if _cur[0] is not None:
                nc.leave_named_scope(_cur[0][0], _cur[0][1], False)
            _cur[0] = None
            if name is not None:
                sid, _ = nc.enter_named_scope(name, False)
                _cur[0] = (name, sid)

    def chk(tag):
        if os.environ.get("KSTOP") == tag:
            scope(None)
            kb.finish()
            raise _Stop()

    def din(name, shape, dt=F32):
        return nc.dram_tensor(name, list(shape), dt, kind="ExternalInput").ap()

    def dout(name, shape, dt=F32):
        return nc.dram_tensor(name, list(shape), dt, kind="ExternalOutput").ap()

    x = din("x", [T, D])
    w_in = din("w_in", [D, N_IN])
    rope = din("rope", [T, 96])
    ident_f_d = din("ident_f", [128, 128])
    cmask_d = din("cmask", [128, 128])
    k_out = dout("k_out", [T, 256])
    v_out = dout("v_out", [T, 256])
    ki_out = dout("ki_out", [T, 64])
    w_out = din("w_out", [D, D])
    norms = din("norms", [4, D])
    peer_wq = din("peer_wq", [D, D])
    peer_sk = din("peer_sk", [16 * 128, 128])
    peer_u = din("peer_u", [16384, D])
    peer_v = din("peer_v", [16384, D])
    ple_gate = din("ple_gate", [D, D])
    ple_proj = din("ple_proj", [256, D])
    pvec = din("pvec", [T, 256])
    iota_d = din("iota128", [128, 128])
    ck = din("ck", [2560 * 128, 256])
    cv = din("cv", [2560 * 128, 256])
    cki = din("cki", [2560 * 128, 64])
    pt_d = din("pt", [NS * 16], I32)
    cmaskS_d = din("cmaskS", [64, 64])
    selS_d = din("selS", [64, 256])
    pidx_d = din("pidx", [128, 1])
    y_out = dout("y", [T, D])
    Gd = nc.dram_tensor("Gd", [128, 128, T], BF16, kind="Internal").ap()
    uTd = nc.dram_tensor("uTd", [128, 128, 16, 128], BF16, kind="Internal").ap()
    vd = nc.dram_tensor("vd", [128, 128, D], BF16, kind="Internal").ap()
    conv_w = din("conv_w", [31, 1024])
    conv_b = din("conv_b", [1024])
    conv_ln_g = din("conv_ln_g", [1024])
    conv_ln_b = din("conv_ln_b", [1024])
    state = din("state", [NS * 30, 1024])
    conv_p = dout("conv_p", [30, 1024])
    conv_s = dout("conv_s", [NS, 30, 1024])
    dbg = {n: dout("dbg_" + n, shp) for n, shp in debug}

    with contextlib.ExitStack() as es:
        kb = KB(nc, es)
        op, dma = kb.op, kb.dma
        banks = [Buf(es.enter_context(nc.psum_tensor("bank%d" % i, [128, 512], F32)), "bank%d" % i) for i in range(8)]
        for b_ in banks:
            b_.res.excl = True

        ident_f = kb.sb(es, "ident_f", [128, 128], F32)
        ident_b = kb.sb(es, "ident_b", [128, 128], BF16)
        cmask = kb.sb(es, "cmask", [128, 128], F32)
        gcols = kb.sb(es, "gcols", [128, 4, 16], F32)
        gcol = gcols[:, 0, :]
        gcol_r = gcols
        fgb = kb.sb(es, "fgb", [128, D], F32)
        iotaC = kb.sb(es, "iotaC", [128, 128], BF16)
        iota16 = kb.sb(es, "iota16", [128, 16], F32)
        skT = kb.sb(es, "skT", [128, 16, 128], BF16)
        dma("sp", ident_f[:], ident_f_d, writes=[ident_f])
        dma("sp", cmask[:], cmask_d, writes=[cmask])
        for w_ in range(4):
            dma("sp", gcols[:, w_, :], norms[w_].rearrange("(kc p) -> p kc", p=128), writes=[gcols], allow_slow_non_contiguous=True)
        dma("sp", fgb[:], norms[3].partition_broadcast(128), writes=[fgb])
        op("dve", lambda e: e.tensor_copy(out=ident_b[:], in_=ident_f[:]), reads=[ident_f], writes=[ident_b])
        with contextlib.ExitStack() as c1s:
            io32 = kb.sb(c1s, "io32", [128, 128], F32)
            dma("sp", io32[:], iota_d, writes=[io32])
            op("dve", lambda e: e.tensor_copy(out=iotaC[:], in_=io32[:]), reads=[io32], writes=[iotaC])
            op("dve", lambda e: e.tensor_copy(out=iota16[:], in_=io32[:, 0:16]), reads=[io32], writes=[iota16])
            sk32 = kb.sb(c1s, "sk32", [128, 16, 128], F32)
            dma("sp", sk32[:], peer_sk.rearrange("(b k) d -> k b d", k=128), writes=[sk32])
            for q4 in range(4):
                for b4 in range(4):
                    op("pe", lambda e: e.transpose(out=banks[q4].t[:, b4 * 128:(b4 + 1) * 128], in_=sk32[:, q4 * 4 + b4, :], identity=ident_f[:]),
                       reads=[sk32, ident_f], writes=[banks[q4]])
                op("act", lambda e: e.activation(out=skT[:, q4 * 4:q4 * 4 + 4, :], in_=banks[q4].t[:, :].rearrange("p (b k) -> p b k", b=4), func=AF.Copy),
                   reads=[banks[q4]], writes=[skT])
        chk("p0")
        ones_f = kb.sb(es, "ones_f", [128, 128], F32)
        ones_b = kb.sb(es, "ones_b", [128, 128], BF16)
        I4 = kb.sb(es, "I4", [128, 4, 128], BF16)
        thr0 = kb.sb(es, "thr0", [128, 1], F32)
        op("pool", lambda e: e.memset(ones_b[:], 1.0), writes=[ones_b])
        op("pool", lambda e: e.memset(thr0[:], -1.0e29), writes=[thr0])
        op("dve", lambda e: e.tensor_copy(out=I4[:], in_=ident_f[:].unsqueeze(1).broadcast_to([128, 4, 128])), reads=[ident_f], writes=[I4])
        op("pool", lambda e: e.memset(ones_f[:], 1.0), writes=[ones_f])
        cwT = kb.sb(es, "cwT", [128, 8, 31], F32)
        ccol = kb.sb(es, "ccol", [128, 3, 8], F32)
        halo = kb.sb(es, "halo", [128, 8, 30], BF16)
        with contextlib.ExitStack() as c0s:
            cw_sb = kb.sb(c0s, "cw_sb", [31, 1024], F32)
            dma("sp", cw_sb[:], conv_w, writes=[cw_sb])
            for i_, src in enumerate((conv_b, conv_ln_g, conv_ln_b)):
                dma("sp", ccol[:, i_, :], src.rearrange("(g p) -> p g", p=128), writes=[ccol], allow_slow_non_contiguous=True)
            for g in range(8):
                op("pe", lambda e: e.transpose(out=banks[0].t[:, g * 31:(g + 1) * 31], in_=cw_sb[:31, g * 128:(g + 1) * 128],
                                               identity=ident_f[:31, :31]), reads=[cw_sb, ident_f], writes=[banks[0]])
            op("act", lambda e: e.activation(out=cwT[:], in_=banks[0].t[:, 0:248].rearrange("p (g j) -> p g j", g=8), func=AF.Copy),
               reads=[banks[0]], writes=[cwT])

        Gd_res = Res("Gd")
        uTd_res = Res("uTd")
        vd_res = Res("vd")
        kT = kb.sb(es, "kT", [128, 2, T], BF16)
        Vb = kb.sb(es, "Vb", [128, 17, 256], BF16)
        kiT = kb.sb(es, "kiT", [64, T], BF16)
        wiS = kb.sb(es, "wiS", [128, 17, 8], F32)

        tiles_all = [(i * 128, 128) for i in range(16)] + [(SEQ, TS)]
        passes = [tiles_all[:6], tiles_all[6:12], tiles_all[12:]]
        import os
        if os.environ.get("KPASS"):
            szs = [int(v_) for v_ in os.environ["KPASS"].split(",")]
            passes, o_ = [], 0
            for z_ in szs:
                passes.append(tiles_all[o_:o_ + z_])
                o_ += z_
        if os.environ.get("KSEL"):
            passes = [[tiles_all[int(v_)] for v_ in grp.split(",")] for grp in os.environ["KSEL"].split(";")]
        if os.environ.get("KTILES"):
            passes = [tiles_all[:int(os.environ["KTILES"])]]

        for pi, tiles in enumerate(passes):
            c0 = tiles[0][0]
            Tp = sum(r for _, r in tiles)
            Tpp = sum(r for t0_, r in tiles if t0_ < SEQ)
            has_s = any(t0_ >= SEQ for t0_, _ in tiles)
            last_pass = (tiles[-1][0] + tiles[-1][1] >= SEQ)
            chunks = []
            cc_ = 0
            while cc_ < Tpp:
                n_ = min(512, Tpp - cc_)
                chunks.append((cc_, n_, False))
                cc_ += n_
            if has_s:
                chunks.append((Tpp, TS, True))

            def tiles_of(cl0, n):
                return [i for i, (t0_, r_) in enumerate(tiles) if (t0_ - c0) < cl0 + n and (t0_ - c0 + r_) > cl0]

            with contextlib.ExitStack() as ps:
                AX = kb.sb(ps, "AX%d" % pi, [128, 16, Tp], BF16)
                xnT = AX
                actT = AX
                xnT_t = [AX.sub("AX%d_%d" % (pi, i)) for i in range(len(tiles))]
                actT_t = xnT_t
                pa = contextlib.ExitStack()
                ps_real = ps
                ps = pa
                gluT = kb.sb(ps, "gluT%d" % pi, [128, 8, 30 + Tpp], BF16)
                gluT_g = [gluT.sub("gluT%d_%d" % (pi, g)) for g in range(8)]
                if has_s:
                    gluS = kb.sb(ps, "gluS", [128, 8, NS, 34], BF16)
                    gluS_g = [gluS.sub("gluS_%d" % g) for g in range(8)]
                if last_pass:
                    gl32 = kb.sb(ps, "gl32", [128, 8, 96], F32)
                qT = kb.sb(ps, "qT%d" % pi, [128, 8, Tp], BF16)
                qiT = kb.sb(ps, "qiT%d" % pi, [64, 8, Tp], BF16)
                ps = ps_real
                qT_t = [qT.sub("qT%d_%d" % (pi, i)) for i in range(len(tiles))]
                qiT_t = [qiT.sub("qiT%d_%d" % (pi, i)) for i in range(len(tiles))]
                scope("P1_%d" % pi)
                with contextlib.ExitStack() as p1:
                    xt = [kb.sb(p1, "xt%d" % i, [128, D], F32) for i in range(2)]
                    junk = kb.sb(p1, "junk", [128, D], BF16)
                    xs = [kb.sb(p1, "xs%d" % i, [128, D], BF16) for i in range(2)]
                    st = [kb.sb(p1, "st%d" % i, [128, 4], F32) for i in range(2)]
                    for ti, (t0, rows) in enumerate(tiles):
                        s = ti % 2
                        lc = t0 - c0
                        dma("sp", xt[s][:rows, :], x[t0:t0 + rows, :], writes=[xt[s]])
                        op("act", lambda e: e.activation(out=junk[:rows, :], in_=xt[s][:rows, :], func=AF.Square,
                                                         accum_out=st[s][:rows, 0:1]), reads=[xt[s]], writes=[junk, st[s]])
                        op("dve", lambda e: e.tensor_scalar(out=st[s][:rows, 1:2], in0=st[s][:rows, 0:1], scalar1=1.0 / D,
                                                            scalar2=EPS, op0=ALU.mult, op1=ALU.add), reads=[st[s]], writes=[st[s]])
                        op("act", lambda e: e.activation(out=st[s][:rows, 2:3], in_=st[s][:rows, 1:2], func=AF.Sqrt),
                           reads=[st[s]], writes=[st[s]])
                        op("dve", lambda e: e.reciprocal(out=st[s][:rows, 3:4], in_=st[s][:rows, 2:3]), reads=[st[s]], writes=[st[s]])
                        op("act", lambda e: e.activation(out=xs[s][:rows, :], in_=xt[s][:rows, :], func=AF.Copy,
                                                         scale=st[s][:rows, 3:4]), reads=[xt[s], st[s]], writes=[xs[s]])
                        for half in range(2):
                            bk = banks[(2 * ti + half) % 4]
                            tp = bk.t[:].bitcast(BF16)
                            for k8 in range(8):
                                kc = half * 8 + k8
                                op("pe", lambda e: e.transpose(out=tp[:, k8 * 128:k8 * 128 + rows], in_=xs[s][:rows, kc * 128:(kc + 1) * 128],
                                                               identity=ident_b[:rows, :rows]), reads=[xs[s], ident_b], writes=[bk])
                            op("dve", lambda e: e.tensor_tensor(
                                out=xnT[:, half * 8:half * 8 + 8, lc:lc + rows],
                                in0=tp.rearrange("p (k t) -> p k t", k=8)[:, :, :rows],
                                in1=gcols[:, 0, half * 8:half * 8 + 8].unsqueeze(2).broadcast_to([128, 8, rows]), op=ALU.mult),
                               reads=[bk, gcols], writes=[xnT_t[ti]])

                chk("p1")
                if "xnT" in dbg and pi == 0:
                    dma("pool", dbg["xnT"][:, :, 0:Tp], xnT[:], reads=xnT_t, is_output=True)

                chk("p1d")
                scope("P2_%d" % pi)
                with contextlib.ExitStack() as p2:
                    wblk = [kb.sb(p2, "wblk%d" % i, [128, 16, 512], BF16) for i in range(2)]
                    rp = [kb.sb(p2, "rp%d" % i, [128, 96], F32) for i in range(2)]
                    zb = [kb.sb(p2, "zb%d" % i, [128, 512], BF16) for i in range(2)]
                    z32 = [kb.sb(p2, "z32%d" % i, [128, 512], F32) for i in range(2)]
                    ra = [kb.sb(p2, "ra%d" % i, [128, 256], F32) for i in range(2)]
                    rb = [kb.sb(p2, "rb%d" % i, [128, 256], F32) for i in range(2)]
                    blocks = [("q", 0, 512), ("q", 512, 512), ("kv", 1024, 512), ("qi", 1536, 512), ("kw", 2048, 72)]
                    it = 0
                    for bi, (kind, col0, ncol) in enumerate(blocks):
                        wb = wblk[bi % 2]
                        dma("pool", wb[:, :, :ncol], w_in[:, col0:col0 + ncol].rearrange("(kc p) n -> p kc n", p=128), writes=[wb])
                        for ti, (t0, rows) in enumerate(tiles):
                            lc = t0 - c0
                            tile_id = t0 // 128
                            s = it % 2
                            it += 1
                            zp = banks[4 + s]
                            for kc in range(16):
                                op("pe", lambda e: e.matmul(zp.t[:rows, :ncol], lhsT=xnT[:, kc, lc:lc + rows], rhs=wb[:, kc, :ncol],
                                                            start=(kc == 0), stop=(kc == 15)), reads=[xnT_t[ti], wb], writes=[zp])
                            chk("m")
                            dma("sp", rp[s][:rows, :], rope[t0:t0 + rows, :], writes=[rp[s]])
                            chk("rd")

                            def do_rope(dst, H, Dh, R, tb, col_off=0):
                                half = R // 2
                                zv = zp.t[:rows, col_off:col_off + H * Dh].rearrange("p (h d) -> p h d", h=H)
                                cs = rp[s][:rows, tb:tb + R].unsqueeze(1).broadcast_to([rows, H, R])
                                sn1 = rp[s][:rows, tb + R:tb + R + half].unsqueeze(1).broadcast_to([rows, H, half])
                                sn2 = rp[s][:rows, tb + R + half:tb + 2 * R].unsqueeze(1).broadcast_to([rows, H, half])
                                A = ra[s][:rows, :H * R].rearrange("p (h r) -> p h r", h=H)
                                B = rb[s][:rows, :H * R].rearrange("p (h r) -> p h r", h=H)
                                op("dve", lambda e: e.tensor_tensor(out=A, in0=zv[:, :, 0:R], in1=cs, op=ALU.mult), reads=[zp, rp[s]], writes=[ra[s]])
                                op("dve", lambda e: e.tensor_tensor(out=B[:, :, 0:half], in0=zv[:, :, half:R], in1=sn1, op=ALU.mult), reads=[zp, rp[s]], writes=[rb[s]])
                                op("dve", lambda e: e.tensor_tensor(out=B[:, :, half:R], in0=zv[:, :, 0:half], in1=sn2, op=ALU.mult), reads=[zp, rp[s]], writes=[rb[s]])
                                return A, B

                            if kind == "q":
                                h0 = col0 // 128
                                A, B = do_rope(None, 4, 128, 32, 0)
                                chk("r")
                                op("act", lambda e: e.activation(out=zb[s][:rows, :], in_=zp.t[:rows, :], func=AF.Copy), reads=[zp], writes=[zb[s]])
                                op("dve", lambda e: e.tensor_tensor(out=zb[s][:rows, :].rearrange("p (h d) -> p h d", h=4)[:, :, 0:32], in0=A, in1=B, op=ALU.add),
                                   reads=[ra[s], rb[s]], writes=[zb[s]])
                                chk("z")
                                tb_ = banks[(it % 2)]
                                tpv = tb_.t[:].bitcast(BF16)
                                for h in range(4):
                                    op("pe", lambda e: e.transpose(out=tpv[:, h * 128:h * 128 + rows], in_=zb[s][:rows, h * 128:(h + 1) * 128],
                                                                   identity=ident_b[:rows, :rows]), reads=[zb[s], ident_b], writes=[tb_])
                                chk("t")
                                op("act", lambda e: e.activation(out=qT[:, h0:h0 + 4, lc:lc + rows],
                                                                 in_=tpv[:, 0:512].rearrange("p (h t) -> p h t", h=4)[:, :, :rows], func=AF.Copy),
                                   reads=[tb_], writes=[qT_t[ti]])
                            elif kind == "kv":
                                A, B = do_rope(None, 2, 128, 32, 0)
                                op("act", lambda e: e.activation(out=z32[s][:rows, :], in_=zp.t[:rows, :], func=AF.Copy), reads=[zp], writes=[z32[s]])
                                op("dve", lambda e: e.tensor_tensor(out=z32[s][:rows, 0:256].rearrange("p (h d) -> p h d", h=2)[:, :, 0:32], in0=A, in1=B, op=ALU.add),
                                   reads=[ra[s], rb[s]], writes=[z32[s]])
                                dma("sp", k_out[t0:t0 + rows, :], z32[s][:rows, 0:256], reads=[z32[s]], is_output=True)
                                dma("sp", v_out[t0:t0 + rows, :], z32[s][:rows, 256:512], reads=[z32[s]], is_output=True)
                                op("act", lambda e: e.activation(out=Vb[:rows, tile_id, :], in_=z32[s][:rows, 256:512], func=AF.Copy), reads=[z32[s]], writes=[Vb])
                                op("act", lambda e: e.activation(out=zb[s][:rows, 0:256], in_=z32[s][:rows, 0:256], func=AF.Copy), reads=[z32[s]], writes=[zb[s]])
                                tb_ = banks[(it % 2)]
                                tpv = tb_.t[:].bitcast(BF16)
                                for g in range(2):
                                    op("pe", lambda e: e.transpose(out=tpv[:, g * 128:g * 128 + rows], in_=zb[s][:rows, g * 128:(g + 1) * 128],
                                                                   identity=ident_b[:rows, :rows]), reads=[zb[s], ident_b], writes=[tb_])
                                op("act", lambda e: e.activation(out=kT[:, :, t0:t0 + rows],
                                                                 in_=tpv[:, 0:256].rearrange("p (h t) -> p h t", h=2)[:, :, :rows], func=AF.Copy),
                                   reads=[tb_], writes=[kT])
                            elif kind == "qi":
                                A, B = do_rope(None, 8, 64, 16, 64)
                                op("act", lambda e: e.activation(out=zb[s][:rows, :], in_=zp.t[:rows, :], func=AF.Copy), reads=[zp], writes=[zb[s]])
                                op("dve", lambda e: e.tensor_tensor(out=zb[s][:rows, :].rearrange("p (h d) -> p h d", h=8)[:, :, 0:16], in0=A, in1=B, op=ALU.add),
                                   reads=[ra[s], rb[s]], writes=[zb[s]])
                                tb_ = banks[(it % 2)]
                                tpv = tb_.t[:].bitcast(BF16)
                                for h in range(8):
                                    op("pe", lambda e: e.transpose(out=tpv[0:64, h * 128:h * 128 + rows], in_=zb[s][:rows, h * 64:(h + 1) * 64],
                                                                   identity=ident_b[:rows, :rows]), reads=[zb[s], ident_b], writes=[tb_])
                                op("act", lambda e: e.activation(out=qiT[:, :, lc:lc + rows],
                                                                 in_=tpv[0:64, :].rearrange("p (h t) -> p h t", h=8)[:, :, :rows], func=AF.Copy),
                                   reads=[tb_], writes=[qiT_t[ti]])
                            else:
                                A, B = do_rope(None, 1, 64, 16, 64)
                                op("act", lambda e: e.activation(out=z32[s][:rows, 0:72], in_=zp.t[:rows, 0:72], func=AF.Copy), reads=[zp], writes=[z32[s]])
                                op("dve", lambda e: e.tensor_tensor(out=z32[s][:rows, 0:16], in0=A[:, 0, :], in1=B[:, 0, :], op=ALU.add),
                                   reads=[ra[s], rb[s]], writes=[z32[s]])
                                dma("sp", ki_out[t0:t0 + rows, :], z32[s][:rows, 0:64], reads=[z32[s]], is_output=True)
                                op("dve", lambda e: e.tensor_scalar(out=wiS[:rows, tile_id, :], in0=z32[s][:rows, 64:72], scalar1=float(64 ** -0.5 * 8 ** -0.5),
                                                                    scalar2=None, op0=ALU.mult), reads=[z32[s]], writes=[wiS])
                                op("act", lambda e: e.activation(out=zb[s][:rows, 0:64], in_=z32[s][:rows, 0:64], func=AF.Copy), reads=[z32[s]], writes=[zb[s]])
                                tb_ = banks[(it % 2)]
                                tpv = tb_.t[:].bitcast(BF16)
                                op("pe", lambda e: e.transpose(out=tpv[0:64, 0:rows], in_=zb[s][:rows, 0:64],
                                                               identity=ident_b[:rows, :rows]), reads=[zb[s], ident_b], writes=[tb_])
                                op("act", lambda e: e.activation(out=kiT[:, t0:t0 + rows], in_=tpv[0:64, 0:rows], func=AF.Copy), reads=[tb_], writes=[kiT])

                        chk("b%d" % bi)
                scope("P3a_%d" % pi)
                with contextlib.ExitStack() as p3:
                    wga = [kb.sb(p3, "wga%d" % i, [128, 16, 256], BF16) for i in range(2)]
                    sg = [kb.sb(p3, "sg%d" % i, [128, 512], F32) for i in range(2)]
                    if pi == 0:
                        op("pool", lambda e: e.memset(gluT[:, :, 0:30], 0.0), writes=gluT_g)
                    else:
                        op("dve", lambda e: e.tensor_copy(out=gluT[:, :, 0:30], in_=halo[:]), reads=[halo], writes=gluT_g)
                    if has_s:
                        stt = [kb.sb(p3, "stt%d" % i, [120, 1024], F32) for i in range(2)]
                        for q4 in range(4):
                            st_ = stt[q4 % 2]
                            dma("sp", st_[:, :], state[q4 * 120:(q4 + 1) * 120, :], writes=[st_])
                            for sl in range(4):
                                dma("sp", conv_s[q4 * 4 + sl, 0:26, :], st_[sl * 30 + 4:sl * 30 + 30, :], reads=[st_], is_output=True)
                            for gh in range(2):
                                bk = banks[gh]
                                for g4 in range(4):
                                    g = gh * 4 + g4
                                    op("pe", lambda e: e.transpose(out=bk.t[:, g4 * 128:g4 * 128 + 120], in_=st_[:120, g * 128:(g + 1) * 128],
                                                                   identity=ident_f[:120, :120]), reads=[st_, ident_f], writes=[bk])
                                op("act", lambda e: e.activation(
                                    out=gluS[:, gh * 4:gh * 4 + 4, q4 * 4:q4 * 4 + 4, 0:30],
                                    in_=bk.t[:, :].rearrange("p (g x) -> p g x", g=4)[:, :, 0:120].rearrange("p g (s r) -> p g s r", s=4),
                                    func=AF.Copy), reads=[bk], writes=gluS_g[gh * 4:gh * 4 + 4])
                    it3 = 0
                    for g in range(8):
                        wg = wga[g % 2]
                        ca = OFF_GLU + g * 128
                        dma("pool", wg[:, :, 0:128], w_in[:, ca:ca + 128].rearrange("(kc p) n -> p kc n", p=128), writes=[wg])
                        dma("pool", wg[:, :, 128:256], w_in[:, ca + 1024:ca + 1152].rearrange("(kc p) n -> p kc n", p=128), writes=[wg])
                        for (cl0, n, is_s) in chunks:
                            s3 = it3 % 2
                            it3 += 1
                            bA, bB = banks[4 + s3], banks[6 + s3]
                            rt = [xnT_t[i] for i in tiles_of(cl0, n)]
                            for kc in range(16):
                                op("pe", lambda e: e.matmul(bA.t[:, :n], lhsT=wg[:, kc, 0:128], rhs=xnT[:, kc, cl0:cl0 + n],
                                                            start=(kc == 0), stop=(kc == 15)), reads=rt + [wg], writes=[bA])
                            for kc in range(16):
                                op("pe", lambda e: e.matmul(bB.t[:, :n], lhsT=wg[:, kc, 128:256], rhs=xnT[:, kc, cl0:cl0 + n],
                                                            start=(kc == 0), stop=(kc == 15)), reads=rt + [wg], writes=[bB])
                            op("act", lambda e: e.activation(out=sg[s3][:, :n], in_=bB.t[:, :n], func=AF.Sigmoid), reads=[bB], writes=[sg[s3]])
                            if not is_s:
                                op("dve", lambda e: e.tensor_tensor(out=gluT[:, g, 30 + cl0:30 + cl0 + n], in0=bA.t[:, :n], in1=sg[s3][:, :n], op=ALU.mult),
                                   reads=[bA, sg[s3]], writes=[gluT_g[g]])
                                if last_pass and cl0 + n == Tpp:
                                    op("dve", lambda e: e.tensor_tensor(out=gl32[:, g, 0:32], in0=bA.t[:, n - 32:n], in1=sg[s3][:, n - 32:n], op=ALU.mult),
                                       reads=[bA, sg[s3]], writes=[gl32])
                            else:
                                op("dve", lambda e: e.tensor_tensor(out=gluS[:, g, :, 30:34], in0=bA.t[:, :n].rearrange("p (s i) -> p s i", i=4),
                                                                    in1=sg[s3][:, :n].rearrange("p (s i) -> p s i", i=4), op=ALU.mult),
                                   reads=[bA, sg[s3]], writes=[gluS_g[g]])
                                op("dve", lambda e: e.tensor_tensor(out=gl32[:, g, 32:96], in0=bA.t[:, :n], in1=sg[s3][:, :n], op=ALU.mult),
                                   reads=[bA, sg[s3]], writes=[gl32])
                    if not last_pass:
                        op("dve", lambda e: e.tensor_copy(out=halo[:], in_=gluT[:, :, Tpp:Tpp + 30]), reads=gluT_g, writes=[halo])
                chk("p3a")
                actT_c = [[xnT_t[i] for i in tiles_of(cl0_, n_)] for (cl0_, n_, _s) in chunks]

                scope("P3b_%d" % pi)
                with contextlib.ExitStack() as p3:
                    Dg = [kb.sb(p3, "Dg%d" % i, [128, 31, 128], BF16) for i in range(2)]
                    yb = kb.sb(p3, "yb", [128, 8, 512], F32)
                    yb_g = [yb.sub("yb_%d" % g) for g in range(8)]
                    ysq = [kb.sb(p3, "ysq%d" % i, [128, 512], F32) for i in range(2)]
                    mu = kb.sb(p3, "mu", [128, 512], F32)
                    rs = kb.sb(p3, "rs", [128, 512], F32)
                    tmp = kb.sb(p3, "tmp", [128, 512], F32)
                    it3 = 0
                    for ci, (cl0, n, is_s) in enumerate(chunks):
                        S1, S2 = banks[2], banks[3]
                        for g in range(8):
                            s3 = it3 % 2
                            it3 += 1
                            dg = Dg[s3]
                            op("pool", lambda e: e.tensor_tensor(out=dg[:], in0=ident_b[:].unsqueeze(1).broadcast_to([128, 31, 128]),
                                                                 in1=cwT[:, g, :].unsqueeze(2).broadcast_to([128, 31, 128]), op=ALU.mult),
                               reads=[ident_b, cwT], writes=[dg])
                            bY = banks[s3]
                            for j in range(31):
                                if not is_s:
                                    rhs = gluT[:, g, cl0 + j:cl0 + j + n]
                                    rr = [gluT_g[g]]
                                    outp = bY.t[:, :n]
                                else:
                                    rhs = gluS[:, g, :, j:j + 4]
                                    rr = [gluS_g[g]]
                                    outp = bY.t[:, :n].rearrange("p (s i) -> p s i", i=4)
                                op("pe", lambda e: e.matmul(outp, lhsT=dg[:, j, :], rhs=rhs, start=(j == 0), stop=(j == 30)),
                                   reads=rr + [dg], writes=[bY])
                            op("act", lambda e: e.activation(out=yb[:, g, :n], in_=bY.t[:, :n], func=AF.Identity, bias=ccol[:, 0, g:g + 1]),
                               reads=[bY, ccol], writes=[yb_g[g]])
                            op("act", lambda e: e.activation(out=ysq[s3][:, :n], in_=bY.t[:, :n], func=AF.Square, bias=ccol[:, 0, g:g + 1]),
                               reads=[bY, ccol], writes=[ysq[s3]])
                            op("pe", lambda e: e.matmul(S1.t[:, :n], lhsT=ones_f[:], rhs=yb[:, g, :n], start=(g == 0), stop=(g == 7)),
                               reads=[ones_f, yb_g[g]], writes=[S1])
                            op("pe", lambda e: e.matmul(S2.t[:, :n], lhsT=ones_f[:], rhs=ysq[s3][:, :n], start=(g == 0), stop=(g == 7)),
                               reads=[ones_f, ysq[s3]], writes=[S2])
                        op("dve", lambda e: e.tensor_scalar(out=mu[:, :n], in0=S1.t[:, :n], scalar1=1.0 / 1024, scalar2=None, op0=ALU.mult), reads=[S1], writes=[mu])
                        op("dve", lambda e: e.tensor_tensor(out=tmp[:, :n], in0=mu[:, :n], in1=mu[:, :n], op=ALU.mult), reads=[mu], writes=[tmp])
                        op("dve", lambda e: e.scalar_tensor_tensor(out=tmp[:, :n], in0=S2.t[:, :n], scalar=1.0 / 1024, in1=tmp[:, :n], op0=ALU.mult, op1=ALU.subtract),
                           reads=[S2, tmp], writes=[tmp])
                        op("dve", lambda e: e.tensor_scalar(out=tmp[:, :n], in0=tmp[:, :n], scalar1=EPS, scalar2=None, op0=ALU.add), reads=[tmp], writes=[tmp])
                        op("act", lambda e: e.activation(out=tmp[:, :n], in_=tmp[:, :n], func=AF.Sqrt), reads=[tmp], writes=[tmp])
                        op("dve", lambda e: e.reciprocal(out=rs[:, :n], in_=tmp[:, :n]), reads=[tmp], writes=[rs])
                        for g in range(8):
                            op("dve", lambda e: e.tensor_tensor(out=yb[:, g, :n], in0=yb[:, g, :n], in1=mu[:, :n], op=ALU.subtract), reads=[yb_g[g], mu], writes=[yb_g[g]])
                            op("dve", lambda e: e.tensor_tensor(out=yb[:, g, :n], in0=yb[:, g, :n], in1=rs[:, :n], op=ALU.mult), reads=[yb_g[g], rs], writes=[yb_g[g]])
                            op("act", lambda e: e.activation(out=actT[:, 8 + g, cl0:cl0 + n], in_=yb[:, g, :n], func=AF.Silu,
                                                             scale=ccol[:, 1, g:g + 1], bias=ccol[:, 2, g:g + 1]),
                               reads=[yb_g[g], ccol], writes=actT_c[ci])
                    if last_pass:
                        cst = kb.sb(p3, "cst", [64, 1024], F32)
                        for (c_lo, c_n, which) in ((0, 32, "p"), (32, 64, "s")):
                            if which == "s" and not has_s:
                                continue
                            for gh in range(2):
                                bk = banks[4 + gh]
                                for g4 in range(4):
                                    g = gh * 4 + g4
                                    op("pe", lambda e: e.transpose(out=bk.t[:c_n, g4 * 128:(g4 + 1) * 128], in_=gl32[:, g, c_lo:c_lo + c_n],
                                                                   identity=ident_f[:, :]), reads=[gl32, ident_f], writes=[bk])
                                op("act", lambda e: e.activation(out=cst[:c_n, gh * 512:(gh + 1) * 512], in_=bk.t[:c_n, :], func=AF.Copy), reads=[bk], writes=[cst])
                            if which == "p":
                                dma("sp", conv_p[:, :], cst[2:32, :], reads=[cst], is_output=True)
                            else:
                                for s_ in range(NS):
                                    dma("sp", conv_s[s_, 26:30, :], cst[4 * s_:4 * s_ + 4, :], reads=[cst], is_output=True)
                chk("p3b")
                if "convT" in dbg:
                    for g in range(8):
                        dma("pool", dbg["convT"][:, g, c0:c0 + Tp], actT[:, 8 + g, :], reads=xnT_t, is_output=True)
                scope("P4_%d" % pi)
                with contextlib.ExitStack() as p4:
                    Rh = [kb.sb(p4, "Rh%d" % i, [128, 512], BF16) for i in range(3)]
                    Dw = [kb.sb(p4, "Dw%d" % i, [128, 8, 128], BF16) for i in range(2)]
                    iscA = [kb.sb(p4, "iscA%d" % i, [128, 2048], F32) for i in range(2)]
                    iscW = kb.sb(p4, "iscW", [128, 2048], F32)
                    mx8 = [kb.sb(p4, "mx8_%d" % i, [128, 8], F32) for i in range(2)]
                    maskb = [kb.sb(p4, "maskb%d" % i, [128, 2048], BF16) for i in range(2)]
                    PT = [kb.sb(p4, "PT%d" % i, [128, 512], BF16) for i in range(3)]
                    rsum = [kb.sb(p4, "rsum%d" % i, [128, 512], F32) for i in range(2)]
                    cnt = {"S": 0, "R": 0, "L": 0, "P": 0, "G": 0}
                    ptl = [(ti, t0) for ti, (t0, rows) in enumerate(tiles) if t0 < SEQ]

                    def p4_indexer(ti, t0):
                        j = t0 // 128
                        lc = t0 - c0
                        L = (j + 1) * 128
                        dw, ia = Dw[ti % 2], iscA[ti % 2]
                        op("pool", lambda e: e.tensor_tensor(out=dw[:], in0=ident_b[:].unsqueeze(1).broadcast_to([128, 8, 128]),
                                                             in1=wiS[:, j, :].unsqueeze(2).broadcast_to([128, 8, 128]), op=ALU.mult),
                           reads=[ident_b, wiS], writes=[dw])
                        nch = (L + 511) // 512
                        items = [(c4, h) for c4 in range(nch) for h in range(8)]
                        slots = {}

                        def S_(i):
                            c4, h = items[i]
                            l0 = c4 * 512
                            n = min(512, L - l0)
                            Sb = banks[cnt["S"] % 2]
                            cnt["S"] += 1
                            slots[i] = Sb
                            op("pe", lambda e: e.matmul(Sb.t[:, :n], lhsT=qiT[:, h, lc:lc + 128], rhs=kiT[:, l0:l0 + n], start=True, stop=True),
                               reads=[qiT_t[ti], kiT], writes=[Sb])

                        S_(0)
                        for i, (c4, h) in enumerate(items):
                            if i + 1 < len(items):
                                S_(i + 1)
                            l0 = c4 * 512
                            n = min(512, L - l0)
                            Sb = slots.pop(i)
                            iP = banks[2 + c4 % 2]
                            rh = Rh[cnt["R"] % 3]
                            cnt["R"] += 1
                            op("act", lambda e: e.activation(out=rh[:, :n], in_=Sb.t[:, :n], func=AF.Relu), reads=[Sb], writes=[rh])
                            op("pe", lambda e: e.matmul(iP.t[:, :n], lhsT=dw[:, h, :], rhs=rh[:, :n], start=(h == 0), stop=(h == 7)),
                               reads=[dw, rh], writes=[iP])
                            if h == 7:
                                op("act", lambda e: e.activation(out=ia[:, l0:l0 + n], in_=iP.t[:, :n], func=AF.Copy), reads=[iP], writes=[ia])
                        op("dve", lambda e: e.tensor_tensor(out=ia[:, j * 128:(j + 1) * 128], in0=ia[:, j * 128:(j + 1) * 128], in1=cmask[:], op=ALU.add),
                           reads=[ia, cmask], writes=[ia])
                        if "isc" in dbg and j == int(os.environ.get("KDBGJ", "3")):
                            dma("sp", dbg["isc"][:, 0:L], ia[:, 0:L], reads=[ia], is_output=True)

                    def p4_topk(ti, t0):
                        j = t0 // 128
                        L = (j + 1) * 128
                        ia, mb = iscA[ti % 2], maskb[ti % 2]
                        if j >= 2:
                            src = ia
                            for r in range(32):
                                m8 = mx8[r % 2]
                                op("dve", lambda e: e.max(out=m8[:], in_=src[:, :L]), reads=[src], writes=[m8])
                                if r < 31:
                                    op("dve", lambda e: e.match_replace(out=iscW[:, :L], in_to_replace=m8[:], in_values=src[:, :L], imm_value=NEG),
                                       reads=[src, m8], writes=[iscW])
                                    src = iscW
                            thr_ap, thr_r = m8[:, 7:8], m8
                        else:
                            thr_ap, thr_r = thr0[:, 0:1], thr0
                        op("dve", lambda e: e.tensor_scalar(out=mb[:, :L], in0=ia[:, :L], scalar1=thr_ap, scalar2=NEG, op0=ALU.is_lt, op1=ALU.mult),
                           reads=[ia, thr_r], writes=[mb])

                    def p4_attn(ti, t0):
                        j = t0 // 128
                        lc = t0 - c0
                        mb = maskb[ti % 2]
                        OT, SM = banks[6], banks[7]
                        items = [(g, lb) for g in range(2) for lb in range(j + 1)]
                        slots = {}

                        def LT_(i):
                            g, lb = items[i]
                            LT = banks[4 + cnt["L"] % 2]
                            cnt["L"] += 1
                            slots[i] = LT
                            op("pe", lambda e: e.matmul(LT.t[:, :], lhsT=kT[:, g, lb * 128:(lb + 1) * 128], rhs=qT[:, 4 * g:4 * g + 4, lc:lc + 128],
                                                        start=True, stop=False), reads=[kT, qT_t[ti]], writes=[LT])
                            op("pe", lambda e: e.matmul(LT.t[:, :], lhsT=mb[:, lb * 128:(lb + 1) * 128], rhs=I4[:], start=False, stop=True),
                               reads=[mb, I4], writes=[LT])

                        LT_(0)
                        for i, (g, lb) in enumerate(items):
                            if i + 1 < len(items):
                                LT_(i + 1)
                            LT = slots.pop(i)
                            pt = PT[cnt["P"] % 3]
                            cnt["P"] += 1
                            op("act", lambda e: e.activation(out=pt[:], in_=LT.t[:, :], func=AF.Exp, scale=float(128 ** -0.5)), reads=[LT], writes=[pt])
                            op("pe", lambda e: e.matmul(OT.t[:, :], lhsT=Vb[:, lb, g * 128:(g + 1) * 128], rhs=pt[:], start=(lb == 0), stop=(lb == j)),
                               reads=[Vb, pt], writes=[OT])
                            op("pe", lambda e: e.matmul(SM.t[:, :], lhsT=ones_b[:], rhs=pt[:], start=(lb == 0), stop=(lb == j)),
                               reads=[ones_b, pt], writes=[SM])
                            if lb == j:
                                rsm = rsum[cnt["G"] % 2]
                                cnt["G"] += 1
                                op("dve", lambda e: e.reciprocal(out=rsm[:], in_=SM.t[:, :]), reads=[SM], writes=[rsm])
                                op("dve", lambda e: e.tensor_tensor(out=actT[:, 4 * g:4 * g + 4, lc:lc + 128], in0=OT.t[:, :].rearrange("p (h t) -> p h t", h=4),
                                                                    in1=rsm[:].rearrange("p (h t) -> p h t", h=4), op=ALU.mult),
                                   reads=[OT, rsm], writes=[actT_t[ti]])

                    if ptl:
                        p4_indexer(*ptl[0])
                    for k_, (ti, t0) in enumerate(ptl):
                        if k_ + 1 < len(ptl):
                            p4_indexer(*ptl[k_ + 1])
                        p4_topk(ti, t0)
                        p4_attn(ti, t0)
                scope("P4s_%d" % pi)
                if has_s:
                    lcS = SEQ - c0
                    tiS = len(tiles) - 1
                    with contextlib.ExitStack() as p4s:
                        ptb_ = kb.sb(p4s, "ptb_", [128, 256], I32)
                        ptf = kb.sb(p4s, "ptf", [128, 256], F32)
                        pcol = kb.sb(p4s, "pcol", [128, 1], F32)
                        idxs = kb.sb(p4s, "idxs", [128, 256], U32)
                        cmS = kb.sb(p4s, "cmS", [64, 64], F32)
                        sel32 = kb.sb(p4s, "sel32", [64, 256], F32)
                        selS = kb.sb(p4s, "selS", [64, 256], BF16)
                        dma("sp", ptb_[:], pt_d.partition_broadcast(128), writes=[ptb_])
                        dma("sp", pcol[:], pidx_d, writes=[pcol])
                        dma("sp", cmS[:], cmaskS_d, writes=[cmS])
                        dma("sp", sel32[:], selS_d, writes=[sel32])
                        op("dve", lambda e: e.tensor_copy(out=selS[:], in_=sel32[:]), reads=[sel32], writes=[selS])
                        op("dve", lambda e: e.tensor_copy(out=ptf[:], in_=ptb_[:]), reads=[ptb_], writes=[ptf])
                        op("dve", lambda e: e.tensor_scalar(out=ptf[:], in0=ptf[:], scalar1=128.0, scalar2=pcol[:, 0:1], op0=ALU.mult, op1=ALU.add), reads=[ptf, pcol], writes=[ptf])
                        op("dve", lambda e: e.tensor_copy(out=idxs[:], in_=ptf[:]), reads=[ptf], writes=[idxs])
                        iscS = kb.sb(p4s, "iscS", [64, 2112], F32)
                        iscWs = kb.sb(p4s, "iscWs", [64, 2112], F32)
                        mbS = kb.sb(p4s, "mbS", [64, 2112], BF16)
                        m8s = [kb.sb(p4s, "m8s%d" % i, [64, 8], F32) for i in range(2)]
                        DwS = kb.sb(p4s, "DwS", [64, 8, 64], BF16)
                        op("pool", lambda e: e.tensor_tensor(out=DwS[:], in0=ident_b[0:64, 0:64].unsqueeze(1).broadcast_to([64, 8, 64]),
                                                             in1=wiS[0:64, 16, :].unsqueeze(2).broadcast_to([64, 8, 64]), op=ALU.mult), reads=[ident_b, wiS], writes=[DwS])
                        with contextlib.ExitStack() as pix:
                            qiZ = kb.sb(pix, "qiZ", [64, 8, NS, 64], BF16)
                            op("pool", lambda e: e.memset(qiZ[:], 0.0), writes=[qiZ])
                            for s_ in range(NS):
                                op("act", lambda e: e.activation(out=qiZ[:, :, s_, 4 * s_:4 * s_ + 4], in_=qiT[:, :, lcS + 4 * s_:lcS + 4 * s_ + 4], func=AF.Copy),
                                   reads=[qiT_t[tiS]], writes=[qiZ])
                            kis = [kb.sb(pix, "kis%d" % i, [128, 16, 64], BF16) for i in range(2)]
                            kiTs = [kb.sb(pix, "kiTs%d" % i, [64, 512], BF16) for i in range(2)]
                            RhS = [kb.sb(pix, "RhS%d" % i, [64, 512], BF16) for i in range(3)]
                            for h in range(8):
                                Sb = banks[4 + h % 2]
                                rh = RhS[h % 3]
                                op("pe", lambda e: e.matmul(Sb.t[0:64, 0:64], lhsT=qiT[:, h, lcS:lcS + 64], rhs=kiT[:, SEQ:SEQ + 64], start=True, stop=True),
                                   reads=[qiT_t[tiS], kiT], writes=[Sb])
                                op("act", lambda e: e.activation(out=rh[:, 0:64], in_=Sb.t[0:64, 0:64], func=AF.Relu), reads=[Sb], writes=[rh])
                                op("pe", lambda e: e.matmul(banks[6].t[0:64, 0:64], lhsT=DwS[:, h, :], rhs=rh[:, 0:64], start=(h == 0), stop=(h == 7)),
                                   reads=[DwS, rh], writes=[banks[6]])
                            op("dve", lambda e: e.tensor_tensor(out=iscS[:, 2048:2112], in0=banks[6].t[0:64, 0:64], in1=cmS[:], op=ALU.add), reads=[banks[6], cmS], writes=[iscS])
                            groups = [(s_, c4) for s_ in range(NS) for c4 in range(4)]
                            gk = {}
                            cq = {"q": 0, "S": 0, "R": 0}

                            def prep_(gi):
                                s_, c4 = groups[gi]
                                ks_ = kis[s_ % 2]
                                if c4 == 0:
                                    for pg in range(16):
                                        kb.idma(ks_[:, pg, :], cki, idxs[:, s_ * 16 + pg:s_ * 16 + pg + 1], reads=[idxs], writes=[ks_])
                                tb_ = banks[6 + cq["q"] % 2]
                                kt_ = kiTs[cq["q"] % 2]
                                cq["q"] += 1
                                tpv = tb_.t[:].bitcast(BF16)
                                for p4_ in range(4):
                                    op("pe", lambda e: e.transpose(out=tpv[0:64, p4_ * 128:(p4_ + 1) * 128], in_=ks_[:, c4 * 4 + p4_, :], identity=ident_b[:]),
                                       reads=[ks_, ident_b], writes=[tb_])
                                op("dve", lambda e: e.tensor_copy(out=kt_[:], in_=tpv[0:64, 0:512]), reads=[tb_], writes=[kt_])
                                gk[gi] = kt_

                            prep_(0)
                            for gi, (s_, c4) in enumerate(groups):
                                if gi + 1 < len(groups):
                                    prep_(gi + 1)
                                kt_ = gk.pop(gi)
                                sl = {}

                                def S2_(h):
                                    Sb = banks[4 + cq["S"] % 2]
                                    cq["S"] += 1
                                    sl[h] = Sb
                                    op("pe", lambda e: e.matmul(Sb.t[0:64, :], lhsT=qiZ[:, h, s_, :], rhs=kt_[:], start=True, stop=True), reads=[qiZ, kt_], writes=[Sb])

                                S2_(0)
                                for h in range(8):
                                    if h + 1 < 8:
                                        S2_(h + 1)
                                    Sb = sl.pop(h)
                                    rh = RhS[cq["R"] % 3]
                                    cq["R"] += 1
                                    op("act", lambda e: e.activation(out=rh[:], in_=Sb.t[0:64, :], func=AF.Relu), reads=[Sb], writes=[rh])
                                    op("pe", lambda e: e.matmul(banks[c4].t[0:64, :], lhsT=DwS[:, h, :], rhs=rh[:], start=(s_ == 0 and h == 0), stop=(s_ == NS - 1 and h == 7)),
                                       reads=[DwS, rh], writes=[banks[c4]])
                            for c4 in range(4):
                                op("act", lambda e: e.activation(out=iscS[:, c4 * 512:(c4 + 1) * 512], in_=banks[c4].t[0:64, :], func=AF.Copy), reads=[banks[c4]], writes=[iscS])
                        if "iscS" in dbg:
                            dma("sp", dbg["iscS"], iscS[:], reads=[iscS], is_output=True)
                        src = iscS
                        for r in range(32):
                            m8 = m8s[r % 2]
                            op("dve", lambda e: e.max(out=m8[:], in_=src[:, :]), reads=[src], writes=[m8])
                            if r < 31:
                                op("dve", lambda e: e.match_replace(out=iscWs[:, :], in_to_replace=m8[:], in_values=src[:, :], imm_value=NEG), reads=[src, m8], writes=[iscWs])
                                src = iscWs
                        op("dve", lambda e: e.tensor_scalar(out=mbS[:], in0=iscS[:], scalar1=m8[:, 7:8], scalar2=NEG, op0=ALU.is_lt, op1=ALU.mult), reads=[iscS, m8], writes=[mbS])
                        with contextlib.ExitStack() as pat:
                            Ks = [kb.sb(pat, "Ks%d" % i, [128, 16, 256], BF16) for i in range(2)]
                            Vs = [kb.sb(pat, "Vs%d" % i, [128, 16, 256], BF16) for i in range(2)]
                            kTs = [kb.sb(pat, "kTs%d" % i, [128, 2, 2048], BF16) for i in range(2)]
                            PTs = [kb.sb(pat, "PTs%d" % i, [128, 272], BF16) for i in range(2)]
                            rsS = [kb.sb(pat, "rsS%d" % i, [128, 32], F32) for i in range(2)]
                            ca = {"t": 0, "l": 0}

                            def prepK(s_):
                                K_, V_, kT_ = Ks[s_ % 2], Vs[s_ % 2], kTs[s_ % 2]
                                for pg in range(16):
                                    kb.idma(K_[:, pg, :], ck, idxs[:, s_ * 16 + pg:s_ * 16 + pg + 1], reads=[idxs], writes=[K_])
                                    kb.idma(V_[:, pg, :], cv, idxs[:, s_ * 16 + pg:s_ * 16 + pg + 1], reads=[idxs], writes=[V_])
                                for g in range(2):
                                    for p8 in range(2):
                                        tb_ = banks[ca["t"] % 2]
                                        ca["t"] += 1
                                        tpv = tb_.t[:].bitcast(BF16)
                                        for k8 in range(8):
                                            pg = p8 * 8 + k8
                                            op("pe", lambda e: e.transpose(out=tpv[:, k8 * 128:(k8 + 1) * 128], in_=K_[:, pg, g * 128:(g + 1) * 128], identity=ident_b[:]),
                                               reads=[K_, ident_b], writes=[tb_])
                                        op("act", lambda e: e.activation(out=kT_[:, g, p8 * 1024:(p8 + 1) * 1024], in_=tpv[:, :], func=AF.Copy), reads=[tb_], writes=[kT_])

                            def attnS(s_):
                                K_, V_, kT_ = Ks[s_ % 2], Vs[s_ % 2], kTs[s_ % 2]
                                OT, SM = banks[4], banks[5]
                                sv = selS[:, s_ * 16:(s_ + 1) * 16]
                                lts = []
                                for g in range(2):
                                    LT = banks[2 + g]
                                    qv = qT[:, 4 * g:4 * g + 4, lcS + 4 * s_:lcS + 4 * s_ + 4]
                                    for lb in range(16):
                                        op("pe", lambda e: e.matmul(LT.t[:, lb * 16:(lb + 1) * 16], lhsT=kT_[:, g, lb * 128:(lb + 1) * 128], rhs=qv, start=True, stop=False),
                                           reads=[kT_, qT_t[tiS]], writes=[LT])
                                        op("pe", lambda e: e.matmul(LT.t[:, lb * 16:(lb + 1) * 16], lhsT=mbS[:, lb * 128:(lb + 1) * 128], rhs=sv, start=False, stop=True),
                                           reads=[mbS, selS], writes=[LT])
                                    op("pe", lambda e: e.matmul(LT.t[0:64, 256:272], lhsT=kT[:, g, SEQ:SEQ + 64], rhs=qv, start=True, stop=False), reads=[kT, qT_t[tiS]], writes=[LT])
                                    op("pe", lambda e: e.matmul(LT.t[0:64, 256:272], lhsT=mbS[:, 2048:2112], rhs=sv, start=False, stop=True), reads=[mbS, selS], writes=[LT])
                                for g in range(2):
                                    LT = banks[2 + g]
                                    pt = PTs[g]
                                    op("act", lambda e: e.activation(out=pt[:, 0:256], in_=LT.t[:, 0:256], func=AF.Exp, scale=float(128 ** -0.5)), reads=[LT], writes=[pt])
                                    op("act", lambda e: e.activation(out=pt[0:64, 256:272], in_=LT.t[0:64, 256:272], func=AF.Exp, scale=float(128 ** -0.5)), reads=[LT], writes=[pt])
                                for g in range(2):
                                    pt = PTs[g]
                                    for lb in range(17):
                                        if lb < 16:
                                            lv, pv, on = V_[:, lb, g * 128:(g + 1) * 128], pt[:, lb * 16:(lb + 1) * 16], ones_b[:]
                                        else:
                                            lv, pv, on = Vb[0:64, 16, g * 128:(g + 1) * 128], pt[0:64, 256:272], ones_b[0:64, :]
                                        op("pe", lambda e: e.matmul(OT.t[:, g * 16:(g + 1) * 16], lhsT=lv, rhs=pv, start=(lb == 0), stop=(lb == 16)), reads=[V_, Vb, pt], writes=[OT])
                                        op("pe", lambda e: e.matmul(SM.t[:, g * 16:(g + 1) * 16], lhsT=on, rhs=pv, start=(lb == 0), stop=(lb == 16)), reads=[ones_b, pt], writes=[SM])
                                rs_ = rsS[s_ % 2]
                                op("dve", lambda e: e.reciprocal(out=rs_[:], in_=SM.t[:, 0:32]), reads=[SM], writes=[rs_])
                                op("dve", lambda e: e.tensor_tensor(out=actT[:, 0:8, lcS + 4 * s_:lcS + 4 * s_ + 4], in0=OT.t[:, 0:32].rearrange("p (h i) -> p h i", i=4),
                                                                    in1=rs_[:].rearrange("p (h i) -> p h i", i=4), op=ALU.mult), reads=[OT, rs_], writes=[actT_t[tiS]])

                            prepK(0)
                            for s_ in range(NS):
                                if s_ + 1 < NS:
                                    prepK(s_ + 1)
                                attnS(s_)
                chk("p4")
                if "attnT" in dbg:
                    for h in range(8):
                        dma("pool", dbg["attnT"][:, h, c0:c0 + Tp], actT[:, h, :], reads=actT_t, is_output=True)
                if "qT" in dbg and pi == 0:
                    dma("pool", dbg["qT"][:, :, 0:Tp], qT[:], reads=qT_t, is_output=True)
                if "qiT" in dbg and pi == 0:
                    dma("pool", dbg["qiT"][:, :, 0:Tp], qiT[:], reads=qiT_t, is_output=True)
                pa.close()

                scope("P5_%d" % pi)
                acc = kb.sb(ps, "acc%d" % pi, [128, len(tiles), D], F32)
                acc_t = [acc.sub("acc%d_%d" % (pi, i)) for i in range(len(tiles))]

                def proj_resid(wsrc, nm):
                    with contextlib.ExitStack() as p5:
                        wblk = [kb.sb(p5, "w5_%d" % i, [128, 16, 512], BF16) for i in range(2)]
                        xb = [kb.sb(p5, "xb%d" % i, [128, 512], F32) for i in range(3)]
                        it5 = 0
                        for nb in range(4):
                            wb = wblk[nb % 2]
                            dma("pool", wb[:], wsrc[:, nb * 512:(nb + 1) * 512].rearrange("(kc p) n -> p kc n", p=128), writes=[wb])
                            for ti, (t0, rows) in enumerate(tiles):
                                lc = t0 - c0
                                bk = banks[it5 % 4]
                                xs_ = xb[it5 % 3]
                                it5 += 1
                                for kc in range(16):
                                    op("pe", lambda e: e.matmul(bk.t[:rows, :], lhsT=AX[:, kc, lc:lc + rows], rhs=wb[:, kc, :],
                                                                start=(kc == 0), stop=(kc == 15)), reads=[xnT_t[ti], wb], writes=[bk])
                                dma("sp", xs_[:rows, :], x[t0:t0 + rows, nb * 512:(nb + 1) * 512], writes=[xs_])
                                op("dve", lambda e: e.tensor_tensor(out=acc[:rows, ti, nb * 512:(nb + 1) * 512], in0=bk.t[:rows, :], in1=xs_[:rows, :], op=ALU.add),
                                   reads=[bk, xs_], writes=[acc_t[ti]])

                proj_resid(w_out, "wout")
                chk("p5")

                def norm_to_AX(which):
                    with contextlib.ExitStack() as pn:
                        junk = kb.sb(pn, "njunk", [128, D], BF16)
                        xs = [kb.sb(pn, "nxs%d" % i, [128, D], BF16) for i in range(2)]
                        st = [kb.sb(pn, "nst%d" % i, [128, 4], F32) for i in range(2)]
                        for ti, (t0, rows) in enumerate(tiles):
                            s_ = ti % 2
                            lc = t0 - c0
                            op("act", lambda e: e.activation(out=junk[:rows, :], in_=acc[:rows, ti, :], func=AF.Square,
                                                             accum_out=st[s_][:rows, 0:1]), reads=[acc_t[ti]], writes=[junk, st[s_]])
                            op("dve", lambda e: e.tensor_scalar(out=st[s_][:rows, 1:2], in0=st[s_][:rows, 0:1], scalar1=1.0 / D,
                                                                scalar2=EPS, op0=ALU.mult, op1=ALU.add), reads=[st[s_]], writes=[st[s_]])
                            op("act", lambda e: e.activation(out=st[s_][:rows, 2:3], in_=st[s_][:rows, 1:2], func=AF.Sqrt), reads=[st[s_]], writes=[st[s_]])
                            op("dve", lambda e: e.reciprocal(out=st[s_][:rows, 3:4], in_=st[s_][:rows, 2:3]), reads=[st[s_]], writes=[st[s_]])
                            op("act", lambda e: e.activation(out=xs[s_][:rows, :], in_=acc[:rows, ti, :], func=AF.Copy,
                                                             scale=st[s_][:rows, 3:4]), reads=[acc_t[ti], st[s_]], writes=[xs[s_]])
                            for half in range(2):
                                bk = banks[(2 * ti + half) % 4]
                                tp = bk.t[:].bitcast(BF16)
                                for k8 in range(8):
                                    kc = half * 8 + k8
                                    op("pe", lambda e: e.transpose(out=tp[:, k8 * 128:k8 * 128 + rows], in_=xs[s_][:rows, kc * 128:(kc + 1) * 128],
                                                                   identity=ident_b[:rows, :rows]), reads=[xs[s_], ident_b], writes=[bk])
                                op("dve", lambda e: e.tensor_tensor(
                                    out=AX[:, half * 8:half * 8 + 8, lc:lc + rows],
                                    in0=tp.rearrange("p (k t) -> p k t", k=8)[:, :, :rows],
                                    in1=gcols[:, which, half * 8:half * 8 + 8].unsqueeze(2).broadcast_to([128, 8, rows]), op=ALU.mult),
                                   reads=[bk, gcols], writes=[xnT_t[ti]])

                scope("P6n_%d" % pi)
                norm_to_AX(1)
                nt = len(tiles)
                with contextlib.ExitStack() as p6:
                    scope("P6a_%d" % pi)
                    v16 = kb.sb(p6, "v16", [128, nt, 16, 16], F32)
                    I16 = kb.sb(p6, "I16", [128, nt, 16, 16], U32)
                    v16_t = [v16.sub("v16_%d" % i) for i in range(nt)]
                    with contextlib.ExitStack() as p6a:
                        wqb = [kb.sb(p6a, "wqb%d" % i, [128, 16, 512], BF16) for i in range(2)]
                        qhb = [kb.sb(p6a, "qhb%d" % i, [128, Tp], BF16) for i in range(2)]
                        stmp = [kb.sb(p6a, "stmp%d" % i, [128, 128], F32) for i in range(2)]
                        it6 = 0
                        for b4 in range(4):
                            wb = wqb[b4 % 2]
                            dma("pool", wb[:], peer_wq[:, b4 * 512:(b4 + 1) * 512].rearrange("(kc p) n -> p kc n", p=128), writes=[wb])
                            for bl in range(4):
                                blk = b4 * 4 + bl
                                qb = qhb[blk % 2]
                                for (cl0, n, is_s) in chunks:
                                    bk = banks[it6 % 2]
                                    it6 += 1
                                    rt = [xnT_t[i] for i in tiles_of(cl0, n)]
                                    for kc in range(16):
                                        op("pe", lambda e: e.matmul(bk.t[:, :n], lhsT=wb[:, kc, bl * 128:(bl + 1) * 128], rhs=AX[:, kc, cl0:cl0 + n],
                                                                    start=(kc == 0), stop=(kc == 15)), reads=rt + [wb], writes=[bk])
                                    op("act", lambda e: e.activation(out=qb[:, cl0:cl0 + n], in_=bk.t[:, :n], func=AF.Copy), reads=[bk], writes=[qb])
                                for ti, (t0, rows) in enumerate(tiles):
                                    lc = t0 - c0
                                    sP = banks[2 + (it6 % 2)]
                                    it6 += 1
                                    sm = stmp[it6 % 2]
                                    op("pe", lambda e: e.matmul(sP.t[:rows, 0:128], lhsT=qb[:, lc:lc + rows], rhs=skT[:, blk, :], start=True, stop=True),
                                       reads=[qb, skT], writes=[sP])
                                    op("dve", lambda e: e.max(out=v16[:rows, ti, blk, 0:8], in_=sP.t[:rows, 0:128]), reads=[sP], writes=[v16_t[ti]])
                                    op("dve", lambda e: e.max_index(out=I16[:rows, ti, blk, 0:8], in_max=v16[:rows, ti, blk, 0:8], in_values=sP.t[:rows, 0:128]),
                                       reads=[sP, v16_t[ti]], writes=[v16_t[ti]])
                                    op("dve", lambda e: e.match_replace(out=sm[:rows, :], in_to_replace=v16[:rows, ti, blk, 0:8], in_values=sP.t[:rows, 0:128], imm_value=NEG),
                                       reads=[sP, v16_t[ti]], writes=[sm])
                                    op("dve", lambda e: e.max(out=v16[:rows, ti, blk, 8:16], in_=sm[:rows, :]), reads=[sm], writes=[v16_t[ti]])
                                    op("dve", lambda e: e.max_index(out=I16[:rows, ti, blk, 8:16], in_max=v16[:rows, ti, blk, 8:16], in_values=sm[:rows, :]),
                                       reads=[sm, v16_t[ti]], writes=[v16_t[ti]])
                    chk("p6a")
                    scope("P6b_%d" % pi)
                    with contextlib.ExitStack() as p6b:
                        I16f = kb.sb(p6b, "I16f", [128, 16, 16], F32)
                        cand = kb.sb(p6b, "cand", [128, 8, 256], F32)
                        ctmp = kb.sb(p6b, "ctmp", [128, 256], F32)
                        c16 = kb.sb(p6b, "c16", [128, 8, 16], F32)
                        P16 = kb.sb(p6b, "P16", [128, 8, 16], U32)
                        ab_u = kb.sb(p6b, "ab_u", [128, 2, 8, 16], U32)
                        ab_f = kb.sb(p6b, "ab_f", [128, 2, 8, 16], F32)
                        eq = kb.sb(p6b, "eq", [128, 8, 16, 16], F32)
                        sel3 = kb.sb(p6b, "sel3", [128, 3, 128], F32)
                        zst = kb.sb(p6b, "zst", [128, 8, 2], F32)
                        selT = [kb.sb(p6b, "selT%d" % i, [128, 3, 128], BF16) for i in range(2)]
                        OH2 = [kb.sb(p6b, "OH2_%d" % i, [128, 16, 128], BF16) for i in range(2)]
                        OH1 = [kb.sb(p6b, "OH1_%d" % i, [128, 16, 128], BF16) for i in range(2)]
                        GS = kb.sb(p6b, "GS", [128, 128, 128], BF16)
                        itb = itg = 0
                        for ti, (t0, rows) in enumerate(tiles):
                            vv = v16[:rows, ti].rearrange("p (h n) a -> p h n a", n=2)
                            op("dve", lambda e: e.tensor_copy(out=I16f[:rows], in_=I16[:rows, ti]), reads=[v16_t[ti]], writes=[I16f])
                            op("dve", lambda e: e.tensor_tensor(out=cand[:rows].rearrange("p h (a b) -> p h a b", a=16),
                                                                in0=vv[:, :, 0, :].unsqueeze(3).broadcast_to([rows, 8, 16, 16]),
                                                                in1=vv[:, :, 1, :].unsqueeze(2).broadcast_to([rows, 8, 16, 16]), op=ALU.add),
                               reads=[v16_t[ti]], writes=[cand])
                            for h in range(8):
                                op("dve", lambda e: e.max(out=c16[:rows, h, 0:8], in_=cand[:rows, h, :]), reads=[cand], writes=[c16])
                                op("dve", lambda e: e.max_index(out=P16[:rows, h, 0:8], in_max=c16[:rows, h, 0:8], in_values=cand[:rows, h, :]), reads=[cand, c16], writes=[P16])
                                op("dve", lambda e: e.match_replace(out=ctmp[:rows, :], in_to_replace=c16[:rows, h, 0:8], in_values=cand[:rows, h, :], imm_value=NEG),
                                   reads=[cand, c16], writes=[ctmp])
                                op("dve", lambda e: e.max(out=c16[:rows, h, 8:16], in_=ctmp[:rows, :]), reads=[ctmp], writes=[c16])
                                op("dve", lambda e: e.max_index(out=P16[:rows, h, 8:16], in_max=c16[:rows, h, 8:16], in_values=ctmp[:rows, :]), reads=[ctmp, c16], writes=[P16])
                            op("dve", lambda e: e.tensor_scalar(out=ab_u[:rows, 0], in0=P16[:rows], scalar1=4, scalar2=None, op0=ALU.logical_shift_right), reads=[P16], writes=[ab_u])
                            op("dve", lambda e: e.tensor_scalar(out=ab_u[:rows, 1], in0=P16[:rows], scalar1=15, scalar2=None, op0=ALU.bitwise_and), reads=[P16], writes=[ab_u])
                            op("dve", lambda e: e.tensor_copy(out=ab_f[:rows], in_=ab_u[:rows]), reads=[ab_u], writes=[ab_f])
                            If = I16f[:rows].rearrange("p (h n) a -> p h n a", n=2)
                            for w_ in range(2):
                                op("dve", lambda e: e.tensor_tensor(out=eq[:rows], in0=ab_f[:rows, w_].unsqueeze(3).broadcast_to([rows, 8, 16, 16]),
                                                                    in1=iota16[:rows, :].unsqueeze(1).unsqueeze(1).broadcast_to([rows, 8, 16, 16]), op=ALU.is_equal),
                                   reads=[ab_f, iota16], writes=[eq])
                                op("dve", lambda e: e.tensor_tensor(out=eq[:rows], in0=eq[:rows], in1=If[:, :, w_, :].unsqueeze(2).broadcast_to([rows, 8, 16, 16]), op=ALU.mult),
                                   reads=[eq, I16f], writes=[eq])
                                op("dve", lambda e: e.tensor_reduce(out=sel3[:rows, w_, :].rearrange("p (h j) -> p h j", h=8), in_=eq[:rows], axis=AX_.X, op=ALU.add),
                                   reads=[eq], writes=[sel3])
                            wv = sel3[:rows, 2, :].rearrange("p (h j) -> p h j", h=8)
                            op("dve", lambda e: e.tensor_tensor(out=wv, in0=c16[:rows], in1=c16[:rows, :, 0:1].broadcast_to([rows, 8, 16]), op=ALU.subtract),
                               reads=[c16], writes=[sel3])
                            op("act", lambda e: e.activation(out=wv, in_=wv, func=AF.Exp), reads=[sel3], writes=[sel3])
                            op("dve", lambda e: e.tensor_reduce(out=zst[:rows, :, 0], in_=wv, axis=AX_.X, op=ALU.add), reads=[sel3], writes=[zst])
                            op("dve", lambda e: e.reciprocal(out=zst[:rows, :, 1], in_=zst[:rows, :, 0]), reads=[zst], writes=[zst])
                            op("dve", lambda e: e.tensor_tensor(out=wv, in0=wv, in1=zst[:rows, :, 1:2].broadcast_to([rows, 8, 16]), op=ALU.mult),
                               reads=[sel3, zst], writes=[sel3])
                            if "sel3" in dbg and t0 == 0:
                                dma("sp", dbg["sel3"], sel3[:], reads=[sel3], is_output=True)
                            sT = selT[ti % 2]
                            bkT = banks[4]
                            for w_ in range(3):
                                op("pe", lambda e: e.transpose(out=bkT.t[:, w_ * 128:w_ * 128 + rows], in_=sel3[:rows, w_, :], identity=ident_f[:rows, :rows]),
                                   reads=[sel3, ident_f], writes=[bkT])
                            op("act", lambda e: e.activation(out=sT[:, :, :rows], in_=bkT.t[:, 0:384].rearrange("p (w t) -> p w t", w=3)[:, :, :rows], func=AF.Copy),
                               reads=[bkT], writes=[sT])
                            for sub in range((rows + 15) // 16):
                                tl0 = sub * 16
                                o2, o1 = OH2[itb % 2], OH1[itb % 2]
                                itb += 1
                                io_b = iotaC[:].unsqueeze(1).broadcast_to([128, 16, 128])
                                op("dve", lambda e: e.tensor_tensor(out=o2[:], in0=io_b, in1=sT[:, 1, tl0:tl0 + 16].unsqueeze(2).broadcast_to([128, 16, 128]), op=ALU.is_equal),
                                   reads=[iotaC, sT], writes=[o2])
                                op("dve", lambda e: e.tensor_tensor(out=o1[:], in0=io_b, in1=sT[:, 0, tl0:tl0 + 16].unsqueeze(2).broadcast_to([128, 16, 128]), op=ALU.is_equal),
                                   reads=[iotaC, sT], writes=[o1])
                                op("pool", lambda e: e.tensor_tensor(out=o1[:], in0=o1[:], in1=sT[:, 2, tl0:tl0 + 16].unsqueeze(2).broadcast_to([128, 16, 128]), op=ALU.mult),
                                   reads=[o1, sT], writes=[o1])
                                for q4 in range(4):
                                    gb = banks[5 + itg % 3]
                                    itg += 1
                                    for t4 in range(4):
                                        tl = q4 * 4 + t4
                                        op("pe", lambda e: e.matmul(gb.t[:, t4 * 128:(t4 + 1) * 128], lhsT=o2[:, tl, :], rhs=o1[:, tl, :], start=True, stop=True),
                                           reads=[o2, o1], writes=[gb])
                                    tg = tl0 + q4 * 4
                                    op("act", lambda e: e.activation(out=GS[:, :, tg:tg + 4], in_=gb.t[:, :].rearrange("p (t c) -> p c t", t=4), func=AF.Copy),
                                       reads=[gb], writes=[GS])
                            for c8 in range(8):
                                dma("sp", Gd[c8 * 16:(c8 + 1) * 16, :, t0:t0 + rows].rearrange("c i t -> i c t"), GS[:, c8 * 16:(c8 + 1) * 16, :rows],
                                    reads=[GS], writes=[Gd_res], allow_slow_non_contiguous=False)
                    chk("p6b")
                    if "G" in dbg and pi == 0:
                        with contextlib.ExitStack() as pd:
                            gtmp = kb.sb(pd, "gtmp", [128, 128, 128], BF16)
                            dma("sp", gtmp[:], Gd[:, :, 0:128].rearrange("c i t -> i c t"), reads=[Gd_res], writes=[gtmp])
                            for c8 in range(8):
                                dma("pool", dbg["G"][:, c8 * 16:(c8 + 1) * 16, :], gtmp[:, c8 * 16:(c8 + 1) * 16, :], reads=[gtmp], is_output=True)
                    chk("p6c")
                    scope("P6d_%d" % pi)
                    with contextlib.ExitStack() as p6d:
                        ES = 4
                        ub = [kb.sb(p6d, "ub%d" % i, [128, D], BF16) for i in range(2)]
                        uT = [kb.sb(p6d, "uT%d" % i, [128, 16, 128], BF16) for i in range(2)]
                        gt = [kb.sb(p6d, "gt%d" % i, [128, Tp], BF16) for i in range(2)]
                        gl = [kb.sb(p6d, "gl%d" % i, [128, 512], BF16) for i in range(2)]
                        actS = [kb.sb(p6d, "actS%d" % i, [128, ES, Tp], BF16) for i in range(2)]
                        vS = [kb.sb(p6d, "vS%d" % i, [128, ES, D], BF16) for i in range(2)]
                        ith = itv = 0
                        nsc = 128 // ES
                        if os.environ.get("KNSC"):
                            nsc = int(os.environ["KNSC"])
                        cu = {"h": 0, "v": 0}
                        prepped = {}

                        def prepU(c):
                            sc, ec = divmod(c, ES)
                            vs = vS[sc % 2]
                            u_, uT_, g_ = ub[c % 2], uT[c % 2], gt[c % 2]
                            dma("sp", g_[:, :], Gd[c, :, c0:c0 + Tp], reads=[Gd_res], writes=[g_])
                            if pi > 0:
                                dma("sp", uT_[:], uTd[c], writes=[uT_])
                                dma("sp", vs[:, ec, :], vd[c], writes=[vs])
                                return
                            dma("pool", u_[:], peer_u[c * 128:(c + 1) * 128, :], writes=[u_])
                            dma("pool", vs[:, ec, :], peer_v[c * 128:(c + 1) * 128, :], writes=[vs])
                            dma("sp", vd[c], vs[:, ec, :], reads=[vs])
                            for half in range(2):
                                bk = banks[half]
                                tp = bk.t[:].bitcast(BF16)
                                for k8 in range(8):
                                    kc = half * 8 + k8
                                    op("pe", lambda e: e.transpose(out=tp[:, k8 * 128:(k8 + 1) * 128], in_=u_[:, kc * 128:(kc + 1) * 128], identity=ident_b[:]),
                                       reads=[u_, ident_b], writes=[bk])
                                op("act", lambda e: e.activation(out=uT_[:, half * 8:half * 8 + 8, :], in_=tp.rearrange("p (k e) -> p k e", k=8), func=AF.Copy),
                                   reads=[bk], writes=[uT_])
                            dma("sp", uTd[c], uT_[:], reads=[uT_])

                        def uside(c):
                            sc, ec = divmod(c, ES)
                            aS = actS[sc % 2]
                            uT_, g_ = uT[c % 2], gt[c % 2]
                            for (cl0, n, is_s) in chunks:
                                hp = banks[2 + cu["h"] % 2]
                                gl_ = gl[cu["h"] % 2]
                                cu["h"] += 1
                                rt = [xnT_t[i] for i in tiles_of(cl0, n)]
                                for kc in range(16):
                                    op("pe", lambda e: e.matmul(hp.t[:, :n], lhsT=uT_[:, kc, :], rhs=AX[:, kc, cl0:cl0 + n], start=(kc == 0), stop=(kc == 15)),
                                       reads=rt + [uT_], writes=[hp])
                                op("act", lambda e: e.activation(out=gl_[:, :n], in_=hp.t[:, :n], func=AF.Gelu), reads=[hp], writes=[gl_])
                                op("pool", lambda e: e.tensor_tensor(out=aS[:, ec, cl0:cl0 + n], in0=gl_[:, :n], in1=g_[:, cl0:cl0 + n], op=ALU.mult),
                                   reads=[gl_, g_], writes=[aS])

                        def vside(sc):
                            aS, vs = actS[sc % 2], vS[sc % 2]
                            for ti, (t0, rows) in enumerate(tiles):
                                lc = t0 - c0
                                for dq4 in range(4):
                                    bk = banks[4 + cu["v"] % 4]
                                    cu["v"] += 1
                                    d0 = dq4 * 512
                                    for ec in range(ES):
                                        op("pe", lambda e: e.matmul(bk.t[:rows, :], lhsT=aS[:, ec, lc:lc + rows], rhs=vs[:, ec, d0:d0 + 512], start=(ec == 0), stop=(ec == ES - 1)),
                                           reads=[aS, vs], writes=[bk])
                                    op("dve", lambda e: e.tensor_tensor(out=acc[:rows, ti, d0:d0 + 512], in0=bk.t[:rows, :], in1=acc[:rows, ti, d0:d0 + 512], op=ALU.add),
                                       reads=[bk, acc_t[ti]], writes=[acc_t[ti]])

                        nchunk = nsc * ES
                        prepU(0)
                        for c in range(nchunk):
                            if c + 1 < nchunk:
                                prepU(c + 1)
                            uside(c)
                            if (c + 1) % ES == 0:
                                vside(c // ES)
                chk("p6")
                if "h3" in dbg:
                    for ti, (t0, rows) in enumerate(tiles):
                        dma("sp", dbg["h3"][t0:t0 + rows, :], acc[:rows, ti, :], reads=[acc_t[ti]], is_output=True)

                scope("P7_%d" % pi)
                norm_to_AX(2)
                with contextlib.ExitStack() as p7:
                    pT = kb.sb(p7, "pT", [128, 2, Tp], BF16)
                    pt32 = [kb.sb(p7, "pt32_%d" % i, [128, 256], F32) for i in range(2)]
                    ptb = [kb.sb(p7, "ptb%d" % i, [128, 256], BF16) for i in range(2)]
                    for ti, (t0, rows) in enumerate(tiles):
                        lc = t0 - c0
                        s_ = ti % 2
                        dma("sp", pt32[s_][:rows, :], pvec[t0:t0 + rows, :], writes=[pt32[s_]])
                        op("act", lambda e: e.activation(out=ptb[s_][:rows, :], in_=pt32[s_][:rows, :], func=AF.Copy), reads=[pt32[s_]], writes=[ptb[s_]])
                        bk = banks[s_]
                        tp = bk.t[:].bitcast(BF16)
                        for k2 in range(2):
                            op("pe", lambda e: e.transpose(out=tp[:, k2 * 128:k2 * 128 + rows], in_=ptb[s_][:rows, k2 * 128:(k2 + 1) * 128], identity=ident_b[:rows, :rows]),
                               reads=[ptb[s_], ident_b], writes=[bk])
                        op("act", lambda e: e.activation(out=pT[:, :, lc:lc + rows], in_=tp[:, 0:256].rearrange("p (k t) -> p k t", k=2)[:, :, :rows], func=AF.Copy),
                           reads=[bk], writes=[pT])
                    wgb = [kb.sb(p7, "wgb%d" % i, [128, 16, 512], BF16) for i in range(2)]
                    wpb = [kb.sb(p7, "wpb%d" % i, [128, 2, 512], BF16) for i in range(2)]
                    gsb = [kb.sb(p7, "gsb%d" % i, [128, 512], F32) for i in range(2)]
                    it7 = 0
                    for nb in range(4):
                        wg_, wp_ = wgb[nb % 2], wpb[nb % 2]
                        dma("pool", wg_[:], ple_gate[:, nb * 512:(nb + 1) * 512].rearrange("(kc p) n -> p kc n", p=128), writes=[wg_])
                        dma("pool", wp_[:], ple_proj[:, nb * 512:(nb + 1) * 512].rearrange("(kc p) n -> p kc n", p=128), writes=[wp_])
                        for ti, (t0, rows) in enumerate(tiles):
                            lc = t0 - c0
                            bG, bP = banks[2 + it7 % 2], banks[4 + it7 % 2]
                            gs_ = gsb[it7 % 2]
                            it7 += 1
                            for kc in range(16):
                                op("pe", lambda e: e.matmul(bG.t[:rows, :], lhsT=AX[:, kc, lc:lc + rows], rhs=wg_[:, kc, :], start=(kc == 0), stop=(kc == 15)),
                                   reads=[xnT_t[ti], wg_], writes=[bG])
                            for k2 in range(2):
                                op("pe", lambda e: e.matmul(bP.t[:rows, :], lhsT=pT[:, k2, lc:lc + rows], rhs=wp_[:, k2, :], start=(k2 == 0), stop=(k2 == 1)),
                                   reads=[pT, wp_], writes=[bP])
                            op("act", lambda e: e.activation(out=gs_[:rows, :], in_=bG.t[:rows, :], func=AF.Sigmoid), reads=[bG], writes=[gs_])
                            op("dve", lambda e: e.tensor_tensor(out=gs_[:rows, :], in0=gs_[:rows, :], in1=bP.t[:rows, :], op=ALU.mult), reads=[gs_, bP], writes=[gs_])
                            op("dve", lambda e: e.tensor_tensor(out=acc[:rows, ti, nb * 512:(nb + 1) * 512], in0=acc[:rows, ti, nb * 512:(nb + 1) * 512], in1=gs_[:rows, :], op=ALU.add),
                               reads=[gs_, acc_t[ti]], writes=[acc_t[ti]])
                chk("p7")
                if "h4" in dbg:
                    for ti, (t0, rows) in enumerate(tiles):
                        dma("sp", dbg["h4"][t0:t0 + rows, :], acc[:rows, ti, :], reads=[acc_t[ti]], is_output=True)
                scope("PF_%d" % pi)
                with contextlib.ExitStack() as pf:
                    junk = kb.sb(pf, "fjunk", [128, D], BF16)
                    yt = [kb.sb(pf, "yt%d" % i, [128, D], F32) for i in range(2)]
                    st = [kb.sb(pf, "fst%d" % i, [128, 4], F32) for i in range(2)]
                    for ti, (t0, rows) in enumerate(tiles):
                        s_ = ti % 2
                        op("act", lambda e: e.activation(out=junk[:rows, :], in_=acc[:rows, ti, :], func=AF.Square, accum_out=st[s_][:rows, 0:1]),
                           reads=[acc_t[ti]], writes=[junk, st[s_]])
                        op("dve", lambda e: e.tensor_scalar(out=st[s_][:rows, 1:2], in0=st[s_][:rows, 0:1], scalar1=1.0 / D, scalar2=EPS, op0=ALU.mult, op1=ALU.add),
                           reads=[st[s_]], writes=[st[s_]])
                        op("act", lambda e: e.activation(out=st[s_][:rows, 2:3], in_=st[s_][:rows, 1:2], func=AF.Sqrt), reads=[st[s_]], writes=[st[s_]])
                        op("dve", lambda e: e.reciprocal(out=st[s_][:rows, 3:4], in_=st[s_][:rows, 2:3]), reads=[st[s_]], writes=[st[s_]])
                        op("dve", lambda e: e.scalar_tensor_tensor(out=yt[s_][:rows, :], in0=acc[:rows, ti, :], scalar=st[s_][:rows, 3:4], in1=fgb[:rows, :], op0=ALU.mult, op1=ALU.mult),
                           reads=[acc_t[ti], st[s_], fgb], writes=[yt[s_]])
                        dma("sp", y_out[t0:t0 + rows, :], yt[s_][:rows, :], reads=[yt[s_]], is_output=True)

                if "h2" in dbg:
                    for ti, (t0, rows) in enumerate(tiles):
                        dma("sp", dbg["h2"][t0:t0 + rows, :], acc[:rows, ti, :], reads=[acc_t[ti]], is_output=True)
        scope(None)
        kb.finish()


def make_in_maps(inputs):
    cst = consts()
    maps = []
    for c in range(NC):
        m = dict(cst)
        m["x"] = np.ascontiguousarray(np.concatenate(
            [inputs["x_prompt"][c], inputs["x_sample"][NS * c:NS * (c + 1)].reshape(TS, D)], 0))
        m["w_in"] = np.ascontiguousarray(inputs["w_in"][0])
        m["norms"] = np.ascontiguousarray(np.stack([inputs["attn_norm"][0], inputs["ffn_norm"][0], inputs["ple_norm"][0], inputs["final_norm"]]))
        m["peer_wq"] = np.ascontiguousarray(inputs["peer_wq"][0])
        m["peer_sk"] = np.ascontiguousarray(inputs["peer_subkeys"][0].reshape(16 * 128, 128))
        m["peer_u"] = np.ascontiguousarray(inputs["peer_u"][0])
        m["peer_v"] = np.ascontiguousarray(inputs["peer_v"][0])
        m["ple_gate"] = np.ascontiguousarray(inputs["ple_gate"][0])
        m["ple_proj"] = np.ascontiguousarray(inputs["ple_proj"][0])
        m["ck"] = inputs["cache_k"][0].reshape(2560 * 128, 256)
        m["cv"] = inputs["cache_v"][0].reshape(2560 * 128, 256)
        m["cki"] = inputs["cache_kidx"][0].reshape(2560 * 128, 64)
        m["pt"] = np.ascontiguousarray(inputs["page_table"][NS * c:NS * (c + 1)].reshape(-1).astype(np.int32))
        m["pvec"] = np.ascontiguousarray(np.concatenate([inputs["p_prompt"][0, c], inputs["p_sample"][0, NS * c:NS * (c + 1)].reshape(TS, 256)], 0))
        for k_ in ("conv_w", "conv_b", "conv_ln_g", "conv_ln_b", "w_out"):
            m[k_] = np.ascontiguousarray(inputs[k_][0])
        m["state"] = np.ascontiguousarray(inputs["state_conv"][0, NS * c:NS * (c + 1)].reshape(NS * 30, 1024))
        maps.append(m)
    return maps


def kernel(**inputs):
    inputs = {k: np.asarray(v) for k, v in inputs.items()}
    nc = build()
    maps = make_in_maps(inputs)
    res = run_bass_kernel_spmd(nc, maps, core_ids=list(range(NC)))
    R = res.results
    B = 8
    k_p = np.stack([R[c]["k_out"][:SEQ].reshape(SEQ, 2, 128) for c in range(NC)])[None]
    v_p = np.stack([R[c]["v_out"][:SEQ].reshape(SEQ, 2, 128) for c in range(NC)])[None]
    ki_p = np.stack([R[c]["ki_out"][:SEQ] for c in range(NC)])[None]
    k_s = np.concatenate([R[c]["k_out"][SEQ:].reshape(NS, 4, 2, 128) for c in range(NC)])[None]
    v_s = np.concatenate([R[c]["v_out"][SEQ:].reshape(NS, 4, 2, 128) for c in range(NC)])[None]
    ki_s = np.concatenate([R[c]["ki_out"][SEQ:].reshape(NS, 4, 64) for c in range(NC)])[None]
    y_p = np.stack([R[c]["y"][:SEQ] for c in range(NC)])
    y_s = np.concatenate([R[c]["y"][SEQ:].reshape(NS, 4, D) for c in range(NC)])
    cp = np.stack([R[c]["conv_p"] for c in range(NC)])[None]
    cs = np.concatenate([R[c]["conv_s"] for c in range(NC)])[None]
    return (y_p, y_s, k_p, v_p, ki_p, cp, k_s, v_s, ki_s, cs)
```

```python
import contextlib
import numpy as np
import concourse.bass as bass
import concourse.mybir as mybir
from concourse.bass_utils import run_bass_kernel_spmd

F32 = mybir.dt.float32
BF16 = mybir.dt.bfloat16
I32 = mybir.dt.int32
U32 = mybir.dt.uint32
AF = mybir.ActivationFunctionType
ALU = mybir.AluOpType
AX_ = mybir.AxisListType

D = 2048
NC = 8
SEQ = 2048
NS = 16
TS = 64
T = SEQ + TS
N_IN = 4168
OFF_K, OFF_V, OFF_QI, OFF_KI, OFF_WI, OFF_GLU = 1024, 1280, 1536, 2048, 2112, 2120
EPS = 1e-6
NEG = -1.0e30


class Res:
    __slots__ = ("name", "w", "r", "excl")

    def __init__(self, name, excl=False):
        self.name = name
        self.w = None
        self.r = {}
        self.excl = excl


class Buf:
    def __init__(self, t, name):
        self.t = t
        self.res = Res(name)
        self.subs = []

    def sub(self, name, excl=False):
        r = Res(name, excl)
        self.subs.append(r)
        return r

    def __getitem__(self, idx):
        return self.t[idx]


class KB:
    def __init__(self, nc, es):
        self.nc = nc
        self.es = es
        self.E = {"pe": nc.tensor, "act": nc.scalar, "dve": nc.vector, "pool": nc.gpsimd, "sp": nc.sync}
        self.esem = {k: es.enter_context(nc.semaphore("sem_" + k)) for k in self.E}
        self.ecnt = {k: 0 for k in self.E}
        self.seen = {k: {} for k in self.E}
        self.dsem = {}
        self.dcnt = {}
        self.semname = {}
        self.out_tags = []
        self.nsem = 5

    def sb(self, es, name, shape, dtype):
        self.nalloc = getattr(self, "nalloc", 0) + 1
        b = Buf(es.enter_context(self.nc.sbuf_tensor("s%d_%s" % (self.nalloc, name), list(shape), dtype)), name)
        es.callback(self.release, b)
        return b

    def release(self, buf):
        tags = []
        for res in [buf.res] + buf.subs:
            tags += list(res.r.values()) + ([res.w] if res.w is not None else [])
        for eng in self.E:
            for tg in tags:
                self._wait(eng, tg)
        for res in [buf.res] + buf.subs:
            for kind in ("w", "r"):
                for qc in ("sw", "hw"):
                    s_ = self.dsem.pop((id(res), kind, qc), None)
                    if s_ is not None:
                        if not hasattr(self, "free_sems"):
                            self.free_sems = {"sw": [], "hw": []}
                        self.free_sems[qc].append(s_)

    def _wait(self, eng, dep):
        sem, val = dep
        key = id(sem)
        if eng == "pe" and sem is self.esem["pe"]:
            return
        if self.seen[eng].get(key, 0) >= val:
            return
        self.E[eng].wait_ge(sem, val)
        self.seen[eng][key] = val

    def _deps(self, eng, reads, writes, skip_waw_sem=None):
        for r in reads:
            if r.w is not None:
                self._wait(eng, r.w)
            if r.excl:
                for d in r.r.values():
                    if d[0] is not self.esem.get(eng):
                        self._wait(eng, d)
        for w in writes:
            if w.w is not None and not (skip_waw_sem is not None and w.w[0] is skip_waw_sem):
                self._wait(eng, w.w)
            for d in w.r.values():
                self._wait(eng, d)

    @staticmethod
    def _res(xs):
        return [x.res if isinstance(x, Buf) else x for x in xs]

    def op(self, eng, fn, reads=(), writes=()):
        reads = self._res(reads)
        writes = self._res(writes)
        self._deps(eng, reads, writes)
        ins = fn(self.E[eng])
        self.ecnt[eng] += 1
        ins.then_inc(self.esem[eng], 1)
        tag = (self.esem[eng], self.ecnt[eng])
        for r in reads:
            r.r[id(tag[0])] = tag
        for w in writes:
            w.w = tag
            w.r = {}
        return ins

    def _dma_sem(self, res, kind, q="sp"):
        qc = "sw" if q == "pool" else "hw"
        key = (id(res), kind, qc)
        if key not in self.dsem:
            if not hasattr(self, "free_sems"):
                self.free_sems = {"sw": [], "hw": []}
            free = self.free_sems[qc]
            if free:
                s = free.pop()
            else:
                self.nsem += 1
                s = self.es.enter_context(self.nc.semaphore("d%d" % self.nsem))
                self.dcnt[id(s)] = 0
            self.dsem[key] = s
        return self.dsem[key]

    def dma(self, q, out, in_, reads=(), writes=(), is_output=False, **kw):
        reads = self._res(reads)
        writes = self._res(writes)
        if writes:
            sem = self._dma_sem(writes[0], "w", q)
        else:
            sem = self._dma_sem(reads[0], "r", q)
        self._deps(q, reads, writes, skip_waw_sem=sem)
        if q == "pool":
            kw.setdefault("max_dma_last_dim", 2048)
        ins = self.E[q].dma_start(out=out, in_=in_, **kw)
        self.dcnt[id(sem)] += 16
        ins.then_inc(sem, 16)
        tag = (sem, self.dcnt[id(sem)])
        for r in reads:
            r.r[id(sem)] = tag
        for w in writes:
            w.w = tag
            w.r = {}
        if is_output:
            self.out_tags.append(tag)
        return ins

    def idma(self, out, in_, idx_ap, reads=(), writes=()):
        reads = self._res(reads)
        writes = self._res(writes)
        sem = self._dma_sem(writes[0], "w", "pool")
        self._deps("pool", reads, writes, skip_waw_sem=sem)
        ins = self.nc.gpsimd.indirect_dma_start(out=out, out_offset=None, in_=in_,
                                                in_offset=bass.IndirectOffsetOnAxis(ap=idx_ap, axis=0))
        self.dcnt[id(sem)] += 16
        ins.then_inc(sem, 16)
        tag = (sem, self.dcnt[id(sem)])
        for r in reads:
            r.r[id(sem)] = tag
        for w in writes:
            w.w = tag
            w.r = {}
        return ins

    def finish(self):
        last = {}
        for sem, val in self.out_tags:
            k = id(sem)
            if k not in last or last[k][1] < val:
                last[k] = (sem, val)
        for tag in last.values():
            self._wait("sp", tag)
        for e in ("pe", "act", "dve", "pool"):
            if self.ecnt[e]:
                self._wait("sp", (self.esem[e], self.ecnt[e]))


def rope_tables():
    pos = np.concatenate([np.arange(SEQ, dtype=np.float32), np.tile(2048.0 + np.arange(4, dtype=np.float32), NS)])
    out = np.zeros((T, 96), np.float32)
    for rot, off in ((32, 0), (16, 64)):
        half = rot // 2
        inv = (np.float32(500000.0) ** (-np.arange(half, dtype=np.float32) * np.float32(2.0) / np.float32(rot))).astype(np.float32)
        ang = pos[:, None] * inv[None, :]
        c, s = np.cos(ang), np.sin(ang)
        out[:, off:off + rot] = np.concatenate([c, c], 1)
        out[:, off + rot:off + 2 * rot] = np.concatenate([-s, s], 1)
    return out


def consts():
    cm = np.where(np.arange(128)[None, :] <= np.arange(128)[:, None], 0.0, NEG).astype(np.float32)
    r_ = np.arange(64)
    cms = np.where((r_[:, None] // 4 == r_[None, :] // 4) & (r_[None, :] % 4 <= r_[:, None] % 4), 0.0, NEG).astype(np.float32)
    sel = np.zeros((64, 16, 4, 4), np.float32)
    for s_ in range(16):
        for i_ in range(4):
            sel[4 * s_ + i_, s_, :, i_] = 1.0
    return {"ident_f": np.eye(128, dtype=np.float32), "cmask": cm, "rope": rope_tables(), "cmaskS": cms, "selS": sel.reshape(64, 256),
            "pidx": np.arange(128, dtype=np.float32).reshape(128, 1),
            "iota128": np.tile(np.arange(128, dtype=np.float32)[None, :], (128, 1))}


class _Stop(Exception):
    pass


def build(stage=99, debug=()):
    nc = bass.Bass("TRN2", target_bir_lowering=False)
    try:
        _build(nc, stage, debug)
    except _Stop:
        pass
    return nc


def _build(nc, stage, debug):
    import os

    _cur = [None]

    def scope(name):
        if os.environ.get("KSCOPE"):
            if _cur[0] is not None:
                nc.leave_named_scope(_cur[0][0], _cur[0][1], False)
            _cur[0] = None
            if name is not None:
                sid, _ = nc.enter_named_scope(name, False)
                _cur[0] = (name, sid)

    def chk(tag):
        if os.environ.get("KSTOP") == tag:
            scope(None)
            kb.finish()
            raise _Stop()

    def din(name, shape, dt=F32):
        return nc.dram_tensor(name, list(shape), dt, kind="ExternalInput").ap()

    def dout(name, shape, dt=F32):
        return nc.dram_tensor(name, list(shape), dt, kind="ExternalOutput").ap()

    x = din("x", [T, D])
    w_in = din("w_in", [D, N_IN])
    rope = din("rope", [T, 96])
    ident_f_d = din("ident_f", [128, 128])
    cmask_d = din("cmask", [128, 128])
    k_out = dout("k_out", [T, 256])
    v_out = dout("v_out", [T, 256])
    ki_out = dout("ki_out", [T, 64])
    w_out = din("w_out", [D, D])
    norms = din("norms", [4, D])
    peer_wq = din("peer_wq", [D, D])
    peer_sk = din("peer_sk", [16 * 128, 128])
    peer_u = din("peer_u", [16384, D])
    peer_v = din("peer_v", [16384, D])
    ple_gate = din("ple_gate", [D, D])
    ple_proj = din("ple_proj", [256, D])
    pvec = din("pvec", [T, 256])
    iota_d = din("iota128", [128, 128])
    ck = din("ck", [2560 * 128, 256])
    cv = din("cv", [2560 * 128, 256])
    cki = din("cki", [2560 * 128, 64])
    pt_d = din("pt", [NS * 16], I32)
    cmaskS_d = din("cmaskS", [64, 64])
    selS_d = din("selS", [64, 256])
    pidx_d = din("pidx", [128, 1])
    y_out = dout("y", [T, D])
    Gd = nc.dram_tensor("Gd", [128, 128, T], BF16, kind="Internal").ap()
    uTd = nc.dram_tensor("uTd", [128, 128, 16, 128], BF16, kind="Internal").ap()
    vd = nc.dram_tensor("vd", [128, 128, D], BF16, kind="Internal").ap()
    conv_w = din("conv_w", [31, 1024])
    conv_b = din("conv_b", [1024])
    conv_ln_g = din("conv_ln_g", [1024])
    conv_ln_b = din("conv_ln_b", [1024])
    state = din("state", [NS * 30, 1024])
    conv_p = dout("conv_p", [30, 1024])
    conv_s = dout("conv_s", [NS, 30, 1024])
    dbg = {n: dout("dbg_" + n, shp) for n, shp in debug}

    with contextlib.ExitStack() as es:
        kb = KB(nc, es)
        op, dma = kb.op, kb.dma
        banks = [Buf(es.enter_context(nc.psum_tensor("bank%d" % i, [128, 512], F32)), "bank%d" % i) for i in range(8)]
        for b_ in banks:
            b_.res.excl = True

        ident_f = kb.sb(es, "ident_f", [128, 128], F32)
        ident_b = kb.sb(es, "ident_b", [128, 128], BF16)
        cmask = kb.sb(es, "cmask", [128, 128], F32)
        gcols = kb.sb(es, "gcols", [128, 4, 16], F32)
        gcol = gcols[:, 0, :]
        gcol_r = gcols
        fgb = kb.sb(es, "fgb", [128, D], F32)
        iotaC = kb.sb(es, "iotaC", [128, 128], BF16)
        iota16 = kb.sb(es, "iota16", [128, 16], F32)
        skT = kb.sb(es, "skT", [128, 16, 128], BF16)
        dma("sp", ident_f[:], ident_f_d, writes=[ident_f])
        dma("sp", cmask[:], cmask_d, writes=[cmask])
        for w_ in range(4):
            dma("sp", gcols[:, w_, :], norms[w_].rearrange("(kc p) -> p kc", p=128), writes=[gcols], allow_slow_non_contiguous=True)
        dma("sp", fgb[:], norms[3].partition_broadcast(128), writes=[fgb])
        op("dve", lambda e: e.tensor_copy(out=ident_b[:], in_=ident_f[:]), reads=[ident_f], writes=[ident_b])
        with contextlib.ExitStack() as c1s:
            io32 = kb.sb(c1s, "io32", [128, 128], F32)
            dma("sp", io32[:], iota_d, writes=[io32])
            op("dve", lambda e: e.tensor_copy(out=iotaC[:], in_=io32[:]), reads=[io32], writes=[iotaC])
            op("dve", lambda e: e.tensor_copy(out=iota16[:], in_=io32[:, 0:16]), reads=[io32], writes=[iota16])
            sk32 = kb.sb(c1s, "sk32", [128, 16, 128], F32)
            dma("sp", sk32[:], peer_sk.rearrange("(b k) d -> k b d", k=128), writes=[sk32])
            for q4 in range(4):
                for b4 in range(4):
                    op("pe", lambda e: e.transpose(out=banks[q4].t[:, b4 * 128:(b4 + 1) * 128], in_=sk32[:, q4 * 4 + b4, :], identity=ident_f[:]),
                       reads=[sk32, ident_f], writes=[banks[q4]])
                op("act", lambda e: e.activation(out=skT[:, q4 * 4:q4 * 4 + 4, :], in_=banks[q4].t[:, :].rearrange("p (b k) -> p b k", b=4), func=AF.Copy),
                   reads=[banks[q4]], writes=[skT])
        chk("p0")
        ones_f = kb.sb(es, "ones_f", [128, 128], F32)
        ones_b = kb.sb(es, "ones_b", [128, 128], BF16)
        I4 = kb.sb(es, "I4", [128, 4, 128], BF16)
        thr0 = kb.sb(es, "thr0", [128, 1], F32)
        op("pool", lambda e: e.memset(ones_b[:], 1.0), writes=[ones_b])
        op("pool", lambda e: e.memset(thr0[:], -1.0e29), writes=[thr0])
        op("dve", lambda e: e.tensor_copy(out=I4[:], in_=ident_f[:].unsqueeze(1).broadcast_to([128, 4, 128])), reads=[ident_f], writes=[I4])
        op("pool", lambda e: e.memset(ones_f[:], 1.0), writes=[ones_f])
        cwT = kb.sb(es, "cwT", [128, 8, 31], F32)
        ccol = kb.sb(es, "ccol", [128, 3, 8], F32)
        halo = kb.sb(es, "halo", [128, 8, 30], BF16)
        with contextlib.ExitStack() as c0s:
            cw_sb = kb.sb(c0s, "cw_sb", [31, 1024], F32)
            dma("sp", cw_sb[:], conv_w, writes=[cw_sb])
            for i_, src in enumerate((conv_b, conv_ln_g, conv_ln_b)):
                dma("sp", ccol[:, i_, :], src.rearrange("(g p) -> p g", p=128), writes=[ccol], allow_slow_non_contiguous=True)
            for g in range(8):
                op("pe", lambda e: e.transpose(out=banks[0].t[:, g * 31:(g + 1) * 31], in_=cw_sb[:31, g * 128:(g + 1) * 128],
                                               identity=ident_f[:31, :31]), reads=[cw_sb, ident_f], writes=[banks[0]])
            op("act", lambda e: e.activation(out=cwT[:], in_=banks[0].t[:, 0:248].rearrange("p (g j) -> p g j", g=8), func=AF.Copy),
               reads=[banks[0]], writes=[cwT])

        Gd_res = Res("Gd")
        uTd_res = Res("uTd")
        vd_res = Res("vd")
        kT = kb.sb(es, "kT", [128, 2, T], BF16)
        Vb = kb.sb(es, "Vb", [128, 17, 256], BF16)
        kiT = kb.sb(es, "kiT", [64, T], BF16)
        wiS = kb.sb(es, "wiS", [128, 17, 8], F32)

        tiles_all = [(i * 128, 128) for i in range(16)] + [(SEQ, TS)]
        passes = [tiles_all[:6], tiles_all[6:12], tiles_all[12:]]
        import os
        if os.environ.get("KPASS"):
            szs = [int(v_) for v_ in os.environ["KPASS"].split(",")]
            passes, o_ = [], 0
            for z_ in szs:
                passes.append(tiles_all[o_:o_ + z_])
                o_ += z_
        if os.environ.get("KSEL"):
            passes = [[tiles_all[int(v_)] for v_ in grp.split(",")] for grp in os.environ["KSEL"].split(";")]
        if os.environ.get("KTILES"):
            passes = [tiles_all[:int(os.environ["KTILES"])]]

        for pi, tiles in enumerate(passes):
            c0 = tiles[0][0]
            Tp = sum(r for _, r in tiles)
            Tpp = sum(r for t0_, r in tiles if t0_ < SEQ)
            has_s = any(t0_ >= SEQ for t0_, _ in tiles)
            last_pass = (tiles[-1][0] + tiles[-1][1] >= SEQ)
            chunks = []
            cc_ = 0
            while cc_ < Tpp:
                n_ = min(512, Tpp - cc_)
                chunks.append((cc_, n_, False))
                cc_ += n_
            if has_s:
                chunks.append((Tpp, TS, True))

            def tiles_of(cl0, n):
                return [i for i, (t0_, r_) in enumerate(tiles) if (t0_ - c0) < cl0 + n and (t0_ - c0 + r_) > cl0]

            with contextlib.ExitStack() as ps:
                AX = kb.sb(ps, "AX%d" % pi, [128, 16, Tp], BF16)
                xnT = AX
                actT = AX
                xnT_t = [AX.sub("AX%d_%d" % (pi, i)) for i in range(len(tiles))]
                actT_t = xnT_t
                pa = contextlib.ExitStack()
                ps_real = ps
                ps = pa
                gluT = kb.sb(ps, "gluT%d" % pi, [128, 8, 30 + Tpp], BF16)
                gluT_g = [gluT.sub("gluT%d_%d" % (pi, g)) for g in range(8)]
                if has_s:
                    gluS = kb.sb(ps, "gluS", [128, 8, NS, 34], BF16)
                    gluS_g = [gluS.sub("gluS_%d" % g) for g in range(8)]
                if last_pass:
                    gl32 = kb.sb(ps, "gl32", [128, 8, 96], F32)
                qT = kb.sb(ps, "qT%d" % pi, [128, 8, Tp], BF16)
                qiT = kb.sb(ps, "qiT%d" % pi, [64, 8, Tp], BF16)
                ps = ps_real
                qT_t = [qT.sub("qT%d_%d" % (pi, i)) for i in range(len(tiles))]
                qiT_t = [qiT.sub("qiT%d_%d" % (pi, i)) for i in range(len(tiles))]
                scope("P1_%d" % pi)
                with contextlib.ExitStack() as p1:
                    xt = [kb.sb(p1, "xt%d" % i, [128, D], F32) for i in range(2)]
                    junk = kb.sb(p1, "junk", [128, D], BF16)
                    xs = [kb.sb(p1, "xs%d" % i, [128, D], BF16) for i in range(2)]
                    st = [kb.sb(p1, "st%d" % i, [128, 4], F32) for i in range(2)]
                    for ti, (t0, rows) in enumerate(tiles):
                        s = ti % 2
                        lc = t0 - c0
                        dma("sp", xt[s][:rows, :], x[t0:t0 + rows, :], writes=[xt[s]])
                        op("act", lambda e: e.activation(out=junk[:rows, :], in_=xt[s][:rows, :], func=AF.Square,
                                                         accum_out=st[s][:rows, 0:1]), reads=[xt[s]], writes=[junk, st[s]])
                        op("dve", lambda e: e.tensor_scalar(out=st[s][:rows, 1:2], in0=st[s][:rows, 0:1], scalar1=1.0 / D,
                                                            scalar2=EPS, op0=ALU.mult, op1=ALU.add), reads=[st[s]], writes=[st[s]])
                        op("act", lambda e: e.activation(out=st[s][:rows, 2:3], in_=st[s][:rows, 1:2], func=AF.Sqrt),
                           reads=[st[s]], writes=[st[s]])
                        op("dve", lambda e: e.reciprocal(out=st[s][:rows, 3:4], in_=st[s][:rows, 2:3]), reads=[st[s]], writes=[st[s]])
                        op("act", lambda e: e.activation(out=xs[s][:rows, :], in_=xt[s][:rows, :], func=AF.Copy,
                                                         scale=st[s][:rows, 3:4]), reads=[xt[s], st[s]], writes=[xs[s]])
                        for half in range(2):
                            bk = banks[(2 * ti + half) % 4]
                            tp = bk.t[:].bitcast(BF16)
                            for k8 in range(8):
                                kc = half * 8 + k8
                                op("pe", lambda e: e.transpose(out=tp[:, k8 * 128:k8 * 128 + rows], in_=xs[s][:rows, kc * 128:(kc + 1) * 128],
                                                               identity=ident_b[:rows, :rows]), reads=[xs[s], ident_b], writes=[bk])
                            op("dve", lambda e: e.tensor_tensor(
                                out=xnT[:, half * 8:half * 8 + 8, lc:lc + rows],
                                in0=tp.rearrange("p (k t) -> p k t", k=8)[:, :, :rows],
                                in1=gcols[:, 0, half * 8:half * 8 + 8].unsqueeze(2).broadcast_to([128, 8, rows]), op=ALU.mult),
                               reads=[bk, gcols], writes=[xnT_t[ti]])

                chk("p1")
                if "xnT" in dbg and pi == 0:
                    dma("pool", dbg["xnT"][:, :, 0:Tp], xnT[:], reads=xnT_t, is_output=True)

                chk("p1d")
                scope("P2_%d" % pi)
                with contextlib.ExitStack() as p2:
                    wblk = [kb.sb(p2, "wblk%d" % i, [128, 16, 512], BF16) for i in range(2)]
                    rp = [kb.sb(p2, "rp%d" % i, [128, 96], F32) for i in range(2)]
                    zb = [kb.sb(p2, "zb%d" % i, [128, 512], BF16) for i in range(2)]
                    z32 = [kb.sb(p2, "z32%d" % i, [128, 512], F32) for i in range(2)]
                    ra = [kb.sb(p2, "ra%d" % i, [128, 256], F32) for i in range(2)]
                    rb = [kb.sb(p2, "rb%d" % i, [128, 256], F32) for i in range(2)]
                    blocks = [("q", 0, 512), ("q", 512, 512), ("kv", 1024, 512), ("qi", 1536, 512), ("kw", 2048, 72)]
                    it = 0
                    for bi, (kind, col0, ncol) in enumerate(blocks):
                        wb = wblk[bi % 2]
                        dma("pool", wb[:, :, :ncol], w_in[:, col0:col0 + ncol].rearrange("(kc p) n -> p kc n", p=128), writes=[wb])
                        for ti, (t0, rows) in enumerate(tiles):
                            lc = t0 - c0
                            tile_id = t0 // 128
                            s = it % 2
                            it += 1
                            zp = banks[4 + s]
                            for kc in range(16):
                                op("pe", lambda e: e.matmul(zp.t[:rows, :ncol], lhsT=xnT[:, kc, lc:lc + rows], rhs=wb[:, kc, :ncol],
                                                            start=(kc == 0), stop=(kc == 15)), reads=[xnT_t[ti], wb], writes=[zp])
                            chk("m")
                            dma("sp", rp[s][:rows, :], rope[t0:t0 + rows, :], writes=[rp[s]])
                            chk("rd")

                            def do_rope(dst, H, Dh, R, tb, col_off=0):
                                half = R // 2
                                zv = zp.t[:rows, col_off:col_off + H * Dh].rearrange("p (h d) -> p h d", h=H)
                                cs = rp[s][:rows, tb:tb + R].unsqueeze(1).broadcast_to([rows, H, R])
                                sn1 = rp[s][:rows, tb + R:tb + R + half].unsqueeze(1).broadcast_to([rows, H, half])
                                sn2 = rp[s][:rows, tb + R + half:tb + 2 * R].unsqueeze(1).broadcast_to([rows, H, half])
                                A = ra[s][:rows, :H * R].rearrange("p (h r) -> p h r", h=H)
                                B = rb[s][:rows, :H * R].rearrange("p (h r) -> p h r", h=H)
                                op("dve", lambda e: e.tensor_tensor(out=A, in0=zv[:, :, 0:R], in1=cs, op=ALU.mult), reads=[zp, rp[s]], writes=[ra[s]])
                                op("dve", lambda e: e.tensor_tensor(out=B[:, :, 0:half], in0=zv[:, :, half:R], in1=sn1, op=ALU.mult), reads=[zp, rp[s]], writes=[rb[s]])
                                op("dve", lambda e: e.tensor_tensor(out=B[:, :, half:R], in0=zv[:, :, 0:half], in1=sn2, op=ALU.mult), reads=[zp, rp[s]], writes=[rb[s]])
                                return A, B

                            if kind == "q":
                                h0 = col0 // 128
                                A, B = do_rope(None, 4, 128, 32, 0)
                                chk("r")
                                op("act", lambda e: e.activation(out=zb[s][:rows, :], in_=zp.t[:rows, :], func=AF.Copy), reads=[zp], writes=[zb[s]])
                                op("dve", lambda e: e.tensor_tensor(out=zb[s][:rows, :].rearrange("p (h d) -> p h d", h=4)[:, :, 0:32], in0=A, in1=B, op=ALU.add),
                                   reads=[ra[s], rb[s]], writes=[zb[s]])
                                chk("z")
                                tb_ = banks[(it % 2)]
                                tpv = tb_.t[:].bitcast(BF16)
                                for h in range(4):
                                    op("pe", lambda e: e.transpose(out=tpv[:, h * 128:h * 128 + rows], in_=zb[s][:rows, h * 128:(h + 1) * 128],
                                                                   identity=ident_b[:rows, :rows]), reads=[zb[s], ident_b], writes=[tb_])
                                chk("t")
                                op("act", lambda e: e.activation(out=qT[:, h0:h0 + 4, lc:lc + rows],
                                                                 in_=tpv[:, 0:512].rearrange("p (h t) -> p h t", h=4)[:, :, :rows], func=AF.Copy),
                                   reads=[tb_], writes=[qT_t[ti]])
                            elif kind == "kv":
                                A, B = do_rope(None, 2, 128, 32, 0)
                                op("act", lambda e: e.activation(out=z32[s][:rows, :], in_=zp.t[:rows, :], func=AF.Copy), reads=[zp], writes=[z32[s]])
                                op("dve", lambda e: e.tensor_tensor(out=z32[s][:rows, 0:256].rearrange("p (h d) -> p h d", h=2)[:, :, 0:32], in0=A, in1=B, op=ALU.add),
                                   reads=[ra[s], rb[s]], writes=[z32[s]])
                                dma("sp", k_out[t0:t0 + rows, :], z32[s][:rows, 0:256], reads=[z32[s]], is_output=True)
                                dma("sp", v_out[t0:t0 + rows, :], z32[s][:rows, 256:512], reads=[z32[s]], is_output=True)
                                op("act", lambda e: e.activation(out=Vb[:rows, tile_id, :], in_=z32[s][:rows, 256:512], func=AF.Copy), reads=[z32[s]], writes=[Vb])
                                op("act", lambda e: e.activation(out=zb[s][:rows, 0:256], in_=z32[s][:rows, 0:256], func=AF.Copy), reads=[z32[s]], writes=[zb[s]])
                                tb_ = banks[(it % 2)]
                                tpv = tb_.t[:].bitcast(BF16)
                                for g in range(2):
                                    op("pe", lambda e: e.transpose(out=tpv[:, g * 128:g * 128 + rows], in_=zb[s][:rows, g * 128:(g + 1) * 128],
                                                                   identity=ident_b[:rows, :rows]), reads=[zb[s], ident_b], writes=[tb_])
                                op("act", lambda e: e.activation(out=kT[:, :, t0:t0 + rows],
                                                                 in_=tpv[:, 0:256].rearrange("p (h t) -> p h t", h=2)[:, :, :rows], func=AF.Copy),
                                   reads=[tb_], writes=[kT])
                            elif kind == "qi":
                                A, B = do_rope(None, 8, 64, 16, 64)
                                op("act", lambda e: e.activation(out=zb[s][:rows, :], in_=zp.t[:rows, :], func=AF.Copy), reads=[zp], writes=[zb[s]])
                                op("dve", lambda e: e.tensor_tensor(out=zb[s][:rows, :].rearrange("p (h d) -> p h d", h=8)[:, :, 0:16], in0=A, in1=B, op=ALU.add),
                                   reads=[ra[s], rb[s]], writes=[zb[s]])
                                tb_ = banks[(it % 2)]
                                tpv = tb_.t[:].bitcast(BF16)
                                for h in range(8):
                                    op("pe", lambda e: e.transpose(out=tpv[0:64, h * 128:h * 128 + rows], in_=zb[s][:rows, h * 64:(h + 1) * 64],
                                                                   identity=ident_b[:rows, :rows]), reads=[zb[s], ident_b], writes=[tb_])
                                op("act", lambda e: e.activation(out=qiT[:, :, lc:lc + rows],
                                                                 in_=tpv[0:64, :].rearrange("p (h t) -> p h t", h=8)[:, :, :rows], func=AF.Copy),
                                   reads=[tb_], writes=[qiT_t[ti]])
                            else:
                                A, B = do_rope(None, 1, 64, 16, 64)
                                op("act", lambda e: e.activation(out=z32[s][:rows, 0:72], in_=zp.t[:rows, 0:72], func=AF.Copy), reads=[zp], writes=[z32[s]])
                                op("dve", lambda e: e.tensor_tensor(out=z32[s][:rows, 0:16], in0=A[:, 0, :], in1=B[:, 0, :], op=ALU.add),
                                   reads=[ra[s], rb[s]], writes=[z32[s]])
                                dma("sp", ki_out[t0:t0 + rows, :], z32[s][:rows, 0:64], reads=[z32[s]], is_output=True)
                                op("dve", lambda e: e.tensor_scalar(out=wiS[:rows, tile_id, :], in0=z32[s][:rows, 64:72], scalar1=float(64 ** -0.5 * 8 ** -0.5),
                                                                    scalar2=None, op0=ALU.mult), reads=[z32[s]], writes=[wiS])
                                op("act", lambda e: e.activation(out=zb[s][:rows, 0:64], in_=z32[s][:rows, 0:64], func=AF.Copy), reads=[z32[s]], writes=[zb[s]])
                                tb_ = banks[(it % 2)]
                                tpv = tb_.t[:].bitcast(BF16)
                                op("pe", lambda e: e.transpose(out=tpv[0:64, 0:rows], in_=zb[s][:rows, 0:64],
                                                               identity=ident_b[:rows, :rows]), reads=[zb[s], ident_b], writes=[tb_])
                                op("act", lambda e: e.activation(out=kiT[:, t0:t0 + rows], in_=tpv[0:64, 0:rows], func=AF.Copy), reads=[tb_], writes=[kiT])

                        chk("b%d" % bi)
                scope("P3a_%d" % pi)
                with contextlib.ExitStack() as p3:
                    wga = [kb.sb(p3, "wga%d" % i, [128, 16, 256], BF16) for i in range(2)]
                    sg = [kb.sb(p3, "sg%d" % i, [128, 512], F32) for i in range(2)]
                    if pi == 0:
                        op("pool", lambda e: e.memset(gluT[:, :, 0:30], 0.0), writes=gluT_g)
                    else:
                        op("dve", lambda e: e.tensor_copy(out=gluT[:, :, 0:30], in_=halo[:]), reads=[halo], writes=gluT_g)
                    if has_s:
                        stt = [kb.sb(p3, "stt%d" % i, [120, 1024], F32) for i in range(2)]
                        for q4 in range(4):
                            st_ = stt[q4 % 2]
                            dma("sp", st_[:, :], state[q4 * 120:(q4 + 1) * 120, :], writes=[st_])
                            for sl in range(4):
                                dma("sp", conv_s[q4 * 4 + sl, 0:26, :], st_[sl * 30 + 4:sl * 30 + 30, :], reads=[st_], is_output=True)
                            for gh in range(2):
                                bk = banks[gh]
                                for g4 in range(4):
                                    g = gh * 4 + g4
                                    op("pe", lambda e: e.transpose(out=bk.t[:, g4 * 128:g4 * 128 + 120], in_=st_[:120, g * 128:(g + 1) * 128],
                                                                   identity=ident_f[:120, :120]), reads=[st_, ident_f], writes=[bk])
                                op("act", lambda e: e.activation(
                                    out=gluS[:, gh * 4:gh * 4 + 4, q4 * 4:q4 * 4 + 4, 0:30],
                                    in_=bk.t[:, :].rearrange("p (g x) -> p g x", g=4)[:, :, 0:120].rearrange("p g (s r) -> p g s r", s=4),
                                    func=AF.Copy), reads=[bk], writes=gluS_g[gh * 4:gh * 4 + 4])
                    it3 = 0
                    for g in range(8):
                        wg = wga[g % 2]
                        ca = OFF_GLU + g * 128
                        dma("pool", wg[:, :, 0:128], w_in[:, ca:ca + 128].rearrange("(kc p) n -> p kc n", p=128), writes=[wg])
                        dma("pool", wg[:, :, 128:256], w_in[:, ca + 1024:ca + 1152].rearrange("(kc p) n -> p kc n", p=128), writes=[wg])
                        for (cl0, n, is_s) in chunks:
                            s3 = it3 % 2
                            it3 += 1
                            bA, bB = banks[4 + s3], banks[6 + s3]
                            rt = [xnT_t[i] for i in tiles_of(cl0, n)]
                            for kc in range(16):
                                op("pe", lambda e: e.matmul(bA.t[:, :n], lhsT=wg[:, kc, 0:128], rhs=xnT[:, kc, cl0:cl0 + n],
                                                            start=(kc == 0), stop=(kc == 15)), reads=rt + [wg], writes=[bA])
                            for kc in range(16):
                                op("pe", lambda e: e.matmul(bB.t[:, :n], lhsT=wg[:, kc, 128:256], rhs=xnT[:, kc, cl0:cl0 + n],
                                                            start=(kc == 0), stop=(kc == 15)), reads=rt + [wg], writes=[bB])
                            op("act", lambda e: e.activation(out=sg[s3][:, :n], in_=bB.t[:, :n], func=AF.Sigmoid), reads=[bB], writes=[sg[s3]])
                            if not is_s:
                                op("dve", lambda e: e.tensor_tensor(out=gluT[:, g, 30 + cl0:30 + cl0 + n], in0=bA.t[:, :n], in1=sg[s3][:, :n], op=ALU.mult),
                                   reads=[bA, sg[s3]], writes=[gluT_g[g]])
                                if last_pass and cl0 + n == Tpp:
                                    op("dve", lambda e: e.tensor_tensor(out=gl32[:, g, 0:32], in0=bA.t[:, n - 32:n], in1=sg[s3][:, n - 32:n], op=ALU.mult),
                                       reads=[bA, sg[s3]], writes=[gl32])
                            else:
                                op("dve", lambda e: e.tensor_tensor(out=gluS[:, g, :, 30:34], in0=bA.t[:, :n].rearrange("p (s i) -> p s i", i=4),
                                                                    in1=sg[s3][:, :n].rearrange("p (s i) -> p s i", i=4), op=ALU.mult),
                                   reads=[bA, sg[s3]], writes=[gluS_g[g]])
                                op("dve", lambda e: e.tensor_tensor(out=gl32[:, g, 32:96], in0=bA.t[:, :n], in1=sg[s3][:, :n], op=ALU.mult),
                                   reads=[bA, sg[s3]], writes=[gl32])
                    if not last_pass:
                        op("dve", lambda e: e.tensor_copy(out=halo[:], in_=gluT[:, :, Tpp:Tpp + 30]), reads=gluT_g, writes=[halo])
                chk("p3a")
                actT_c = [[xnT_t[i] for i in tiles_of(cl0_, n_)] for (cl0_, n_, _s) in chunks]

                scope("P3b_%d" % pi)
                with contextlib.ExitStack() as p3:
                    Dg = [kb.sb(p3, "Dg%d" % i, [128, 31, 128], BF16) for i in range(2)]
                    yb = kb.sb(p3, "yb", [128, 8, 512], F32)
                    yb_g = [yb.sub("yb_%d" % g) for g in range(8)]
                    ysq = [kb.sb(p3, "ysq%d" % i, [128, 512], F32) for i in range(2)]
                    mu = kb.sb(p3, "mu", [128, 512], F32)
                    rs = kb.sb(p3, "rs", [128, 512], F32)
                    tmp = kb.sb(p3, "tmp", [128, 512], F32)
                    it3 = 0
                    for ci, (cl0, n, is_s) in enumerate(chunks):
                        S1, S2 = banks[2], banks[3]
                        for g in range(8):
                            s3 = it3 % 2
                            it3 += 1
                            dg = Dg[s3]
                            op("pool", lambda e: e.tensor_tensor(out=dg[:], in0=ident_b[:].unsqueeze(1).broadcast_to([128, 31, 128]),
                                                                 in1=cwT[:, g, :].unsqueeze(2).broadcast_to([128, 31, 128]), op=ALU.mult),
                               reads=[ident_b, cwT], writes=[dg])
                            bY = banks[s3]
                            for j in range(31):
                                if not is_s:
                                    rhs = gluT[:, g, cl0 + j:cl0 + j + n]
                                    rr = [gluT_g[g]]
                                    outp = bY.t[:, :n]
                                else:
                                    rhs = gluS[:, g, :, j:j + 4]
                                    rr = [gluS_g[g]]
                                    outp = bY.t[:, :n].rearrange("p (s i) -> p s i", i=4)
                                op("pe", lambda e: e.matmul(outp, lhsT=dg[:, j, :], rhs=rhs, start=(j == 0), stop=(j == 30)),
                                   reads=rr + [dg], writes=[bY])
                            op("act", lambda e: e.activation(out=yb[:, g, :n], in_=bY.t[:, :n], func=AF.Identity, bias=ccol[:, 0, g:g + 1]),
                               reads=[bY, ccol], writes=[yb_g[g]])
                            op("act", lambda e: e.activation(out=ysq[s3][:, :n], in_=bY.t[:, :n], func=AF.Square, bias=ccol[:, 0, g:g + 1]),
                               reads=[bY, ccol], writes=[ysq[s3]])
                            op("pe", lambda e: e.matmul(S1.t[:, :n], lhsT=ones_f[:], rhs=yb[:, g, :n], start=(g == 0), stop=(g == 7)),
                               reads=[ones_f, yb_g[g]], writes=[S1])
                            op("pe", lambda e: e.matmul(S2.t[:, :n], lhsT=ones_f[:], rhs=ysq[s3][:, :n], start=(g == 0), stop=(g == 7)),
                               reads=[ones_f, ysq[s3]], writes=[S2])
                        op("dve", lambda e: e.tensor_scalar(out=mu[:, :n], in0=S1.t[:, :n], scalar1=1.0 / 1024, scalar2=None, op0=ALU.mult), reads=[S1], writes=[mu])
                        op("dve", lambda e: e.tensor_tensor(out=tmp[:, :n], in0=mu[:, :n], in1=mu[:, :n], op=ALU.mult), reads=[mu], writes=[tmp])
                        op("dve", lambda e: e.scalar_tensor_tensor(out=tmp[:, :n], in0=S2.t[:, :n], scalar=1.0 / 1024, in1=tmp[:, :n], op0=ALU.mult, op1=ALU.subtract),
                           reads=[S2, tmp], writes=[tmp])
                        op("dve", lambda e: e.tensor_scalar(out=tmp[:, :n], in0=tmp[:, :n], scalar1=EPS, scalar2=None, op0=ALU.add), reads=[tmp], writes=[tmp])
                        op("act", lambda e: e.activation(out=tmp[:, :n], in_=tmp[:, :n], func=AF.Sqrt), reads=[tmp], writes=[tmp])
                        op("dve", lambda e: e.reciprocal(out=rs[:, :n], in_=tmp[:, :n]), reads=[tmp], writes=[rs])
                        for g in range(8):
                            op("dve", lambda e: e.tensor_tensor(out=yb[:, g, :n], in0=yb[:, g, :n], in1=mu[:, :n], op=ALU.subtract), reads=[yb_g[g], mu], writes=[yb_g[g]])
                            op("dve", lambda e: e.tensor_tensor(out=yb[:, g, :n], in0=yb[:, g, :n], in1=rs[:, :n], op=ALU.mult), reads=[yb_g[g], rs], writes=[yb_g[g]])
                            op("act", lambda e: e.activation(out=actT[:, 8 + g, cl0:cl0 + n], in_=yb[:, g, :n], func=AF.Silu,
                                                             scale=ccol[:, 1, g:g + 1], bias=ccol[:, 2, g:g + 1]),
                               reads=[yb_g[g], ccol], writes=actT_c[ci])
                    if last_pass:
                        cst = kb.sb(p3, "cst", [64, 1024], F32)
                        for (c_lo, c_n, which) in ((0, 32, "p"), (32, 64, "s")):
                            if which == "s" and not has_s:
                                continue
                            for gh in range(2):
                                bk = banks[4 + gh]
                                for g4 in range(4):
                                    g = gh * 4 + g4
                                    op("pe", lambda e: e.transpose(out=bk.t[:c_n, g4 * 128:(g4 + 1) * 128], in_=gl32[:, g, c_lo:c_lo + c_n],
                                                                   identity=ident_f[:, :]), reads=[gl32, ident_f], writes=[bk])
                                op("act", lambda e: e.activation(out=cst[:c_n, gh * 512:(gh + 1) * 512], in_=bk.t[:c_n, :], func=AF.Copy), reads=[bk], writes=[cst])
                            if which == "p":
                                dma("sp", conv_p[:, :], cst[2:32, :], reads=[cst], is_output=True)
                            else:
                                for s_ in range(NS):
                                    dma("sp", conv_s[s_, 26:30, :], cst[4 * s_:4 * s_ + 4, :], reads=[cst], is_output=True)
                chk("p3b")
                if "convT" in dbg:
                    for g in range(8):
                        dma("pool", dbg["convT"][:, g, c0:c0 + Tp], actT[:, 8 + g, :], reads=xnT_t, is_output=True)
                scope("P4_%d" % pi)
                with contextlib.ExitStack() as p4:
                    Rh = [kb.sb(p4, "Rh%d" % i, [128, 512], BF16) for i in range(3)]
                    Dw = [kb.sb(p4, "Dw%d" % i, [128, 8, 128], BF16) for i in range(2)]
                    iscA = [kb.sb(p4, "iscA%d" % i, [128, 2048], F32) for i in range(2)]
                    iscW = kb.sb(p4, "iscW", [128, 2048], F32)
                    mx8 = [kb.sb(p4, "mx8_%d" % i, [128, 8], F32) for i in range(2)]
                    maskb = [kb.sb(p4, "maskb%d" % i, [128, 2048], BF16) for i in range(2)]
                    PT = [kb.sb(p4, "PT%d" % i, [128, 512], BF16) for i in range(3)]
                    rsum = [kb.sb(p4, "rsum%d" % i, [128, 512], F32) for i in range(2)]
                    cnt = {"S": 0, "R": 0, "L": 0, "P": 0, "G": 0}
                    ptl = [(ti, t0) for ti, (t0, rows) in enumerate(tiles) if t0 < SEQ]

                    def p4_indexer(ti, t0):
                        j = t0 // 128
                        lc = t0 - c0
                        L = (j + 1) * 128
                        dw, ia = Dw[ti % 2], iscA[ti % 2]
                        op("pool", lambda e: e.tensor_tensor(out=dw[:], in0=ident_b[:].unsqueeze(1).broadcast_to([128, 8, 128]),
                                                             in1=wiS[:, j, :].unsqueeze(2).broadcast_to([128, 8, 128]), op=ALU.mult),
                           reads=[ident_b, wiS], writes=[dw])
                        nch = (L + 511) // 512
                        items = [(c4, h) for c4 in range(nch) for h in range(8)]
                        slots = {}

                        def S_(i):
                            c4, h = items[i]
                            l0 = c4 * 512
                            n = min(512, L - l0)
                            Sb = banks[cnt["S"] % 2]
                            cnt["S"] += 1
                            slots[i] = Sb
                            op("pe", lambda e: e.matmul(Sb.t[:, :n], lhsT=qiT[:, h, lc:lc + 128], rhs=kiT[:, l0:l0 + n], start=True, stop=True),
                               reads=[qiT_t[ti], kiT], writes=[Sb])

                        S_(0)
                        for i, (c4, h) in enumerate(items):
                            if i + 1 < len(items):
                                S_(i + 1)
                            l0 = c4 * 512
                            n = min(512, L - l0)
                            Sb = slots.pop(i)
                            iP = banks[2 + c4 % 2]
                            rh = Rh[cnt["R"] % 3]
                            cnt["R"] += 1
                            op("act", lambda e: e.activation(out=rh[:, :n], in_=Sb.t[:, :n], func=AF.Relu), reads=[Sb], writes=[rh])
                            op("pe", lambda e: e.matmul(iP.t[:, :n], lhsT=dw[:, h, :], rhs=rh[:, :n], start=(h == 0), stop=(h == 7)),
                               reads=[dw, rh], writes=[iP])
                            if h == 7:
                                op("act", lambda e: e.activation(out=ia[:, l0:l0 + n], in_=iP.t[:, :n], func=AF.Copy), reads=[iP], writes=[ia])
                        op("dve", lambda e: e.tensor_tensor(out=ia[:, j * 128:(j + 1) * 128], in0=ia[:, j * 128:(j + 1) * 128], in1=cmask[:], op=ALU.add),
                           reads=[ia, cmask], writes=[ia])
                        if "isc" in dbg and j == int(os.environ.get("KDBGJ", "3")):
                            dma("sp", dbg["isc"][:, 0:L], ia[:, 0:L], reads=[ia], is_output=True)

                    def p4_topk(ti, t0):
                        j = t0 // 128
                        L = (j + 1) * 128
                        ia, mb = iscA[ti % 2], maskb[ti % 2]
                        if j >= 2:
                            src = ia
                            for r in range(32):
                                m8 = mx8[r % 2]
                                op("dve", lambda e: e.max(out=m8[:], in_=src[:, :L]), reads=[src], writes=[m8])
                                if r < 31:
                                    op("dve", lambda e: e.match_replace(out=iscW[:, :L], in_to_replace=m8[:], in_values=src[:, :L], imm_value=NEG),
                                       reads=[src, m8], writes=[iscW])
                                    src = iscW
                            thr_ap, thr_r = m8[:, 7:8], m8
                        else:
                            thr_ap, thr_r = thr0[:, 0:1], thr0
                        op("dve", lambda e: e.tensor_scalar(out=mb[:, :L], in0=ia[:, :L], scalar1=thr_ap, scalar2=NEG, op0=ALU.is_lt, op1=ALU.mult),
                           reads=[ia, thr_r], writes=[mb])

                    def p4_attn(ti, t0):
                        j = t0 // 128
                        lc = t0 - c0
                        mb = maskb[ti % 2]
                        OT, SM = banks[6], banks[7]
                        items = [(g, lb) for g in range(2) for lb in range(j + 1)]
                        slots = {}

                        def LT_(i):
                            g, lb = items[i]
                            LT = banks[4 + cnt["L"] % 2]
                            cnt["L"] += 1
                            slots[i] = LT
                            op("pe", lambda e: e.matmul(LT.t[:, :], lhsT=kT[:, g, lb * 128:(lb + 1) * 128], rhs=qT[:, 4 * g:4 * g + 4, lc:lc + 128],
                                                        start=True, stop=False), reads=[kT, qT_t[ti]], writes=[LT])
                            op("pe", lambda e: e.matmul(LT.t[:, :], lhsT=mb[:, lb * 128:(lb + 1) * 128], rhs=I4[:], start=False, stop=True),
                               reads=[mb, I4], writes=[LT])

                        LT_(0)
                        for i, (g, lb) in enumerate(items):
                            if i + 1 < len(items):
                                LT_(i + 1)
                            LT = slots.pop(i)
                            pt = PT[cnt["P"] % 3]
                            cnt["P"] += 1
                            op("act", lambda e: e.activation(out=pt[:], in_=LT.t[:, :], func=AF.Exp, scale=float(128 ** -0.5)), reads=[LT], writes=[pt])
                            op("pe", lambda e: e.matmul(OT.t[:, :], lhsT=Vb[:, lb, g * 128:(g + 1) * 128], rhs=pt[:], start=(lb == 0), stop=(lb == j)),
                               reads=[Vb, pt], writes=[OT])
                            op("pe", lambda e: e.matmul(SM.t[:, :], lhsT=ones_b[:], rhs=pt[:], start=(lb == 0), stop=(lb == j)),
                               reads=[ones_b, pt], writes=[SM])
                            if lb == j:
                                rsm = rsum[cnt["G"] % 2]
                                cnt["G"] += 1
                                op("dve", lambda e: e.reciprocal(out=rsm[:], in_=SM.t[:, :]), reads=[SM], writes=[rsm])
                                op("dve", lambda e: e.tensor_tensor(out=actT[:, 4 * g:4 * g + 4, lc:lc + 128], in0=OT.t[:, :].rearrange("p (h t) -> p h t", h=4),
                                                                    in1=rsm[:].rearrange("p (h t) -> p h t", h=4), op=ALU.mult),
                                   reads=[OT, rsm], writes=[actT_t[ti]])

                    if ptl:
                        p4_indexer(*ptl[0])
                    for k_, (ti, t0) in enumerate(ptl):
                        if k_ + 1 < len(ptl):
                            p4_indexer(*ptl[k_ + 1])
                        p4_topk(ti, t0)
                        p4_attn(ti, t0)
                scope("P4s_%d" % pi)
                if has_s:
                    lcS = SEQ - c0
                    tiS = len(tiles) - 1
                    with contextlib.ExitStack() as p4s:
                        ptb_ = kb.sb(p4s, "ptb_", [128, 256], I32)
                        ptf = kb.sb(p4s, "ptf", [128, 256], F32)
                        pcol = kb.sb(p4s, "pcol", [128, 1], F32)
                        idxs = kb.sb(p4s, "idxs", [128, 256], U32)
                        cmS = kb.sb(p4s, "cmS", [64, 64], F32)
                        sel32 = kb.sb(p4s, "sel32", [64, 256], F32)
                        selS = kb.sb(p4s, "selS", [64, 256], BF16)
                        dma("sp", ptb_[:], pt_d.partition_broadcast(128), writes=[ptb_])
                        dma("sp", pcol[:], pidx_d, writes=[pcol])
                        dma("sp", cmS[:], cmaskS_d, writes=[cmS])
                        dma("sp", sel32[:], selS_d, writes=[sel32])
                        op("dve", lambda e: e.tensor_copy(out=selS[:], in_=sel32[:]), reads=[sel32], writes=[selS])
                        op("dve", lambda e: e.tensor_copy(out=ptf[:], in_=ptb_[:]), reads=[ptb_], writes=[ptf])
                        op("dve", lambda e: e.tensor_scalar(out=ptf[:], in0=ptf[:], scalar1=128.0, scalar2=pcol[:, 0:1], op0=ALU.mult, op1=ALU.add), reads=[ptf, pcol], writes=[ptf])
                        op("dve", lambda e: e.tensor_copy(out=idxs[:], in_=ptf[:]), reads=[ptf], writes=[idxs])
                        iscS = kb.sb(p4s, "iscS", [64, 2112], F32)
                        iscWs = kb.sb(p4s, "iscWs", [64, 2112], F32)
                        mbS = kb.sb(p4s, "mbS", [64, 2112], BF16)
                        m8s = [kb.sb(p4s, "m8s%d" % i, [64, 8], F32) for i in range(2)]
                        DwS = kb.sb(p4s, "DwS", [64, 8, 64], BF16)
                        op("pool", lambda e: e.tensor_tensor(out=DwS[:], in0=ident_b[0:64, 0:64].unsqueeze(1).broadcast_to([64, 8, 64]),
                                                             in1=wiS[0:64, 16, :].unsqueeze(2).broadcast_to([64, 8, 64]), op=ALU.mult), reads=[ident_b, wiS], writes=[DwS])
                        with contextlib.ExitStack() as pix:
                            qiZ = kb.sb(pix, "qiZ", [64, 8, NS, 64], BF16)
                            op("pool", lambda e: e.memset(qiZ[:], 0.0), writes=[qiZ])
                            for s_ in range(NS):
                                op("act", lambda e: e.activation(out=qiZ[:, :, s_, 4 * s_:4 * s_ + 4], in_=qiT[:, :, lcS + 4 * s_:lcS + 4 * s_ + 4], func=AF.Copy),
                                   reads=[qiT_t[tiS]], writes=[qiZ])
                            kis = [kb.sb(pix, "kis%d" % i, [128, 16, 64], BF16) for i in range(4)]
                            kiTs = [kb.sb(pix, "kiTs%d" % i, [64, 512], BF16) for i in range(2)]
                            RhS = [kb.sb(pix, "RhS%d" % i, [64, 512], BF16) for i in range(3)]
                            for h in range(8):
                                Sb = banks[4 + h % 2]
                                rh = RhS[h % 3]
                                op("pe", lambda e: e.matmul(Sb.t[0:64, 0:64], lhsT=qiT[:, h, lcS:lcS + 64], rhs=kiT[:, SEQ:SEQ + 64], start=True, stop=True),
                                   reads=[qiT_t[tiS], kiT], writes=[Sb])
                                op("act", lambda e: e.activation(out=rh[:, 0:64], in_=Sb.t[0:64, 0:64], func=AF.Relu), reads=[Sb], writes=[rh])
                                op("pe", lambda e: e.matmul(banks[6].t[0:64, 0:64], lhsT=DwS[:, h, :], rhs=rh[:, 0:64], start=(h == 0), stop=(h == 7)),
                                   reads=[DwS, rh], writes=[banks[6]])
                            op("dve", lambda e: e.tensor_tensor(out=iscS[:, 2048:2112], in0=banks[6].t[0:64, 0:64], in1=cmS[:], op=ALU.add), reads=[banks[6], cmS], writes=[iscS])
                            groups = [(s_, c4) for s_ in range(NS) for c4 in range(4)]
                            gk = {}
                            cq = {"q": 0, "S": 0, "R": 0}

                            def prep_(gi):
                                s_, c4 = groups[gi]
                                ks_ = kis[s_ % 4]
                                if c4 == 0:
                                    for pg in range(16):
                                        kb.idma(ks_[:, pg, :], cki, idxs[:, s_ * 16 + pg:s_ * 16 + pg + 1], reads=[idxs], writes=[ks_])
                                tb_ = banks[6 + cq["q"] % 2]
                                kt_ = kiTs[cq["q"] % 2]
                                cq["q"] += 1
                                tpv = tb_.t[:].bitcast(BF16)
                                for p4_ in range(4):
                                    op("pe", lambda e: e.transpose(out=tpv[0:64, p4_ * 128:(p4_ + 1) * 128], in_=ks_[:, c4 * 4 + p4_, :], identity=ident_b[:]),
                                       reads=[ks_, ident_b], writes=[tb_])
                                op("dve", lambda e: e.tensor_copy(out=kt_[:], in_=tpv[0:64, 0:512]), reads=[tb_], writes=[kt_])
                                gk[gi] = kt_

                            prep_(0)
                            for gi, (s_, c4) in enumerate(groups):
                                if gi + 1 < len(groups):
                                    prep_(gi + 1)
                                kt_ = gk.pop(gi)
                                sl = {}

                                def S2_(h):
                                    Sb = banks[4 + cq["S"] % 2]
                                    cq["S"] += 1
                                    sl[h] = Sb
                                    op("pe", lambda e: e.matmul(Sb.t[0:64, :], lhsT=qiZ[:, h, s_, :], rhs=kt_[:], start=True, stop=True), reads=[qiZ, kt_], writes=[Sb])

                                S2_(0)
                                for h in range(8):
                                    if h + 1 < 8:
                                        S2_(h + 1)
                                    Sb = sl.pop(h)
                                    rh = RhS[cq["R"] % 3]
                                    cq["R"] += 1
                                    op("act", lambda e: e.activation(out=rh[:], in_=Sb.t[0:64, :], func=AF.Relu), reads=[Sb], writes=[rh])
                                    op("pe", lambda e: e.matmul(banks[c4].t[0:64, :], lhsT=DwS[:, h, :], rhs=rh[:], start=(s_ == 0 and h == 0), stop=(s_ == NS - 1 and h == 7)),
                                       reads=[DwS, rh], writes=[banks[c4]])
                            for c4 in range(4):
                                op("act", lambda e: e.activation(out=iscS[:, c4 * 512:(c4 + 1) * 512], in_=banks[c4].t[0:64, :], func=AF.Copy), reads=[banks[c4]], writes=[iscS])
                        if "iscS" in dbg:
                            dma("sp", dbg["iscS"], iscS[:], reads=[iscS], is_output=True)
                        src = iscS
                        for r in range(32):
                            m8 = m8s[r % 2]
                            op("dve", lambda e: e.max(out=m8[:], in_=src[:, :]), reads=[src], writes=[m8])
                            if r < 31:
                                op("dve", lambda e: e.match_replace(out=iscWs[:, :], in_to_replace=m8[:], in_values=src[:, :], imm_value=NEG), reads=[src, m8], writes=[iscWs])
                                src = iscWs
                        op("dve", lambda e: e.tensor_scalar(out=mbS[:], in0=iscS[:], scalar1=m8[:, 7:8], scalar2=NEG, op0=ALU.is_lt, op1=ALU.mult), reads=[iscS, m8], writes=[mbS])
                        with contextlib.ExitStack() as pat:
                            Ks = [kb.sb(pat, "Ks%d" % i, [128, 16, 256], BF16) for i in range(4)]
                            Vs = [kb.sb(pat, "Vs%d" % i, [128, 16, 256], BF16) for i in range(4)]
                            kTs = [kb.sb(pat, "kTs%d" % i, [128, 2, 2048], BF16) for i in range(2)]
                            PTs = [kb.sb(pat, "PTs%d" % i, [128, 272], BF16) for i in range(2)]
                            rsS = [kb.sb(pat, "rsS%d" % i, [128, 32], F32) for i in range(2)]
                            ca = {"t": 0, "l": 0}

                            def gatherKV(s_):
                                K_, V_ = Ks[s_ % 4], Vs[s_ % 4]
                                for pg in range(16):
                                    kb.idma(K_[:, pg, :], ck, idxs[:, s_ * 16 + pg:s_ * 16 + pg + 1], reads=[idxs], writes=[K_])
                                    kb.idma(V_[:, pg, :], cv, idxs[:, s_ * 16 + pg:s_ * 16 + pg + 1], reads=[idxs], writes=[V_])

                            def prepK(s_):
                                K_, V_, kT_ = Ks[s_ % 4], Vs[s_ % 4], kTs[s_ % 2]
                                if s_ + 2 < NS:
                                    gatherKV(s_ + 2)
                                for g in range(2):
                                    for p8 in range(2):
                                        tb_ = banks[ca["t"] % 2]
                                        ca["t"] += 1
                                        tpv = tb_.t[:].bitcast(BF16)
                                        for k8 in range(8):
                                            pg = p8 * 8 + k8
                                            op("pe", lambda e: e.transpose(out=tpv[:, k8 * 128:(k8 + 1) * 128], in_=K_[:, pg, g * 128:(g + 1) * 128], identity=ident_b[:]),
                                               reads=[K_, ident_b], writes=[tb_])
                                        op("act", lambda e: e.activation(out=kT_[:, g, p8 * 1024:(p8 + 1) * 1024], in_=tpv[:, :], func=AF.Copy), reads=[tb_], writes=[kT_])

                            def attnS(s_):
                                K_, V_, kT_ = Ks[s_ % 4], Vs[s_ % 4], kTs[s_ % 2]
                                OT, SM = banks[4], banks[5]
                                sv = selS[:, s_ * 16:(s_ + 1) * 16]
                                lts = []
                                for g in range(2):
                                    LT = banks[2 + g]
                                    qv = qT[:, 4 * g:4 * g + 4, lcS + 4 * s_:lcS + 4 * s_ + 4]
                                    for lb in range(16):
                                        op("pe", lambda e: e.matmul(LT.t[:, lb * 16:(lb + 1) * 16], lhsT=kT_[:, g, lb * 128:(lb + 1) * 128], rhs=qv, start=True, stop=False),
                                           reads=[kT_, qT_t[tiS]], writes=[LT])
                                        op("pe", lambda e: e.matmul(LT.t[:, lb * 16:(lb + 1) * 16], lhsT=mbS[:, lb * 128:(lb + 1) * 128], rhs=sv, start=False, stop=True),
                                           reads=[mbS, selS], writes=[LT])
                                    op("pe", lambda e: e.matmul(LT.t[0:64, 256:272], lhsT=kT[:, g, SEQ:SEQ + 64], rhs=qv, start=True, stop=False), reads=[kT, qT_t[tiS]], writes=[LT])
                                    op("pe", lambda e: e.matmul(LT.t[0:64, 256:272], lhsT=mbS[:, 2048:2112], rhs=sv, start=False, stop=True), reads=[mbS, selS], writes=[LT])
                                for g in range(2):
                                    LT = banks[2 + g]
                                    pt = PTs[g]
                                    op("act", lambda e: e.activation(out=pt[:, 0:256], in_=LT.t[:, 0:256], func=AF.Exp, scale=float(128 ** -0.5)), reads=[LT], writes=[pt])
                                    op("act", lambda e: e.activation(out=pt[0:64, 256:272], in_=LT.t[0:64, 256:272], func=AF.Exp, scale=float(128 ** -0.5)), reads=[LT], writes=[pt])
                                for g in range(2):
                                    pt = PTs[g]
                                    for lb in range(17):
                                        if lb < 16:
                                            lv, pv, on = V_[:, lb, g * 128:(g + 1) * 128], pt[:, lb * 16:(lb + 1) * 16], ones_b[:]
                                        else:
                                            lv, pv, on = Vb[0:64, 16, g * 128:(g + 1) * 128], pt[0:64, 256:272], ones_b[0:64, :]
                                        op("pe", lambda e: e.matmul(OT.t[:, g * 16:(g + 1) * 16], lhsT=lv, rhs=pv, start=(lb == 0), stop=(lb == 16)), reads=[V_, Vb, pt], writes=[OT])
                                        op("pe", lambda e: e.matmul(SM.t[:, g * 16:(g + 1) * 16], lhsT=on, rhs=pv, start=(lb == 0), stop=(lb == 16)), reads=[ones_b, pt], writes=[SM])
                                rs_ = rsS[s_ % 2]
                                op("dve", lambda e: e.reciprocal(out=rs_[:], in_=SM.t[:, 0:32]), reads=[SM], writes=[rs_])
                                op("dve", lambda e: e.tensor_tensor(out=actT[:, 0:8, lcS + 4 * s_:lcS + 4 * s_ + 4], in0=OT.t[:, 0:32].rearrange("p (h i) -> p h i", i=4),
                                                                    in1=rs_[:].rearrange("p (h i) -> p h i", i=4), op=ALU.mult), reads=[OT, rs_], writes=[actT_t[tiS]])

                            gatherKV(0)
                            gatherKV(1)
                            prepK(0)
                            for s_ in range(NS):
                                if s_ + 1 < NS:
                                    prepK(s_ + 1)
                                attnS(s_)
                chk("p4")
                if "attnT" in dbg:
                    for h in range(8):
                        dma("pool", dbg["attnT"][:, h, c0:c0 + Tp], actT[:, h, :], reads=actT_t, is_output=True)
                if "qT" in dbg and pi == 0:
                    dma("pool", dbg["qT"][:, :, 0:Tp], qT[:], reads=qT_t, is_output=True)
                if "qiT" in dbg and pi == 0:
                    dma("pool", dbg["qiT"][:, :, 0:Tp], qiT[:], reads=qiT_t, is_output=True)
                pa.close()

                scope("P5_%d" % pi)
                acc = kb.sb(ps, "acc%d" % pi, [128, len(tiles), D], F32)
                acc_t = [acc.sub("acc%d_%d" % (pi, i)) for i in range(len(tiles))]

                def proj_resid(wsrc, nm):
                    with contextlib.ExitStack() as p5:
                        wblk = [kb.sb(p5, "w5_%d" % i, [128, 16, 512], BF16) for i in range(2)]
                        xb = [kb.sb(p5, "xb%d" % i, [128, 512], F32) for i in range(3)]
                        it5 = 0
                        for nb in range(4):
                            wb = wblk[nb % 2]
                            dma("pool", wb[:], wsrc[:, nb * 512:(nb + 1) * 512].rearrange("(kc p) n -> p kc n", p=128), writes=[wb])
                            for ti, (t0, rows) in enumerate(tiles):
                                lc = t0 - c0
                                bk = banks[it5 % 4]
                                xs_ = xb[it5 % 3]
                                it5 += 1
                                for kc in range(16):
                                    op("pe", lambda e: e.matmul(bk.t[:rows, :], lhsT=AX[:, kc, lc:lc + rows], rhs=wb[:, kc, :],
                                                                start=(kc == 0), stop=(kc == 15)), reads=[xnT_t[ti], wb], writes=[bk])
                                dma("sp", xs_[:rows, :], x[t0:t0 + rows, nb * 512:(nb + 1) * 512], writes=[xs_])
                                op("dve", lambda e: e.tensor_tensor(out=acc[:rows, ti, nb * 512:(nb + 1) * 512], in0=bk.t[:rows, :], in1=xs_[:rows, :], op=ALU.add),
                                   reads=[bk, xs_], writes=[acc_t[ti]])

                proj_resid(w_out, "wout")
                chk("p5")

                def norm_to_AX(which):
                    with contextlib.ExitStack() as pn:
                        junk = kb.sb(pn, "njunk", [128, D], BF16)
                        xs = [kb.sb(pn, "nxs%d" % i, [128, D], BF16) for i in range(2)]
                        st = [kb.sb(pn, "nst%d" % i, [128, 4], F32) for i in range(2)]
                        for ti, (t0, rows) in enumerate(tiles):
                            s_ = ti % 2
                            lc = t0 - c0
                            op("act", lambda e: e.activation(out=junk[:rows, :], in_=acc[:rows, ti, :], func=AF.Square,
                                                             accum_out=st[s_][:rows, 0:1]), reads=[acc_t[ti]], writes=[junk, st[s_]])
                            op("dve", lambda e: e.tensor_scalar(out=st[s_][:rows, 1:2], in0=st[s_][:rows, 0:1], scalar1=1.0 / D,
                                                                scalar2=EPS, op0=ALU.mult, op1=ALU.add), reads=[st[s_]], writes=[st[s_]])
                            op("act", lambda e: e.activation(out=st[s_][:rows, 2:3], in_=st[s_][:rows, 1:2], func=AF.Sqrt), reads=[st[s_]], writes=[st[s_]])
                            op("dve", lambda e: e.reciprocal(out=st[s_][:rows, 3:4], in_=st[s_][:rows, 2:3]), reads=[st[s_]], writes=[st[s_]])
                            op("act", lambda e: e.activation(out=xs[s_][:rows, :], in_=acc[:rows, ti, :], func=AF.Copy,
                                                             scale=st[s_][:rows, 3:4]), reads=[acc_t[ti], st[s_]], writes=[xs[s_]])
                            for half in range(2):
                                bk = banks[(2 * ti + half) % 4]
                                tp = bk.t[:].bitcast(BF16)
                                for k8 in range(8):
                                    kc = half * 8 + k8
                                    op("pe", lambda e: e.transpose(out=tp[:, k8 * 128:k8 * 128 + rows], in_=xs[s_][:rows, kc * 128:(kc + 1) * 128],
                                                                   identity=ident_b[:rows, :rows]), reads=[xs[s_], ident_b], writes=[bk])
                                op("dve", lambda e: e.tensor_tensor(
                                    out=AX[:, half * 8:half * 8 + 8, lc:lc + rows],
                                    in0=tp.rearrange("p (k t) -> p k t", k=8)[:, :, :rows],
                                    in1=gcols[:, which, half * 8:half * 8 + 8].unsqueeze(2).broadcast_to([128, 8, rows]), op=ALU.mult),
                                   reads=[bk, gcols], writes=[xnT_t[ti]])

                scope("P6n_%d" % pi)
                norm_to_AX(1)
                nt = len(tiles)
                with contextlib.ExitStack() as p6:
                    scope("P6a_%d" % pi)
                    p6ab = contextlib.ExitStack()
                    v16 = kb.sb(p6ab, "v16", [128, nt, 16, 16], F32)
                    I16 = kb.sb(p6ab, "I16", [128, nt, 16, 16], U32)
                    v16_t = [v16.sub("v16_%d" % i) for i in range(nt)]
                    with contextlib.ExitStack() as p6a:
                        wqb = [kb.sb(p6a, "wqb%d" % i, [128, 16, 512], BF16) for i in range(2)]
                        qhb = [kb.sb(p6a, "qhb%d" % i, [128, Tp], BF16) for i in range(2)]
                        stmp = [kb.sb(p6a, "stmp%d" % i, [128, 128], F32) for i in range(2)]
                        it6 = 0
                        for b4 in range(4):
                            wb = wqb[b4 % 2]
                            dma("pool", wb[:], peer_wq[:, b4 * 512:(b4 + 1) * 512].rearrange("(kc p) n -> p kc n", p=128), writes=[wb])
                            for bl in range(4):
                                blk = b4 * 4 + bl
                                qb = qhb[blk % 2]
                                for (cl0, n, is_s) in chunks:
                                    bk = banks[it6 % 2]
                                    it6 += 1
                                    rt = [xnT_t[i] for i in tiles_of(cl0, n)]
                                    for kc in range(16):
                                        op("pe", lambda e: e.matmul(bk.t[:, :n], lhsT=wb[:, kc, bl * 128:(bl + 1) * 128], rhs=AX[:, kc, cl0:cl0 + n],
                                                                    start=(kc == 0), stop=(kc == 15)), reads=rt + [wb], writes=[bk])
                                    op("act", lambda e: e.activation(out=qb[:, cl0:cl0 + n], in_=bk.t[:, :n], func=AF.Copy), reads=[bk], writes=[qb])
                                for ti, (t0, rows) in enumerate(tiles):
                                    lc = t0 - c0
                                    sP = banks[2 + (it6 % 2)]
                                    it6 += 1
                                    sm = stmp[it6 % 2]
                                    op("pe", lambda e: e.matmul(sP.t[:rows, 0:128], lhsT=qb[:, lc:lc + rows], rhs=skT[:, blk, :], start=True, stop=True),
                                       reads=[qb, skT], writes=[sP])
                                    op("dve", lambda e: e.max(out=v16[:rows, ti, blk, 0:8], in_=sP.t[:rows, 0:128]), reads=[sP], writes=[v16_t[ti]])
                                    op("dve", lambda e: e.max_index(out=I16[:rows, ti, blk, 0:8], in_max=v16[:rows, ti, blk, 0:8], in_values=sP.t[:rows, 0:128]),
                                       reads=[sP, v16_t[ti]], writes=[v16_t[ti]])
                                    op("dve", lambda e: e.match_replace(out=sm[:rows, :], in_to_replace=v16[:rows, ti, blk, 0:8], in_values=sP.t[:rows, 0:128], imm_value=NEG),
                                       reads=[sP, v16_t[ti]], writes=[sm])
                                    op("dve", lambda e: e.max(out=v16[:rows, ti, blk, 8:16], in_=sm[:rows, :]), reads=[sm], writes=[v16_t[ti]])
                                    op("dve", lambda e: e.max_index(out=I16[:rows, ti, blk, 8:16], in_max=v16[:rows, ti, blk, 8:16], in_values=sm[:rows, :]),
                                       reads=[sm, v16_t[ti]], writes=[v16_t[ti]])
                    chk("p6a")
                    scope("P6b_%d" % pi)
                    with contextlib.ExitStack() as p6b:
                        I16f = kb.sb(p6b, "I16f", [128, 16, 16], F32)
                        cand = kb.sb(p6b, "cand", [128, 8, 256], F32)
                        ctmp = kb.sb(p6b, "ctmp", [128, 256], F32)
                        c16 = kb.sb(p6b, "c16", [128, 8, 16], F32)
                        P16 = kb.sb(p6b, "P16", [128, 8, 16], U32)
                        ab_u = kb.sb(p6b, "ab_u", [128, 2, 8, 16], U32)
                        ab_f = kb.sb(p6b, "ab_f", [128, 2, 8, 16], F32)
                        eq = kb.sb(p6b, "eq", [128, 8, 16, 16], F32)
                        sel3 = kb.sb(p6b, "sel3", [128, 3, 128], F32)
                        zst = kb.sb(p6b, "zst", [128, 8, 2], F32)
                        selT = [kb.sb(p6b, "selT%d" % i, [128, 3, 128], BF16) for i in range(2)]
                        OH2 = [kb.sb(p6b, "OH2_%d" % i, [128, 16, 128], BF16) for i in range(2)]
                        OH1 = [kb.sb(p6b, "OH1_%d" % i, [128, 16, 128], BF16) for i in range(2)]
                        GS = kb.sb(p6b, "GS", [128, 128, 128], BF16)
                        itb = itg = 0
                        for ti, (t0, rows) in enumerate(tiles):
                            vv = v16[:rows, ti].rearrange("p (h n) a -> p h n a", n=2)
                            op("dve", lambda e: e.tensor_copy(out=I16f[:rows], in_=I16[:rows, ti]), reads=[v16_t[ti]], writes=[I16f])
                            op("dve", lambda e: e.tensor_tensor(out=cand[:rows].rearrange("p h (a b) -> p h a b", a=16),
                                                                in0=vv[:, :, 0, :].unsqueeze(3).broadcast_to([rows, 8, 16, 16]),
                                                                in1=vv[:, :, 1, :].unsqueeze(2).broadcast_to([rows, 8, 16, 16]), op=ALU.add),
                               reads=[v16_t[ti]], writes=[cand])
                            for h in range(8):
                                op("dve", lambda e: e.max(out=c16[:rows, h, 0:8], in_=cand[:rows, h, :]), reads=[cand], writes=[c16])
                                op("dve", lambda e: e.max_index(out=P16[:rows, h, 0:8], in_max=c16[:rows, h, 0:8], in_values=cand[:rows, h, :]), reads=[cand, c16], writes=[P16])
                                op("dve", lambda e: e.match_replace(out=ctmp[:rows, :], in_to_replace=c16[:rows, h, 0:8], in_values=cand[:rows, h, :], imm_value=NEG),
                                   reads=[cand, c16], writes=[ctmp])
                                op("dve", lambda e: e.max(out=c16[:rows, h, 8:16], in_=ctmp[:rows, :]), reads=[ctmp], writes=[c16])
                                op("dve", lambda e: e.max_index(out=P16[:rows, h, 8:16], in_max=c16[:rows, h, 8:16], in_values=ctmp[:rows, :]), reads=[ctmp, c16], writes=[P16])
                            op("dve", lambda e: e.tensor_scalar(out=ab_u[:rows, 0], in0=P16[:rows], scalar1=4, scalar2=None, op0=ALU.logical_shift_right), reads=[P16], writes=[ab_u])
                            op("dve", lambda e: e.tensor_scalar(out=ab_u[:rows, 1], in0=P16[:rows], scalar1=15, scalar2=None, op0=ALU.bitwise_and), reads=[P16], writes=[ab_u])
                            op("dve", lambda e: e.tensor_copy(out=ab_f[:rows], in_=ab_u[:rows]), reads=[ab_u], writes=[ab_f])
                            If = I16f[:rows].rearrange("p (h n) a -> p h n a", n=2)
                            for w_ in range(2):
                                op("dve", lambda e: e.tensor_tensor(out=eq[:rows], in0=ab_f[:rows, w_].unsqueeze(3).broadcast_to([rows, 8, 16, 16]),
                                                                    in1=iota16[:rows, :].unsqueeze(1).unsqueeze(1).broadcast_to([rows, 8, 16, 16]), op=ALU.is_equal),
                                   reads=[ab_f, iota16], writes=[eq])
                                op("dve", lambda e: e.tensor_tensor(out=eq[:rows], in0=eq[:rows], in1=If[:, :, w_, :].unsqueeze(2).broadcast_to([rows, 8, 16, 16]), op=ALU.mult),
                                   reads=[eq, I16f], writes=[eq])
                                op("dve", lambda e: e.tensor_reduce(out=sel3[:rows, w_, :].rearrange("p (h j) -> p h j", h=8), in_=eq[:rows], axis=AX_.X, op=ALU.add),
                                   reads=[eq], writes=[sel3])
                            wv = sel3[:rows, 2, :].rearrange("p (h j) -> p h j", h=8)
                            op("dve", lambda e: e.tensor_tensor(out=wv, in0=c16[:rows], in1=c16[:rows, :, 0:1].broadcast_to([rows, 8, 16]), op=ALU.subtract),
                               reads=[c16], writes=[sel3])
                            op("act", lambda e: e.activation(out=wv, in_=wv, func=AF.Exp), reads=[sel3], writes=[sel3])
                            op("dve", lambda e: e.tensor_reduce(out=zst[:rows, :, 0], in_=wv, axis=AX_.X, op=ALU.add), reads=[sel3], writes=[zst])
                            op("dve", lambda e: e.reciprocal(out=zst[:rows, :, 1], in_=zst[:rows, :, 0]), reads=[zst], writes=[zst])
                            op("dve", lambda e: e.tensor_tensor(out=wv, in0=wv, in1=zst[:rows, :, 1:2].broadcast_to([rows, 8, 16]), op=ALU.mult),
                               reads=[sel3, zst], writes=[sel3])
                            if "sel3" in dbg and t0 == 0:
                                dma("sp", dbg["sel3"], sel3[:], reads=[sel3], is_output=True)
                            sT = selT[ti % 2]
                            bkT = banks[4]
                            for w_ in range(3):
                                op("pe", lambda e: e.transpose(out=bkT.t[:, w_ * 128:w_ * 128 + rows], in_=sel3[:rows, w_, :], identity=ident_f[:rows, :rows]),
                                   reads=[sel3, ident_f], writes=[bkT])
                            op("act", lambda e: e.activation(out=sT[:, :, :rows], in_=bkT.t[:, 0:384].rearrange("p (w t) -> p w t", w=3)[:, :, :rows], func=AF.Copy),
                               reads=[bkT], writes=[sT])
                            for sub in range((rows + 15) // 16):
                                tl0 = sub * 16
                                o2, o1 = OH2[itb % 2], OH1[itb % 2]
                                itb += 1
                                io_b = iotaC[:].unsqueeze(1).broadcast_to([128, 16, 128])
                                op("dve", lambda e: e.tensor_tensor(out=o2[:], in0=io_b, in1=sT[:, 1, tl0:tl0 + 16].unsqueeze(2).broadcast_to([128, 16, 128]), op=ALU.is_equal),
                                   reads=[iotaC, sT], writes=[o2])
                                op("dve", lambda e: e.tensor_tensor(out=o1[:], in0=io_b, in1=sT[:, 0, tl0:tl0 + 16].unsqueeze(2).broadcast_to([128, 16, 128]), op=ALU.is_equal),
                                   reads=[iotaC, sT], writes=[o1])
                                op("pool", lambda e: e.tensor_tensor(out=o1[:], in0=o1[:], in1=sT[:, 2, tl0:tl0 + 16].unsqueeze(2).broadcast_to([128, 16, 128]), op=ALU.mult),
                                   reads=[o1, sT], writes=[o1])
                                for q4 in range(4):
                                    gb = banks[5 + itg % 3]
                                    itg += 1
                                    for t4 in range(4):
                                        tl = q4 * 4 + t4
                                        op("pe", lambda e: e.matmul(gb.t[:, t4 * 128:(t4 + 1) * 128], lhsT=o2[:, tl, :], rhs=o1[:, tl, :], start=True, stop=True),
                                           reads=[o2, o1], writes=[gb])
                                    tg = tl0 + q4 * 4
                                    op("act", lambda e: e.activation(out=GS[:, :, tg:tg + 4], in_=gb.t[:, :].rearrange("p (t c) -> p c t", t=4), func=AF.Copy),
                                       reads=[gb], writes=[GS])
                            for c8 in range(8):
                                dma("sp", Gd[c8 * 16:(c8 + 1) * 16, :, t0:t0 + rows].rearrange("c i t -> i c t"), GS[:, c8 * 16:(c8 + 1) * 16, :rows],
                                    reads=[GS], writes=[Gd_res], allow_slow_non_contiguous=False)
                    chk("p6b")
                    if "G" in dbg and pi == 0:
                        with contextlib.ExitStack() as pd:
                            gtmp = kb.sb(pd, "gtmp", [128, 128, 128], BF16)
                            dma("sp", gtmp[:], Gd[:, :, 0:128].rearrange("c i t -> i c t"), reads=[Gd_res], writes=[gtmp])
                            for c8 in range(8):
                                dma("pool", dbg["G"][:, c8 * 16:(c8 + 1) * 16, :], gtmp[:, c8 * 16:(c8 + 1) * 16, :], reads=[gtmp], is_output=True)
                    chk("p6c")
                    p6ab.close()
                    scope("P6d_%d" % pi)
                    with contextlib.ExitStack() as p6d:
                        ES = 4
                        NUB = 4
                        ub = [kb.sb(p6d, "ub%d" % i, [128, D], BF16) for i in range(NUB)] if pi == 0 else [None] * NUB
                        uT = [kb.sb(p6d, "uT%d" % i, [128, 16, 128], BF16) for i in range(2)]
                        gt = [kb.sb(p6d, "gt%d" % i, [128, Tp], BF16) for i in range(2)]
                        gl = [kb.sb(p6d, "gl%d" % i, [128, 512], BF16) for i in range(2)]
                        actS = [kb.sb(p6d, "actS%d" % i, [128, ES, Tp], BF16) for i in range(2)]
                        vS = [kb.sb(p6d, "vS%d" % i, [128, ES, D], BF16) for i in range(2)]
                        vsub = [[b_.sub("vS%d_%d" % (i, e_)) for e_ in range(ES)] for i, b_ in enumerate(vS)]
                        ith = itv = 0
                        nsc = 128 // ES
                        if os.environ.get("KNSC"):
                            nsc = int(os.environ["KNSC"])
                        cu = {"h": 0, "v": 0}
                        prepped = {}

                        def prepU(c):
                            sc, ec = divmod(c, ES)
                            vs = vS[sc % 2]
                            u_, uT_, g_ = ub[c % NUB], uT[c % 2], gt[c % 2]
                            dma("sp", g_[:, :], Gd[c, :, c0:c0 + Tp], reads=[Gd_res], writes=[g_])
                            if pi > 0:
                                dma("sp", uT_[:], uTd[c], writes=[uT_])
                                dma("sp", vs[:, ec, :], vd[c], writes=[vsub[sc % 2][ec]])
                                return
                            for half in range(2):
                                bk = banks[half]
                                tp = bk.t[:].bitcast(BF16)
                                for k8 in range(8):
                                    kc = half * 8 + k8
                                    op("pe", lambda e: e.transpose(out=tp[:, k8 * 128:(k8 + 1) * 128], in_=u_[:, kc * 128:(kc + 1) * 128], identity=ident_b[:]),
                                       reads=[u_, ident_b], writes=[bk])
                                op("act", lambda e: e.activation(out=uT_[:, half * 8:half * 8 + 8, :], in_=tp.rearrange("p (k e) -> p k e", k=8), func=AF.Copy),
                                   reads=[bk], writes=[uT_])
                            dma("sp", uTd[c], uT_[:], reads=[uT_])

                        def loadUV(c):
                            sc, ec = divmod(c, ES)
                            vs = vS[sc % 2]
                            dma("pool", ub[c % NUB][:], peer_u[c * 128:(c + 1) * 128, :], writes=[ub[c % NUB]], max_dma_last_dim=8192)
                            dma("pool", vs[:, ec, :], peer_v[c * 128:(c + 1) * 128, :], writes=[vsub[sc % 2][ec]], max_dma_last_dim=8192)
                            dma("sp", vd[c], vs[:, ec, :], reads=[vsub[sc % 2][ec]])

                        def uside(c):
                            sc, ec = divmod(c, ES)
                            aS = actS[sc % 2]
                            uT_, g_ = uT[c % 2], gt[c % 2]
                            for (cl0, n, is_s) in chunks:
                                hp = banks[2 + cu["h"] % 2]
                                gl_ = gl[cu["h"] % 2]
                                cu["h"] += 1
                                rt = [xnT_t[i] for i in tiles_of(cl0, n)]
                                for kc in range(16):
                                    op("pe", lambda e: e.matmul(hp.t[:, :n], lhsT=uT_[:, kc, :], rhs=AX[:, kc, cl0:cl0 + n], start=(kc == 0), stop=(kc == 15)),
                                       reads=rt + [uT_], writes=[hp])
                                op("act", lambda e: e.activation(out=gl_[:, :n], in_=hp.t[:, :n], func=AF.Gelu), reads=[hp], writes=[gl_])
                                op("pool", lambda e: e.tensor_tensor(out=aS[:, ec, cl0:cl0 + n], in0=gl_[:, :n], in1=g_[:, cl0:cl0 + n], op=ALU.mult),
                                   reads=[gl_, g_], writes=[aS])

                        def vside(sc):
                            aS, vs = actS[sc % 2], vS[sc % 2]
                            for ti, (t0, rows) in enumerate(tiles):
                                lc = t0 - c0
                                for dq4 in range(4):
                                    bk = banks[4 + cu["v"] % 4]
                                    cu["v"] += 1
                                    d0 = dq4 * 512
                                    for ec in range(ES):
                                        op("pe", lambda e: e.matmul(bk.t[:rows, :], lhsT=aS[:, ec, lc:lc + rows], rhs=vs[:, ec, d0:d0 + 512], start=(ec == 0), stop=(ec == ES - 1)),
                                           reads=[aS, vsub[sc % 2][ec]], writes=[bk])
                                    op("dve", lambda e: e.tensor_tensor(out=acc[:rows, ti, d0:d0 + 512], in0=bk.t[:rows, :], in1=acc[:rows, ti, d0:d0 + 512], op=ALU.add),
                                       reads=[bk, acc_t[ti]], writes=[acc_t[ti]])

                        nchunk = nsc * ES
                        if pi == 0:
                            for c_ in range(min(NUB - 1, nchunk)):
                                loadUV(c_)
                        prepU(0)
                        for c in range(nchunk):
                            if pi == 0 and c + NUB - 1 < nchunk:
                                loadUV(c + NUB - 1)
                            if c + 1 < nchunk:
                                prepU(c + 1)
                            uside(c)
                            if (c + 1) % ES == 0:
                                vside(c // ES)
                chk("p6")
                if "h3" in dbg:
                    for ti, (t0, rows) in enumerate(tiles):
                        dma("sp", dbg["h3"][t0:t0 + rows, :], acc[:rows, ti, :], reads=[acc_t[ti]], is_output=True)

                scope("P7_%d" % pi)
                norm_to_AX(2)
                with contextlib.ExitStack() as p7:
                    pT = kb.sb(p7, "pT", [128, 2, Tp], BF16)
                    pt32 = [kb.sb(p7, "pt32_%d" % i, [128, 256], F32) for i in range(2)]
                    ptb = [kb.sb(p7, "ptb%d" % i, [128, 256], BF16) for i in range(2)]
                    for ti, (t0, rows) in enumerate(tiles):
                        lc = t0 - c0
                        s_ = ti % 2
                        dma("sp", pt32[s_][:rows, :], pvec[t0:t0 + rows, :], writes=[pt32[s_]])
                        op("act", lambda e: e.activation(out=ptb[s_][:rows, :], in_=pt32[s_][:rows, :], func=AF.Copy), reads=[pt32[s_]], writes=[ptb[s_]])
                        bk = banks[s_]
                        tp = bk.t[:].bitcast(BF16)
                        for k2 in range(2):
                            op("pe", lambda e: e.transpose(out=tp[:, k2 * 128:k2 * 128 + rows], in_=ptb[s_][:rows, k2 * 128:(k2 + 1) * 128], identity=ident_b[:rows, :rows]),
                               reads=[ptb[s_], ident_b], writes=[bk])
                        op("act", lambda e: e.activation(out=pT[:, :, lc:lc + rows], in_=tp[:, 0:256].rearrange("p (k t) -> p k t", k=2)[:, :, :rows], func=AF.Copy),
                           reads=[bk], writes=[pT])
                    wgb = [kb.sb(p7, "wgb%d" % i, [128, 16, 512], BF16) for i in range(2)]
                    wpb = [kb.sb(p7, "wpb%d" % i, [128, 2, 512], BF16) for i in range(2)]
                    gsb = [kb.sb(p7, "gsb%d" % i, [128, 512], F32) for i in range(2)]
                    it7 = 0
                    for nb in range(4):
                        wg_, wp_ = wgb[nb % 2], wpb[nb % 2]
                        dma("pool", wg_[:], ple_gate[:, nb * 512:(nb + 1) * 512].rearrange("(kc p) n -> p kc n", p=128), writes=[wg_])
                        dma("pool", wp_[:], ple_proj[:, nb * 512:(nb + 1) * 512].rearrange("(kc p) n -> p kc n", p=128), writes=[wp_])
                        for ti, (t0, rows) in enumerate(tiles):
                            lc = t0 - c0
                            bG, bP = banks[2 + it7 % 2], banks[4 + it7 % 2]
                            gs_ = gsb[it7 % 2]
                            it7 += 1
                            for kc in range(16):
                                op("pe", lambda e: e.matmul(bG.t[:rows, :], lhsT=AX[:, kc, lc:lc + rows], rhs=wg_[:, kc, :], start=(kc == 0), stop=(kc == 15)),
                                   reads=[xnT_t[ti], wg_], writes=[bG])
                            for k2 in range(2):
                                op("pe", lambda e: e.matmul(bP.t[:rows, :], lhsT=pT[:, k2, lc:lc + rows], rhs=wp_[:, k2, :], start=(k2 == 0), stop=(k2 == 1)),
                                   reads=[pT, wp_], writes=[bP])
                            op("act", lambda e: e.activation(out=gs_[:rows, :], in_=bG.t[:rows, :], func=AF.Sigmoid), reads=[bG], writes=[gs_])
                            op("dve", lambda e: e.tensor_tensor(out=gs_[:rows, :], in0=gs_[:rows, :], in1=bP.t[:rows, :], op=ALU.mult), reads=[gs_, bP], writes=[gs_])
                            op("dve", lambda e: e.tensor_tensor(out=acc[:rows, ti, nb * 512:(nb + 1) * 512], in0=acc[:rows, ti, nb * 512:(nb + 1) * 512], in1=gs_[:rows, :], op=ALU.add),
                               reads=[gs_, acc_t[ti]], writes=[acc_t[ti]])
                chk("p7")
                if "h4" in dbg:
                    for ti, (t0, rows) in enumerate(tiles):
                        dma("sp", dbg["h4"][t0:t0 + rows, :], acc[:rows, ti, :], reads=[acc_t[ti]], is_output=True)
                scope("PF_%d" % pi)
                with contextlib.ExitStack() as pf:
                    junk = kb.sb(pf, "fjunk", [128, D], BF16)
                    yt = [kb.sb(pf, "yt%d" % i, [128, D], F32) for i in range(2)]
                    st = [kb.sb(pf, "fst%d" % i, [128, 4], F32) for i in range(2)]
                    for ti, (t0, rows) in enumerate(tiles):
                        s_ = ti % 2
                        op("act", lambda e: e.activation(out=junk[:rows, :], in_=acc[:rows, ti, :], func=AF.Square, accum_out=st[s_][:rows, 0:1]),
                           reads=[acc_t[ti]], writes=[junk, st[s_]])
                        op("dve", lambda e: e.tensor_scalar(out=st[s_][:rows, 1:2], in0=st[s_][:rows, 0:1], scalar1=1.0 / D, scalar2=EPS, op0=ALU.mult, op1=ALU.add),
                           reads=[st[s_]], writes=[st[s_]])
                        op("act", lambda e: e.activation(out=st[s_][:rows, 2:3], in_=st[s_][:rows, 1:2], func=AF.Sqrt), reads=[st[s_]], writes=[st[s_]])
                        op("dve", lambda e: e.reciprocal(out=st[s_][:rows, 3:4], in_=st[s_][:rows, 2:3]), reads=[st[s_]], writes=[st[s_]])
                        op("dve", lambda e: e.scalar_tensor_tensor(out=yt[s_][:rows, :], in0=acc[:rows, ti, :], scalar=st[s_][:rows, 3:4], in1=fgb[:rows, :], op0=ALU.mult, op1=ALU.mult),
                           reads=[acc_t[ti], st[s_], fgb], writes=[yt[s_]])
                        dma("sp", y_out[t0:t0 + rows, :], yt[s_][:rows, :], reads=[yt[s_]], is_output=True)

                if "h2" in dbg:
                    for ti, (t0, rows) in enumerate(tiles):
                        dma("sp", dbg["h2"][t0:t0 + rows, :], acc[:rows, ti, :], reads=[acc_t[ti]], is_output=True)
        scope(None)
        kb.finish()


def make_in_maps(inputs):
    cst = consts()
    maps = []
    for c in range(NC):
        m = dict(cst)
        m["x"] = np.ascontiguousarray(np.concatenate(
            [inputs["x_prompt"][c], inputs["x_sample"][NS * c:NS * (c + 1)].reshape(TS, D)], 0))
        m["w_in"] = np.ascontiguousarray(inputs["w_in"][0])
        m["norms"] = np.ascontiguousarray(np.stack([inputs["attn_norm"][0], inputs["ffn_norm"][0], inputs["ple_norm"][0], inputs["final_norm"]]))
        m["peer_wq"] = np.ascontiguousarray(inputs["peer_wq"][0])
        m["peer_sk"] = np.ascontiguousarray(inputs["peer_subkeys"][0].reshape(16 * 128, 128))
        m["peer_u"] = np.ascontiguousarray(inputs["peer_u"][0])
        m["peer_v"] = np.ascontiguousarray(inputs["peer_v"][0])
        m["ple_gate"] = np.ascontiguousarray(inputs["ple_gate"][0])
        m["ple_proj"] = np.ascontiguousarray(inputs["ple_proj"][0])
        m["ck"] = inputs["cache_k"][0].reshape(2560 * 128, 256)
        m["cv"] = inputs["cache_v"][0].reshape(2560 * 128, 256)
        m["cki"] = inputs["cache_kidx"][0].reshape(2560 * 128, 64)
        m["pt"] = np.ascontiguousarray(inputs["page_table"][NS * c:NS * (c + 1)].reshape(-1).astype(np.int32))
        m["pvec"] = np.ascontiguousarray(np.concatenate([inputs["p_prompt"][0, c], inputs["p_sample"][0, NS * c:NS * (c + 1)].reshape(TS, 256)], 0))
        for k_ in ("conv_w", "conv_b", "conv_ln_g", "conv_ln_b", "w_out"):
            m[k_] = np.ascontiguousarray(inputs[k_][0])
        m["state"] = np.ascontiguousarray(inputs["state_conv"][0, NS * c:NS * (c + 1)].reshape(NS * 30, 1024))
        maps.append(m)
    return maps


def kernel(**inputs):
    inputs = {k: np.asarray(v) for k, v in inputs.items()}
    nc = build()
    maps = make_in_maps(inputs)
    res = run_bass_kernel_spmd(nc, maps, core_ids=list(range(NC)))
    R = res.results
    B = 8
    k_p = np.stack([R[c]["k_out"][:SEQ].reshape(SEQ, 2, 128) for c in range(NC)])[None]
    v_p = np.stack([R[c]["v_out"][:SEQ].reshape(SEQ, 2, 128) for c in range(NC)])[None]
    ki_p = np.stack([R[c]["ki_out"][:SEQ] for c in range(NC)])[None]
    k_s = np.concatenate([R[c]["k_out"][SEQ:].reshape(NS, 4, 2, 128) for c in range(NC)])[None]
    v_s = np.concatenate([R[c]["v_out"][SEQ:].reshape(NS, 4, 2, 128) for c in range(NC)])[None]
    ki_s = np.concatenate([R[c]["ki_out"][SEQ:].reshape(NS, 4, 64) for c in range(NC)])[None]
    y_p = np.stack([R[c]["y"][:SEQ] for c in range(NC)])
    y_s = np.concatenate([R[c]["y"][SEQ:].reshape(NS, 4, D) for c in range(NC)])
    cp = np.stack([R[c]["conv_p"] for c in range(NC)])[None]
    cs = np.concatenate([R[c]["conv_s"] for c in range(NC)])[None]
    return (y_p, y_s, k_p, v_p, ki_p, cp, k_s, v_s, ki_s, cs)
```

```python
import contextlib
import numpy as np
import concourse.bass as bass
import concourse.mybir as mybir
from concourse.bass_utils import run_bass_kernel_spmd

F32 = mybir.dt.float32
BF16 = mybir.dt.bfloat16
I32 = mybir.dt.int32
U32 = mybir.dt.uint32
AF = mybir.ActivationFunctionType
ALU = mybir.AluOpType
AX_ = mybir.AxisListType

D = 2048
NC = 8
SEQ = 2048
NS = 16
TS = 64
T = SEQ + TS
N_IN = 4168
OFF_K, OFF_V, OFF_QI, OFF_KI, OFF_WI, OFF_GLU = 1024, 1280, 1536, 2048, 2112, 2120
EPS = 1e-6
NEG = -1.0e30


class Res:
    __slots__ = ("name", "w", "r", "excl")

    def __init__(self, name, excl=False):
        self.name = name
        self.w = None
        self.r = {}
        self.excl = excl


class Buf:
    def __init__(self, t, name):
        self.t = t
        self.res = Res(name)
        self.subs = []

    def sub(self, name, excl=False):
        r = Res(name, excl)
        self.subs.append(r)
        return r

    def __getitem__(self, idx):
        return self.t[idx]


class KB:
    def __init__(self, nc, es):
        self.nc = nc
        self.es = es
        self.E = {"pe": nc.tensor, "act": nc.scalar, "dve": nc.vector, "pool": nc.gpsimd, "sp": nc.sync}
        self.esem = {k: es.enter_context(nc.semaphore("sem_" + k)) for k in self.E}
        self.ecnt = {k: 0 for k in self.E}
        self.seen = {k: {} for k in self.E}
        self.dsem = {}
        self.dcnt = {}
        self.semname = {}
        self.out_tags = []
        self.nsem = 5

    def sb(self, es, name, shape, dtype):
        self.nalloc = getattr(self, "nalloc", 0) + 1
        b = Buf(es.enter_context(self.nc.sbuf_tensor("s%d_%s" % (self.nalloc, name), list(shape), dtype)), name)
        es.callback(self.release, b)
        return b

    def release(self, buf):
        tags = []
        for res in [buf.res] + buf.subs:
            tags += list(res.r.values()) + ([res.w] if res.w is not None else [])
        for eng in self.E:
            for tg in tags:
                self._wait(eng, tg)
        for res in [buf.res] + buf.subs:
            for kind in ("w", "r"):
                for qc in ("sw", "hw"):
                    s_ = self.dsem.pop((id(res), kind, qc), None)
                    if s_ is not None:
                        if not hasattr(self, "free_sems"):
                            self.free_sems = {"sw": [], "hw": []}
                        self.free_sems[qc].append(s_)

    def _wait(self, eng, dep):
        sem, val = dep
        key = id(sem)
        if eng == "pe" and sem is self.esem["pe"]:
            return
        if self.seen[eng].get(key, 0) >= val:
            return
        self.E[eng].wait_ge(sem, val)
        self.seen[eng][key] = val

    def _deps(self, eng, reads, writes, skip_waw_sem=None):
        for r in reads:
            if r.w is not None:
                self._wait(eng, r.w)
            if r.excl:
                for d in r.r.values():
                    if d[0] is not self.esem.get(eng):
                        self._wait(eng, d)
        for w in writes:
            if w.w is not None and not (skip_waw_sem is not None and w.w[0] is skip_waw_sem):
                self._wait(eng, w.w)
            for d in w.r.values():
                self._wait(eng, d)

    @staticmethod
    def _res(xs):
        return [x.res if isinstance(x, Buf) else x for x in xs]

    def op(self, eng, fn, reads=(), writes=()):
        reads = self._res(reads)
        writes = self._res(writes)
        self._deps(eng, reads, writes)
        ins = fn(self.E[eng])
        self.ecnt[eng] += 1
        ins.then_inc(self.esem[eng], 1)
        tag = (self.esem[eng], self.ecnt[eng])
        for r in reads:
            r.r[id(tag[0])] = tag
        for w in writes:
            w.w = tag
            w.r = {}
        return ins

    def _dma_sem(self, res, kind, q="sp"):
        qc = "sw" if q == "pool" else "hw"
        key = (id(res), kind, qc)
        if key not in self.dsem:
            if not hasattr(self, "free_sems"):
                self.free_sems = {"sw": [], "hw": []}
            free = self.free_sems[qc]
            if free:
                s = free.pop()
            else:
                self.nsem += 1
                s = self.es.enter_context(self.nc.semaphore("d%d" % self.nsem))
                self.dcnt[id(s)] = 0
            self.dsem[key] = s
        return self.dsem[key]

    def dma(self, q, out, in_, reads=(), writes=(), is_output=False, **kw):
        reads = self._res(reads)
        writes = self._res(writes)
        if writes:
            sem = self._dma_sem(writes[0], "w", q)
        else:
            sem = self._dma_sem(reads[0], "r", q)
        self._deps(q, reads, writes, skip_waw_sem=sem)
        if q == "pool":
            kw.setdefault("max_dma_last_dim", 2048)
        ins = self.E[q].dma_start(out=out, in_=in_, **kw)
        self.dcnt[id(sem)] += 16
        ins.then_inc(sem, 16)
        tag = (sem, self.dcnt[id(sem)])
        for r in reads:
            r.r[id(sem)] = tag
        for w in writes:
            w.w = tag
            w.r = {}
        if is_output:
            self.out_tags.append(tag)
        return ins

    def idma(self, out, in_, idx_ap, reads=(), writes=()):
        reads = self._res(reads)
        writes = self._res(writes)
        sem = self._dma_sem(writes[0], "w", "pool")
        self._deps("pool", reads, writes, skip_waw_sem=sem)
        ins = self.nc.gpsimd.indirect_dma_start(out=out, out_offset=None, in_=in_,
                                                in_offset=bass.IndirectOffsetOnAxis(ap=idx_ap, axis=0))
        self.dcnt[id(sem)] += 16
        ins.then_inc(sem, 16)
        tag = (sem, self.dcnt[id(sem)])
        for r in reads:
            r.r[id(sem)] = tag
        for w in writes:
            w.w = tag
            w.r = {}
        return ins

    def finish(self):
        last = {}
        for sem, val in self.out_tags:
            k = id(sem)
            if k not in last or last[k][1] < val:
                last[k] = (sem, val)
        for tag in last.values():
            self._wait("sp", tag)
        for e in ("pe", "act", "dve", "pool"):
            if self.ecnt[e]:
                self._wait("sp", (self.esem[e], self.ecnt[e]))


def rope_tables():
    pos = np.concatenate([np.arange(SEQ, dtype=np.float32), np.tile(2048.0 + np.arange(4, dtype=np.float32), NS)])
    out = np.zeros((T, 96), np.float32)
    for rot, off in ((32, 0), (16, 64)):
        half = rot // 2
        inv = (np.float32(500000.0) ** (-np.arange(half, dtype=np.float32) * np.float32(2.0) / np.float32(rot))).astype(np.float32)
        ang = pos[:, None] * inv[None, :]
        c, s = np.cos(ang), np.sin(ang)
        out[:, off:off + rot] = np.concatenate([c, c], 1)
        out[:, off + rot:off + 2 * rot] = np.concatenate([-s, s], 1)
    return out


def consts():
    cm = np.where(np.arange(128)[None, :] <= np.arange(128)[:, None], 0.0, NEG).astype(np.float32)
    r_ = np.arange(64)
    cms = np.where((r_[:, None] // 4 == r_[None, :] // 4) & (r_[None, :] % 4 <= r_[:, None] % 4), 0.0, NEG).astype(np.float32)
    sel = np.zeros((64, 16, 4, 4), np.float32)
    for s_ in range(16):
        for i_ in range(4):
            sel[4 * s_ + i_, s_, :, i_] = 1.0
    return {"ident_f": np.eye(128, dtype=np.float32), "cmask": cm, "rope": rope_tables(), "cmaskS": cms, "selS": sel.reshape(64, 256),
            "pidx": np.arange(128, dtype=np.float32).reshape(128, 1),
            "iota128": np.tile(np.arange(128, dtype=np.float32)[None, :], (128, 1))}


class _Stop(Exception):
    pass


def build(stage=99, debug=()):
    nc = bass.Bass("TRN2", target_bir_lowering=False)
    try:
        _build(nc, stage, debug)
    except _Stop:
        pass
    return nc


def _build(nc, stage, debug):
    import os

    _cur = [None]

    def scope(name):
        if os.environ.get("KSCOPE"):
            if _cur[0] is not None:
                nc.leave_named_scope(_cur[0][0], _cur[0][1], False)
            _cur[0] = None
            if name is not None:
                sid, _ = nc.enter_named_scope(name, False)
                _cur[0] = (name, sid)

    def chk(tag):
        if os.environ.get("KSTOP") == tag:
            scope(None)
            kb.finish()
            raise _Stop()

    def din(name, shape, dt=F32):
        return nc.dram_tensor(name, list(shape), dt, kind="ExternalInput").ap()

    def dout(name, shape, dt=F32):
        return nc.dram_tensor(name, list(shape), dt, kind="ExternalOutput").ap()

    x = din("x", [T, D])
    w_in = din("w_in", [D, N_IN])
    rope = din("rope", [T, 96])
    ident_f_d = din("ident_f", [128, 128])
    cmask_d = din("cmask", [128, 128])
    k_out = dout("k_out", [T, 256])
    v_out = dout("v_out", [T, 256])
    ki_out = dout("ki_out", [T, 64])
    w_out = din("w_out", [D, D])
    norms = din("norms", [4, D])
    peer_wq = din("peer_wq", [D, D])
    peer_sk = din("peer_sk", [16 * 128, 128])
    peer_u = din("peer_u", [16384, D])
    peer_v = din("peer_v", [16384, D])
    ple_gate = din("ple_gate", [D, D])
    ple_proj = din("ple_proj", [256, D])
    pvec = din("pvec", [T, 256])
    iota_d = din("iota128", [128, 128])
    ck = din("ck", [2560 * 128, 256])
    cv = din("cv", [2560 * 128, 256])
    cki = din("cki", [2560 * 128, 64])
    pt_d = din("pt", [NS * 16], I32)
    cmaskS_d = din("cmaskS", [64, 64])
    selS_d = din("selS", [64, 256])
    pidx_d = din("pidx", [128, 1])
    y_out = dout("y", [T, D])
    Gd = nc.dram_tensor("Gd", [128, 128, T], BF16, kind="Internal").ap()
    uTd = nc.dram_tensor("uTd", [128, 128, 16, 128], BF16, kind="Internal").ap()
    vd = nc.dram_tensor("vd", [128, 128, D], BF16, kind="Internal").ap()
    conv_w = din("conv_w", [31, 1024])
    conv_b = din("conv_b", [1024])
    conv_ln_g = din("conv_ln_g", [1024])
    conv_ln_b = din("conv_ln_b", [1024])
    state = din("state", [NS * 30, 1024])
    conv_p = dout("conv_p", [30, 1024])
    conv_s = dout("conv_s", [NS, 30, 1024])
    dbg = {n: dout("dbg_" + n, shp) for n, shp in debug}

    with contextlib.ExitStack() as es:
        kb = KB(nc, es)
        op, dma = kb.op, kb.dma
        banks = [Buf(es.enter_context(nc.psum_tensor("bank%d" % i, [128, 512], F32)), "bank%d" % i) for i in range(8)]
        for b_ in banks:
            b_.res.excl = True

        ident_f = kb.sb(es, "ident_f", [128, 128], F32)
        ident_b = kb.sb(es, "ident_b", [128, 128], BF16)
        cmask = kb.sb(es, "cmask", [128, 128], F32)
        gcols = kb.sb(es, "gcols", [128, 4, 16], F32)
        gcol = gcols[:, 0, :]
        gcol_r = gcols
        fgb = kb.sb(es, "fgb", [128, D], F32)
        iotaC = kb.sb(es, "iotaC", [128, 128], BF16)
        iota16 = kb.sb(es, "iota16", [128, 16], F32)
        skT = kb.sb(es, "skT", [128, 16, 128], BF16)
        dma("sp", ident_f[:], ident_f_d, writes=[ident_f])
        dma("sp", cmask[:], cmask_d, writes=[cmask])
        for w_ in range(4):
            dma("sp", gcols[:, w_, :], norms[w_].rearrange("(kc p) -> p kc", p=128), writes=[gcols], allow_slow_non_contiguous=True)
        dma("sp", fgb[:], norms[3].partition_broadcast(128), writes=[fgb])
        op("dve", lambda e: e.tensor_copy(out=ident_b[:], in_=ident_f[:]), reads=[ident_f], writes=[ident_b])
        with contextlib.ExitStack() as c1s:
            io32 = kb.sb(c1s, "io32", [128, 128], F32)
            dma("sp", io32[:], iota_d, writes=[io32])
            op("dve", lambda e: e.tensor_copy(out=iotaC[:], in_=io32[:]), reads=[io32], writes=[iotaC])
            op("dve", lambda e: e.tensor_copy(out=iota16[:], in_=io32[:, 0:16]), reads=[io32], writes=[iota16])
            sk32 = kb.sb(c1s, "sk32", [128, 16, 128], F32)
            dma("sp", sk32[:], peer_sk.rearrange("(b k) d -> k b d", k=128), writes=[sk32])
            for q4 in range(4):
                for b4 in range(4):
                    op("pe", lambda e: e.transpose(out=banks[q4].t[:, b4 * 128:(b4 + 1) * 128], in_=sk32[:, q4 * 4 + b4, :], identity=ident_f[:]),
                       reads=[sk32, ident_f], writes=[banks[q4]])
                op("act", lambda e: e.activation(out=skT[:, q4 * 4:q4 * 4 + 4, :], in_=banks[q4].t[:, :].rearrange("p (b k) -> p b k", b=4), func=AF.Copy),
                   reads=[banks[q4]], writes=[skT])
        chk("p0")
        ones_f = kb.sb(es, "ones_f", [128, 128], F32)
        ones_b = kb.sb(es, "ones_b", [128, 128], BF16)
        I4 = kb.sb(es, "I4", [128, 4, 128], BF16)
        thr0 = kb.sb(es, "thr0", [128, 1], F32)
        op("pool", lambda e: e.memset(ones_b[:], 1.0), writes=[ones_b])
        op("pool", lambda e: e.memset(thr0[:], -1.0e29), writes=[thr0])
        op("dve", lambda e: e.tensor_copy(out=I4[:], in_=ident_f[:].unsqueeze(1).broadcast_to([128, 4, 128])), reads=[ident_f], writes=[I4])
        op("pool", lambda e: e.memset(ones_f[:], 1.0), writes=[ones_f])
        cwT = kb.sb(es, "cwT", [128, 8, 31], F32)
        ccol = kb.sb(es, "ccol", [128, 3, 8], F32)
        halo = kb.sb(es, "halo", [128, 8, 30], BF16)
        with contextlib.ExitStack() as c0s:
            cw_sb = kb.sb(c0s, "cw_sb", [31, 1024], F32)
            dma("sp", cw_sb[:], conv_w, writes=[cw_sb])
            for i_, src in enumerate((conv_b, conv_ln_g, conv_ln_b)):
                dma("sp", ccol[:, i_, :], src.rearrange("(g p) -> p g", p=128), writes=[ccol], allow_slow_non_contiguous=True)
            for g in range(8):
                op("pe", lambda e: e.transpose(out=banks[0].t[:, g * 31:(g + 1) * 31], in_=cw_sb[:31, g * 128:(g + 1) * 128],
                                               identity=ident_f[:31, :31]), reads=[cw_sb, ident_f], writes=[banks[0]])
            op("act", lambda e: e.activation(out=cwT[:], in_=banks[0].t[:, 0:248].rearrange("p (g j) -> p g j", g=8), func=AF.Copy),
               reads=[banks[0]], writes=[cwT])

        Gd_res = Res("Gd")
        uTd_res = Res("uTd")
        vd_res = Res("vd")
        kT = kb.sb(es, "kT", [128, 2, T], BF16)
        Vb = kb.sb(es, "Vb", [128, 17, 256], BF16)
        kiT = kb.sb(es, "kiT", [64, T], BF16)
        wiS = kb.sb(es, "wiS", [128, 17, 8], F32)

        tiles_all = [(i * 128, 128) for i in range(16)] + [(SEQ, TS)]
        passes = [tiles_all[:6], tiles_all[6:12], tiles_all[12:]]
        import os
        if os.environ.get("KPASS"):
            szs = [int(v_) for v_ in os.environ["KPASS"].split(",")]
            passes, o_ = [], 0
            for z_ in szs:
                passes.append(tiles_all[o_:o_ + z_])
                o_ += z_
        if os.environ.get("KSEL"):
            passes = [[tiles_all[int(v_)] for v_ in grp.split(",")] for grp in os.environ["KSEL"].split(";")]
        if os.environ.get("KTILES"):
            passes = [tiles_all[:int(os.environ["KTILES"])]]

        for pi, tiles in enumerate(passes):
            c0 = tiles[0][0]
            Tp = sum(r for _, r in tiles)
            Tpp = sum(r for t0_, r in tiles if t0_ < SEQ)
            has_s = any(t0_ >= SEQ for t0_, _ in tiles)
            last_pass = (tiles[-1][0] + tiles[-1][1] >= SEQ)
            chunks = []
            cc_ = 0
            while cc_ < Tpp:
                n_ = min(512, Tpp - cc_)
                chunks.append((cc_, n_, False))
                cc_ += n_
            if has_s:
                chunks.append((Tpp, TS, True))

            def tiles_of(cl0, n):
                return [i for i, (t0_, r_) in enumerate(tiles) if (t0_ - c0) < cl0 + n and (t0_ - c0 + r_) > cl0]

            with contextlib.ExitStack() as ps:
                AX = kb.sb(ps, "AX%d" % pi, [128, 16, Tp], BF16)
                xnT = AX
                actT = AX
                xnT_t = [AX.sub("AX%d_%d" % (pi, i)) for i in range(len(tiles))]
                actT_t = xnT_t
                pa = contextlib.ExitStack()
                ps_real = ps
                ps = pa
                gluT = kb.sb(ps, "gluT%d" % pi, [128, 8, 30 + Tpp], BF16)
                gluT_g = [gluT.sub("gluT%d_%d" % (pi, g)) for g in range(8)]
                if has_s:
                    gluS = kb.sb(ps, "gluS", [128, 8, NS, 34], BF16)
                    gluS_g = [gluS.sub("gluS_%d" % g) for g in range(8)]
                if last_pass:
                    gl32 = kb.sb(ps, "gl32", [128, 8, 96], F32)
                qT = kb.sb(ps, "qT%d" % pi, [128, 8, Tp], BF16)
                qiT = kb.sb(ps, "qiT%d" % pi, [64, 8, Tp], BF16)
                ps = ps_real
                qT_t = [qT.sub("qT%d_%d" % (pi, i)) for i in range(len(tiles))]
                qiT_t = [qiT.sub("qiT%d_%d" % (pi, i)) for i in range(len(tiles))]
                scope("P1_%d" % pi)
                with contextlib.ExitStack() as p1:
                    xt = [kb.sb(p1, "xt%d" % i, [128, D], F32) for i in range(2)]
                    junk = kb.sb(p1, "junk", [128, D], BF16)
                    xs = [kb.sb(p1, "xs%d" % i, [128, D], BF16) for i in range(2)]
                    st = [kb.sb(p1, "st%d" % i, [128, 4], F32) for i in range(2)]
                    for ti, (t0, rows) in enumerate(tiles):
                        s = ti % 2
                        lc = t0 - c0
                        dma("sp", xt[s][:rows, :], x[t0:t0 + rows, :], writes=[xt[s]])
                        op("act", lambda e: e.activation(out=junk[:rows, :], in_=xt[s][:rows, :], func=AF.Square,
                                                         accum_out=st[s][:rows, 0:1]), reads=[xt[s]], writes=[junk, st[s]])
                        op("dve", lambda e: e.tensor_scalar(out=st[s][:rows, 1:2], in0=st[s][:rows, 0:1], scalar1=1.0 / D,
                                                            scalar2=EPS, op0=ALU.mult, op1=ALU.add), reads=[st[s]], writes=[st[s]])
                        op("act", lambda e: e.activation(out=st[s][:rows, 2:3], in_=st[s][:rows, 1:2], func=AF.Sqrt),
                           reads=[st[s]], writes=[st[s]])
                        op("dve", lambda e: e.reciprocal(out=st[s][:rows, 3:4], in_=st[s][:rows, 2:3]), reads=[st[s]], writes=[st[s]])
                        op("act", lambda e: e.activation(out=xs[s][:rows, :], in_=xt[s][:rows, :], func=AF.Copy,
                                                         scale=st[s][:rows, 3:4]), reads=[xt[s], st[s]], writes=[xs[s]])
                        for half in range(2):
                            bk = banks[(2 * ti + half) % 4]
                            tp = bk.t[:].bitcast(BF16)
                            for k8 in range(8):
                                kc = half * 8 + k8
                                op("pe", lambda e: e.transpose(out=tp[:, k8 * 128:k8 * 128 + rows], in_=xs[s][:rows, kc * 128:(kc + 1) * 128],
                                                               identity=ident_b[:rows, :rows]), reads=[xs[s], ident_b], writes=[bk])
                            op("dve", lambda e: e.tensor_tensor(
                                out=xnT[:, half * 8:half * 8 + 8, lc:lc + rows],
                                in0=tp.rearrange("p (k t) -> p k t", k=8)[:, :, :rows],
                                in1=gcols[:, 0, half * 8:half * 8 + 8].unsqueeze(2).broadcast_to([128, 8, rows]), op=ALU.mult),
                               reads=[bk, gcols], writes=[xnT_t[ti]])

                chk("p1")
                if "xnT" in dbg and pi == 0:
                    dma("pool", dbg["xnT"][:, :, 0:Tp], xnT[:], reads=xnT_t, is_output=True)

                chk("p1d")
                scope("P2_%d" % pi)
                with contextlib.ExitStack() as p2:
                    wblk = [kb.sb(p2, "wblk%d" % i, [128, 16, 512], BF16) for i in range(2)]
                    rp = [kb.sb(p2, "rp%d" % i, [128, 96], F32) for i in range(2)]
                    zb = [kb.sb(p2, "zb%d" % i, [128, 512], BF16) for i in range(2)]
                    z32 = [kb.sb(p2, "z32%d" % i, [128, 512], F32) for i in range(2)]
                    ra = [kb.sb(p2, "ra%d" % i, [128, 256], F32) for i in range(2)]
                    rb = [kb.sb(p2, "rb%d" % i, [128, 256], F32) for i in range(2)]
                    blocks = [("q", 0, 512), ("q", 512, 512), ("kv", 1024, 512), ("qi", 1536, 512), ("kw", 2048, 72)]
                    it = 0
                    for bi, (kind, col0, ncol) in enumerate(blocks):
                        wb = wblk[bi % 2]
                        dma("pool", wb[:, :, :ncol], w_in[:, col0:col0 + ncol].rearrange("(kc p) n -> p kc n", p=128), writes=[wb])
                        for ti, (t0, rows) in enumerate(tiles):
                            lc = t0 - c0
                            tile_id = t0 // 128
                            s = it % 2
                            it += 1
                            zp = banks[4 + s]
                            for kc in range(16):
                                op("pe", lambda e: e.matmul(zp.t[:rows, :ncol], lhsT=xnT[:, kc, lc:lc + rows], rhs=wb[:, kc, :ncol],
                                                            start=(kc == 0), stop=(kc == 15)), reads=[xnT_t[ti], wb], writes=[zp])
                            chk("m")
                            dma("sp", rp[s][:rows, :], rope[t0:t0 + rows, :], writes=[rp[s]])
                            chk("rd")

                            def do_rope(dst, H, Dh, R, tb, col_off=0):
                                half = R // 2
                                zv = zp.t[:rows, col_off:col_off + H * Dh].rearrange("p (h d) -> p h d", h=H)
                                cs = rp[s][:rows, tb:tb + R].unsqueeze(1).broadcast_to([rows, H, R])
                                sn1 = rp[s][:rows, tb + R:tb + R + half].unsqueeze(1).broadcast_to([rows, H, half])
                                sn2 = rp[s][:rows, tb + R + half:tb + 2 * R].unsqueeze(1).broadcast_to([rows, H, half])
                                A = ra[s][:rows, :H * R].rearrange("p (h r) -> p h r", h=H)
                                B = rb[s][:rows, :H * R].rearrange("p (h r) -> p h r", h=H)
                                op("dve", lambda e: e.tensor_tensor(out=A, in0=zv[:, :, 0:R], in1=cs, op=ALU.mult), reads=[zp, rp[s]], writes=[ra[s]])
                                op("dve", lambda e: e.tensor_tensor(out=B[:, :, 0:half], in0=zv[:, :, half:R], in1=sn1, op=ALU.mult), reads=[zp, rp[s]], writes=[rb[s]])
                                op("dve", lambda e: e.tensor_tensor(out=B[:, :, half:R], in0=zv[:, :, 0:half], in1=sn2, op=ALU.mult), reads=[zp, rp[s]], writes=[rb[s]])
                                return A, B

                            if kind == "q":
                                h0 = col0 // 128
                                A, B = do_rope(None, 4, 128, 32, 0)
                                chk("r")
                                op("act", lambda e: e.activation(out=zb[s][:rows, :], in_=zp.t[:rows, :], func=AF.Copy), reads=[zp], writes=[zb[s]])
                                op("dve", lambda e: e.tensor_tensor(out=zb[s][:rows, :].rearrange("p (h d) -> p h d", h=4)[:, :, 0:32], in0=A, in1=B, op=ALU.add),
                                   reads=[ra[s], rb[s]], writes=[zb[s]])
                                chk("z")
                                tb_ = banks[(it % 2)]
                                tpv = tb_.t[:].bitcast(BF16)
                                for h in range(4):
                                    op("pe", lambda e: e.transpose(out=tpv[:, h * 128:h * 128 + rows], in_=zb[s][:rows, h * 128:(h + 1) * 128],
                                                                   identity=ident_b[:rows, :rows]), reads=[zb[s], ident_b], writes=[tb_])
                                chk("t")
                                op("act", lambda e: e.activation(out=qT[:, h0:h0 + 4, lc:lc + rows],
                                                                 in_=tpv[:, 0:512].rearrange("p (h t) -> p h t", h=4)[:, :, :rows], func=AF.Copy),
                                   reads=[tb_], writes=[qT_t[ti]])
                            elif kind == "kv":
                                A, B = do_rope(None, 2, 128, 32, 0)
                                op("act", lambda e: e.activation(out=z32[s][:rows, :], in_=zp.t[:rows, :], func=AF.Copy), reads=[zp], writes=[z32[s]])
                                op("dve", lambda e: e.tensor_tensor(out=z32[s][:rows, 0:256].rearrange("p (h d) -> p h d", h=2)[:, :, 0:32], in0=A, in1=B, op=ALU.add),
                                   reads=[ra[s], rb[s]], writes=[z32[s]])
                                dma("sp", k_out[t0:t0 + rows, :], z32[s][:rows, 0:256], reads=[z32[s]], is_output=True)
                                dma("sp", v_out[t0:t0 + rows, :], z32[s][:rows, 256:512], reads=[z32[s]], is_output=True)
                                op("act", lambda e: e.activation(out=Vb[:rows, tile_id, :], in_=z32[s][:rows, 256:512], func=AF.Copy), reads=[z32[s]], writes=[Vb])
                                op("act", lambda e: e.activation(out=zb[s][:rows, 0:256], in_=z32[s][:rows, 0:256], func=AF.Copy), reads=[z32[s]], writes=[zb[s]])
                                tb_ = banks[(it % 2)]
                                tpv = tb_.t[:].bitcast(BF16)
                                for g in range(2):
                                    op("pe", lambda e: e.transpose(out=tpv[:, g * 128:g * 128 + rows], in_=zb[s][:rows, g * 128:(g + 1) * 128],
                                                                   identity=ident_b[:rows, :rows]), reads=[zb[s], ident_b], writes=[tb_])
                                op("act", lambda e: e.activation(out=kT[:, :, t0:t0 + rows],
                                                                 in_=tpv[:, 0:256].rearrange("p (h t) -> p h t", h=2)[:, :, :rows], func=AF.Copy),
                                   reads=[tb_], writes=[kT])
                            elif kind == "qi":
                                A, B = do_rope(None, 8, 64, 16, 64)
                                op("act", lambda e: e.activation(out=zb[s][:rows, :], in_=zp.t[:rows, :], func=AF.Copy), reads=[zp], writes=[zb[s]])
                                op("dve", lambda e: e.tensor_tensor(out=zb[s][:rows, :].rearrange("p (h d) -> p h d", h=8)[:, :, 0:16], in0=A, in1=B, op=ALU.add),
                                   reads=[ra[s], rb[s]], writes=[zb[s]])
                                tb_ = banks[(it % 2)]
                                tpv = tb_.t[:].bitcast(BF16)
                                for h in range(8):
                                    op("pe", lambda e: e.transpose(out=tpv[0:64, h * 128:h * 128 + rows], in_=zb[s][:rows, h * 64:(h + 1) * 64],
                                                                   identity=ident_b[:rows, :rows]), reads=[zb[s], ident_b], writes=[tb_])
                                op("act", lambda e: e.activation(out=qiT[:, :, lc:lc + rows],
                                                                 in_=tpv[0:64, :].rearrange("p (h t) -> p h t", h=8)[:, :, :rows], func=AF.Copy),
                                   reads=[tb_], writes=[qiT_t[ti]])
                            else:
                                A, B = do_rope(None, 1, 64, 16, 64)
                                op("act", lambda e: e.activation(out=z32[s][:rows, 0:72], in_=zp.t[:rows, 0:72], func=AF.Copy), reads=[zp], writes=[z32[s]])
                                op("dve", lambda e: e.tensor_tensor(out=z32[s][:rows, 0:16], in0=A[:, 0, :], in1=B[:, 0, :], op=ALU.add),
                                   reads=[ra[s], rb[s]], writes=[z32[s]])
                                dma("sp", ki_out[t0:t0 + rows, :], z32[s][:rows, 0:64], reads=[z32[s]], is_output=True)
                                op("dve", lambda e: e.tensor_scalar(out=wiS[:rows, tile_id, :], in0=z32[s][:rows, 64:72], scalar1=float(64 ** -0.5 * 8 ** -0.5),
                                                                    scalar2=None, op0=ALU.mult), reads=[z32[s]], writes=[wiS])
                                op("act", lambda e: e.activation(out=zb[s][:rows, 0:64], in_=z32[s][:rows, 0:64], func=AF.Copy), reads=[z32[s]], writes=[zb[s]])
                                tb_ = banks[(it % 2)]
                                tpv = tb_.t[:].bitcast(BF16)
                                op("pe", lambda e: e.transpose(out=tpv[0:64, 0:rows], in_=zb[s][:rows, 0:64],
                                                               identity=ident_b[:rows, :rows]), reads=[zb[s], ident_b], writes=[tb_])
                                op("act", lambda e: e.activation(out=kiT[:, t0:t0 + rows], in_=tpv[0:64, 0:rows], func=AF.Copy), reads=[tb_], writes=[kiT])

                        chk("b%d" % bi)
                scope("P3a_%d" % pi)
                with contextlib.ExitStack() as p3:
                    wga = [kb.sb(p3, "wga%d" % i, [128, 16, 256], BF16) for i in range(2)]
                    sg = [kb.sb(p3, "sg%d" % i, [128, 512], F32) for i in range(2)]
                    if pi == 0:
                        op("pool", lambda e: e.memset(gluT[:, :, 0:30], 0.0), writes=gluT_g)
                    else:
                        op("dve", lambda e: e.tensor_copy(out=gluT[:, :, 0:30], in_=halo[:]), reads=[halo], writes=gluT_g)
                    if has_s:
                        stt = [kb.sb(p3, "stt%d" % i, [120, 1024], F32) for i in range(2)]
                        for q4 in range(4):
                            st_ = stt[q4 % 2]
                            dma("sp", st_[:, :], state[q4 * 120:(q4 + 1) * 120, :], writes=[st_])
                            for sl in range(4):
                                dma("sp", conv_s[q4 * 4 + sl, 0:26, :], st_[sl * 30 + 4:sl * 30 + 30, :], reads=[st_], is_output=True)
                            for gh in range(2):
                                bk = banks[gh]
                                for g4 in range(4):
                                    g = gh * 4 + g4
                                    op("pe", lambda e: e.transpose(out=bk.t[:, g4 * 128:g4 * 128 + 120], in_=st_[:120, g * 128:(g + 1) * 128],
                                                                   identity=ident_f[:120, :120]), reads=[st_, ident_f], writes=[bk])
                                op("act", lambda e: e.activation(
                                    out=gluS[:, gh * 4:gh * 4 + 4, q4 * 4:q4 * 4 + 4, 0:30],
                                    in_=bk.t[:, :].rearrange("p (g x) -> p g x", g=4)[:, :, 0:120].rearrange("p g (s r) -> p g s r", s=4),
                                    func=AF.Copy), reads=[bk], writes=gluS_g[gh * 4:gh * 4 + 4])
                    it3 = 0
                    for g in range(8):
                        wg = wga[g % 2]
                        ca = OFF_GLU + g * 128
                        dma("pool", wg[:, :, 0:128], w_in[:, ca:ca + 128].rearrange("(kc p) n -> p kc n", p=128), writes=[wg])
                        dma("pool", wg[:, :, 128:256], w_in[:, ca + 1024:ca + 1152].rearrange("(kc p) n -> p kc n", p=128), writes=[wg])
                        for (cl0, n, is_s) in chunks:
                            s3 = it3 % 2
                            it3 += 1
                            bA, bB = banks[4 + s3], banks[6 + s3]
                            rt = [xnT_t[i] for i in tiles_of(cl0, n)]
                            for kc in range(16):
                                op("pe", lambda e: e.matmul(bA.t[:, :n], lhsT=wg[:, kc, 0:128], rhs=xnT[:, kc, cl0:cl0 + n],
                                                            start=(kc == 0), stop=(kc == 15)), reads=rt + [wg], writes=[bA])
                            for kc in range(16):
                                op("pe", lambda e: e.matmul(bB.t[:, :n], lhsT=wg[:, kc, 128:256], rhs=xnT[:, kc, cl0:cl0 + n],
                                                            start=(kc == 0), stop=(kc == 15)), reads=rt + [wg], writes=[bB])
                            op("act", lambda e: e.activation(out=sg[s3][:, :n], in_=bB.t[:, :n], func=AF.Sigmoid), reads=[bB], writes=[sg[s3]])
                            if not is_s:
                                op("dve", lambda e: e.tensor_tensor(out=gluT[:, g, 30 + cl0:30 + cl0 + n], in0=bA.t[:, :n], in1=sg[s3][:, :n], op=ALU.mult),
                                   reads=[bA, sg[s3]], writes=[gluT_g[g]])
                                if last_pass and cl0 + n == Tpp:
                                    op("dve", lambda e: e.tensor_tensor(out=gl32[:, g, 0:32], in0=bA.t[:, n - 32:n], in1=sg[s3][:, n - 32:n], op=ALU.mult),
                                       reads=[bA, sg[s3]], writes=[gl32])
                            else:
                                op("dve", lambda e: e.tensor_tensor(out=gluS[:, g, :, 30:34], in0=bA.t[:, :n].rearrange("p (s i) -> p s i", i=4),
                                                                    in1=sg[s3][:, :n].rearrange("p (s i) -> p s i", i=4), op=ALU.mult),
                                   reads=[bA, sg[s3]], writes=[gluS_g[g]])
                                op("dve", lambda e: e.tensor_tensor(out=gl32[:, g, 32:96], in0=bA.t[:, :n], in1=sg[s3][:, :n], op=ALU.mult),
                                   reads=[bA, sg[s3]], writes=[gl32])
                    if not last_pass:
                        op("dve", lambda e: e.tensor_copy(out=halo[:], in_=gluT[:, :, Tpp:Tpp + 30]), reads=gluT_g, writes=[halo])
                chk("p3a")
                actT_c = [[xnT_t[i] for i in tiles_of(cl0_, n_)] for (cl0_, n_, _s) in chunks]

                scope("P3b_%d" % pi)
                with contextlib.ExitStack() as p3:
                    Dg = [kb.sb(p3, "Dg%d" % i, [128, 31, 128], BF16) for i in range(2)]
                    yb = kb.sb(p3, "yb", [128, 8, 512], F32)
                    yb_g = [yb.sub("yb_%d" % g) for g in range(8)]
                    ysq = [kb.sb(p3, "ysq%d" % i, [128, 512], F32) for i in range(2)]
                    mu = kb.sb(p3, "mu", [128, 512], F32)
                    rs = kb.sb(p3, "rs", [128, 512], F32)
                    tmp = kb.sb(p3, "tmp", [128, 512], F32)
                    it3 = 0
                    for ci, (cl0, n, is_s) in enumerate(chunks):
                        S1, S2 = banks[2], banks[3]
                        for g in range(8):
                            s3 = it3 % 2
                            it3 += 1
                            dg = Dg[s3]
                            op("pool", lambda e: e.tensor_tensor(out=dg[:], in0=ident_b[:].unsqueeze(1).broadcast_to([128, 31, 128]),
                                                                 in1=cwT[:, g, :].unsqueeze(2).broadcast_to([128, 31, 128]), op=ALU.mult),
                               reads=[ident_b, cwT], writes=[dg])
                            bY = banks[s3]
                            for j in range(31):
                                if not is_s:
                                    rhs = gluT[:, g, cl0 + j:cl0 + j + n]
                                    rr = [gluT_g[g]]
                                    outp = bY.t[:, :n]
                                else:
                                    rhs = gluS[:, g, :, j:j + 4]
                                    rr = [gluS_g[g]]
                                    outp = bY.t[:, :n].rearrange("p (s i) -> p s i", i=4)
                                op("pe", lambda e: e.matmul(outp, lhsT=dg[:, j, :], rhs=rhs, start=(j == 0), stop=(j == 30)),
                                   reads=rr + [dg], writes=[bY])
                            op("act", lambda e: e.activation(out=yb[:, g, :n], in_=bY.t[:, :n], func=AF.Identity, bias=ccol[:, 0, g:g + 1]),
                               reads=[bY, ccol], writes=[yb_g[g]])
                            op("act", lambda e: e.activation(out=ysq[s3][:, :n], in_=bY.t[:, :n], func=AF.Square, bias=ccol[:, 0, g:g + 1]),
                               reads=[bY, ccol], writes=[ysq[s3]])
                            op("pe", lambda e: e.matmul(S1.t[:, :n], lhsT=ones_f[:], rhs=yb[:, g, :n], start=(g == 0), stop=(g == 7)),
                               reads=[ones_f, yb_g[g]], writes=[S1])
                            op("pe", lambda e: e.matmul(S2.t[:, :n], lhsT=ones_f[:], rhs=ysq[s3][:, :n], start=(g == 0), stop=(g == 7)),
                               reads=[ones_f, ysq[s3]], writes=[S2])
                        op("dve", lambda e: e.tensor_scalar(out=mu[:, :n], in0=S1.t[:, :n], scalar1=1.0 / 1024, scalar2=None, op0=ALU.mult), reads=[S1], writes=[mu])
                        op("dve", lambda e: e.tensor_tensor(out=tmp[:, :n], in0=mu[:, :n], in1=mu[:, :n], op=ALU.mult), reads=[mu], writes=[tmp])
                        op("dve", lambda e: e.scalar_tensor_tensor(out=tmp[:, :n], in0=S2.t[:, :n], scalar=1.0 / 1024, in1=tmp[:, :n], op0=ALU.mult, op1=ALU.subtract),
                           reads=[S2, tmp], writes=[tmp])
                        op("dve", lambda e: e.tensor_scalar(out=tmp[:, :n], in0=tmp[:, :n], scalar1=EPS, scalar2=None, op0=ALU.add), reads=[tmp], writes=[tmp])
                        op("act", lambda e: e.activation(out=tmp[:, :n], in_=tmp[:, :n], func=AF.Sqrt), reads=[tmp], writes=[tmp])
                        op("dve", lambda e: e.reciprocal(out=rs[:, :n], in_=tmp[:, :n]), reads=[tmp], writes=[rs])
                        for g in range(8):
                            op("dve", lambda e: e.tensor_tensor(out=yb[:, g, :n], in0=yb[:, g, :n], in1=mu[:, :n], op=ALU.subtract), reads=[yb_g[g], mu], writes=[yb_g[g]])
                            op("dve", lambda e: e.tensor_tensor(out=yb[:, g, :n], in0=yb[:, g, :n], in1=rs[:, :n], op=ALU.mult), reads=[yb_g[g], rs], writes=[yb_g[g]])
                            op("act", lambda e: e.activation(out=actT[:, 8 + g, cl0:cl0 + n], in_=yb[:, g, :n], func=AF.Silu,
                                                             scale=ccol[:, 1, g:g + 1], bias=ccol[:, 2, g:g + 1]),
                               reads=[yb_g[g], ccol], writes=actT_c[ci])
                    if last_pass:
                        cst = kb.sb(p3, "cst", [64, 1024], F32)
                        for (c_lo, c_n, which) in ((0, 32, "p"), (32, 64, "s")):
                            if which == "s" and not has_s:
                                continue
                            for gh in range(2):
                                bk = banks[4 + gh]
                                for g4 in range(4):
                                    g = gh * 4 + g4
                                    op("pe", lambda e: e.transpose(out=bk.t[:c_n, g4 * 128:(g4 + 1) * 128], in_=gl32[:, g, c_lo:c_lo + c_n],
                                                                   identity=ident_f[:, :]), reads=[gl32, ident_f], writes=[bk])
                                op("act", lambda e: e.activation(out=cst[:c_n, gh * 512:(gh + 1) * 512], in_=bk.t[:c_n, :], func=AF.Copy), reads=[bk], writes=[cst])
                            if which == "p":
                                dma("sp", conv_p[:, :], cst[2:32, :], reads=[cst], is_output=True)
                            else:
                                for s_ in range(NS):
                                    dma("sp", conv_s[s_, 26:30, :], cst[4 * s_:4 * s_ + 4, :], reads=[cst], is_output=True)
                chk("p3b")
                if "convT" in dbg:
                    for g in range(8):
                        dma("pool", dbg["convT"][:, g, c0:c0 + Tp], actT[:, 8 + g, :], reads=xnT_t, is_output=True)
                scope("P4_%d" % pi)
                with contextlib.ExitStack() as p4:
                    Rh = [kb.sb(p4, "Rh%d" % i, [128, 512], BF16) for i in range(3)]
                    Dw = [kb.sb(p4, "Dw%d" % i, [128, 8, 128], BF16) for i in range(2)]
                    iscA = [kb.sb(p4, "iscA%d" % i, [128, 2048], F32) for i in range(4)]
                    iscWs_ = [kb.sb(p4, "iscW%d" % i, [128, 2048], F32) for i in range(2)]
                    mx8 = [kb.sb(p4, "mx8_%d" % i, [128, 8], F32) for i in range(4)]
                    maskb = [kb.sb(p4, "maskb%d" % i, [128, 2048], BF16) for i in range(2)]
                    PT = [kb.sb(p4, "PT%d" % i, [128, 512], BF16) for i in range(3)]
                    rsum = [kb.sb(p4, "rsum%d" % i, [128, 512], F32) for i in range(2)]
                    cnt = {"S": 0, "R": 0, "L": 0, "P": 0, "G": 0}
                    ptl = [(ti, t0) for ti, (t0, rows) in enumerate(tiles) if t0 < SEQ]

                    def p4_indexer(ti, t0):
                        j = t0 // 128
                        lc = t0 - c0
                        L = (j + 1) * 128
                        dw, ia = Dw[ti % 2], iscA[ti % 4]
                        op("pool", lambda e: e.tensor_tensor(out=dw[:], in0=ident_b[:].unsqueeze(1).broadcast_to([128, 8, 128]),
                                                             in1=wiS[:, j, :].unsqueeze(2).broadcast_to([128, 8, 128]), op=ALU.mult),
                           reads=[ident_b, wiS], writes=[dw])
                        nch = (L + 511) // 512
                        items = [(c4, h) for c4 in range(nch) for h in range(8)]
                        slots = {}

                        def S_(i):
                            c4, h = items[i]
                            l0 = c4 * 512
                            n = min(512, L - l0)
                            Sb = banks[cnt["S"] % 2]
                            cnt["S"] += 1
                            slots[i] = Sb
                            op("pe", lambda e: e.matmul(Sb.t[:, :n], lhsT=qiT[:, h, lc:lc + 128], rhs=kiT[:, l0:l0 + n], start=True, stop=True),
                               reads=[qiT_t[ti], kiT], writes=[Sb])

                        S_(0)
                        for i, (c4, h) in enumerate(items):
                            if i + 1 < len(items):
                                S_(i + 1)
                            l0 = c4 * 512
                            n = min(512, L - l0)
                            Sb = slots.pop(i)
                            iP = banks[2 + c4 % 2]
                            rh = Rh[cnt["R"] % 3]
                            cnt["R"] += 1
                            op("act", lambda e: e.activation(out=rh[:, :n], in_=Sb.t[:, :n], func=AF.Relu), reads=[Sb], writes=[rh])
                            op("pe", lambda e: e.matmul(iP.t[:, :n], lhsT=dw[:, h, :], rhs=rh[:, :n], start=(h == 0), stop=(h == 7)),
                               reads=[dw, rh], writes=[iP])
                            if h == 7:
                                op("act", lambda e: e.activation(out=ia[:, l0:l0 + n], in_=iP.t[:, :n], func=AF.Copy), reads=[iP], writes=[ia])
                        op("dve", lambda e: e.tensor_tensor(out=ia[:, j * 128:(j + 1) * 128], in0=ia[:, j * 128:(j + 1) * 128], in1=cmask[:], op=ALU.add),
                           reads=[ia, cmask], writes=[ia])
                        if "isc" in dbg and j == int(os.environ.get("KDBGJ", "3")):
                            dma("sp", dbg["isc"][:, 0:L], ia[:, 0:L], reads=[ia], is_output=True)

                    def p4_topk_group(grp):
                        st_ = []
                        for gi_, (ti, t0) in enumerate(grp):
                            j = t0 // 128
                            st_.append({"ti": ti, "j": j, "L": (j + 1) * 128, "ia": iscA[ti % 4], "src": iscA[ti % 4], "w": iscWs_[gi_],
                                        "m": [mx8[2 * gi_], mx8[2 * gi_ + 1]], "mb": maskb[ti % 2]})
                        for r in range(32):
                            for d_ in st_:
                                if d_["j"] >= 2:
                                    m8, src, L = d_["m"][r % 2], d_["src"], d_["L"]
                                    op("dve", lambda e: e.max(out=m8[:], in_=src[:, :L]), reads=[src], writes=[m8])
                            if r < 31:
                                for d_ in st_:
                                    if d_["j"] >= 2:
                                        m8, src, L, w_ = d_["m"][r % 2], d_["src"], d_["L"], d_["w"]
                                        op("dve", lambda e: e.match_replace(out=w_[:, :L], in_to_replace=m8[:], in_values=src[:, :L], imm_value=NEG),
                                           reads=[src, m8], writes=[w_])
                                        d_["src"] = w_
                        for d_ in st_:
                            L, ia, mb = d_["L"], d_["ia"], d_["mb"]
                            if d_["j"] >= 2:
                                m8 = d_["m"][31 % 2]
                                thr_ap, thr_r = m8[:, 7:8], m8
                            else:
                                thr_ap, thr_r = thr0[:, 0:1], thr0
                            op("dve", lambda e: e.tensor_scalar(out=mb[:, :L], in0=ia[:, :L], scalar1=thr_ap, scalar2=NEG, op0=ALU.is_lt, op1=ALU.mult),
                               reads=[ia, thr_r], writes=[mb])

                    def p4_attn(ti, t0):
                        j = t0 // 128
                        lc = t0 - c0
                        mb = maskb[ti % 2]
                        OT, SM = banks[6], banks[7]
                        items = [(g, lb) for g in range(2) for lb in range(j + 1)]
                        slots = {}

                        def LT_(i):
                            g, lb = items[i]
                            LT = banks[4 + cnt["L"] % 2]
                            cnt["L"] += 1
                            slots[i] = LT
                            op("pe", lambda e: e.matmul(LT.t[:, :], lhsT=kT[:, g, lb * 128:(lb + 1) * 128], rhs=qT[:, 4 * g:4 * g + 4, lc:lc + 128],
                                                        start=True, stop=False), reads=[kT, qT_t[ti]], writes=[LT])
                            op("pe", lambda e: e.matmul(LT.t[:, :], lhsT=mb[:, lb * 128:(lb + 1) * 128], rhs=I4[:], start=False, stop=True),
                               reads=[mb, I4], writes=[LT])

                        LT_(0)
                        for i, (g, lb) in enumerate(items):
                            if i + 1 < len(items):
                                LT_(i + 1)
                            LT = slots.pop(i)
                            pt = PT[cnt["P"] % 3]
                            cnt["P"] += 1
                            op("act", lambda e: e.activation(out=pt[:], in_=LT.t[:, :], func=AF.Exp, scale=float(128 ** -0.5)), reads=[LT], writes=[pt])
                            op("pe", lambda e: e.matmul(OT.t[:, :], lhsT=Vb[:, lb, g * 128:(g + 1) * 128], rhs=pt[:], start=(lb == 0), stop=(lb == j)),
                               reads=[Vb, pt], writes=[OT])
                            op("pe", lambda e: e.matmul(SM.t[:, :], lhsT=ones_b[:], rhs=pt[:], start=(lb == 0), stop=(lb == j)),
                               reads=[ones_b, pt], writes=[SM])
                            if lb == j:
                                rsm = rsum[cnt["G"] % 2]
                                cnt["G"] += 1
                                op("dve", lambda e: e.reciprocal(out=rsm[:], in_=SM.t[:, :]), reads=[SM], writes=[rsm])
                                op("dve", lambda e: e.tensor_tensor(out=actT[:, 4 * g:4 * g + 4, lc:lc + 128], in0=OT.t[:, :].rearrange("p (h t) -> p h t", h=4),
                                                                    in1=rsm[:].rearrange("p (h t) -> p h t", h=4), op=ALU.mult),
                                   reads=[OT, rsm], writes=[actT_t[ti]])

                    groups4 = [ptl[i_:i_ + 2] for i_ in range(0, len(ptl), 2)]
                    if groups4:
                        for tt_ in groups4[0]:
                            p4_indexer(*tt_)
                    for k_, grp in enumerate(groups4):
                        if k_ + 1 < len(groups4):
                            for tt_ in groups4[k_ + 1]:
                                p4_indexer(*tt_)
                        p4_topk_group(grp)
                        for tt_ in grp:
                            p4_attn(*tt_)
                scope("P4s_%d" % pi)
                if has_s:
                    lcS = SEQ - c0
                    tiS = len(tiles) - 1
                    with contextlib.ExitStack() as p4s:
                        ptb_ = kb.sb(p4s, "ptb_", [128, 256], I32)
                        ptf = kb.sb(p4s, "ptf", [128, 256], F32)
                        pcol = kb.sb(p4s, "pcol", [128, 1], F32)
                        idxs = kb.sb(p4s, "idxs", [128, 256], U32)
                        cmS = kb.sb(p4s, "cmS", [64, 64], F32)
                        sel32 = kb.sb(p4s, "sel32", [64, 256], F32)
                        selS = kb.sb(p4s, "selS", [64, 256], BF16)
                        dma("sp", ptb_[:], pt_d.partition_broadcast(128), writes=[ptb_])
                        dma("sp", pcol[:], pidx_d, writes=[pcol])
                        dma("sp", cmS[:], cmaskS_d, writes=[cmS])
                        dma("sp", sel32[:], selS_d, writes=[sel32])
                        op("dve", lambda e: e.tensor_copy(out=selS[:], in_=sel32[:]), reads=[sel32], writes=[selS])
                        op("dve", lambda e: e.tensor_copy(out=ptf[:], in_=ptb_[:]), reads=[ptb_], writes=[ptf])
                        op("dve", lambda e: e.tensor_scalar(out=ptf[:], in0=ptf[:], scalar1=128.0, scalar2=pcol[:, 0:1], op0=ALU.mult, op1=ALU.add), reads=[ptf, pcol], writes=[ptf])
                        op("dve", lambda e: e.tensor_copy(out=idxs[:], in_=ptf[:]), reads=[ptf], writes=[idxs])
                        iscS = kb.sb(p4s, "iscS", [64, 2112], F32)
                        iscWs = kb.sb(p4s, "iscWs", [64, 2112], F32)
                        mbS = kb.sb(p4s, "mbS", [64, 2112], BF16)
                        m8s = [kb.sb(p4s, "m8s%d" % i, [64, 8], F32) for i in range(2)]
                        DwS = kb.sb(p4s, "DwS", [64, 8, 64], BF16)
                        op("pool", lambda e: e.tensor_tensor(out=DwS[:], in0=ident_b[0:64, 0:64].unsqueeze(1).broadcast_to([64, 8, 64]),
                                                             in1=wiS[0:64, 16, :].unsqueeze(2).broadcast_to([64, 8, 64]), op=ALU.mult), reads=[ident_b, wiS], writes=[DwS])
                        with contextlib.ExitStack() as pix:
                            qiZ = kb.sb(pix, "qiZ", [64, 8, NS, 64], BF16)
                            op("pool", lambda e: e.memset(qiZ[:], 0.0), writes=[qiZ])
                            for s_ in range(NS):
                                op("act", lambda e: e.activation(out=qiZ[:, :, s_, 4 * s_:4 * s_ + 4], in_=qiT[:, :, lcS + 4 * s_:lcS + 4 * s_ + 4], func=AF.Copy),
                                   reads=[qiT_t[tiS]], writes=[qiZ])
                            kis = [kb.sb(pix, "kis%d" % i, [128, 16, 64], BF16) for i in range(4)]
                            kiTs = [kb.sb(pix, "kiTs%d" % i, [64, 512], BF16) for i in range(2)]
                            RhS = [kb.sb(pix, "RhS%d" % i, [64, 512], BF16) for i in range(3)]
                            for h in range(8):
                                Sb = banks[4 + h % 2]
                                rh = RhS[h % 3]
                                op("pe", lambda e: e.matmul(Sb.t[0:64, 0:64], lhsT=qiT[:, h, lcS:lcS + 64], rhs=kiT[:, SEQ:SEQ + 64], start=True, stop=True),
                                   reads=[qiT_t[tiS], kiT], writes=[Sb])
                                op("act", lambda e: e.activation(out=rh[:, 0:64], in_=Sb.t[0:64, 0:64], func=AF.Relu), reads=[Sb], writes=[rh])
                                op("pe", lambda e: e.matmul(banks[6].t[0:64, 0:64], lhsT=DwS[:, h, :], rhs=rh[:, 0:64], start=(h == 0), stop=(h == 7)),
                                   reads=[DwS, rh], writes=[banks[6]])
                            op("dve", lambda e: e.tensor_tensor(out=iscS[:, 2048:2112], in0=banks[6].t[0:64, 0:64], in1=cmS[:], op=ALU.add), reads=[banks[6], cmS], writes=[iscS])
                            groups = [(s_, c4) for s_ in range(NS) for c4 in range(4)]
                            gk = {}
                            cq = {"q": 0, "S": 0, "R": 0}

                            def prep_(gi):
                                s_, c4 = groups[gi]
                                ks_ = kis[s_ % 4]
                                if c4 == 0:
                                    for pg in range(16):
                                        kb.idma(ks_[:, pg, :], cki, idxs[:, s_ * 16 + pg:s_ * 16 + pg + 1], reads=[idxs], writes=[ks_])
                                tb_ = banks[6 + cq["q"] % 2]
                                kt_ = kiTs[cq["q"] % 2]
                                cq["q"] += 1
                                tpv = tb_.t[:].bitcast(BF16)
                                for p4_ in range(4):
                                    op("pe", lambda e: e.transpose(out=tpv[0:64, p4_ * 128:(p4_ + 1) * 128], in_=ks_[:, c4 * 4 + p4_, :], identity=ident_b[:]),
                                       reads=[ks_, ident_b], writes=[tb_])
                                op("dve", lambda e: e.tensor_copy(out=kt_[:], in_=tpv[0:64, 0:512]), reads=[tb_], writes=[kt_])
                                gk[gi] = kt_

                            prep_(0)
                            for gi, (s_, c4) in enumerate(groups):
                                if gi + 1 < len(groups):
                                    prep_(gi + 1)
                                kt_ = gk.pop(gi)
                                sl = {}

                                def S2_(h):
                                    Sb = banks[4 + cq["S"] % 2]
                                    cq["S"] += 1
                                    sl[h] = Sb
                                    op("pe", lambda e: e.matmul(Sb.t[0:64, :], lhsT=qiZ[:, h, s_, :], rhs=kt_[:], start=True, stop=True), reads=[qiZ, kt_], writes=[Sb])

                                S2_(0)
                                for h in range(8):
                                    if h + 1 < 8:
                                        S2_(h + 1)
                                    Sb = sl.pop(h)
                                    rh = RhS[cq["R"] % 3]
                                    cq["R"] += 1
                                    op("act", lambda e: e.activation(out=rh[:], in_=Sb.t[0:64, :], func=AF.Relu), reads=[Sb], writes=[rh])
                                    op("pe", lambda e: e.matmul(banks[c4].t[0:64, :], lhsT=DwS[:, h, :], rhs=rh[:], start=(s_ == 0 and h == 0), stop=(s_ == NS - 1 and h == 7)),
                                       reads=[DwS, rh], writes=[banks[c4]])
                            for c4 in range(4):
                                op("act", lambda e: e.activation(out=iscS[:, c4 * 512:(c4 + 1) * 512], in_=banks[c4].t[0:64, :], func=AF.Copy), reads=[banks[c4]], writes=[iscS])
                        if "iscS" in dbg:
                            dma("sp", dbg["iscS"], iscS[:], reads=[iscS], is_output=True)
                        src = iscS
                        for r in range(32):
                            m8 = m8s[r % 2]
                            op("dve", lambda e: e.max(out=m8[:], in_=src[:, :]), reads=[src], writes=[m8])
                            if r < 31:
                                op("dve", lambda e: e.match_replace(out=iscWs[:, :], in_to_replace=m8[:], in_values=src[:, :], imm_value=NEG), reads=[src, m8], writes=[iscWs])
                                src = iscWs
                        op("dve", lambda e: e.tensor_scalar(out=mbS[:], in0=iscS[:], scalar1=m8[:, 7:8], scalar2=NEG, op0=ALU.is_lt, op1=ALU.mult), reads=[iscS, m8], writes=[mbS])
                        with contextlib.ExitStack() as pat:
                            Ks = [kb.sb(pat, "Ks%d" % i, [128, 16, 256], BF16) for i in range(4)]
                            Vs = [kb.sb(pat, "Vs%d" % i, [128, 16, 256], BF16) for i in range(4)]
                            kTs = [kb.sb(pat, "kTs%d" % i, [128, 2, 2048], BF16) for i in range(2)]
                            PTs = [kb.sb(pat, "PTs%d" % i, [128, 272], BF16) for i in range(2)]
                            rsS = [kb.sb(pat, "rsS%d" % i, [128, 32], F32) for i in range(2)]
                            ca = {"t": 0, "l": 0}

                            def gatherKV(s_):
                                K_, V_ = Ks[s_ % 4], Vs[s_ % 4]
                                for pg in range(16):
                                    kb.idma(K_[:, pg, :], ck, idxs[:, s_ * 16 + pg:s_ * 16 + pg + 1], reads=[idxs], writes=[K_])
                                    kb.idma(V_[:, pg, :], cv, idxs[:, s_ * 16 + pg:s_ * 16 + pg + 1], reads=[idxs], writes=[V_])

                            def prepK(s_):
                                K_, V_, kT_ = Ks[s_ % 4], Vs[s_ % 4], kTs[s_ % 2]
                                if s_ + 2 < NS:
                                    gatherKV(s_ + 2)
                                for g in range(2):
                                    for p8 in range(2):
                                        tb_ = banks[ca["t"] % 2]
                                        ca["t"] += 1
                                        tpv = tb_.t[:].bitcast(BF16)
                                        for k8 in range(8):
                                            pg = p8 * 8 + k8
                                            op("pe", lambda e: e.transpose(out=tpv[:, k8 * 128:(k8 + 1) * 128], in_=K_[:, pg, g * 128:(g + 1) * 128], identity=ident_b[:]),
                                               reads=[K_, ident_b], writes=[tb_])
                                        op("act", lambda e: e.activation(out=kT_[:, g, p8 * 1024:(p8 + 1) * 1024], in_=tpv[:, :], func=AF.Copy), reads=[tb_], writes=[kT_])

                            def attnS(s_):
                                K_, V_, kT_ = Ks[s_ % 4], Vs[s_ % 4], kTs[s_ % 2]
                                OT, SM = banks[4], banks[5]
                                sv = selS[:, s_ * 16:(s_ + 1) * 16]
                                lts = []
                                for g in range(2):
                                    LT = banks[2 + g]
                                    qv = qT[:, 4 * g:4 * g + 4, lcS + 4 * s_:lcS + 4 * s_ + 4]
                                    for lb in range(16):
                                        op("pe", lambda e: e.matmul(LT.t[:, lb * 16:(lb + 1) * 16], lhsT=kT_[:, g, lb * 128:(lb + 1) * 128], rhs=qv, start=True, stop=False),
                                           reads=[kT_, qT_t[tiS]], writes=[LT])
                                        op("pe", lambda e: e.matmul(LT.t[:, lb * 16:(lb + 1) * 16], lhsT=mbS[:, lb * 128:(lb + 1) * 128], rhs=sv, start=False, stop=True),
                                           reads=[mbS, selS], writes=[LT])
                                    op("pe", lambda e: e.matmul(LT.t[0:64, 256:272], lhsT=kT[:, g, SEQ:SEQ + 64], rhs=qv, start=True, stop=False), reads=[kT, qT_t[tiS]], writes=[LT])
                                    op("pe", lambda e: e.matmul(LT.t[0:64, 256:272], lhsT=mbS[:, 2048:2112], rhs=sv, start=False, stop=True), reads=[mbS, selS], writes=[LT])
                                for g in range(2):
                                    LT = banks[2 + g]
                                    pt = PTs[g]
                                    op("act", lambda e: e.activation(out=pt[:, 0:256], in_=LT.t[:, 0:256], func=AF.Exp, scale=float(128 ** -0.5)), reads=[LT], writes=[pt])
                                    op("act", lambda e: e.activation(out=pt[0:64, 256:272], in_=LT.t[0:64, 256:272], func=AF.Exp, scale=float(128 ** -0.5)), reads=[LT], writes=[pt])
                                for g in range(2):
                                    pt = PTs[g]
                                    for lb in range(17):
                                        if lb < 16:
                                            lv, pv, on = V_[:, lb, g * 128:(g + 1) * 128], pt[:, lb * 16:(lb + 1) * 16], ones_b[:]
                                        else:
                                            lv, pv, on = Vb[0:64, 16, g * 128:(g + 1) * 128], pt[0:64, 256:272], ones_b[0:64, :]
                                        op("pe", lambda e: e.matmul(OT.t[:, g * 16:(g + 1) * 16], lhsT=lv, rhs=pv, start=(lb == 0), stop=(lb == 16)), reads=[V_, Vb, pt], writes=[OT])
                                        op("pe", lambda e: e.matmul(SM.t[:, g * 16:(g + 1) * 16], lhsT=on, rhs=pv, start=(lb == 0), stop=(lb == 16)), reads=[ones_b, pt], writes=[SM])
                                rs_ = rsS[s_ % 2]
                                op("dve", lambda e: e.reciprocal(out=rs_[:], in_=SM.t[:, 0:32]), reads=[SM], writes=[rs_])
                                op("dve", lambda e: e.tensor_tensor(out=actT[:, 0:8, lcS + 4 * s_:lcS + 4 * s_ + 4], in0=OT.t[:, 0:32].rearrange("p (h i) -> p h i", i=4),
                                                                    in1=rs_[:].rearrange("p (h i) -> p h i", i=4), op=ALU.mult), reads=[OT, rs_], writes=[actT_t[tiS]])

                            gatherKV(0)
                            gatherKV(1)
                            prepK(0)
                            for s_ in range(NS):
                                if s_ + 1 < NS:
                                    prepK(s_ + 1)
                                attnS(s_)
                chk("p4")
                if "attnT" in dbg:
                    for h in range(8):
                        dma("pool", dbg["attnT"][:, h, c0:c0 + Tp], actT[:, h, :], reads=actT_t, is_output=True)
                if "qT" in dbg and pi == 0:
                    dma("pool", dbg["qT"][:, :, 0:Tp], qT[:], reads=qT_t, is_output=True)
                if "qiT" in dbg and pi == 0:
                    dma("pool", dbg["qiT"][:, :, 0:Tp], qiT[:], reads=qiT_t, is_output=True)
                pa.close()

                scope("P5_%d" % pi)
                acc = kb.sb(ps, "acc%d" % pi, [128, len(tiles), D], F32)
                acc_t = [acc.sub("acc%d_%d" % (pi, i)) for i in range(len(tiles))]

                def proj_resid(wsrc, nm):
                    with contextlib.ExitStack() as p5:
                        wblk = [kb.sb(p5, "w5_%d" % i, [128, 16, 512], BF16) for i in range(2)]
                        xb = [kb.sb(p5, "xb%d" % i, [128, 512], F32) for i in range(3)]
                        it5 = 0
                        for nb in range(4):
                            wb = wblk[nb % 2]
                            dma("pool", wb[:], wsrc[:, nb * 512:(nb + 1) * 512].rearrange("(kc p) n -> p kc n", p=128), writes=[wb])
                            for ti, (t0, rows) in enumerate(tiles):
                                lc = t0 - c0
                                bk = banks[it5 % 4]
                                xs_ = xb[it5 % 3]
                                it5 += 1
                                for kc in range(16):
                                    op("pe", lambda e: e.matmul(bk.t[:rows, :], lhsT=AX[:, kc, lc:lc + rows], rhs=wb[:, kc, :],
                                                                start=(kc == 0), stop=(kc == 15)), reads=[xnT_t[ti], wb], writes=[bk])
                                dma("sp", xs_[:rows, :], x[t0:t0 + rows, nb * 512:(nb + 1) * 512], writes=[xs_])
                                op("dve", lambda e: e.tensor_tensor(out=acc[:rows, ti, nb * 512:(nb + 1) * 512], in0=bk.t[:rows, :], in1=xs_[:rows, :], op=ALU.add),
                                   reads=[bk, xs_], writes=[acc_t[ti]])

                proj_resid(w_out, "wout")
                chk("p5")

                def norm_to_AX(which):
                    with contextlib.ExitStack() as pn:
                        junk = kb.sb(pn, "njunk", [128, D], BF16)
                        xs = [kb.sb(pn, "nxs%d" % i, [128, D], BF16) for i in range(2)]
                        st = [kb.sb(pn, "nst%d" % i, [128, 4], F32) for i in range(2)]
                        for ti, (t0, rows) in enumerate(tiles):
                            s_ = ti % 2
                            lc = t0 - c0
                            op("act", lambda e: e.activation(out=junk[:rows, :], in_=acc[:rows, ti, :], func=AF.Square,
                                                             accum_out=st[s_][:rows, 0:1]), reads=[acc_t[ti]], writes=[junk, st[s_]])
                            op("dve", lambda e: e.tensor_scalar(out=st[s_][:rows, 1:2], in0=st[s_][:rows, 0:1], scalar1=1.0 / D,
                                                                scalar2=EPS, op0=ALU.mult, op1=ALU.add), reads=[st[s_]], writes=[st[s_]])
                            op("act", lambda e: e.activation(out=st[s_][:rows, 2:3], in_=st[s_][:rows, 1:2], func=AF.Sqrt), reads=[st[s_]], writes=[st[s_]])
                            op("dve", lambda e: e.reciprocal(out=st[s_][:rows, 3:4], in_=st[s_][:rows, 2:3]), reads=[st[s_]], writes=[st[s_]])
                            op("act", lambda e: e.activation(out=xs[s_][:rows, :], in_=acc[:rows, ti, :], func=AF.Copy,
                                                             scale=st[s_][:rows, 3:4]), reads=[acc_t[ti], st[s_]], writes=[xs[s_]])
                            for half in range(2):
                                bk = banks[(2 * ti + half) % 4]
                                tp = bk.t[:].bitcast(BF16)
                                for k8 in range(8):
                                    kc = half * 8 + k8
                                    op("pe", lambda e: e.transpose(out=tp[:, k8 * 128:k8 * 128 + rows], in_=xs[s_][:rows, kc * 128:(kc + 1) * 128],
                                                                   identity=ident_b[:rows, :rows]), reads=[xs[s_], ident_b], writes=[bk])
                                op("dve", lambda e: e.tensor_tensor(
                                    out=AX[:, half * 8:half * 8 + 8, lc:lc + rows],
                                    in0=tp.rearrange("p (k t) -> p k t", k=8)[:, :, :rows],
                                    in1=gcols[:, which, half * 8:half * 8 + 8].unsqueeze(2).broadcast_to([128, 8, rows]), op=ALU.mult),
                                   reads=[bk, gcols], writes=[xnT_t[ti]])

                scope("P6n_%d" % pi)
                norm_to_AX(1)
                nt = len(tiles)
                with contextlib.ExitStack() as p6:
                    scope("P6a_%d" % pi)
                    p6ab = contextlib.ExitStack()
                    v16 = kb.sb(p6ab, "v16", [128, nt, 16, 16], F32)
                    I16 = kb.sb(p6ab, "I16", [128, nt, 16, 16], U32)
                    v16_t = [v16.sub("v16_%d" % i) for i in range(nt)]
                    with contextlib.ExitStack() as p6a:
                        wqb = [kb.sb(p6a, "wqb%d" % i, [128, 16, 512], BF16) for i in range(2)]
                        qhb = [kb.sb(p6a, "qhb%d" % i, [128, Tp], BF16) for i in range(2)]
                        stmp = [kb.sb(p6a, "stmp%d" % i, [128, 128], F32) for i in range(2)]
                        it6 = 0
                        for b4 in range(4):
                            wb = wqb[b4 % 2]
                            dma("pool", wb[:], peer_wq[:, b4 * 512:(b4 + 1) * 512].rearrange("(kc p) n -> p kc n", p=128), writes=[wb])
                            for bl in range(4):
                                blk = b4 * 4 + bl
                                qb = qhb[blk % 2]
                                for (cl0, n, is_s) in chunks:
                                    bk = banks[it6 % 2]
                                    it6 += 1
                                    rt = [xnT_t[i] for i in tiles_of(cl0, n)]
                                    for kc in range(16):
                                        op("pe", lambda e: e.matmul(bk.t[:, :n], lhsT=wb[:, kc, bl * 128:(bl + 1) * 128], rhs=AX[:, kc, cl0:cl0 + n],
                                                                    start=(kc == 0), stop=(kc == 15)), reads=rt + [wb], writes=[bk])
                                    op("act", lambda e: e.activation(out=qb[:, cl0:cl0 + n], in_=bk.t[:, :n], func=AF.Copy), reads=[bk], writes=[qb])
                                for ti, (t0, rows) in enumerate(tiles):
                                    lc = t0 - c0
                                    sP = banks[2 + (it6 % 2)]
                                    it6 += 1
                                    sm = stmp[it6 % 2]
                                    op("pe", lambda e: e.matmul(sP.t[:rows, 0:128], lhsT=qb[:, lc:lc + rows], rhs=skT[:, blk, :], start=True, stop=True),
                                       reads=[qb, skT], writes=[sP])
                                    op("dve", lambda e: e.max(out=v16[:rows, ti, blk, 0:8], in_=sP.t[:rows, 0:128]), reads=[sP], writes=[v16_t[ti]])
                                    op("dve", lambda e: e.max_index(out=I16[:rows, ti, blk, 0:8], in_max=v16[:rows, ti, blk, 0:8], in_values=sP.t[:rows, 0:128]),
                                       reads=[sP, v16_t[ti]], writes=[v16_t[ti]])
                                    op("dve", lambda e: e.match_replace(out=sm[:rows, :], in_to_replace=v16[:rows, ti, blk, 0:8], in_values=sP.t[:rows, 0:128], imm_value=NEG),
                                       reads=[sP, v16_t[ti]], writes=[sm])
                                    op("dve", lambda e: e.max(out=v16[:rows, ti, blk, 8:16], in_=sm[:rows, :]), reads=[sm], writes=[v16_t[ti]])
                                    op("dve", lambda e: e.max_index(out=I16[:rows, ti, blk, 8:16], in_max=v16[:rows, ti, blk, 8:16], in_values=sm[:rows, :]),
                                       reads=[sm, v16_t[ti]], writes=[v16_t[ti]])
                    chk("p6a")
                    scope("P6b_%d" % pi)
                    with contextlib.ExitStack() as p6b:
                        I16f = kb.sb(p6b, "I16f", [128, 16, 16], F32)
                        cand = kb.sb(p6b, "cand", [128, 8, 256], F32)
                        ctmp = kb.sb(p6b, "ctmp", [128, 256], F32)
                        c16 = kb.sb(p6b, "c16", [128, 8, 16], F32)
                        P16 = kb.sb(p6b, "P16", [128, 8, 16], U32)
                        ab_u = kb.sb(p6b, "ab_u", [128, 2, 8, 16], U32)
                        ab_f = kb.sb(p6b, "ab_f", [128, 2, 8, 16], F32)
                        eq = kb.sb(p6b, "eq", [128, 8, 16, 16], F32)
                        sel3 = kb.sb(p6b, "sel3", [128, 3, 128], F32)
                        zst = kb.sb(p6b, "zst", [128, 8, 2], F32)
                        selT = [kb.sb(p6b, "selT%d" % i, [128, 3, 128], BF16) for i in range(2)]
                        OH2 = [kb.sb(p6b, "OH2_%d" % i, [128, 16, 128], BF16) for i in range(2)]
                        OH1 = [kb.sb(p6b, "OH1_%d" % i, [128, 16, 128], BF16) for i in range(2)]
                        GS = kb.sb(p6b, "GS", [128, 128, 128], BF16)
                        itb = itg = 0
                        for ti, (t0, rows) in enumerate(tiles):
                            vv = v16[:rows, ti].rearrange("p (h n) a -> p h n a", n=2)
                            op("dve", lambda e: e.tensor_copy(out=I16f[:rows], in_=I16[:rows, ti]), reads=[v16_t[ti]], writes=[I16f])
                            op("dve", lambda e: e.tensor_tensor(out=cand[:rows].rearrange("p h (a b) -> p h a b", a=16),
                                                                in0=vv[:, :, 0, :].unsqueeze(3).broadcast_to([rows, 8, 16, 16]),
                                                                in1=vv[:, :, 1, :].unsqueeze(2).broadcast_to([rows, 8, 16, 16]), op=ALU.add),
                               reads=[v16_t[ti]], writes=[cand])
                            for h in range(8):
                                op("dve", lambda e: e.max(out=c16[:rows, h, 0:8], in_=cand[:rows, h, :]), reads=[cand], writes=[c16])
                                op("dve", lambda e: e.max_index(out=P16[:rows, h, 0:8], in_max=c16[:rows, h, 0:8], in_values=cand[:rows, h, :]), reads=[cand, c16], writes=[P16])
                                op("dve", lambda e: e.match_replace(out=ctmp[:rows, :], in_to_replace=c16[:rows, h, 0:8], in_values=cand[:rows, h, :], imm_value=NEG),
                                   reads=[cand, c16], writes=[ctmp])
                                op("dve", lambda e: e.max(out=c16[:rows, h, 8:16], in_=ctmp[:rows, :]), reads=[ctmp], writes=[c16])
                                op("dve", lambda e: e.max_index(out=P16[:rows, h, 8:16], in_max=c16[:rows, h, 8:16], in_values=ctmp[:rows, :]), reads=[ctmp, c16], writes=[P16])
                            op("dve", lambda e: e.tensor_scalar(out=ab_u[:rows, 0], in0=P16[:rows], scalar1=4, scalar2=None, op0=ALU.logical_shift_right), reads=[P16], writes=[ab_u])
                            op("dve", lambda e: e.tensor_scalar(out=ab_u[:rows, 1], in0=P16[:rows], scalar1=15, scalar2=None, op0=ALU.bitwise_and), reads=[P16], writes=[ab_u])
                            op("dve", lambda e: e.tensor_copy(out=ab_f[:rows], in_=ab_u[:rows]), reads=[ab_u], writes=[ab_f])
                            If = I16f[:rows].rearrange("p (h n) a -> p h n a", n=2)
                            for w_ in range(2):
                                op("dve", lambda e: e.tensor_tensor(out=eq[:rows], in0=ab_f[:rows, w_].unsqueeze(3).broadcast_to([rows, 8, 16, 16]),
                                                                    in1=iota16[:rows, :].unsqueeze(1).unsqueeze(1).broadcast_to([rows, 8, 16, 16]), op=ALU.is_equal),
                                   reads=[ab_f, iota16], writes=[eq])
                                op("dve", lambda e: e.tensor_tensor(out=eq[:rows], in0=eq[:rows], in1=If[:, :, w_, :].unsqueeze(2).broadcast_to([rows, 8, 16, 16]), op=ALU.mult),
                                   reads=[eq, I16f], writes=[eq])
                                op("dve", lambda e: e.tensor_reduce(out=sel3[:rows, w_, :].rearrange("p (h j) -> p h j", h=8), in_=eq[:rows], axis=AX_.X, op=ALU.add),
                                   reads=[eq], writes=[sel3])
                            wv = sel3[:rows, 2, :].rearrange("p (h j) -> p h j", h=8)
                            op("dve", lambda e: e.tensor_tensor(out=wv, in0=c16[:rows], in1=c16[:rows, :, 0:1].broadcast_to([rows, 8, 16]), op=ALU.subtract),
                               reads=[c16], writes=[sel3])
                            op("act", lambda e: e.activation(out=wv, in_=wv, func=AF.Exp), reads=[sel3], writes=[sel3])
                            op("dve", lambda e: e.tensor_reduce(out=zst[:rows, :, 0], in_=wv, axis=AX_.X, op=ALU.add), reads=[sel3], writes=[zst])
                            op("dve", lambda e: e.reciprocal(out=zst[:rows, :, 1], in_=zst[:rows, :, 0]), reads=[zst], writes=[zst])
                            op("dve", lambda e: e.tensor_tensor(out=wv, in0=wv, in1=zst[:rows, :, 1:2].broadcast_to([rows, 8, 16]), op=ALU.mult),
                               reads=[sel3, zst], writes=[sel3])
                            if "sel3" in dbg and t0 == 0:
                                dma("sp", dbg["sel3"], sel3[:], reads=[sel3], is_output=True)
                            sT = selT[ti % 2]
                            bkT = banks[4]
                            for w_ in range(3):
                                op("pe", lambda e: e.transpose(out=bkT.t[:, w_ * 128:w_ * 128 + rows], in_=sel3[:rows, w_, :], identity=ident_f[:rows, :rows]),
                                   reads=[sel3, ident_f], writes=[bkT])
                            op("act", lambda e: e.activation(out=sT[:, :, :rows], in_=bkT.t[:, 0:384].rearrange("p (w t) -> p w t", w=3)[:, :, :rows], func=AF.Copy),
                               reads=[bkT], writes=[sT])
                            for sub in range((rows + 15) // 16):
                                tl0 = sub * 16
                                o2, o1 = OH2[itb % 2], OH1[itb % 2]
                                itb += 1
                                io_b = iotaC[:].unsqueeze(1).broadcast_to([128, 16, 128])
                                op("dve", lambda e: e.tensor_tensor(out=o2[:], in0=io_b, in1=sT[:, 1, tl0:tl0 + 16].unsqueeze(2).broadcast_to([128, 16, 128]), op=ALU.is_equal),
                                   reads=[iotaC, sT], writes=[o2])
                                op("dve", lambda e: e.tensor_tensor(out=o1[:], in0=io_b, in1=sT[:, 0, tl0:tl0 + 16].unsqueeze(2).broadcast_to([128, 16, 128]), op=ALU.is_equal),
                                   reads=[iotaC, sT], writes=[o1])
                                op("pool", lambda e: e.tensor_tensor(out=o1[:], in0=o1[:], in1=sT[:, 2, tl0:tl0 + 16].unsqueeze(2).broadcast_to([128, 16, 128]), op=ALU.mult),
                                   reads=[o1, sT], writes=[o1])
                                for q4 in range(4):
                                    gb = banks[5 + itg % 3]
                                    itg += 1
                                    for t4 in range(4):
                                        tl = q4 * 4 + t4
                                        op("pe", lambda e: e.matmul(gb.t[:, t4 * 128:(t4 + 1) * 128], lhsT=o2[:, tl, :], rhs=o1[:, tl, :], start=True, stop=True),
                                           reads=[o2, o1], writes=[gb])
                                    tg = tl0 + q4 * 4
                                    op("act", lambda e: e.activation(out=GS[:, :, tg:tg + 4], in_=gb.t[:, :].rearrange("p (t c) -> p c t", t=4), func=AF.Copy),
                                       reads=[gb], writes=[GS])
                            for c8 in range(8):
                                dma("sp", Gd[c8 * 16:(c8 + 1) * 16, :, t0:t0 + rows].rearrange("c i t -> i c t"), GS[:, c8 * 16:(c8 + 1) * 16, :rows],
                                    reads=[GS], writes=[Gd_res], allow_slow_non_contiguous=False)
                    chk("p6b")
                    if "G" in dbg and pi == 0:
                        with contextlib.ExitStack() as pd:
                            gtmp = kb.sb(pd, "gtmp", [128, 128, 128], BF16)
                            dma("sp", gtmp[:], Gd[:, :, 0:128].rearrange("c i t -> i c t"), reads=[Gd_res], writes=[gtmp])
                            for c8 in range(8):
                                dma("pool", dbg["G"][:, c8 * 16:(c8 + 1) * 16, :], gtmp[:, c8 * 16:(c8 + 1) * 16, :], reads=[gtmp], is_output=True)
                    chk("p6c")
                    p6ab.close()
                    scope("P6d_%d" % pi)
                    with contextlib.ExitStack() as p6d:
                        ES = 4
                        NUB = 4
                        ub = [kb.sb(p6d, "ub%d" % i, [128, D], BF16) for i in range(NUB)] if pi == 0 else [None] * NUB
                        NUT = 2 if pi == 0 else 3
                        uT = [kb.sb(p6d, "uT%d" % i, [128, 16, 128], BF16) for i in range(NUT)]
                        gt = [kb.sb(p6d, "gt%d" % i, [128, Tp], BF16) for i in range(NUT)]
                        gl = [kb.sb(p6d, "gl%d" % i, [128, 512], BF16) for i in range(2)]
                        actS = [kb.sb(p6d, "actS%d" % i, [128, ES, Tp], BF16) for i in range(2)]
                        vS = [kb.sb(p6d, "vS%d" % i, [128, ES, D], BF16) for i in range(2)]
                        vsub = [[b_.sub("vS%d_%d" % (i, e_)) for e_ in range(ES)] for i, b_ in enumerate(vS)]
                        ith = itv = 0
                        nsc = 128 // ES
                        if os.environ.get("KNSC"):
                            nsc = int(os.environ["KNSC"])
                        cu = {"h": 0, "v": 0}
                        prepped = {}

                        def prepU(c):
                            sc, ec = divmod(c, ES)
                            vs = vS[sc % 2]
                            u_, uT_, g_ = ub[c % NUB], uT[c % NUT], gt[c % NUT]
                            dma("sp", g_[:, :], Gd[c, :, c0:c0 + Tp], reads=[Gd_res], writes=[g_])
                            if pi > 0:
                                dma("sp", uT_[:], uTd[c], writes=[uT_])
                                dma("sp", vs[:, ec, :], vd[c], writes=[vsub[sc % 2][ec]])
                                return
                            for half in range(2):
                                bk = banks[half]
                                tp = bk.t[:].bitcast(BF16)
                                for k8 in range(8):
                                    kc = half * 8 + k8
                                    op("pe", lambda e: e.transpose(out=tp[:, k8 * 128:(k8 + 1) * 128], in_=u_[:, kc * 128:(kc + 1) * 128], identity=ident_b[:]),
                                       reads=[u_, ident_b], writes=[bk])
                                op("act", lambda e: e.activation(out=uT_[:, half * 8:half * 8 + 8, :], in_=tp.rearrange("p (k e) -> p k e", k=8), func=AF.Copy),
                                   reads=[bk], writes=[uT_])
                            dma("sp", uTd[c], uT_[:], reads=[uT_])

                        def loadUV(c):
                            sc, ec = divmod(c, ES)
                            vs = vS[sc % 2]
                            dma("pool", ub[c % NUB][:], peer_u[c * 128:(c + 1) * 128, :], writes=[ub[c % NUB]], max_dma_last_dim=8192)
                            dma("pool", vs[:, ec, :], peer_v[c * 128:(c + 1) * 128, :], writes=[vsub[sc % 2][ec]], max_dma_last_dim=8192)
                            dma("sp", vd[c], vs[:, ec, :], reads=[vsub[sc % 2][ec]])

                        def uside(c):
                            sc, ec = divmod(c, ES)
                            aS = actS[sc % 2]
                            uT_, g_ = uT[c % NUT], gt[c % NUT]
                            for (cl0, n, is_s) in chunks:
                                hp = banks[2 + cu["h"] % 2]
                                gl_ = gl[cu["h"] % 2]
                                cu["h"] += 1
                                rt = [xnT_t[i] for i in tiles_of(cl0, n)]
                                for kc in range(16):
                                    op("pe", lambda e: e.matmul(hp.t[:, :n], lhsT=uT_[:, kc, :], rhs=AX[:, kc, cl0:cl0 + n], start=(kc == 0), stop=(kc == 15)),
                                       reads=rt + [uT_], writes=[hp])
                                op("act", lambda e: e.activation(out=gl_[:, :n], in_=hp.t[:, :n], func=AF.Gelu), reads=[hp], writes=[gl_])
                                op("pool", lambda e: e.tensor_tensor(out=aS[:, ec, cl0:cl0 + n], in0=gl_[:, :n], in1=g_[:, cl0:cl0 + n], op=ALU.mult),
                                   reads=[gl_, g_], writes=[aS])

                        def vside(sc):
                            aS, vs = actS[sc % 2], vS[sc % 2]
                            for ti, (t0, rows) in enumerate(tiles):
                                lc = t0 - c0
                                for dq4 in range(4):
                                    bk = banks[4 + cu["v"] % 4]
                                    cu["v"] += 1
                                    d0 = dq4 * 512
                                    for ec in range(ES):
                                        op("pe", lambda e: e.matmul(bk.t[:rows, :], lhsT=aS[:, ec, lc:lc + rows], rhs=vs[:, ec, d0:d0 + 512], start=(ec == 0), stop=(ec == ES - 1)),
                                           reads=[aS, vsub[sc % 2][ec]], writes=[bk])
                                    op("dve", lambda e: e.tensor_tensor(out=acc[:rows, ti, d0:d0 + 512], in0=bk.t[:rows, :], in1=acc[:rows, ti, d0:d0 + 512], op=ALU.add),
                                       reads=[bk, acc_t[ti]], writes=[acc_t[ti]])

                        nchunk = nsc * ES
                        if pi == 0:
                            for c_ in range(min(NUB - 1, nchunk)):
                                loadUV(c_)
                        AH = NUT - 1
                        for c_ in range(min(AH, nchunk)):
                            prepU(c_)
                        for c in range(nchunk):
                            if pi == 0 and c + NUB - 1 < nchunk:
                                loadUV(c + NUB - 1)
                            if c + AH < nchunk:
                                prepU(c + AH)
                            uside(c)
                            if (c + 1) % ES == 0:
                                vside(c // ES)
                chk("p6")
                if "h3" in dbg:
                    for ti, (t0, rows) in enumerate(tiles):
                        dma("sp", dbg["h3"][t0:t0 + rows, :], acc[:rows, ti, :], reads=[acc_t[ti]], is_output=True)

                scope("P7_%d" % pi)
                norm_to_AX(2)
                with contextlib.ExitStack() as p7:
                    pT = kb.sb(p7, "pT", [128, 2, Tp], BF16)
                    pt32 = [kb.sb(p7, "pt32_%d" % i, [128, 256], F32) for i in range(2)]
                    ptb = [kb.sb(p7, "ptb%d" % i, [128, 256], BF16) for i in range(2)]
                    for ti, (t0, rows) in enumerate(tiles):
                        lc = t0 - c0
                        s_ = ti % 2
                        dma("sp", pt32[s_][:rows, :], pvec[t0:t0 + rows, :], writes=[pt32[s_]])
                        op("act", lambda e: e.activation(out=ptb[s_][:rows, :], in_=pt32[s_][:rows, :], func=AF.Copy), reads=[pt32[s_]], writes=[ptb[s_]])
                        bk = banks[s_]
                        tp = bk.t[:].bitcast(BF16)
                        for k2 in range(2):
                            op("pe", lambda e: e.transpose(out=tp[:, k2 * 128:k2 * 128 + rows], in_=ptb[s_][:rows, k2 * 128:(k2 + 1) * 128], identity=ident_b[:rows, :rows]),
                               reads=[ptb[s_], ident_b], writes=[bk])
                        op("act", lambda e: e.activation(out=pT[:, :, lc:lc + rows], in_=tp[:, 0:256].rearrange("p (k t) -> p k t", k=2)[:, :, :rows], func=AF.Copy),
                           reads=[bk], writes=[pT])
                    wgb = [kb.sb(p7, "wgb%d" % i, [128, 16, 512], BF16) for i in range(2)]
                    wpb = [kb.sb(p7, "wpb%d" % i, [128, 2, 512], BF16) for i in range(2)]
                    gsb = [kb.sb(p7, "gsb%d" % i, [128, 512], F32) for i in range(2)]
                    it7 = 0
                    for nb in range(4):
                        wg_, wp_ = wgb[nb % 2], wpb[nb % 2]
                        dma("pool", wg_[:], ple_gate[:, nb * 512:(nb + 1) * 512].rearrange("(kc p) n -> p kc n", p=128), writes=[wg_])
                        dma("pool", wp_[:], ple_proj[:, nb * 512:(nb + 1) * 512].rearrange("(kc p) n -> p kc n", p=128), writes=[wp_])
                        for ti, (t0, rows) in enumerate(tiles):
                            lc = t0 - c0
                            bG, bP = banks[2 + it7 % 2], banks[4 + it7 % 2]
                            gs_ = gsb[it7 % 2]
                            it7 += 1
                            for kc in range(16):
                                op("pe", lambda e: e.matmul(bG.t[:rows, :], lhsT=AX[:, kc, lc:lc + rows], rhs=wg_[:, kc, :], start=(kc == 0), stop=(kc == 15)),
                                   reads=[xnT_t[ti], wg_], writes=[bG])
                            for k2 in range(2):
                                op("pe", lambda e: e.matmul(bP.t[:rows, :], lhsT=pT[:, k2, lc:lc + rows], rhs=wp_[:, k2, :], start=(k2 == 0), stop=(k2 == 1)),
                                   reads=[pT, wp_], writes=[bP])
                            op("act", lambda e: e.activation(out=gs_[:rows, :], in_=bG.t[:rows, :], func=AF.Sigmoid), reads=[bG], writes=[gs_])
                            op("dve", lambda e: e.tensor_tensor(out=gs_[:rows, :], in0=gs_[:rows, :], in1=bP.t[:rows, :], op=ALU.mult), reads=[gs_, bP], writes=[gs_])
                            op("dve", lambda e: e.tensor_tensor(out=acc[:rows, ti, nb * 512:(nb + 1) * 512], in0=acc[:rows, ti, nb * 512:(nb + 1) * 512], in1=gs_[:rows, :], op=ALU.add),
                               reads=[gs_, acc_t[ti]], writes=[acc_t[ti]])
                chk("p7")
                if "h4" in dbg:
                    for ti, (t0, rows) in enumerate(tiles):
                        dma("sp", dbg["h4"][t0:t0 + rows, :], acc[:rows, ti, :], reads=[acc_t[ti]], is_output=True)
                scope("PF_%d" % pi)
                with contextlib.ExitStack() as pf:
                    junk = kb.sb(pf, "fjunk", [128, D], BF16)
                    yt = [kb.sb(pf, "yt%d" % i, [128, D], F32) for i in range(2)]
                    st = [kb.sb(pf, "fst%d" % i, [128, 4], F32) for i in range(2)]
                    for ti, (t0, rows) in enumerate(tiles):
                        s_ = ti % 2
                        op("act", lambda e: e.activation(out=junk[:rows, :], in_=acc[:rows, ti, :], func=AF.Square, accum_out=st[s_][:rows, 0:1]),
                           reads=[acc_t[ti]], writes=[junk, st[s_]])
                        op("dve", lambda e: e.tensor_scalar(out=st[s_][:rows, 1:2], in0=st[s_][:rows, 0:1], scalar1=1.0 / D, scalar2=EPS, op0=ALU.mult, op1=ALU.add),
                           reads=[st[s_]], writes=[st[s_]])
                        op("act", lambda e: e.activation(out=st[s_][:rows, 2:3], in_=st[s_][:rows, 1:2], func=AF.Sqrt), reads=[st[s_]], writes=[st[s_]])
                        op("dve", lambda e: e.reciprocal(out=st[s_][:rows, 3:4], in_=st[s_][:rows, 2:3]), reads=[st[s_]], writes=[st[s_]])
                        op("dve", lambda e: e.scalar_tensor_tensor(out=yt[s_][:rows, :], in0=acc[:rows, ti, :], scalar=st[s_][:rows, 3:4], in1=fgb[:rows, :], op0=ALU.mult, op1=ALU.mult),
                           reads=[acc_t[ti], st[s_], fgb], writes=[yt[s_]])
                        dma("sp", y_out[t0:t0 + rows, :], yt[s_][:rows, :], reads=[yt[s_]], is_output=True)

                if "h2" in dbg:
                    for ti, (t0, rows) in enumerate(tiles):
                        dma("sp", dbg["h2"][t0:t0 + rows, :], acc[:rows, ti, :], reads=[acc_t[ti]], is_output=True)
        scope(None)
        kb.finish()


def make_in_maps(inputs):
    cst = consts()
    maps = []
    for c in range(NC):
        m = dict(cst)
        m["x"] = np.ascontiguousarray(np.concatenate(
            [inputs["x_prompt"][c], inputs["x_sample"][NS * c:NS * (c + 1)].reshape(TS, D)], 0))
        m["w_in"] = np.ascontiguousarray(inputs["w_in"][0])
        m["norms"] = np.ascontiguousarray(np.stack([inputs["attn_norm"][0], inputs["ffn_norm"][0], inputs["ple_norm"][0], inputs["final_norm"]]))
        m["peer_wq"] = np.ascontiguousarray(inputs["peer_wq"][0])
        m["peer_sk"] = np.ascontiguousarray(inputs["peer_subkeys"][0].reshape(16 * 128, 128))
        m["peer_u"] = np.ascontiguousarray(inputs["peer_u"][0])
        m["peer_v"] = np.ascontiguousarray(inputs["peer_v"][0])
        m["ple_gate"] = np.ascontiguousarray(inputs["ple_gate"][0])
        m["ple_proj"] = np.ascontiguousarray(inputs["ple_proj"][0])
        m["ck"] = inputs["cache_k"][0].reshape(2560 * 128, 256)
        m["cv"] = inputs["cache_v"][0].reshape(2560 * 128, 256)
        m["cki"] = inputs["cache_kidx"][0].reshape(2560 * 128, 64)
        m["pt"] = np.ascontiguousarray(inputs["page_table"][NS * c:NS * (c + 1)].reshape(-1).astype(np.int32))
        m["pvec"] = np.ascontiguousarray(np.concatenate([inputs["p_prompt"][0, c], inputs["p_sample"][0, NS * c:NS * (c + 1)].reshape(TS, 256)], 0))
        for k_ in ("conv_w", "conv_b", "conv_ln_g", "conv_ln_b", "w_out"):
            m[k_] = np.ascontiguousarray(inputs[k_][0])
        m["state"] = np.ascontiguousarray(inputs["state_conv"][0, NS * c:NS * (c + 1)].reshape(NS * 30, 1024))
        maps.append(m)
    return maps


def kernel(**inputs):
    inputs = {k: np.asarray(v) for k, v in inputs.items()}
    nc = build()
    maps = make_in_maps(inputs)
    res = run_bass_kernel_spmd(nc, maps, core_ids=list(range(NC)))
    R = res.results
    B = 8
    k_p = np.stack([R[c]["k_out"][:SEQ].reshape(SEQ, 2, 128) for c in range(NC)])[None]
    v_p = np.stack([R[c]["v_out"][:SEQ].reshape(SEQ, 2, 128) for c in range(NC)])[None]
    ki_p = np.stack([R[c]["ki_out"][:SEQ] for c in range(NC)])[None]
    k_s = np.concatenate([R[c]["k_out"][SEQ:].reshape(NS, 4, 2, 128) for c in range(NC)])[None]
    v_s = np.concatenate([R[c]["v_out"][SEQ:].reshape(NS, 4, 2, 128) for c in range(NC)])[None]
    ki_s = np.concatenate([R[c]["ki_out"][SEQ:].reshape(NS, 4, 64) for c in range(NC)])[None]
    y_p = np.stack([R[c]["y"][:SEQ] for c in range(NC)])
    y_s = np.concatenate([R[c]["y"][SEQ:].reshape(NS, 4, D) for c in range(NC)])
    cp = np.stack([R[c]["conv_p"] for c in range(NC)])[None]
    cs = np.concatenate([R[c]["conv_s"] for c in range(NC)])[None]
    return (y_p, y_s, k_p, v_p, ki_p, cp, k_s, v_s, ki_s, cs)
```
